# Optimizing a Trainium2 kernel written in Bass

```python
import math
import jax, jax.numpy as jnp
from jax import lax
import numpy as np

D_MODEL = 1024
BATCH = 4
SEQ = 8192
DEPTH = 2

GRID_W = 64
CTX_LEN = 256
HEAD_DIM = 64
ATT_WIDTH = D_MODEL // 2
ATT_HEADS = ATT_WIDTH // HEAD_DIM
ATT_KV_HEADS = ATT_HEADS // 4
KV_WIDTH = ATT_KV_HEADS * HEAD_DIM
ROPE_HALF = HEAD_DIM // 2
ROPE_AXIS_FREQS = HEAD_DIM // 4
ROPE_THETA = 10000.0
Q_BLOCK = 128
LRU_WIDTH = D_MODEL // 4
LRU_BLOCKS = 4
LRU_BLOCK_W = LRU_WIDTH // LRU_BLOCKS
LRU_C = 8.0
LRU_CONV = 4
LRU_CONV_LEFT = 2
HY_WIDTH = D_MODEL // 4
HY_CONV = 3
HY_CONV_LEFT = 1
HY_EMB = 33
HY_BANDS = (HY_EMB - 1) // 2
HY_FFN = 64
HY_FAST_DECAY = 0.3
HY_SLOW_DECAY = 1.5
HY_DECAY_TARGET = 1e-2
MIX_WIDTH = ATT_WIDTH + LRU_WIDTH + HY_WIDTH
IN_WIDTH = ATT_WIDTH + 2 * KV_WIDTH + 2 * LRU_WIDTH + 3 * HY_WIDTH
N_EXPERTS = 32
TOP_K = 4
D_FF = D_MODEL
SWIGLU_ALPHA = 1.702
SWIGLU_LIMIT = 7.0
EPS = 1e-6

kernel_name = 'hymba_gqa_rglru_hyena_moe_prefix_dit'

F32 = jnp.float32


def rmsnorm(x, g):
    xf = x.astype(F32)
    y = xf * lax.rsqrt(jnp.mean(xf * xf, axis=-1, keepdims=True) + EPS)
    return (y * g.astype(F32)).astype(x.dtype)


def modulate(h, shift, scale):
    return h * (1 + scale) + shift


def grid_rope(L):
    rows_n = L // GRID_W
    rows = jnp.repeat(jnp.arange(rows_n), GRID_W).astype(F32)
    cols = jnp.tile(jnp.arange(GRID_W), rows_n).astype(F32)
    inv = ROPE_THETA ** (-jnp.arange(ROPE_AXIS_FREQS, dtype=F32) / ROPE_AXIS_FREQS)
    ang = jnp.concatenate([rows[:, None] * inv, cols[:, None] * inv], axis=-1)
    return jnp.cos(ang)[:, None, :], jnp.sin(ang)[:, None, :]


def apply_rope(x, cos, sin):
    xf = x.astype(F32)
    x1, x2 = xf[..., :ROPE_HALF], xf[..., ROPE_HALF:]
    return jnp.concatenate([x1 * cos - x2 * sin, x2 * cos + x1 * sin], axis=-1).astype(x.dtype)


def dwconv(x, w, b, left):
    W, C = w.shape
    y = lax.conv_general_dilated(x, w[:, None, :].astype(x.dtype), window_strides=(1,),
                                 padding=[(left, W - 1 - left)],
                                 dimension_numbers=('NWC', 'WIO', 'NWC'), feature_group_count=C)
    return y + b.astype(x.dtype)


def split_proj(u):
    sizes = (ATT_WIDTH, KV_WIDTH, KV_WIDTH, LRU_WIDTH, LRU_WIDTH, 3 * HY_WIDTH)
    idx = np.cumsum(sizes)[:-1].tolist()
    return jnp.split(u, idx, axis=-1)


def gqa(q, k, v):
    s = jnp.einsum('bqkgd,bskd->bkgqs', q, k).astype(F32) * (HEAD_DIM ** -0.5)
    p = jax.nn.softmax(s, axis=-1).astype(v.dtype)
    return jnp.einsum('bkgqs,bskd->bqkgd', p, v)


def lru_coeffs(xc, w, b, lam):
    B, L, C = xc.shape
    xf = xc.astype(F32)
    g = jnp.einsum('blnd,jnde->jblne', xf.reshape(B, L, LRU_BLOCKS, LRU_BLOCK_W), w.astype(F32))
    g = g.reshape(2, B, L, C) + b.astype(F32)[:, None, None, :]
    r = jax.nn.sigmoid(g[0])
    i = jax.nn.sigmoid(g[1])
    log_a = -LRU_C * r * jax.nn.softplus(-lam.astype(F32))
    a = jnp.exp(log_a)
    bx = jnp.sqrt(-jnp.expm1(2.0 * log_a)) * (i * xf)
    return a, bx


def linear_scan(a, bx, h0, reverse):
    idx = -1 if reverse else 0
    bx = bx.at[:, idx].add(a[:, idx] * h0)

    def comb(left, right):
        a_l, b_l = left
        a_r, b_r = right
        return a_l * a_r, a_r * b_l + b_r

    _, h = lax.associative_scan(comb, (a, bx), axis=1, reverse=reverse)
    return h


def hyena_kernel(L, p):
    t01 = jnp.linspace(0.0, 1.0, L, dtype=F32)[:, None]
    bands = jnp.linspace(1e-4, HY_BANDS - 1, HY_BANDS, dtype=F32)
    ang = (2.0 * math.pi / L) * jnp.arange(L, dtype=F32)[:, None] * bands
    z = jnp.concatenate([t01, jnp.cos(ang), -jnp.sin(ang)], axis=-1)
    fr = p['hy_freq'].astype(F32)
    h = jnp.sin(fr * (z @ p['hy_w1'].astype(F32) + p['hy_b1'].astype(F32)))
    h = jnp.sin(fr * (h @ p['hy_w2'].astype(F32) + p['hy_b2'].astype(F32)))
    h = (h @ p['hy_w3'].astype(F32)) * jnp.exp(-t01 * jnp.abs(p['hy_decay'].astype(F32)))
    hf, hb = h[:, :HY_WIDTH], h[:, HY_WIDTH:]
    k = jnp.concatenate([hf, jnp.zeros((1, HY_WIDTH), F32), jnp.flip(hb[1:], axis=0)], axis=0)
    return k / jnp.sum(jnp.abs(k), axis=0, keepdims=True)


def hyena(u, p):
    L = u.shape[1]
    uc = dwconv(u, p['hy_cw'], p['hy_cb'], HY_CONV_LEFT)
    x0, x1, v = jnp.split(uc, 3, axis=-1)
    k = hyena_kernel(L, p)
    v = (v * x1).astype(F32)
    n = 2 * L
    y = jnp.fft.irfft(jnp.fft.rfft(v, n=n, axis=1) * jnp.fft.rfft(k, n=n, axis=0)[None], n=n, axis=1)[:, :L]
    y = y + v * p['hy_bias'].astype(F32)
    return y.astype(u.dtype) * x0


def merge_groups(att, lru, hyo, p):
    g = p['out_g']
    o = jnp.concatenate([rmsnorm(att, g[:ATT_WIDTH]),
                         rmsnorm(lru, g[ATT_WIDTH:ATT_WIDTH + LRU_WIDTH]),
                         rmsnorm(hyo, g[ATT_WIDTH + LRU_WIDTH:])], axis=-1)
    return o @ p['w_out']


def mixer(h_lat, h_ctx, p, ctx_out):
    B, S, _ = h_lat.shape
    Lc = h_ctx.shape[1]
    G = ATT_HEADS // ATT_KV_HEADS
    q, k, v, lx, lg, hy = split_proj(h_lat @ p['w_in'])
    qc, kc, vc, lxc, lgc, hyc = split_proj(h_ctx @ p['w_in'])
    cos, sin = grid_rope(S)
    q = apply_rope(rmsnorm(q.reshape(B, S, ATT_HEADS, HEAD_DIM), p['q_g']), cos, sin)
    k = apply_rope(rmsnorm(k.reshape(B, S, ATT_KV_HEADS, HEAD_DIM), p['k_g']), cos, sin)
    v = v.reshape(B, S, ATT_KV_HEADS, HEAD_DIM)
    kc = rmsnorm(kc.reshape(B, Lc, ATT_KV_HEADS, HEAD_DIM), p['k_g'])
    vc = vc.reshape(B, Lc, ATT_KV_HEADS, HEAD_DIM)
    k_all = jnp.concatenate([kc, k], axis=1)
    v_all = jnp.concatenate([vc, v], axis=1)
    qb = q.reshape(B, S // Q_BLOCK, Q_BLOCK, ATT_KV_HEADS, G, HEAD_DIM).transpose(1, 0, 2, 3, 4, 5)
    att = lax.map(lambda qi: gqa(qi, k_all, v_all), qb)
    att = att.transpose(1, 0, 2, 3, 4, 5).reshape(B, S, ATT_WIDTH)
    xl = dwconv(lx, p['lru_cw'], p['lru_cb'], LRU_CONV_LEFT)
    xcx = dwconv(lxc, p['lru_cw'], p['lru_cb'], LRU_CONV_LEFT)
    h_lat_dirs = []
    h_ctx_dirs = []
    for d, rev in enumerate((False, True)):
        a_c, b_c = lru_coeffs(xcx, p['lru_gw'][d], p['lru_gb'][d], p['lru_lam'][d])
        h_c = linear_scan(a_c, b_c, jnp.zeros_like(b_c[:, 0]), rev)
        h_end = h_c[:, 0] if rev else h_c[:, -1]
        a_l, b_l = lru_coeffs(xl, p['lru_gw'][d], p['lru_gb'][d], p['lru_lam'][d])
        h_lat_dirs.append(linear_scan(a_l, b_l, h_end, rev))
        h_ctx_dirs.append(h_c)
    lru = (h_lat_dirs[0] + h_lat_dirs[1]).astype(h_lat.dtype) * jax.nn.gelu(lg)
    hyo = hyena(hy, p)
    o_lat = merge_groups(att, lru, hyo, p)
    if not ctx_out:
        return o_lat, None
    qc = rmsnorm(qc.reshape(B, Lc, ATT_KV_HEADS, G, HEAD_DIM), p['q_g'])
    att_c = gqa(qc, kc, vc).reshape(B, Lc, ATT_WIDTH)
    lru_c = (h_ctx_dirs[0] + h_ctx_dirs[1]).astype(h_ctx.dtype) * jax.nn.gelu(lgc)
    o_ctx = merge_groups(att_c, lru_c, hyena(hyc, p), p)
    return o_lat, o_ctx


def moe(h, router_w, router_b, w1, b1, w2, b2):
    T = h.shape[0]
    logits = (h @ router_w).astype(F32) + router_b.astype(F32)
    top_v, top_i = lax.top_k(logits, TOP_K)
    gate = jax.nn.softmax(top_v, axis=-1)
    dense_gate = jnp.sum(jax.nn.one_hot(top_i, N_EXPERTS, dtype=F32) * gate[..., None], axis=1)
    out = jnp.zeros((T, h.shape[1]), F32)
    for e in range(N_EXPERTS):
        hu = (h @ w1[e] + b1[e]).astype(F32)
        glu = jnp.minimum(hu[:, ::2], SWIGLU_LIMIT)
        lin = jnp.clip(hu[:, 1::2], -SWIGLU_LIMIT, SWIGLU_LIMIT)
        act = (glu * jax.nn.sigmoid(SWIGLU_ALPHA * glu) * (lin + 1.0)).astype(h.dtype)
        out = out + dense_gate[:, e:e + 1] * (act @ w2[e] + b2[e]).astype(F32)
    return out.astype(h.dtype)


def setup_inputs(seed: int = 0) -> dict:
    key = jax.random.key(seed)
    ks = iter(list(jax.random.split(key, 40)))

    def nrm(shape, scale):
        return jax.random.normal(next(ks), shape, F32) * scale

    D = D_MODEL
    x = nrm((BATCH, SEQ, D), 1.0)
    c = nrm((BATCH, D), 1.0)
    ctx = nrm((BATCH, CTX_LEN, D), 1.0)
    c_ctx = nrm((D,), 1.0)
    w_mod = nrm((DEPTH, D, 6 * D), 0.5 * D ** -0.5)
    b_mod = nrm((DEPTH, 6 * D), 0.02)
    norm_mix_g = 1.0 + nrm((DEPTH, D), 0.05)
    norm_ffn_g = 1.0 + nrm((DEPTH, D), 0.05)
    w_in = nrm((DEPTH, D, IN_WIDTH), D ** -0.5)
    q_norm_g = 1.0 + nrm((DEPTH, HEAD_DIM), 0.05)
    k_norm_g = 1.0 + nrm((DEPTH, HEAD_DIM), 0.05)
    lru_conv_w = nrm((DEPTH, LRU_CONV, LRU_WIDTH), LRU_CONV ** -0.5)
    lru_conv_b = nrm((DEPTH, LRU_WIDTH), 0.02)
    lru_gate_w = nrm((DEPTH, 2, 2, LRU_BLOCKS, LRU_BLOCK_W, LRU_BLOCK_W), LRU_BLOCK_W ** -0.5)
    lru_gate_b = nrm((DEPTH, 2, 2, LRU_WIDTH), 0.02)
    a_pow = jax.random.uniform(next(ks), (DEPTH, 2, LRU_WIDTH), F32, minval=0.9, maxval=0.999)
    a_base = a_pow ** (1.0 / LRU_C)
    lru_lambda = jnp.log(a_base) - jnp.log1p(-a_base)
    hy_conv_w = nrm((DEPTH, HY_CONV, 3 * HY_WIDTH), HY_CONV ** -0.5)
    hy_conv_b = nrm((DEPTH, 3 * HY_WIDTH), 0.02)
    hy_w1 = nrm((DEPTH, HY_EMB, HY_FFN), HY_EMB ** -0.5)
    hy_b1 = nrm((DEPTH, HY_FFN), 0.02)
    hy_w2 = nrm((DEPTH, HY_FFN, HY_FFN), HY_FFN ** -0.5)
    hy_b2 = nrm((DEPTH, HY_FFN), 0.02)
    hy_w3 = nrm((DEPTH, HY_FFN, 2 * HY_WIDTH), HY_FFN ** -0.5)
    hy_freq = 1.0 + nrm((DEPTH, HY_FFN), 0.05)
    min_decay = math.log(HY_DECAY_TARGET) / HY_SLOW_DECAY
    max_decay = math.log(HY_DECAY_TARGET) / HY_FAST_DECAY
    base_decay = jnp.tile(jnp.linspace(min_decay, max_decay, HY_WIDTH, dtype=F32), 2)
    hy_decay = base_decay[None, :] + nrm((DEPTH, 2 * HY_WIDTH), 0.1)
    hy_bias = nrm((DEPTH, HY_WIDTH), 0.5)
    out_norm_g = 1.0 + nrm((DEPTH, MIX_WIDTH), 0.05)
    w_out = nrm((DEPTH, MIX_WIDTH, D), MIX_WIDTH ** -0.5)
    router_w = nrm((DEPTH, D, N_EXPERTS), D ** -0.5)
    router_b = nrm((DEPTH, N_EXPERTS), 0.01)
    moe_w1 = nrm((DEPTH, N_EXPERTS, D, 2 * D_FF), D ** -0.5)
    moe_b1 = nrm((DEPTH, N_EXPERTS, 2 * D_FF), 0.01)
    moe_w2 = nrm((DEPTH, N_EXPERTS, D_FF, D), D_FF ** -0.5)
    moe_b2 = nrm((DEPTH, N_EXPERTS, D), 0.01)
    return {'x': x, 'c': c, 'ctx': ctx, 'c_ctx': c_ctx, 'w_mod': w_mod, 'b_mod': b_mod,
            'norm_mix_g': norm_mix_g, 'norm_ffn_g': norm_ffn_g, 'w_in': w_in,
            'q_norm_g': q_norm_g, 'k_norm_g': k_norm_g, 'lru_conv_w': lru_conv_w, 'lru_conv_b': lru_conv_b,
            'lru_gate_w': lru_gate_w, 'lru_gate_b': lru_gate_b, 'lru_lambda': lru_lambda,
            'hy_conv_w': hy_conv_w, 'hy_conv_b': hy_conv_b, 'hy_w1': hy_w1, 'hy_b1': hy_b1,
            'hy_w2': hy_w2, 'hy_b2': hy_b2, 'hy_w3': hy_w3, 'hy_freq': hy_freq, 'hy_decay': hy_decay,
            'hy_bias': hy_bias, 'out_norm_g': out_norm_g, 'w_out': w_out, 'router_w': router_w,
            'router_b': router_b, 'moe_w1': moe_w1, 'moe_b1': moe_b1, 'moe_w2': moe_w2, 'moe_b2': moe_b2}


def reference(x, c, ctx, c_ctx, w_mod, b_mod, norm_mix_g, norm_ffn_g, w_in, q_norm_g, k_norm_g,
              lru_conv_w, lru_conv_b, lru_gate_w, lru_gate_b, lru_lambda, hy_conv_w, hy_conv_b,
              hy_w1, hy_b1, hy_w2, hy_b2, hy_w3, hy_freq, hy_decay, hy_bias, out_norm_g, w_out,
              router_w, router_b, moe_w1, moe_b1, moe_w2, moe_b2):
    B, S, D = x.shape
    Lc = ctx.shape[1]
    for l in range(DEPTH):
        last = l == DEPTH - 1
        mod = jax.nn.silu(c) @ w_mod[l] + b_mod[l]
        mod_c = jax.nn.silu(c_ctx) @ w_mod[l] + b_mod[l]
        sa, ca, ga, sf, cf, gf = jnp.split(mod[:, None, :], 6, axis=-1)
        sa_c, ca_c, ga_c, sf_c, cf_c, gf_c = jnp.split(mod_c, 6, axis=-1)
        p = {'w_in': w_in[l], 'q_g': q_norm_g[l], 'k_g': k_norm_g[l],
             'lru_cw': lru_conv_w[l], 'lru_cb': lru_conv_b[l], 'lru_gw': lru_gate_w[l],
             'lru_gb': lru_gate_b[l], 'lru_lam': lru_lambda[l],
             'hy_cw': hy_conv_w[l], 'hy_cb': hy_conv_b[l], 'hy_w1': hy_w1[l], 'hy_b1': hy_b1[l],
             'hy_w2': hy_w2[l], 'hy_b2': hy_b2[l], 'hy_w3': hy_w3[l], 'hy_freq': hy_freq[l],
             'hy_decay': hy_decay[l], 'hy_bias': hy_bias[l], 'out_g': out_norm_g[l], 'w_out': w_out[l]}
        h_lat = modulate(rmsnorm(x, norm_mix_g[l]), sa, ca)
        h_ctx = modulate(rmsnorm(ctx, norm_mix_g[l]), sa_c, ca_c)
        o_lat, o_ctx = mixer(h_lat, h_ctx, p, not last)
        x = x + ga * o_lat
        f_lat = modulate(rmsnorm(x, norm_ffn_g[l]), sf, cf).reshape(B * S, D)
        if last:
            y = moe(f_lat, router_w[l], router_b[l], moe_w1[l], moe_b1[l], moe_w2[l], moe_b2[l])
            x = x + gf * y.reshape(B, S, D)
        else:
            ctx = ctx + ga_c * o_ctx
            f_ctx = modulate(rmsnorm(ctx, norm_ffn_g[l]), sf_c, cf_c).reshape(B * Lc, D)
            y = moe(jnp.concatenate([f_lat, f_ctx], axis=0), router_w[l], router_b[l],
                    moe_w1[l], moe_b1[l], moe_w2[l], moe_b2[l])
            x = x + gf * y[:B * S].reshape(B, S, D)
            ctx = ctx + gf_c * y[B * S:].reshape(B, Lc, D)
    return x
```

```python
import math
from contextlib import ExitStack
import numpy as np
import ml_dtypes
import concourse.bass as bass
import concourse.mybir as mybir
from concourse.bass_utils import run_bass_kernel_spmd

F32 = mybir.dt.float32
BF16 = mybir.dt.bfloat16
AF = mybir.ActivationFunctionType
ALU = mybir.AluOpType
AX = mybir.AxisListType

D = 1024
S = 8192
SH = 4096
LC = 256
NTOK = SH + LC
NALL = S + LC
NE = 32
EPS = 1e-6
NFFT = 16384


class Dep:
    __slots__ = ("w", "r")

    def __init__(self):
        self.w = None
        self.r = {}


class MK:
    ENG = ("pe", "act", "dve", "pool", "sp")
    EPOCH = 12000

    def __init__(self, nc, n_dma_sems=48):
        self.nc = nc
        self.e = {"pe": nc.tensor, "act": nc.scalar, "dve": nc.vector, "pool": nc.gpsimd, "sp": nc.sync}
        self.sem = {}
        self.cnt = {}
        self.allsems = []
        self.nsem = 0
        for k in self.ENG:
            self._new_epoch(k)
        self.seen = {k: {} for k in self.ENG}
        self.dma_sems = [nc.alloc_semaphore(f"dq{i}") for i in range(n_dma_sems)]
        self.dma_val = [0] * n_dma_sems
        self.dma_rr = 0
        self.n_inst = 0
        self.n_wait = 0
        self._uid = 0

    def _new_epoch(self, k):
        self.sem[k] = self.nc.alloc_semaphore(f"s_{k}_{self.nsem}")
        self.nsem += 1
        self.cnt[k] = 0

    def uid(self, p="t"):
        self._uid += 1
        return f"{p}{self._uid}"

    def _wait(self, eng, tok):
        if tok is None:
            return
        sem, val = tok
        sid = id(sem)
        if self.seen[eng].get(sid, 0) >= val:
            return
        if sem is self.sem.get(eng) and val > self.cnt[eng]:
            return
        self.e[eng].wait_ge(sem, val)
        self.seen[eng][sid] = val
        self.n_wait += 1

    def _deps(self, eng, reads, writes):
        for d in reads:
            self._wait(eng, d.w)
        for d in writes:
            self._wait(eng, d.w)
            for t in d.r.values():
                self._wait(eng, t)

    def op(self, eng, fn, reads=(), writes=(), inc=True):
        self._deps(eng, reads, writes)
        ins = fn(self.e[eng])
        self.n_inst += 1
        if inc:
            self.cnt[eng] += 1
            ins.then_inc(self.sem[eng], 1)
            tok = (self.sem[eng], self.cnt[eng])
            self.seen[eng][id(self.sem[eng])] = self.seen[eng].get(id(self.sem[eng]), 0)
            if self.cnt[eng] >= self.EPOCH:
                self._new_epoch(eng)
        else:
            tok = (self.sem[eng], self.cnt[eng] + 1)
        for d in reads:
            d.r[eng] = tok
        for d in writes:
            d.w = tok
            d.r = {}
        return ins

    def dma(self, out, in_, reads=(), writes=(), q="sp", **kw):
        self._deps(q, reads, writes)
        i = self.dma_rr
        self.dma_rr = (self.dma_rr + 1) % len(self.dma_sems)
        sem = self.dma_sems[i]
        if self.dma_val[i] > 0:
            self._wait(q, (sem, self.dma_val[i]))
        self.dma_val[i] += 16
        ins = self.e[q].dma_start(out=out, in_=in_, **kw)
        ins.then_inc(sem, 16)
        self.n_inst += 1
        tok = (sem, self.dma_val[i])
        for d in reads:
            d.r["dma%d" % i] = tok
        for d in writes:
            d.w = tok
            d.r = {}
        return tok

    def barrier(self, engines=None):
        engines = engines or self.ENG
        toks = [(self.sem[p], self.cnt[p]) for p in self.ENG if self.cnt[p] > 0]
        toks += [(s, v) for s, v in zip(self.dma_sems, self.dma_val) if v > 0]
        for e in engines:
            for t in toks:
                self._wait(e, t)


class Phase:
    def __init__(self, m):
        self.m = m
        self.st = ExitStack()

    def __enter__(self):
        self.st.__enter__()
        return self

    def sb(self, shape, dt=F32):
        return self.st.enter_context(self.m.nc.sbuf_tensor(self.m.uid("sb"), list(shape), dt))

    def ps(self, shape, dt=F32):
        return self.st.enter_context(self.m.nc.psum_tensor(self.m.uid("ps"), list(shape), dt))

    def __exit__(self, *a):
        self.m.barrier()
        return self.st.__exit__(*a)


_CONST = {}


def _consts():
    if _CONST:
        return _CONST
    bf = ml_dtypes.bfloat16
    c = _CONST
    c["ident"] = np.eye(128, dtype=np.float32)
    pr = np.zeros((128, 128), np.float32)
    for blk in range(2):
        for i in range(32):
            pr[blk * 64 + i + 32, blk * 64 + i] = -1.0
            pr[blk * 64 + i, blk * 64 + i + 32] = 1.0
    c["prot"] = pr
    bo = np.zeros((128, 128), np.float32)
    bo[:64, :64] = 1.0 / 64
    bo[64:, 64:] = 1.0 / 64
    c["blk64"] = bo
    sel = np.zeros((65, 64), np.float32)
    sel[64, :] = 1.0
    c["sel"] = sel
    rows = np.repeat(np.arange(S // 64), 64).astype(np.float32)
    cols = np.tile(np.arange(64), S // 64).astype(np.float32)
    inv = (10000.0 ** (-np.arange(16, dtype=np.float32) / 16)).astype(np.float32)
    ang = np.concatenate([rows[:, None] * inv, cols[:, None] * inv], axis=-1).astype(np.float32)
    c["rope_cos"] = np.ascontiguousarray(np.tile(np.cos(ang).T, (4, 1)).astype(np.float32))
    c["rope_sin"] = np.ascontiguousarray(np.tile(np.sin(ang).T, (4, 1)).astype(np.float32))

    def zfeat(L):
        t01 = np.linspace(0.0, 1.0, L, dtype=np.float32)[:, None]
        bands = np.linspace(1e-4, 15, 16, dtype=np.float32)
        a = (np.float32(2.0 * math.pi / L) * np.arange(L, dtype=np.float32)[:, None] * bands).astype(np.float32)
        z = np.concatenate([t01, np.cos(a), -np.sin(a)], axis=-1).astype(np.float32)
        return np.ascontiguousarray(z.T), np.ascontiguousarray(np.broadcast_to(t01[:, 0][None, :], (128, L)))
    c["zT"], c["t01"] = zfeat(S)
    c["zTc"], c["t01c"] = zfeat(LC)
    a = np.arange(64)[:, None, None]
    b = np.arange(128)[None, :, None]
    cc = np.arange(128)[None, None, :]
    th = 2.0 * np.pi * (((128 * a + b) * cc) % NFFT) / NFFT
    c["F1"] = np.ascontiguousarray(np.stack([np.cos(th), -np.sin(th)], axis=2)).astype(bf)
    bd = 2.0 * np.pi * ((np.arange(128)[:, None] * np.arange(128)[None, :]) % 128) / 128
    c["C2"] = np.stack([np.cos(bd), np.sin(bd), -np.sin(bd)], axis=1).astype(bf)
    cI = np.arange(128)[:, None, None]
    bI = np.arange(128)[None, :, None]
    for hh in range(2):
        a32 = (np.arange(32) + 32 * hh)[None, None, :]
        thi = 2.0 * np.pi * (((128 * a32 + bI) * cI) % NFFT) / NFFT
        c["FI%d" % hh] = np.ascontiguousarray(np.stack([np.cos(thi), -np.sin(thi)], axis=2)).astype(bf)
    return c


def _cols(v, n):
    return np.ascontiguousarray(np.asarray(v, np.float32).reshape(n, 128).T)


def _prep_core(inp, l, b, half, x_core, ctx_core):
    c = _consts()
    r = (lambda a, ax=0: np.flip(a, axis=ax)) if half else (lambda a, ax=0: a)
    d = {}
    d["x"] = np.ascontiguousarray(x_core, np.float32)
    d["ctx"] = np.ascontiguousarray(ctx_core, np.float32)
    d["cvec"] = np.ascontiguousarray(np.concatenate([_cols(inp["c"][b], 8), _cols(inp["c_ctx"], 8)], axis=1))
    d["w_mod"] = inp["w_mod"][l]
    d["bmod"] = _cols(inp["b_mod"][l], 48)
    d["gmix"] = _cols(inp["norm_mix_g"][l], 8)
    d["gffn"] = _cols(inp["norm_ffn_g"][l], 8)
    w_in = inp["w_in"][l]
    perm = np.array([(j + 4 * s) * 64 + dd for j in range(4) for s in range(2) for dd in range(64)])
    d["w_in"] = np.ascontiguousarray(np.concatenate([w_in[:, perm], w_in[:, 512:]], axis=1))
    qg = inp["q_norm_g"][l]
    kg = inp["k_norm_g"][l]
    d["qkg"] = np.ascontiguousarray(np.stack([np.tile(qg, 2), np.tile(kg, 2)], axis=1).astype(np.float32))
    d["qkg_row"] = np.ascontiguousarray(np.broadcast_to(np.concatenate([qg, kg])[None, :], (128, 128)).astype(np.float32))
    d["rope_cos"] = np.ascontiguousarray(r(c["rope_cos"], 1))
    d["rope_sin"] = np.ascontiguousarray(r(c["rope_sin"], 1))
    cw = inp["lru_conv_w"][l]
    w5 = np.zeros((5, 256), np.float32)
    if half:
        w5[1:5] = cw[::-1]
    else:
        w5[0:4] = cw
    d["lru_cw"] = np.ascontiguousarray(w5.T.reshape(2, 128, 5).transpose(1, 0, 2))
    d["lru_cb"] = _cols(inp["lru_conv_b"][l], 2)
    gw = inp["lru_gate_w"][l]
    gb = inp["lru_gate_b"][l]
    lam = inp["lru_lambda"][l]
    if half:
        gw, gb, lam = gw[::-1], gb[::-1], lam[::-1]
    wbd = np.zeros((128, 2, 2, 2, 128), np.float32)
    for dd in range(2):
        for g in range(2):
            for ch in range(2):
                wbd[0:64, dd, g, ch, 0:64] = gw[dd, g, 2 * ch]
                wbd[64:128, dd, g, ch, 64:128] = gw[dd, g, 2 * ch + 1]
    d["lru_wbd"] = wbd.reshape(128, 8, 128)
    d["lru_gb"] = np.ascontiguousarray(np.stack([_cols(gb[dd, g], 2) for dd in range(2) for g in range(2)], axis=1).reshape(128, 8))
    d["lru_lam"] = np.ascontiguousarray(np.stack([_cols(lam[dd], 2) for dd in range(2)], axis=1).reshape(128, 4))
    hw = inp["hy_conv_w"][l]
    if half:
        hw = hw[::-1]
    d["hy_cw"] = np.ascontiguousarray(hw.T.reshape(6, 128, 3).transpose(1, 0, 2))
    d["hy_cb"] = _cols(inp["hy_conv_b"][l], 6)
    d["hy_w1"] = inp["hy_w1"][l]
    d["hy_w2"] = inp["hy_w2"][l]
    d["hy_b12f"] = np.ascontiguousarray(np.stack([inp["hy_b1"][l], inp["hy_b2"][l], inp["hy_freq"][l]], axis=1))
    w3 = inp["hy_w3"][l]
    dec = inp["hy_decay"][l]
    wa, wb_, da, db = w3[:, :256], w3[:, 256:], dec[:256], dec[256:]
    if half:
        wa, wb_, da, db = wb_, wa, db, da
    d["hy_w3"] = np.ascontiguousarray(np.concatenate([wa, wb_, w3[:, :256]], axis=1))
    d["hy_dec"] = np.ascontiguousarray(np.concatenate([_cols(da, 2), _cols(db, 2)], axis=1))
    d["hy_bias"] = _cols(inp["hy_bias"][l], 2)
    d["out_g"] = _cols(inp["out_norm_g"][l], 8)
    d["w_out"] = inp["w_out"][l]
    d["router_w"] = inp["router_w"][l]
    d["router_b"] = np.ascontiguousarray(np.broadcast_to(inp["router_b"][l][None, :], (128, NE)).astype(np.float32))
    d["moe_w1"] = inp["moe_w1"][l]
    b1 = inp["moe_b1"][l]
    d["moe_b1"] = np.ascontiguousarray(np.concatenate(
        [b1[:, 0::2].reshape(NE, 8, 128).transpose(2, 0, 1), b1[:, 1::2].reshape(NE, 8, 128).transpose(2, 0, 1)], axis=2))
    d["moe_w2"] = inp["moe_w2"][l]
    d["moe_b2"] = inp["moe_b2"][l]
    for k in ("ident", "prot", "blk64", "sel", "zT", "t01", "zTc", "t01c", "F1", "C2", "FI0", "FI1"):
        d[k] = c[k]
    return d


IN_SPECS = [
    ("x", [S, D], F32), ("ctx", [LC, D], F32), ("cvec", [128, 16], F32), ("w_mod", [D, 6 * D], F32),
    ("bmod", [128, 48], F32), ("gmix", [128, 8], F32), ("gffn", [128, 8], F32), ("w_in", [D, 2048], F32),
    ("qkg", [128, 2], F32), ("qkg_row", [128, 128], F32), ("rope_cos", [128, S], F32), ("rope_sin", [128, S], F32),
    ("lru_cw", [128, 2, 5], F32), ("lru_cb", [128, 2], F32), ("lru_wbd", [128, 8, 128], F32), ("lru_gb", [128, 8], F32),
    ("lru_lam", [128, 4], F32), ("hy_cw", [128, 6, 3], F32), ("hy_cb", [128, 6], F32), ("hy_w1", [33, 64], F32),
    ("hy_w2", [64, 64], F32), ("hy_b12f", [64, 3], F32), ("hy_w3", [64, 768], F32), ("hy_dec", [128, 4], F32),
    ("hy_bias", [128, 2], F32), ("out_g", [128, 8], F32), ("w_out", [D, D], F32), ("router_w", [D, NE], F32),
    ("router_b", [128, NE], F32), ("moe_w1", [NE, D, 2 * D], F32), ("moe_b1", [128, NE, 16], F32),
    ("moe_w2", [NE, D, D], F32), ("moe_b2", [NE, D], F32),
    ("ident", [128, 128], F32), ("prot", [128, 128], F32), ("blk64", [128, 128], F32), ("sel", [65, 64], F32),
    ("zT", [33, S], F32), ("t01", [128, S], F32), ("zTc", [33, LC], F32), ("t01c", [128, LC], F32),
    ("F1", [64, 128, 2, 128], BF16), ("C2", [128, 3, 128], BF16), ("FI0", [128, 128, 2, 32], BF16), ("FI1", [128, 128, 2, 32], BF16),
]


SHARED = ("x", "ctx", "cvec", "rope_cos", "rope_sin", "ident", "prot", "blk64", "sel", "zT", "t01", "zTc", "t01c", "F1", "C2", "FI0", "FI1")


class LayerProg:
    def __init__(self, layers=((0, S, False), (1, SH, True)), stop_after=None, dbg=()):
        self.nc = nc = bass.Bass("TRN2", target_bir_lowering=False)
        self.m = MK(nc)
        specs = {n: (sh, dt) for n, sh, dt in IN_SPECS}
        prog = self

        class _Lazy(dict):
            def __missing__(d, n):
                base = n.split("@")[0]
                sh, dt = specs[base]
                d[n] = nc.dram_tensor(n.replace("@", "_L"), list(sh), dt, kind="ExternalInput").ap()
                return d[n]

        class _View:
            def __getitem__(v, n):
                return prog.Iall[n if n in SHARED else "%s@%d" % (n, prog.l)]
        self.Iall = _Lazy()
        self.I = _View()
        self.stop_after = stop_after
        self.dbg = set(dbg)
        self.out_names = []
        k = "ExternalOutput" if "scratch" in self.dbg else "Internal"
        sc = lambda n, sh, dt=F32: nc.dram_tensor(n, list(sh), dt, kind=k).ap()
        self.U = sc("sU", [10, 128, NALL])
        self.OT = sc("sOT", [D, NALL])
        self.HF = sc("sHF", [2, 128, NALL])
        self.VV = sc("sVV", [256, S])
        self.X0 = sc("sX0", [256, S])
        self.KF = sc("sKF", [512, S])
        self.XM = sc("sXM", [NALL, D])
        self.FT = sc("sFT", [128, 8, NALL], BF16)
        self.QTs = sc("sQT", [4, 128, S], BF16)
        self.VVb = sc("sVVb", [256, S], BF16)
        self.KFb = sc("sKFb", [512, S], BF16)
        self.X1 = sc("sX1", [S, D])
        self.C1 = sc("sC1", [LC, D])
        if k == "ExternalOutput":
            self.out_names += ["sU", "sOT", "sHF", "sVV", "sX0", "sKF", "sXM", "sFT", "sQT", "sX1", "sC1"]
        self.x_out = nc.dram_tensor("x_out", [SH, D], F32, kind="ExternalOutput").ap()
        self.out_names += ["x_out"]
        self.layers = list(layers)
        self.build()

    def build(self):
        m = self.m
        with Phase(m) as G:
            self.G = G
            self.setup_globals()
            steps = ["mod", "inproj_attn", "lru", "hyena", "merge", "moe"]
            done = False
            for li, (l, nown, last) in enumerate(self.layers):
                self.l, self.nown, self.last = l, nown, last
                self.ntok = nown + (0 if last else LC)
                self.xsrc = self.Iall["x"] if li == 0 else self.X1
                self.ctxsrc = self.Iall["ctx"] if li == 0 else self.C1
                for name in steps:
                    getattr(self, "phase_" + name)()
                    m.barrier()
                    if self.stop_after == "%s%d" % (name, l) or (name == "inproj_attn" and self.stop_after == "inproj%d" % l):
                        done = True
                        break
                if done:
                    break
            m.barrier()

    def setup_globals(self):
        m, G, I = self.m, self.G, self.I
        self.identf = G.sb([128, 128]); self.d_const = Dep()
        self.ident = G.sb([128, 128], BF16)
        self.onesf = G.sb([128, 128])
        self.epsT = G.sb([128, 1])
        m.dma(self.identf[:], I["ident"][:, :], writes=[self.d_const])
        m.op("dve", lambda e: e.tensor_copy(out=self.ident[:], in_=self.identf[:]), reads=[self.d_const], writes=[self.d_const])
        m.op("dve", lambda e: e.memset(self.onesf[:], 1.0), writes=[self.d_const])
        m.op("dve", lambda e: e.memset(self.epsT[:], EPS), writes=[self.d_const])
        self.modL = G.sb([128, 48]); self.modC = G.sb([128, 48]); self.d_mod = Dep()
        self.AB = G.sb([128, 4, 8])
        self.gbc = G.sb([128, 4, D])
        self.d_gbc = Dep()

    def phase_mod(self):
        m, I = self.m, self.I
        with Phase(m) as ph:
            cv = ph.sb([128, 16]); d_cv = Dep()
            m.dma(cv[:], I["cvec"][:, :], writes=[d_cv])
            sc = ph.sb([128, 8, 2]); d_sc = Dep()
            m.op("act", lambda e: e.activation(out=sc[:, :, 0], in_=cv[:, 0:8], func=AF.Silu), reads=[d_cv], writes=[d_sc])
            m.op("act", lambda e: e.activation(out=sc[:, :, 1], in_=cv[:, 8:16], func=AF.Silu), reads=[d_cv], writes=[d_sc])
            bm = ph.sb([128, 48]); gm = ph.sb([128, 2, 8]); d_bm = Dep()
            m.dma(bm[:], I["bmod"][:, :], writes=[d_bm])
            m.dma(gm[:, 0, :], I["gmix"][:, :], writes=[d_bm])
            m.dma(gm[:, 1, :], I["gffn"][:, :], writes=[d_bm])
            pm = ph.ps([128, 48, 2]); d_pm = Dep()
            wv = I["w_mod"].rearrange("(kc p) n -> p kc n", p=128)
            wb = [ph.sb([128, 8, 512]) for _ in range(2)]
            d_wb = [Dep(), Dep()]
            for nb in range(12):
                t, dw = wb[nb % 2], d_wb[nb % 2]
                m.dma(t[:], wv[:, :, nb * 512:(nb + 1) * 512], writes=[dw], q=("sp" if nb % 2 == 0 else "act"))
                for j in range(4):
                    col = nb * 4 + j
                    for kc in range(8):
                        m.op("pe", lambda e: e.matmul(pm[:, col, :], lhsT=t[:, kc, j * 128:(j + 1) * 128], rhs=sc[:, kc, :],
                                                      start=(kc == 0), stop=(kc == 7)),
                             reads=[dw, d_sc], writes=[d_pm], inc=(kc == 7))
            m.op("dve", lambda e: e.tensor_tensor(out=self.modL[:], in0=pm[:, :, 0], in1=bm[:], op=ALU.add), reads=[d_pm, d_bm], writes=[self.d_mod])
            m.op("dve", lambda e: e.tensor_tensor(out=self.modC[:], in0=pm[:, :, 1], in1=bm[:], op=ALU.add), reads=[d_pm, d_bm], writes=[self.d_mod])
            tmp = ph.sb([128, 8]); d_tmp = Dep()
            for i, (mt, c0, gi) in enumerate([(self.modL, 8, 0), (self.modL, 32, 1), (self.modC, 8, 0), (self.modC, 32, 1)]):
                m.op("dve", lambda e: e.tensor_scalar(out=tmp[:], in0=mt[:, c0:c0 + 8], scalar1=1.0, scalar2=None, op0=ALU.add),
                     reads=[self.d_mod], writes=[d_tmp])
                m.op("dve", lambda e: e.tensor_tensor(out=self.AB[:, i, :], in0=tmp[:], in1=gm[:, gi, :], op=ALU.mult),
                     reads=[d_tmp, d_bm], writes=[self.d_mod])
            dg = ph.sb([128, 8, 128]); d_dg = Dep()
            pb = ph.ps([128, D]); d_pb = Dep()
            for i, (mt, c0) in enumerate([(self.modL, 16), (self.modL, 40), (self.modC, 16), (self.modC, 40)]):
                for j in range(8):
                    m.op("dve", lambda e: e.tensor_scalar(out=dg[:, j, :], in0=self.identf[:], scalar1=mt[:, c0 + j:c0 + j + 1], scalar2=None,
                                                          op0=ALU.mult), reads=[self.d_mod, self.d_const], writes=[d_dg])
                for h in range(2):
                    m.op("pe", lambda e: e.matmul(pb[:, h * 512:(h + 1) * 512], lhsT=self.onesf[:], rhs=dg[:, 4 * h:4 * h + 4, :],
                                                  start=True, stop=True), reads=[d_dg, self.d_const], writes=[d_pb])
                m.op("act", lambda e: e.activation(out=self.gbc[:, i, :], in_=pb[:], func=AF.Identity), reads=[d_pb], writes=[self.d_gbc])
            if "mod" in self.dbg:
                o = self.nc.dram_tensor("dbg_mod", [128, 96], F32, kind="ExternalOutput").ap(); self.out_names.append("dbg_mod")
                m.dma(o[:, 0:48], self.modL[:], reads=[self.d_mod])
                m.dma(o[:, 48:96], self.modC[:], reads=[self.d_mod])
                o2 = self.nc.dram_tensor("dbg_gbc", [128, 4, D], F32, kind="ExternalOutput").ap(); self.out_names.append("dbg_gbc")
                m.dma(o2[:, :, :], self.gbc[:], reads=[self.d_gbc])

    def phase_inproj_attn(self):
        m, I = self.m, self.I
        with Phase(m) as P:
            KT = P.sb([128, NALL], BF16); d_KT = Dep()
            QT = None; d_QT = Dep()
            QC = P.sb([128, 4, LC], BF16); d_QC = Dep()
            VA = P.sb([128, 66, 2, 65], BF16); d_VA = Dep()
            negM = P.sb([128, 1]); d_negM = Dep()
            m.op("pool", lambda e: e.memset(VA[:, :, :, 64:65], 1.0), writes=[d_VA])
            self._inproj(KT, d_KT, QT, d_QT, QC, d_QC, VA, d_VA, negM, d_negM)
            m.barrier()
            if self.stop_after == "inproj%d" % self.l:
                return
            self._attention(KT, d_KT, QT, d_QT, QC, d_QC, VA, d_VA, negM, d_negM)

    def _inproj(self, KT, d_KT, QT, d_QT, QC, d_QC, VA, d_VA, negM, d_negM):
        m, I = self.m, self.I
        with Phase(m) as ph:
            w_in = ph.sb([128, 8, 2048], BF16); d_w = Dep()
            wv = I["w_in"].rearrange("(kc p) n -> p kc n", p=128)
            for kc in range(8):
                m.dma(w_in[:, kc, :], wv[:, kc, :], writes=[d_w], q="pool")
            cst = ph.sb([128, 2, 128], BF16); d_cst = Dep()
            m.dma(cst[:, 0, :], I["prot"][:, :], writes=[d_cst], q="pool")
            m.dma(cst[:, 1, :], I["blk64"][:, :], writes=[d_cst], q="pool")
            qkg = ph.sb([128, 2]); grow = ph.sb([128, 128])
            m.dma(qkg[:], I["qkg"][:, :], writes=[d_cst])
            m.dma(grow[:], I["qkg_row"][:, :], writes=[d_cst])
            mq = ph.sb([128, 2])
            m.op("dve", lambda e: e.tensor_reduce(out=mq[:, 0:1], in_=grow[:, 0:64], axis=AX.X, op=ALU.max, apply_absolute_value=True),
                 reads=[d_cst], writes=[d_negM])
            m.op("dve", lambda e: e.tensor_reduce(out=mq[:, 1:2], in_=grow[:, 64:128], axis=AX.X, op=ALU.max, apply_absolute_value=True),
                 reads=[d_cst], writes=[d_negM])
            m.op("dve", lambda e: e.tensor_scalar(out=negM[:], in0=mq[:, 0:1], scalar1=mq[:, 1:2], scalar2=-8.0, op0=ALU.mult, op1=ALU.mult),
                 reads=[d_negM], writes=[d_negM])
            xr = [ph.sb([128, D]) for _ in range(4)]; d_xr = [Dep() for _ in range(4)]
            xn = [ph.sb([128, D], BF16) for _ in range(8)]; d_xn = [Dep() for _ in range(8)]
            junk = ph.sb([128, D], BF16); d_junk = Dep()
            hT = [ph.sb([128, 8, 512], BF16) for _ in range(2)]; d_hT = [Dep(), Dep()]
            ss = [ph.sb([128, 4]) for _ in range(2)]; d_ss = [Dep(), Dep()]
            cs = [ph.sb([128, 2, 512]) for _ in range(2)]; d_cs = [Dep(), Dep()]
            stg = [ph.sb([128, 512]) for _ in range(4)]; d_stg = [Dep() for _ in range(4)]
            sq = [ph.sb([128, 512], BF16) for _ in range(2)]; d_sq = [Dep(), Dep()]
            f1 = [ph.sb([128, 512]) for _ in range(2)]; d_f1 = [Dep(), Dep()]
            f2 = [ph.sb([128, 512]) for _ in range(2)]; d_f2 = [Dep(), Dep()]
            qn = [ph.sb([128, 512], BF16) for _ in range(2)]; d_qn = [Dep(), Dep()]
            pT = ph.ps([128, 4, 512], BF16); d_pT = Dep()
            pr = [ph.ps([128, 512]) for _ in range(5)]; d_pr = [Dep() for _ in range(5)]
            pv = ph.ps([128, 4, 128]); d_pv = Dep()
            cnt = {"pr": 0, "stg": 0, "qk": 0, "x": 0, "xn": 0, "qst": 0}
            qst = [ph.sb([128, 512], BF16) for _ in range(3)]; d_qst = [Dep() for _ in range(3)]

            def nxt(key, n):
                i = cnt[key] % n
                cnt[key] += 1
                return i

            blocks = [("lat", i * 512, 512, i * 512 < self.nown) for i in range(16)] + [("ctx", 0, LC, not self.last)]
            for bi, (kind, t0, ntok, own) in enumerate(blocks):
                src = self.xsrc if kind == "lat" else self.ctxsrc
                ntile = ntok // 128
                mi = 0 if kind == "lat" else 2
                modt = self.modL if kind == "lat" else self.modC
                col0 = t0 if kind == "lat" else S
                h = hT[bi % 2]; dh = d_hT[bi % 2]
                s_ = ss[bi % 2]; ds_ = d_ss[bi % 2]
                xs, xns = [], []
                for tt in range(ntile):
                    xi = nxt("x", 4)
                    m.dma(xr[xi][:], src[t0 + tt * 128:t0 + (tt + 1) * 128, :], writes=[d_xr[xi]], q="sp")
                    m.op("act", lambda e: e.activation(out=junk[:], in_=xr[xi][:], func=AF.Square, accum_out=s_[:, tt:tt + 1]),
                         reads=[d_xr[xi]], writes=[d_junk, ds_])
                    xs.append(xi)
                m.op("dve", lambda e: e.tensor_scalar(out=s_[:, 0:ntile], in0=s_[:, 0:ntile], scalar1=1.0 / D, scalar2=EPS, op0=ALU.mult, op1=ALU.add),
                     reads=[ds_], writes=[ds_])
                m.op("act", lambda e: e.activation(out=s_[:, 0:ntile], in_=s_[:, 0:ntile], func=AF.Sqrt), reads=[ds_], writes=[ds_])
                m.op("dve", lambda e: e.reciprocal(out=s_[:, 0:ntile], in_=s_[:, 0:ntile]), reads=[ds_], writes=[ds_])
                for tt in range(ntile):
                    ni = nxt("xn", 8)
                    m.op("dve", lambda e: e.tensor_scalar(out=xn[ni][:], in0=xr[xs[tt]][:], scalar1=s_[:, tt:tt + 1], scalar2=None, op0=ALU.mult),
                         reads=[d_xr[xs[tt]], ds_], writes=[d_xn[ni]])
                    xns.append(ni)
                for hf in range(2):
                    for kcl in range(4):
                        kc = hf * 4 + kcl
                        for tt in range(ntile):
                            m.op("pe", lambda e: e.transpose(out=pT[:, kcl, tt * 128:(tt + 1) * 128], in_=xn[xns[tt]][:, kc * 128:(kc + 1) * 128],
                                                             identity=self.ident[:]),
                                 reads=[d_xn[xns[tt]], self.d_const], writes=[d_pT], inc=(kcl == 3 and tt == ntile - 1))
                    for kcl in range(4):
                        kc = hf * 4 + kcl
                        m.op("dve", lambda e: e.tensor_scalar(out=h[:, kc, 0:ntok], in0=pT[:, kcl, 0:ntok], scalar1=self.AB[:, mi, kc:kc + 1],
                                                              scalar2=modt[:, kc:kc + 1], op0=ALU.mult, op1=ALU.add),
                             reads=[d_pT, self.d_mod], writes=[dh])
                if "hT" in self.dbg and bi in (0, 16):
                    nm = f"dbg_hT{bi}"
                    o = self.nc.dram_tensor(nm, [128, 8, 512], BF16, kind="ExternalOutput").ap(); self.out_names.append(nm)
                    m.dma(o[:, :, 0:ntok], h[:, :, 0:ntok], reads=[dh])
                if kind == "lat":
                    ci = bi % 2
                    m.dma(cs[ci][:, 0, :], I["rope_cos"][:, t0:t0 + 512], writes=[d_cs[ci]], q="sp")
                    m.dma(cs[ci][:, 1, :], I["rope_sin"][:, t0:t0 + 512], writes=[d_cs[ci]], q="sp")

                def proj(c0):
                    pi = nxt("pr", 5)
                    for kc in range(8):
                        m.op("pe", lambda e: e.matmul(pr[pi][:, 0:ntok], lhsT=w_in[:, kc, c0:c0 + 128], rhs=h[:, kc, 0:ntok],
                                                      start=(kc == 0), stop=(kc == 7)),
                             reads=[d_w, dh], writes=[d_pr[pi]], inc=(kc == 7))
                    return pi

                def qk_post(pi, gcol, rope, out_ap, d_out):
                    k_ = nxt("qk", 2)
                    m.op("act", lambda e: e.activation(out=sq[k_][:, 0:ntok], in_=pr[pi][:, 0:ntok], func=AF.Square),
                         reads=[d_pr[pi]], writes=[d_sq[k_]])
                    p2 = nxt("pr", 5)
                    m.op("pe", lambda e: e.matmul(pr[p2][:, 0:ntok], lhsT=cst[:, 1, :], rhs=sq[k_][:, 0:ntok], start=True, stop=True),
                         reads=[d_cst, d_sq[k_]], writes=[d_pr[p2]])
                    m.op("act", lambda e: e.activation(out=f1[k_][:, 0:ntok], in_=pr[p2][:, 0:ntok], func=AF.Sqrt, bias=self.epsT[:, 0:1]),
                         reads=[d_pr[p2], self.d_const], writes=[d_f1[k_]])
                    m.op("dve", lambda e: e.reciprocal(out=f1[k_][:, 0:ntok], in_=f1[k_][:, 0:ntok]), reads=[d_f1[k_]], writes=[d_f1[k_]])
                    if not rope:
                        m.op("dve", lambda e: e.scalar_tensor_tensor(out=out_ap, in0=pr[pi][:, 0:ntok], scalar=qkg[:, gcol:gcol + 1],
                                                                     in1=f1[k_][:, 0:ntok], op0=ALU.mult, op1=ALU.mult),
                             reads=[d_pr[pi], d_f1[k_], d_cst], writes=[d_out])
                        return
                    m.op("dve", lambda e: e.scalar_tensor_tensor(out=qn[k_][:, 0:ntok], in0=pr[pi][:, 0:ntok], scalar=qkg[:, gcol:gcol + 1],
                                                                 in1=f1[k_][:, 0:ntok], op0=ALU.mult, op1=ALU.mult),
                         reads=[d_pr[pi], d_f1[k_], d_cst], writes=[d_qn[k_]])
                    p3 = nxt("pr", 5)
                    m.op("pe", lambda e: e.matmul(pr[p3][:, 0:ntok], lhsT=cst[:, 0, :], rhs=qn[k_][:, 0:ntok], start=True, stop=True),
                         reads=[d_cst, d_qn[k_]], writes=[d_pr[p3]])
                    ci = bi % 2
                    m.op("dve", lambda e: e.tensor_tensor(out=f1[k_][:, 0:ntok], in0=qn[k_][:, 0:ntok], in1=cs[ci][:, 0, 0:ntok], op=ALU.mult),
                         reads=[d_qn[k_], d_cs[ci]], writes=[d_f1[k_]])
                    m.op("dve", lambda e: e.tensor_tensor(out=f2[k_][:, 0:ntok], in0=pr[p3][:, 0:ntok], in1=cs[ci][:, 1, 0:ntok], op=ALU.mult),
                         reads=[d_pr[p3], d_cs[ci]], writes=[d_f2[k_]])
                    m.op("pool", lambda e: e.tensor_tensor(out=out_ap, in0=f1[k_][:, 0:ntok], in1=f2[k_][:, 0:ntok], op=ALU.add),
                         reads=[d_f1[k_], d_f2[k_]], writes=[d_out])

                if own:
                    for j in range(4):
                        pi = proj(j * 128)
                        if kind == "lat":
                            qs = nxt("qst", 3)
                            qk_post(pi, 0, True, qst[qs][:], d_qst[qs])
                            m.dma(self.QTs[j, :, t0:t0 + 512], qst[qs][:], reads=[d_qst[qs]], q="act")
                        else:
                            qk_post(pi, 0, False, QC[:, j, :], d_QC)
                pi = proj(512)
                qk_post(pi, 1, kind == "lat", KT[:, col0:col0 + ntok], d_KT)
                for tt in range(ntile):
                    for kc in range(8):
                        m.op("pe", lambda e: e.matmul(pv[:, tt, :], lhsT=h[:, kc, tt * 128:(tt + 1) * 128], rhs=w_in[:, kc, 640:768],
                                                      start=(kc == 0), stop=(kc == 7)),
                             reads=[dh, d_w], writes=[d_pv], inc=(kc == 7))
                kt0 = col0 // 128
                m.op("act", lambda e: e.activation(out=VA[:, kt0:kt0 + ntile, :, 0:64],
                                                   in_=pv[:, 0:ntile, :].rearrange("p t (h d) -> p t h d", h=2), func=AF.Identity),
                     reads=[d_pv], writes=[d_VA])
                for ci_ in range(10):
                    pi = proj(768 + ci_ * 128)
                    si = nxt("stg", 4)
                    m.op("act", lambda e: e.activation(out=stg[si][:, 0:ntok], in_=pr[pi][:, 0:ntok], func=AF.Identity),
                         reads=[d_pr[pi]], writes=[d_stg[si]])
                    m.dma(self.U[ci_, :, col0:col0 + ntok], stg[si][:, 0:ntok], reads=[d_stg[si]], q="act")

    def _attention(self, KT, d_KT, QT, d_QT, QC, d_QC, VA, d_VA, negM, d_negM):
        m, I = self.m, self.I
        with Phase(m) as ph:
            self_f = ph.sb([65, 64]); d_sel = Dep()
            m.dma(self_f[:], I["sel"][:, :], writes=[d_sel])
            pS = [[ph.ps([128, 512]) for _ in range(2)] for _ in range(3)]
            d_pS = [[Dep(), Dep()] for _ in range(3)]
            pO = [ph.ps([65, 512]) for _ in range(2)]; d_pO = [Dep(), Dep()]
            Pt = [[ph.sb([128, 512], BF16) for _ in range(2)] for _ in range(3)]
            d_Pt = [[Dep(), Dep()] for _ in range(3)]
            osb = [ph.sb([65, 512]) for _ in range(2)]; d_osb = [Dep(), Dep()]
            att = [ph.sb([64, 512]) for _ in range(4)]; d_att = [Dep() for _ in range(4)]
            acnt = [0]

            qtl = [ph.sb([128, 512], BF16) for _ in range(3)]; d_qtl = [Dep() for _ in range(3)]
            qcnt = [0]

            def run(qsrc, d_q, j, q0, nq, keys, out_col0):
                nk = len(keys)

                def issue_S(i):
                    kc0, _ = keys[i]
                    for hb in range(2):
                        lo = hb * 64
                        m.op("pe", lambda e: e.matmul(pS[i % 3][hb][:, 0:nq], lhsT=KT[lo:lo + 64, kc0:kc0 + 128],
                                                      rhs=qsrc[lo:lo + 64, 0:nq], start=True, stop=True),
                             reads=[d_KT, d_q], writes=[d_pS[i % 3][hb]])
                issue_S(0)
                if nk > 1:
                    issue_S(1)
                for i in range(nk):
                    if i + 2 < nk:
                        issue_S(i + 2)
                    _, vt = keys[i]
                    for hb in range(2):
                        P_ = Pt[i % 3][hb]; dP = d_Pt[i % 3][hb]
                        m.op("act", lambda e: e.activation(out=P_[:, 0:nq], in_=pS[i % 3][hb][:, 0:nq], func=AF.Exp, scale=0.125, bias=negM[:, 0:1]),
                             reads=[d_pS[i % 3][hb], d_negM], writes=[dP])
                        m.op("pe", lambda e: e.matmul(pO[hb][:, 0:nq], lhsT=VA[:, vt, hb, :], rhs=P_[:, 0:nq], start=(i == 0), stop=(i == nk - 1)),
                             reads=[d_VA, dP], writes=[d_pO[hb]], inc=(i == nk - 1))
                for hb in range(2):
                    head = j + 4 * hb
                    o_ = osb[hb]; do = d_osb[hb]
                    m.op("act", lambda e: e.activation(out=o_[:, 0:nq], in_=pO[hb][:, 0:nq], func=AF.Identity), reads=[d_pO[hb]], writes=[do])
                    m.op("dve", lambda e: e.reciprocal(out=o_[64:65, 0:nq], in_=o_[64:65, 0:nq]), reads=[do], writes=[do])
                    pB = pS[nk % 3][hb]; d_pB = d_pS[nk % 3][hb]
                    m.op("pe", lambda e: e.matmul(pB[0:64, 0:nq], lhsT=self_f[:], rhs=o_[:, 0:nq], start=True, stop=True),
                         reads=[do, d_sel], writes=[d_pB])
                    ai = acnt[0] % 4; acnt[0] += 1
                    m.op("dve", lambda e: e.tensor_tensor(out=att[ai][:, 0:nq], in0=o_[0:64, 0:nq], in1=pB[0:64, 0:nq], op=ALU.mult),
                         reads=[do, d_pB], writes=[d_att[ai]])
                    m.dma(self.OT[head * 64:(head + 1) * 64, out_col0:out_col0 + nq], att[ai][:, 0:nq], reads=[d_att[ai]], q="sp")

            all_keys = [(kt * 128, kt) for kt in range(66)]
            ctx_keys = [(S + i * 128, 64 + i) for i in range(2)]
            nqb = self.nown // 512 if "att_short" not in self.dbg else 1
            for qb in range(nqb):
                for j in range(4):
                    qi = qcnt[0] % 3; qcnt[0] += 1
                    m.dma(qtl[qi][:], self.QTs[j, :, qb * 512:(qb + 1) * 512], writes=[d_qtl[qi]], q="sp")
                    run(qtl[qi], d_qtl[qi], j, qb * 512, 512, all_keys, qb * 512)
            if not self.last:
                for j in range(4):
                    run(QC[:, j, :], d_QC, j, 0, LC, ctx_keys, self.nown)

    def phase_lru(self):
        m, I = self.m, self.I
        SEG = 2048
        with Phase(m) as ph:
            d_c = Dep()
            cw = ph.sb([128, 2, 5]); cb = ph.sb([128, 2]); gb = ph.sb([128, 8]); lam = ph.sb([128, 4]); cvec = ph.sb([128, 4])
            wbd = ph.sb([128, 8, 128], BF16)
            m.dma(cw[:], I["lru_cw"][:, :, :], writes=[d_c]); m.dma(cb[:], I["lru_cb"][:, :], writes=[d_c])
            m.dma(gb[:], I["lru_gb"][:, :], writes=[d_c]); m.dma(lam[:], I["lru_lam"][:, :], writes=[d_c])
            m.dma(wbd[:], I["lru_wbd"][:, :, :], writes=[d_c], q="pool")
            m.op("act", lambda e: e.activation(out=cvec[:], in_=lam[:], func=AF.Exp, scale=-1.0), reads=[d_c], writes=[d_c])
            m.op("act", lambda e: e.activation(out=cvec[:], in_=cvec[:], func=AF.Ln, bias=1.0), reads=[d_c], writes=[d_c])
            m.op("dve", lambda e: e.tensor_scalar(out=cvec[:], in0=cvec[:], scalar1=-8.0, scalar2=None, op0=ALU.mult), reads=[d_c], writes=[d_c])
            lxp = [ph.sb([128, SEG + 4]) for _ in range(2)]; d_lxp = [Dep(), Dep()]
            xl = ph.sb([128, SEG]); d_xl = Dep()
            xlb = ph.sb([128, SEG], BF16); d_xlb = Dep()
            aS = ph.sb([128, SEG]); d_aS = Dep()
            bS = ph.sb([128, SEG]); d_bS = Dep()
            hS = [ph.sb([128, SEG]) for _ in range(2)]; d_hS = [Dep(), Dep()]
            hfS = [ph.sb([128, SEG]) for _ in range(2)]; d_hfS = [Dep(), Dep()]
            lgS = [ph.sb([128, SEG]) for _ in range(2)]; d_lgS = [Dep(), Dep()]
            g1 = ph.sb([128, SEG]); d_g1 = Dep()
            g2 = ph.sb([128, SEG]); d_g2 = Dep()
            tR = [ph.sb([128, 512]) for _ in range(2)]; d_tR = [Dep(), Dep()]
            tI = [ph.sb([128, 512]) for _ in range(2)]; d_tI = [Dep(), Dep()]
            tA = [ph.sb([128, 512]) for _ in range(2)]; d_tA = [Dep(), Dep()]
            pG = [[ph.ps([128, 512]) for _ in range(2)] for _ in range(2)]; d_pG = [[Dep(), Dep()], [Dep(), Dep()]]
            cr = ph.sb([128, 1]); d_cr = Dep()
            segs_f = [("ctx", 0, LC)] + [("lat", i * SEG, SEG) for i in range(S // SEG)]
            segs_b = [("ctx", 0, LC)] + [("lat", i * SEG, SEG) for i in reversed(range(S // SEG))]
            sc = 0
            for dirn in range(2):
                for c in range(2):
                    m.op("dve", lambda e: e.memset(cr[:], 0.0), writes=[d_cr])
                    for (kind, t0, ln) in (segs_f if dirn == 0 else segs_b):
                        seqlen = S if kind == "lat" else LC
                        base = 0 if kind == "lat" else S
                        bi = sc % 2; sc += 1
                        lp = lxp[bi]; dlp = d_lxp[bi]
                        lo = max(t0 - 2, 0); hi = min(t0 + ln + 2, seqlen)
                        if t0 - 2 < 0:
                            m.op("pool", lambda e: e.memset(lp[:, 0:2], 0.0), writes=[dlp])
                        if t0 + ln + 2 > seqlen:
                            m.op("pool", lambda e: e.memset(lp[:, ln + 2:ln + 4], 0.0), writes=[dlp])
                        m.dma(lp[:, lo - (t0 - 2):hi - (t0 - 2)], self.U[c, :, base + lo:base + hi], writes=[dlp], q="sp")
                        own = (kind == "ctx" and not self.last) or (kind == "lat" and t0 < self.nown)
                        if dirn == 1:
                            m.dma(hfS[bi][:, 0:ln], self.HF[c, :, base + t0:base + t0 + ln], writes=[d_hfS[bi]], q="sp")
                            if own:
                                m.dma(lgS[bi][:, 0:ln], self.U[2 + c, :, base + t0:base + t0 + ln], writes=[d_lgS[bi]], q="sp")
                        m.op("dve", lambda e: e.tensor_scalar(out=xl[:, 0:ln], in0=lp[:, 0:ln], scalar1=cw[:, c, 0:1], scalar2=cb[:, c:c + 1],
                                                              op0=ALU.mult, op1=ALU.add), reads=[dlp, d_c], writes=[d_xl])
                        for o in range(1, 5):
                            m.op("dve", lambda e: e.scalar_tensor_tensor(out=xl[:, 0:ln], in0=lp[:, o:o + ln], scalar=cw[:, c, o:o + 1], in1=xl[:, 0:ln],
                                                                         op0=ALU.mult, op1=ALU.add), reads=[dlp, d_c, d_xl], writes=[d_xl])
                        m.op("act", lambda e: e.activation(out=xlb[:, 0:ln], in_=xl[:, 0:ln], func=AF.Identity), reads=[d_xl], writes=[d_xlb])
                        for sb_ in range((ln + 511) // 512):
                            s0 = sb_ * 512; n = min(512, ln - s0); k_ = sb_ % 2
                            for g in range(2):
                                m.op("pe", lambda e: e.matmul(pG[k_][g][:, 0:n], lhsT=wbd[:, dirn * 4 + g * 2 + c, :], rhs=xlb[:, s0:s0 + n],
                                                              start=True, stop=True), reads=[d_c, d_xlb], writes=[d_pG[k_][g]])
                            gi = dirn * 4 + c
                            m.op("act", lambda e: e.activation(out=tR[k_][:, 0:n], in_=pG[k_][0][:, 0:n], func=AF.Sigmoid, bias=gb[:, gi:gi + 1]),
                                 reads=[d_pG[k_][0], d_c], writes=[d_tR[k_]])
                            m.op("act", lambda e: e.activation(out=tI[k_][:, 0:n], in_=pG[k_][1][:, 0:n], func=AF.Sigmoid, bias=gb[:, gi + 2:gi + 3]),
                                 reads=[d_pG[k_][1], d_c], writes=[d_tI[k_]])
                            m.op("act", lambda e: e.activation(out=aS[:, s0:s0 + n], in_=tR[k_][:, 0:n], func=AF.Exp, scale=cvec[:, dirn * 2 + c:dirn * 2 + c + 1]),
                                 reads=[d_tR[k_], d_c], writes=[d_aS])
                            m.op("pool", lambda e: e.tensor_tensor(out=tA[k_][:, 0:n], in0=aS[:, s0:s0 + n], in1=aS[:, s0:s0 + n], op=ALU.mult),
                                 reads=[d_aS], writes=[d_tA[k_]])
                            m.op("pool", lambda e: e.tensor_scalar(out=tA[k_][:, 0:n], in0=tA[k_][:, 0:n], scalar1=-1.0, scalar2=1.0, op0=ALU.mult, op1=ALU.add),
                                 reads=[d_tA[k_]], writes=[d_tA[k_]])
                            m.op("act", lambda e: e.activation(out=tA[k_][:, 0:n], in_=tA[k_][:, 0:n], func=AF.Sqrt), reads=[d_tA[k_]], writes=[d_tA[k_]])
                            m.op("pool", lambda e: e.tensor_tensor(out=tI[k_][:, 0:n], in0=tI[k_][:, 0:n], in1=xl[:, s0:s0 + n], op=ALU.mult),
                                 reads=[d_tI[k_], d_xl], writes=[d_tI[k_]])
                            m.op("dve", lambda e: e.tensor_tensor(out=bS[:, s0:s0 + n], in0=tA[k_][:, 0:n], in1=tI[k_][:, 0:n], op=ALU.mult),
                                 reads=[d_tA[k_], d_tI[k_]], writes=[d_bS])
                        h_ = hS[bi]; dh = d_hS[bi]
                        if dirn == 0:
                            m.op("dve", lambda e: e.tensor_tensor_scan(out=h_[:, 0:ln], data0=aS[:, 0:ln], data1=bS[:, 0:ln], initial=cr[:, 0:1],
                                                                       op0=ALU.mult, op1=ALU.add), reads=[d_aS, d_bS, d_cr], writes=[dh])
                            m.op("dve", lambda e: e.tensor_copy(out=cr[:], in_=h_[:, ln - 1:ln]), reads=[dh], writes=[d_cr])
                            m.dma(self.HF[c, :, base + t0:base + t0 + ln], h_[:, 0:ln], reads=[dh], q="act")
                        else:
                            m.op("dve", lambda e: e.tensor_tensor_scan(out=h_[:, ln - 1::-1] if ln == SEG else h_[:, ln - 1::-1],
                                                                       data0=aS[:, ln - 1::-1], data1=bS[:, ln - 1::-1], initial=cr[:, 0:1],
                                                                       op0=ALU.mult, op1=ALU.add), reads=[d_aS, d_bS, d_cr], writes=[dh])
                            m.op("dve", lambda e: e.tensor_copy(out=cr[:], in_=h_[:, 0:1]), reads=[dh], writes=[d_cr])
                            if own:
                                lg_ = lgS[bi]; dlg = d_lgS[bi]
                                m.op("pool", lambda e: e.tensor_tensor(out=h_[:, 0:ln], in0=h_[:, 0:ln], in1=hfS[bi][:, 0:ln], op=ALU.add),
                                     reads=[dh, d_hfS[bi]], writes=[dh])
                                m.op("pool", lambda e: e.tensor_tensor(out=g1[:, 0:ln], in0=lg_[:, 0:ln], in1=lg_[:, 0:ln], op=ALU.mult),
                                     reads=[dlg], writes=[d_g1])
                                m.op("pool", lambda e: e.tensor_scalar(out=g1[:, 0:ln], in0=g1[:, 0:ln], scalar1=0.044715, scalar2=1.0, op0=ALU.mult, op1=ALU.add),
                                     reads=[d_g1], writes=[d_g1])
                                m.op("pool", lambda e: e.tensor_tensor(out=g1[:, 0:ln], in0=g1[:, 0:ln], in1=lg_[:, 0:ln], op=ALU.mult),
                                     reads=[d_g1, dlg], writes=[d_g1])
                                m.op("act", lambda e: e.activation(out=g1[:, 0:ln], in_=g1[:, 0:ln], func=AF.Sigmoid, scale=1.5957691216057308),
                                     reads=[d_g1], writes=[d_g1])
                                m.op("dve", lambda e: e.tensor_tensor(out=g1[:, 0:ln], in0=g1[:, 0:ln], in1=lg_[:, 0:ln], op=ALU.mult),
                                     reads=[d_g1, dlg], writes=[d_g1])
                                m.op("dve", lambda e: e.tensor_tensor(out=g2[:, 0:ln], in0=g1[:, 0:ln], in1=h_[:, 0:ln], op=ALU.mult),
                                     reads=[d_g1, dh], writes=[d_g2])
                                oc = t0 if kind == "lat" else self.nown
                                m.dma(self.OT[512 + c * 128:512 + (c + 1) * 128, oc:oc + ln], g2[:, 0:ln], reads=[d_g2], q="act")
                    m.barrier()

    def phase_hyena(self):
        self._hy_filters_and_conv()
        self.m.barrier()
        self._hy_fft()

    def _sin9(self, m, ph, out, psum, n, sc, bi, d_in, d_out, tmp, d_tmp, d_c):
        m.op("act", lambda e: e.activation(out=out, in_=psum, func=AF.Sin, scale=sc, bias=bi), reads=[d_in, d_c], writes=[d_out])
        for _ in range(2):
            m.op("dve", lambda e: e.tensor_tensor(out=tmp, in0=out, in1=out, op=ALU.mult), reads=[d_out], writes=[d_tmp])
            m.op("dve", lambda e: e.tensor_scalar(out=tmp, in0=tmp, scalar1=-4.0, scalar2=3.0, op0=ALU.mult, op1=ALU.add), reads=[d_tmp], writes=[d_tmp])
            m.op("dve", lambda e: e.tensor_tensor(out=out, in0=out, in1=tmp, op=ALU.mult), reads=[d_out, d_tmp], writes=[d_out])

    def _hy_filters_and_conv(self):
        m, I = self.m, self.I
        G = self.G
        self.hyP = Phase(m); P = self.hyP; P.__enter__()
        self.kc_f = P.sb([128, 4, LC]); self.d_kc = Dep()
        self.nrm = P.sb([128, 2, 4]); self.d_nrm = Dep()
        self.vvc = P.sb([128, 2, LC]); self.x0c = P.sb([128, 2, LC]); self.d_vvc = Dep()
        self.hsc = P.sb([128, 2, 2]); self.d_hsc = Dep()
        with Phase(m) as ph:
            d_c = Dep()
            w1 = ph.sb([33, 64]); w2 = ph.sb([64, 64]); w3 = ph.sb([64, 768]); b12f = ph.sb([64, 3]); dec = ph.sb([128, 4])
            for t_, n_ in ((w1, "hy_w1"), (w2, "hy_w2"), (w3, "hy_w3"), (b12f, "hy_b12f"), (dec, "hy_dec")):
                m.dma(t_[:], I[n_][:, :], writes=[d_c])
            scb = ph.sb([64, 3])
            m.op("dve", lambda e: e.tensor_scalar(out=scb[:, 0:1], in0=b12f[:, 2:3], scalar1=1.0 / 9.0, scalar2=None, op0=ALU.mult), reads=[d_c], writes=[d_c])
            m.op("dve", lambda e: e.tensor_scalar(out=scb[:, 1:3], in0=b12f[:, 0:2], scalar1=scb[:, 0:1], scalar2=None, op0=ALU.mult), reads=[d_c], writes=[d_c])
            nad = ph.sb([128, 4])
            m.op("dve", lambda e: e.tensor_scalar(out=nad[:], in0=dec[:], scalar1=-1.0, scalar2=None, op0=ALU.mult), reads=[d_c], writes=[d_c])
            m.op("dve", lambda e: e.tensor_tensor(out=nad[:], in0=nad[:], in1=dec[:], op=ALU.min), reads=[d_c], writes=[d_c])
            m.op("dve", lambda e: e.memset(self.nrm[:], 0.0), writes=[self.d_nrm])
            zb = [ph.sb([33, 512]) for _ in range(2)]; d_zb = [Dep(), Dep()]
            tb = [ph.sb([128, 512]) for _ in range(2)]; d_tb = [Dep(), Dep()]
            h1 = ph.sb([64, 512]); d_h1 = Dep()
            h2 = ph.sb([64, 512]); d_h2 = Dep()
            tmp = ph.sb([64, 512]); d_tmp = Dep()
            ex = [ph.sb([128, 512]) for _ in range(2)]; d_ex = [Dep(), Dep()]
            kk = [ph.sb([128, 512], BF16) for _ in range(3)]; d_kk = [Dep() for _ in range(3)]
            red = ph.sb([128, 1]); d_red = Dep()
            c0t = ph.sb([128, 2, 2]); d_c0 = Dep()
            p1 = ph.ps([64, 512]); d_p1 = Dep()
            p2 = ph.ps([64, 512]); d_p2 = Dep()
            p3 = [ph.ps([128, 512]) for _ in range(2)]; d_p3 = [Dep(), Dep()]
            pc = ph.ps([128, 2, 2]); d_pc = Dep()
            kc_i = 0
            for which, (zname, tname, L) in enumerate((("zT", "t01", S), ("zTc", "t01c", LC))[:(1 if self.last else 2)]):
                for bi_ in range((L + 511) // 512):
                    c0_ = bi_ * 512; n = min(512, L - c0_); r = bi_ % 2
                    m.dma(zb[r][:, 0:n], I[zname][:, c0_:c0_ + n], writes=[d_zb[r]])
                    m.dma(tb[r][:, 0:n], I[tname][:, c0_:c0_ + n], writes=[d_tb[r]])
                    m.op("pe", lambda e: e.matmul(p1[:, 0:n], lhsT=w1[:], rhs=zb[r][:, 0:n], start=True, stop=True), reads=[d_c, d_zb[r]], writes=[d_p1])
                    self._sin9(m, ph, h1[:, 0:n], p1[:, 0:n], n, scb[:, 0:1], scb[:, 1:2], d_p1, d_h1, tmp[:, 0:n], d_tmp, d_c)
                    m.op("pe", lambda e: e.matmul(p2[:, 0:n], lhsT=w2[:], rhs=h1[:, 0:n], start=True, stop=True), reads=[d_c, d_h1], writes=[d_p2])
                    self._sin9(m, ph, h2[:, 0:n], p2[:, 0:n], n, scb[:, 0:1], scb[:, 2:3], d_p2, d_h2, tmp[:, 0:n], d_tmp, d_c)
                    if bi_ == 0:
                        for c in range(2):
                            m.op("pe", lambda e: e.matmul(pc[:, c, :], lhsT=w3[:, 512 + c * 128:512 + (c + 1) * 128], rhs=h2[:, 0:2], start=True, stop=True),
                                 reads=[d_c, d_h2], writes=[d_pc])
                        m.op("act", lambda e: e.activation(out=c0t[:], in_=pc[:], func=AF.Identity), reads=[d_pc], writes=[d_c0])
                    for ci in range(4):
                        pi = ci % 2
                        m.op("pe", lambda e: e.matmul(p3[pi][:, 0:n], lhsT=w3[:, ci * 128:(ci + 1) * 128], rhs=h2[:, 0:n], start=True, stop=True),
                             reads=[d_c, d_h2], writes=[d_p3[pi]])
                        m.op("act", lambda e: e.activation(out=ex[pi][:, 0:n], in_=tb[r][:, 0:n], func=AF.Exp, scale=nad[:, ci:ci + 1]),
                             reads=[d_tb[r], d_c], writes=[d_ex[pi]])
                        if which == 0:
                            ki = kc_i % 3; kc_i += 1
                            kt_ = kk[ki][:, 0:n]; dk = d_kk[ki]
                        else:
                            kt_ = self.kc_f[:, ci, :]; dk = self.d_kc
                        m.op("dve", lambda e: e.tensor_tensor(out=kt_, in0=p3[pi][:, 0:n], in1=ex[pi][:, 0:n], op=ALU.mult),
                             reads=[d_p3[pi], d_ex[pi]], writes=[dk])
                        if bi_ == 0:
                            if ci < 2:
                                m.op("dve", lambda e: e.tensor_copy(out=kt_[:, 0:1], in_=c0t[:, ci, 0:1]), reads=[d_c0, dk], writes=[dk])
                            else:
                                m.op("dve", lambda e: e.memset(kt_[:, 0:1], 0.0), reads=[dk], writes=[dk])
                        m.op("dve", lambda e: e.tensor_reduce(out=red[:], in_=kt_, axis=AX.X, op=ALU.add, apply_absolute_value=True),
                             reads=[dk], writes=[d_red])
                        m.op("dve", lambda e: e.tensor_tensor(out=self.nrm[:, which, ci:ci + 1], in0=self.nrm[:, which, ci:ci + 1], in1=red[:], op=ALU.add),
                             reads=[d_red, self.d_nrm], writes=[self.d_nrm])
                        if which == 0:
                            m.dma(self.KFb[ci * 128:(ci + 1) * 128, c0_:c0_ + n], kt_, reads=[dk], q="act")
            m.op("dve", lambda e: e.tensor_tensor(out=self.hsc[:], in0=self.nrm[:, :, 0:2], in1=self.nrm[:, :, 2:4], op=ALU.add),
                 reads=[self.d_nrm], writes=[self.d_hsc])
            m.op("dve", lambda e: e.tensor_scalar(out=self.hsc[:, 0, :], in0=self.hsc[:, 0, :], scalar1=float(NFFT), scalar2=None, op0=ALU.mult),
                 reads=[self.d_hsc], writes=[self.d_hsc])
            m.op("dve", lambda e: e.reciprocal(out=self.hsc[:], in_=self.hsc[:]), reads=[self.d_hsc], writes=[self.d_hsc])
        with Phase(m) as ph:
            SEG = 2048
            d_c = Dep()
            cw = ph.sb([128, 6, 3]); cb = ph.sb([128, 6])
            m.dma(cw[:], I["hy_cw"][:, :, :], writes=[d_c]); m.dma(cb[:], I["hy_cb"][:, :], writes=[d_c])
            pad = [[ph.sb([128, SEG + 2]) for _ in range(3)] for _ in range(2)]; d_pad = [[Dep() for _ in range(3)] for _ in range(2)]
            uc = [ph.sb([128, SEG]) for _ in range(3)]; d_uc = [Dep() for _ in range(3)]
            vv = [ph.sb([128, SEG]) for _ in range(2)]; d_vv = [Dep(), Dep()]
            vvb = [ph.sb([128, SEG], BF16) for _ in range(2)]; d_vvb = [Dep(), Dep()]
            sc = 0
            segs = [("lat", i * SEG, SEG) for i in range(S // SEG)] + ([] if self.last else [("ctx", 0, LC)])
            for (kind, t0, ln) in segs:
                seqlen = S if kind == "lat" else LC
                base = 0 if kind == "lat" else S
                for c in range(2):
                    bi = sc % 2; sc += 1
                    for k3 in range(3):
                        ci = 2 * k3 + c
                        lp = pad[bi][k3]; dlp = d_pad[bi][k3]
                        lo = max(t0 - 1, 0); hi = min(t0 + ln + 1, seqlen)
                        if t0 - 1 < 0:
                            m.op("pool", lambda e: e.memset(lp[:, 0:1], 0.0), writes=[dlp])
                        if t0 + ln + 1 > seqlen:
                            m.op("pool", lambda e: e.memset(lp[:, ln + 1:ln + 2], 0.0), writes=[dlp])
                        m.dma(lp[:, lo - (t0 - 1):hi - (t0 - 1)], self.U[4 + ci, :, base + lo:base + hi], writes=[dlp], q="sp")
                        eng = "dve" if k3 != 0 else "pool"
                        u_ = uc[k3]; du = d_uc[k3]
                        if kind == "ctx" and k3 == 0:
                            u_ = self.x0c[:, c, :]
                            du = self.d_vvc
                        else:
                            u_ = u_[:, 0:ln]
                        m.op("dve", lambda e: e.tensor_scalar(out=u_, in0=lp[:, 0:ln], scalar1=cw[:, ci, 0:1], scalar2=cb[:, ci:ci + 1],
                                                              op0=ALU.mult, op1=ALU.add), reads=[dlp, d_c], writes=[du])
                        for o in range(1, 3):
                            m.op("dve", lambda e: e.scalar_tensor_tensor(out=u_, in0=lp[:, o:o + ln], scalar=cw[:, ci, o:o + 1], in1=u_,
                                                                         op0=ALU.mult, op1=ALU.add), reads=[dlp, d_c, du], writes=[du])
                    if kind == "lat":
                        m.op("pool", lambda e: e.tensor_tensor(out=vv[bi][:, 0:ln], in0=uc[1][:, 0:ln], in1=uc[2][:, 0:ln], op=ALU.mult),
                             reads=[d_uc[1], d_uc[2]], writes=[d_vv[bi]])
                        m.dma(self.VV[c * 128:(c + 1) * 128, t0:t0 + ln], vv[bi][:, 0:ln], reads=[d_vv[bi]], q="act")
                        m.op("act", lambda e: e.activation(out=vvb[bi][:, 0:ln], in_=vv[bi][:, 0:ln], func=AF.Identity), reads=[d_vv[bi]], writes=[d_vvb[bi]])
                        m.dma(self.VVb[c * 128:(c + 1) * 128, t0:t0 + ln], vvb[bi][:, 0:ln], reads=[d_vvb[bi]], q="act")
                        if t0 < self.nown:
                            m.dma(self.X0[c * 128:(c + 1) * 128, t0:t0 + ln], uc[0][:, 0:ln], reads=[d_uc[0]], q="act")
                    else:
                        m.op("pool", lambda e: e.tensor_tensor(out=self.vvc[:, c, :], in0=uc[1][:, 0:ln], in1=uc[2][:, 0:ln], op=ALU.mult),
                             reads=[d_uc[1], d_uc[2]], writes=[self.d_vvc])
            yc = ph.sb([128, 2, LC]); d_yc = Dep()
            hb = ph.sb([128, 2]); m.dma(hb[:], I["hy_bias"][:, :], writes=[d_c])
            for c in range(0 if self.last else 2):
                y_ = yc[:, c, :]; v_ = self.vvc[:, c, :]
                m.op("dve", lambda e: e.tensor_scalar(out=y_, in0=v_, scalar1=self.kc_f[:, c, 0:1], scalar2=None, op0=ALU.mult),
                     reads=[self.d_vvc, self.d_kc], writes=[d_yc])
                for dl in range(1, LC):
                    m.op("dve", lambda e: e.scalar_tensor_tensor(out=y_[:, dl:], in0=v_[:, 0:LC - dl], scalar=self.kc_f[:, c, dl:dl + 1], in1=y_[:, dl:],
                                                                 op0=ALU.mult, op1=ALU.add), reads=[d_yc], writes=[d_yc])
                    m.op("dve", lambda e: e.scalar_tensor_tensor(out=y_[:, 0:LC - dl], in0=v_[:, dl:], scalar=self.kc_f[:, 2 + c, dl:dl + 1], in1=y_[:, 0:LC - dl],
                                                                 op0=ALU.mult, op1=ALU.add), reads=[d_yc], writes=[d_yc])
                m.op("dve", lambda e: e.tensor_scalar(out=y_, in0=y_, scalar1=self.hsc[:, 1, c:c + 1], scalar2=None, op0=ALU.mult),
                     reads=[d_yc, self.d_hsc], writes=[d_yc])
                m.op("dve", lambda e: e.scalar_tensor_tensor(out=y_, in0=v_, scalar=hb[:, c:c + 1], in1=y_, op0=ALU.mult, op1=ALU.add),
                     reads=[d_yc, d_c, self.d_vvc], writes=[d_yc])
                m.op("dve", lambda e: e.tensor_tensor(out=y_, in0=y_, in1=self.x0c[:, c, :], op=ALU.mult), reads=[d_yc, self.d_vvc], writes=[d_yc])
                m.dma(self.OT[768 + c * 128:768 + (c + 1) * 128, self.nown:self.nown + LC], y_, reads=[d_yc], q="act")

    def _hy_fft(self):
        m, I = self.m, self.I
        GC = 32
        with Phase(m) as ph:
            d_c = Dep()
            C2 = ph.sb([128, 3, 128], BF16); FI = ph.sb([128, 128, 2, 32], BF16); d_FI = Dep()
            m.dma(C2[:], I["C2"][:, :, :], writes=[d_c])
            fi_loaded = [None]
            x1s = [ph.sb([64, GC, 128], BF16) for _ in range(2)]; d_x1 = [Dep(), Dep()]
            f1p = [ph.sb([64, 16, 2, 128], BF16) for _ in range(2)]; d_f1 = [Dep() for _ in range(2)]
            bP = ph.sb([128, 2, 128, GC], BF16); d_bP = Dep()
            bQ = ph.sb([128, 2, GC, 128], BF16); d_bQ = Dep()
            bZ = ph.sb([128, 2, GC, 128], BF16); d_bZ = Dep()
            Kf = ph.sb([128, 2, GC, 128], BF16); d_Kf = Dep()
            Yb = ph.sb([128, 2, GC, 128], BF16); d_Yb = Dep()
            t4 = [ph.sb([128, 512]) for _ in range(4)]; d_t4 = [Dep() for _ in range(4)]
            ysb = ph.sb([GC, 32, 128]); d_ysb = Dep()
            fv = [ph.sb([GC, 512]) for _ in range(2)]; d_fv = [Dep(), Dep()]
            fx = [ph.sb([GC, 512]) for _ in range(2)]; d_fx = [Dep(), Dep()]
            fo = [ph.sb([GC, 512]) for _ in range(2)]; d_fo = [Dep(), Dep()]
            gsc = ph.sb([GC, 2]); d_gsc = Dep()
            pA = [ph.ps([128, 16, GC]) for _ in range(2)]; d_pA = [Dep(), Dep()]
            pT = [ph.ps([128, 8, 128], BF16) for _ in range(2)]; d_pT = [Dep(), Dep()]
            pX = [[ph.ps([128, 512]) for _ in range(2)] for _ in range(2)]; d_pX = [[Dep(), Dep()], [Dep(), Dep()]]
            cnt = {"x1": 0, "f1": 0, "pA": 0, "pT": 0, "pX": 0, "ev": 0, "fin": 0}

            def nxt(k, n):
                i = cnt[k] % n; cnt[k] += 1
                return i

            def evac(out, in_, reads, writes):
                if nxt("ev", 2) == 0:
                    m.op("act", lambda e: e.activation(out=out, in_=in_, func=AF.Identity), reads=reads, writes=writes)
                else:
                    m.op("dve", lambda e: e.tensor_copy(out=out, in_=in_), reads=reads, writes=writes)

            def fwd(src_rows, mode):
                xi = nxt("x1", 2)
                m.dma(x1s[xi][:], src_rows.rearrange("c (a b) -> a c b", b=128), writes=[d_x1[xi]], q="sp")
                for bg in range(8):
                    fi = nxt("f1", 2)
                    m.dma(f1p[fi][:], I["F1"][:, bg * 16:(bg + 1) * 16, :, :], writes=[d_f1[fi]], q="sp")
                    for hb in range(2):
                        pi = nxt("pA", 2)
                        for bl in range(8):
                            b = bg * 16 + hb * 8 + bl
                            for ri in range(2):
                                m.op("pe", lambda e: e.matmul(pA[pi][:, bl * 2 + ri, :], lhsT=f1p[fi][:, hb * 8 + bl, ri, :], rhs=x1s[xi][:, :, b],
                                                              start=True, stop=True),
                                     reads=[d_f1[fi], d_x1[xi]], writes=[d_pA[pi]], inc=(bl == 7 and ri == 1))
                        b0 = bg * 16 + hb * 8
                        evac(bP[:, :, b0:b0 + 8, :], pA[pi][:].rearrange("p (b r) c -> p r b c", r=2), [d_pA[pi]], [d_bP])
                for ri in range(2):
                    for cg in range(GC // 8):
                        ti = nxt("pT", 2)
                        for k in range(8):
                            ch = cg * 8 + k
                            m.op("pe", lambda e: e.transpose(out=pT[ti][:, k, :], in_=bP[:, ri, :, ch], identity=self.ident[:]),
                                 reads=[d_bP, self.d_const], writes=[d_pT[ti]], inc=(k == 7))
                        evac(bQ[:, ri, cg * 8:(cg + 1) * 8, :], pT[ti][:], [d_pT[ti]], [d_bQ])
                for blk in range(GC // 4):
                    xi_ = nxt("pX", 2)
                    cs_ = slice(blk * 4, blk * 4 + 4)
                    are = bQ[:, 0, cs_, :]; aim = bQ[:, 1, cs_, :]
                    m.op("pe", lambda e: e.matmul(pX[xi_][0][:], lhsT=C2[:, 0, :], rhs=are, start=True, stop=False), reads=[d_c, d_bQ], writes=[d_pX[xi_][0]], inc=False)
                    m.op("pe", lambda e: e.matmul(pX[xi_][0][:], lhsT=C2[:, 1, :], rhs=aim, start=False, stop=True), reads=[d_c, d_bQ], writes=[d_pX[xi_][0]])
                    m.op("pe", lambda e: e.matmul(pX[xi_][1][:], lhsT=C2[:, 0, :], rhs=aim, start=True, stop=False), reads=[d_c, d_bQ], writes=[d_pX[xi_][1]], inc=False)
                    m.op("pe", lambda e: e.matmul(pX[xi_][1][:], lhsT=C2[:, 2, :], rhs=are, start=False, stop=True), reads=[d_c, d_bQ], writes=[d_pX[xi_][1]])
                    pre, pim = pX[xi_][0][:], pX[xi_][1][:]
                    dre, dim_ = d_pX[xi_][0], d_pX[xi_][1]
                    kre = Kf[:, 0, cs_, :]; kim = Kf[:, 1, cs_, :]
                    if mode == "A":
                        evac(kre, pre, [dre], [d_Kf]); evac(kim, pim, [dim_], [d_Kf])
                    elif mode == "B":
                        m.op("dve", lambda e: e.tensor_tensor(out=kre, in0=pre, in1=kre, op=ALU.add), reads=[dre, d_Kf], writes=[d_Kf])
                        m.op("dve", lambda e: e.tensor_tensor(out=kim, in0=kim, in1=pim, op=ALU.subtract), reads=[dim_, d_Kf], writes=[d_Kf])
                    else:
                        a_, b_, c_, e_ = t4
                        m.op("dve", lambda e: e.tensor_tensor(out=a_[:], in0=pre, in1=kre, op=ALU.mult), reads=[dre, d_Kf], writes=[d_t4[0]])
                        m.op("dve", lambda e: e.tensor_tensor(out=b_[:], in0=pim, in1=kim, op=ALU.mult), reads=[dim_, d_Kf], writes=[d_t4[1]])
                        m.op("dve", lambda e: e.tensor_tensor(out=c_[:], in0=pre, in1=kim, op=ALU.mult), reads=[dre, d_Kf], writes=[d_t4[2]])
                        m.op("dve", lambda e: e.tensor_tensor(out=e_[:], in0=pim, in1=kre, op=ALU.mult), reads=[dim_, d_Kf], writes=[d_t4[3]])
                        m.op("dve", lambda e: e.tensor_tensor(out=Yb[:, 0, cs_, :], in0=a_[:], in1=b_[:], op=ALU.subtract),
                             reads=[d_t4[0], d_t4[1]], writes=[d_Yb])
                        m.op("pool", lambda e: e.tensor_tensor(out=Yb[:, 1, cs_, :], in0=c_[:], in1=e_[:], op=ALU.add),
                             reads=[d_t4[2], d_t4[3]], writes=[d_Yb])

            def inv(g):
                for blk in range(GC // 4):
                    xi_ = nxt("pX", 2)
                    cs_ = slice(blk * 4, blk * 4 + 4)
                    yre = Yb[:, 0, cs_, :]; yim = Yb[:, 1, cs_, :]
                    m.op("pe", lambda e: e.matmul(pX[xi_][0][:], lhsT=C2[:, 0, :], rhs=yre, start=True, stop=False), reads=[d_c, d_Yb], writes=[d_pX[xi_][0]], inc=False)
                    m.op("pe", lambda e: e.matmul(pX[xi_][0][:], lhsT=C2[:, 2, :], rhs=yim, start=False, stop=True), reads=[d_c, d_Yb], writes=[d_pX[xi_][0]])
                    m.op("pe", lambda e: e.matmul(pX[xi_][1][:], lhsT=C2[:, 1, :], rhs=yre, start=True, stop=False), reads=[d_c, d_Yb], writes=[d_pX[xi_][1]], inc=False)
                    m.op("pe", lambda e: e.matmul(pX[xi_][1][:], lhsT=C2[:, 0, :], rhs=yim, start=False, stop=True), reads=[d_c, d_Yb], writes=[d_pX[xi_][1]])
                    evac(bZ[:, 0, cs_, :], pX[xi_][0][:], [d_pX[xi_][0]], [d_bZ])
                    evac(bZ[:, 1, cs_, :], pX[xi_][1][:], [d_pX[xi_][1]], [d_bZ])
                for ri in range(2):
                    for cg in range(GC // 8):
                        ti = nxt("pT", 2)
                        for k in range(8):
                            ch = cg * 8 + k
                            m.op("pe", lambda e: e.transpose(out=pT[ti][:, k, :], in_=bZ[:, ri, ch, :], identity=self.ident[:]),
                                 reads=[d_bZ, self.d_const], writes=[d_pT[ti]], inc=(k == 7))
                        evac(bQ[:, ri, cg * 8:(cg + 1) * 8, :], pT[ti][:], [d_pT[ti]], [d_bQ])
                c = (g * GC) // 128; r0 = (g * GC) % 128
                m.dma(gsc[:, 0:1], self.hsc[r0:r0 + GC, 0, c:c + 1], reads=[self.d_hsc], writes=[d_gsc], q="sp", allow_slow_non_contiguous=True)
                m.dma(gsc[:, 1:2], I["hy_bias"][r0:r0 + GC, c:c + 1], writes=[d_gsc], q="sp", allow_slow_non_contiguous=True)
                for ah in range(self.nown // SH):
                    if fi_loaded[0] != ah:
                        m.dma(FI[:], I["FI%d" % ah][:, :, :, :], writes=[d_FI], q="sp")
                        fi_loaded[0] = ah
                    for bg in range(8):
                        pi = nxt("pA", 2)
                        for bl in range(16):
                            b = bg * 16 + bl
                            m.op("pe", lambda e: e.matmul(pA[pi][0:GC, bl, :], lhsT=bQ[:, 0, :, b], rhs=FI[:, b, 0, :], start=True, stop=False),
                                 reads=[d_bQ, d_FI], writes=[d_pA[pi]], inc=False)
                            m.op("pe", lambda e: e.matmul(pA[pi][0:GC, bl, :], lhsT=bQ[:, 1, :, b], rhs=FI[:, b, 1, :], start=False, stop=True),
                                 reads=[d_bQ, d_FI], writes=[d_pA[pi]], inc=(bl == 15))
                        evac(ysb[:, :, bg * 16:(bg + 1) * 16], pA[pi][0:GC, :, :].rearrange("p b a -> p a b"), [d_pA[pi]], [d_ysb])
                    yfl = ysb[:].rearrange("p a b -> p (a b)")
                    for q4 in range(8):
                        fi = nxt("fin", 2)
                        sl = slice(q4 * 512, (q4 + 1) * 512)
                        gl = slice(ah * SH + q4 * 512, ah * SH + (q4 + 1) * 512)
                        m.dma(fv[fi][:], self.VV[g * GC:(g + 1) * GC, gl], writes=[d_fv[fi]], q="sp")
                        m.dma(fx[fi][:], self.X0[g * GC:(g + 1) * GC, gl], writes=[d_fx[fi]], q="sp")
                        m.op("act", lambda e: e.activation(out=fo[fi][:], in_=yfl[:, sl], func=AF.Identity, scale=gsc[:, 0:1]),
                             reads=[d_ysb, d_gsc], writes=[d_fo[fi]])
                        m.op("dve", lambda e: e.scalar_tensor_tensor(out=fo[fi][:], in0=fv[fi][:], scalar=gsc[:, 1:2], in1=fo[fi][:], op0=ALU.mult, op1=ALU.add),
                             reads=[d_fv[fi], d_gsc, d_fo[fi]], writes=[d_fo[fi]])
                        m.op("dve", lambda e: e.tensor_tensor(out=fo[fi][:], in0=fo[fi][:], in1=fx[fi][:], op=ALU.mult),
                             reads=[d_fo[fi], d_fx[fi]], writes=[d_fo[fi]])
                        m.dma(self.OT[768 + g * GC:768 + (g + 1) * GC, gl], fo[fi][:], reads=[d_fo[fi]], q="act")

            ngrp = 256 // GC if "hy_short" not in self.dbg else 1
            for g in range(ngrp):
                fwd(self.KFb[g * GC:(g + 1) * GC, :], "A")
                fwd(self.KFb[256 + g * GC:256 + (g + 1) * GC, :], "B")
                fwd(self.VVb[g * GC:(g + 1) * GC, :], "V")
                inv(g)
        self.hyP.__exit__(None, None, None)

    def phase_merge(self):
        m, I = self.m, self.I
        with Phase(m) as ph:
            d_c = Dep()
            og = ph.sb([128, 8]); m.dma(og[:], I["out_g"][:, :], writes=[d_c])
            wo = ph.sb([128, 8, D], BF16); d_wo = Dep()
            wst = [ph.sb([128, D]) for _ in range(2)]; d_wst = [Dep(), Dep()]
            wv = I["w_out"].rearrange("(kc p) n -> p kc n", p=128)
            for kc in range(8):
                m.dma(wst[kc % 2][:], wv[:, kc, :], writes=[d_wst[kc % 2]])
                m.op("dve", lambda e: e.tensor_scalar(out=wo[:, kc, :], in0=wst[kc % 2][:], scalar1=og[:, kc:kc + 1], scalar2=None, op0=ALU.mult),
                     reads=[d_wst[kc % 2], d_c], writes=[d_wo])
            ones2 = ph.sb([128, 2], BF16)
            m.op("dve", lambda e: e.memset(ones2[:], 1.0), writes=[d_c])
            wrec = ph.sb([128, 3])
            for gi, wd in enumerate((512, 256, 256)):
                m.op("dve", lambda e: e.memset(wrec[:, gi:gi + 1], 1.0 / wd), writes=[d_c])
            ob = [ph.sb([128, 8, 512]) for _ in range(2)]; d_ob = [Dep(), Dep()]
            obb = [ph.sb([128, 8, 512], BF16) for _ in range(2)]; d_obb = [Dep(), Dep()]
            sqb = [ph.sb([128, 8, 512], BF16) for _ in range(2)]; d_sqb = [Dep(), Dep()]
            xt = [ph.sb([128, D]) for _ in range(2)]; d_xt = [Dep(), Dep()]
            acc = [ph.sb([128, D]) for _ in range(2)]; d_acc = [Dep(), Dep()]
            xn = [ph.sb([128, D], BF16) for _ in range(2)]; d_xn = [Dep(), Dep()]
            junk = ph.sb([128, D], BF16); d_junk = Dep()
            rs = [ph.sb([128, 4]) for _ in range(2)]; d_rs = [Dep(), Dep()]
            fT = [ph.sb([128, 8, 512], BF16) for _ in range(2)]; d_fT = [Dep(), Dep()]
            pO = [[ph.ps([128, 512]) for _ in range(3)] for _ in range(2)]; d_pO = [[Dep() for _ in range(3)] for _ in range(2)]
            pT = ph.ps([128, 8, 128], BF16); d_pT = Dep()
            pS = ph.ps([128, 3, 2]); d_pS = Dep()
            groups = [(0, 4), (4, 6), (6, 8)]
            OTv = self.OT.rearrange("(c p) t -> p c t", p=128)
            blocks = [("lat", i * 512, 512) for i in range(self.nown // 512)] + ([] if self.last else [("ctx", self.nown, LC)])
            tcount = 0
            pcount = 0
            for bi, (kind, c0, ntok) in enumerate(blocks):
                o_ = ob[bi % 2]; do = d_ob[bi % 2]
                m.dma(o_[:, :, 0:ntok], OTv[:, :, c0:c0 + ntok], writes=[do], q="sp")
                ob_ = obb[bi % 2]; dob = d_obb[bi % 2]
                sq_ = sqb[bi % 2]; dsq = d_sqb[bi % 2]
                for ch in range(8):
                    m.op("act", lambda e: e.activation(out=sq_[:, ch, 0:ntok], in_=o_[:, ch, 0:ntok], func=AF.Square), reads=[do], writes=[dsq])
                    m.op("pool", lambda e: e.tensor_copy(out=ob_[:, ch, 0:ntok], in_=o_[:, ch, 0:ntok]), reads=[do], writes=[dob])
                f_ = fT[bi % 2]; df = d_fT[bi % 2]
                gi_ga = 0 if kind == "lat" else 2
                mi = 1 if kind == "lat" else 3
                modt = self.modL if kind == "lat" else self.modC
                for tt in range(ntok // 128):
                    ts_ = slice(tt * 128, (tt + 1) * 128)
                    k2 = tcount % 2; tcount += 1
                    r_ = rs[k2]; dr = d_rs[k2]
                    if kind == "lat":
                        m.dma(xt[k2][:], self.xsrc[c0 + tt * 128:c0 + (tt + 1) * 128, :], writes=[d_xt[k2]], q="sp")
                    else:
                        m.dma(xt[k2][:], self.ctxsrc[tt * 128:(tt + 1) * 128, :], writes=[d_xt[k2]], q="sp")
                    for gi, (a, b) in enumerate(groups):
                        for ch in range(a, b):
                            m.op("pe", lambda e: e.matmul(pS[:, gi, :], lhsT=sq_[:, ch, ts_], rhs=ones2[:], start=(ch == a), stop=(ch == b - 1)),
                                 reads=[dsq, d_c], writes=[d_pS], inc=(ch == b - 1))
                    m.op("dve", lambda e: e.tensor_tensor(out=r_[:, 0:3], in0=pS[:, :, 0], in1=wrec[:], op=ALU.mult), reads=[d_pS, d_c], writes=[dr])
                    m.op("act", lambda e: e.activation(out=r_[:, 0:3], in_=r_[:, 0:3], func=AF.Sqrt, bias=self.epsT[:, 0:1]), reads=[dr, self.d_const], writes=[dr])
                    m.op("dve", lambda e: e.reciprocal(out=r_[:, 0:3], in_=r_[:, 0:3]), reads=[dr], writes=[dr])
                    a_ = acc[k2]; da = d_acc[k2]
                    for h in range(2):
                        hs = slice(h * 512, (h + 1) * 512)
                        pk = pcount % 2; pcount += 1
                        for gi, (a, b) in enumerate(groups):
                            for ch in range(a, b):
                                m.op("pe", lambda e: e.matmul(pO[pk][gi][:], lhsT=ob_[:, ch, ts_], rhs=wo[:, ch, hs], start=(ch == a), stop=(ch == b - 1)),
                                     reads=[dob, d_wo], writes=[d_pO[pk][gi]], inc=(ch == b - 1))
                        m.op("dve", lambda e: e.tensor_scalar(out=a_[:, hs], in0=pO[pk][0][:], scalar1=r_[:, 0:1], scalar2=None, op0=ALU.mult),
                             reads=[d_pO[pk][0], dr], writes=[da])
                        for gi in (1, 2):
                            m.op("dve", lambda e: e.scalar_tensor_tensor(out=a_[:, hs], in0=pO[pk][gi][:], scalar=r_[:, gi:gi + 1], in1=a_[:, hs],
                                                                         op0=ALU.mult, op1=ALU.add), reads=[d_pO[pk][gi], dr, da], writes=[da])
                    m.op("pool", lambda e: e.tensor_tensor(out=a_[:], in0=a_[:], in1=self.gbc[:, gi_ga, :], op=ALU.mult), reads=[da, self.d_gbc], writes=[da])
                    m.op("pool", lambda e: e.tensor_tensor(out=a_[:], in0=a_[:], in1=xt[k2][:], op=ALU.add), reads=[da, d_xt[k2]], writes=[da])
                    m.dma(self.XM[c0 + tt * 128:c0 + (tt + 1) * 128, :], a_[:], reads=[da], q="act")
                    m.op("act", lambda e: e.activation(out=junk[:], in_=a_[:], func=AF.Square, accum_out=r_[:, 3:4]), reads=[da], writes=[d_junk, dr])
                    m.op("dve", lambda e: e.tensor_scalar(out=r_[:, 3:4], in0=r_[:, 3:4], scalar1=1.0 / D, scalar2=EPS, op0=ALU.mult, op1=ALU.add),
                         reads=[dr], writes=[dr])
                    m.op("act", lambda e: e.activation(out=r_[:, 3:4], in_=r_[:, 3:4], func=AF.Sqrt), reads=[dr], writes=[dr])
                    m.op("dve", lambda e: e.reciprocal(out=r_[:, 3:4], in_=r_[:, 3:4]), reads=[dr], writes=[dr])
                    m.op("dve", lambda e: e.tensor_scalar(out=xn[k2][:], in0=a_[:], scalar1=r_[:, 3:4], scalar2=None, op0=ALU.mult),
                         reads=[da, dr], writes=[d_xn[k2]])
                    for kc in range(8):
                        m.op("pe", lambda e: e.transpose(out=pT[:, kc, :], in_=xn[k2][:, kc * 128:(kc + 1) * 128], identity=self.ident[:]),
                             reads=[d_xn[k2], self.d_const], writes=[d_pT], inc=(kc == 7))
                    for kc in range(8):
                        m.op("dve", lambda e: e.tensor_scalar(out=f_[:, kc, ts_], in0=pT[:, kc, :], scalar1=self.AB[:, mi, kc:kc + 1],
                                                              scalar2=modt[:, 24 + kc:25 + kc], op0=ALU.mult, op1=ALU.add),
                             reads=[d_pT, self.d_mod], writes=[df])
                m.dma(self.FT[:, :, c0:c0 + ntok], f_[:, :, 0:ntok], reads=[df], q="act")

    def phase_moe(self):
        m, I = self.m, self.I
        NT = self.ntok // 128
        NTOK = self.ntok; SHL = self.nown
        with Phase(m) as P:
            d_c = Dep()
            gate = P.sb([128, NT, NE]); d_gate = Dep()
            b1 = P.sb([128, NE, 16])
            m.dma(b1[:], I["moe_b1"][:, :, :], writes=[d_c])
            m.op("dve", lambda e: e.tensor_scalar(out=b1[:, :, 8:16], in0=b1[:, :, 8:16], scalar1=1.0, scalar2=None, op0=ALU.add), reads=[d_c], writes=[d_c])
            with Phase(m) as ph:
                rw = ph.sb([128, 8, NE], BF16); rb = ph.sb([128, NE])
                m.dma(rw[:], I["router_w"].rearrange("(kc p) n -> p kc n", p=128), writes=[d_c], q="pool")
                m.dma(rb[:], I["router_b"][:, :], writes=[d_c])
                fb = [ph.sb([128, 8, 512], BF16) for _ in range(2)]; d_fb = [Dep(), Dep()]
                lg = [ph.sb([128, NE]) for _ in range(2)]; d_lg = [Dep(), Dep()]
                ex = [ph.sb([128, NE]) for _ in range(2)]; d_ex = [Dep(), Dep()]
                m8 = [ph.sb([128, 8]) for _ in range(2)]; d_m8 = [Dep(), Dep()]
                sm = [ph.sb([128, 2]) for _ in range(2)]; d_sm = [Dep(), Dep()]
                pl = [ph.ps([128, NE]) for _ in range(2)]; d_pl = [Dep(), Dep()]
                tcount = 0
                for bi in range((NTOK + 511) // 512):
                    c0 = bi * 512; n = min(512, NTOK - c0)
                    m.dma(fb[bi % 2][:, :, 0:n], self.FT[:, :, c0:c0 + n], writes=[d_fb[bi % 2]], q="sp")
                    for tt in range(n // 128):
                        tile = c0 // 128 + tt
                        k = tcount % 2; tcount += 1
                        for kc in range(8):
                            m.op("pe", lambda e: e.matmul(pl[k][:], lhsT=fb[bi % 2][:, kc, tt * 128:(tt + 1) * 128], rhs=rw[:, kc, :], start=(kc == 0), stop=(kc == 7)),
                                 reads=[d_fb[bi % 2], d_c], writes=[d_pl[k]], inc=(kc == 7))
                        m.op("dve", lambda e: e.tensor_tensor(out=lg[k][:], in0=pl[k][:], in1=rb[:], op=ALU.add), reads=[d_pl[k], d_c], writes=[d_lg[k]])
                        m.op("dve", lambda e: e.max(out=m8[k][:], in_=lg[k][:]), reads=[d_lg[k]], writes=[d_m8[k]])
                        m.op("dve", lambda e: e.tensor_scalar(out=sm[k][:, 0:1], in0=m8[k][:, 0:1], scalar1=-1.0, scalar2=None, op0=ALU.mult),
                             reads=[d_m8[k]], writes=[d_sm[k]])
                        m.op("act", lambda e: e.activation(out=ex[k][:], in_=lg[k][:], func=AF.Exp, bias=sm[k][:, 0:1]), reads=[d_lg[k], d_sm[k]], writes=[d_ex[k]])
                        m.op("dve", lambda e: e.scalar_tensor_tensor(out=ex[k][:], in0=lg[k][:], scalar=m8[k][:, 3:4], in1=ex[k][:], op0=ALU.is_ge, op1=ALU.mult,
                                                                     accum_out=sm[k][:, 1:2]), reads=[d_lg[k], d_m8[k], d_ex[k]], writes=[d_ex[k], d_sm[k]])
                        m.op("dve", lambda e: e.reciprocal(out=sm[k][:, 1:2], in_=sm[k][:, 1:2]), reads=[d_sm[k]], writes=[d_sm[k]])
                        m.op("dve", lambda e: e.tensor_scalar(out=gate[:, tile, :], in0=ex[k][:], scalar1=sm[k][:, 1:2], scalar2=None, op0=ALU.mult),
                             reads=[d_ex[k], d_sm[k]], writes=[d_gate])
            if "gate" in self.dbg:
                o = self.nc.dram_tensor("dbg_gate", [128, NT, NE], F32, kind="ExternalOutput").ap(); self.out_names.append("dbg_gate")
                m.dma(o[:, :, :], gate[:], reads=[d_gate])
            w1v = I["moe_w1"].rearrange("e (kc p) n -> e p kc n", p=128)
            w2v = I["moe_w2"].rearrange("e (kc p) n -> e p kc n", p=128)
            passes = [(a, min(a + 11, NT)) for a in range(0, NT, 11)]
            if "moe_short" in self.dbg:
                passes = [(NT - 2, NT)]
            n_exp = NE
            for (ta, tb_) in passes:
                ntile = tb_ - ta
                with Phase(m) as pp:
                    fT = pp.sb([128, 8, ntile * 128], BF16); d_fT = Dep()
                    yacc = pp.sb([128, ntile, D]); d_y = [Dep() for _ in range(ntile)]
                    m.dma(fT[:], self.FT[:, :, ta * 128:tb_ * 128], writes=[d_fT], q="sp")
                    with Phase(m) as ph:
                        b2 = ph.sb([NE, D])
                        m.dma(b2[:], I["moe_b2"][:, :], writes=[d_c])
                        gT = [ph.sb([NE, 128]) for _ in range(2)]; d_gT = [Dep(), Dep()]
                        pg = [ph.ps([NE, 128]) for _ in range(2)]; d_pg = [Dep(), Dep()]
                        py = [ph.ps([128, 512]) for _ in range(2)]; d_py = [Dep(), Dep()]
                        for ti in range(ntile):
                            k = ti % 2
                            m.op("pe", lambda e: e.transpose(out=pg[k][:], in_=gate[:, ta + ti, :], identity=self.identf[:]),
                                 reads=[d_gate, self.d_const], writes=[d_pg[k]])
                            m.op("act", lambda e: e.activation(out=gT[k][:], in_=pg[k][:], func=AF.Identity), reads=[d_pg[k]], writes=[d_gT[k]])
                            for h in range(2):
                                m.op("pe", lambda e: e.matmul(py[h][:], lhsT=gT[k][:], rhs=b2[:, h * 512:(h + 1) * 512], start=True, stop=True),
                                     reads=[d_gT[k], d_c], writes=[d_py[h]])
                                m.op("dve", lambda e: e.tensor_copy(out=yacc[:, ti, h * 512:(h + 1) * 512], in_=py[h][:]), reads=[d_py[h]], writes=[d_y[ti]])
                    with Phase(m) as ph:
                        w1 = [ph.sb([128, 8, 2 * D], BF16) for _ in range(2)]; d_w1 = [[Dep() for _ in range(8)] for _ in range(2)]
                        w2 = ph.sb([128, 8, D], BF16); d_w2 = [Dep() for _ in range(8)]
                        act = [ph.sb([128, 8, 512], BF16) for _ in range(2)]; d_act = [Dep(), Dep()]
                        tg = [ph.sb([128, 512]) for _ in range(2)]; d_tg = [Dep(), Dep()]
                        tsg = [ph.sb([128, 512]) for _ in range(2)]; d_tsg = [Dep(), Dep()]
                        tl = [ph.sb([128, 512]) for _ in range(2)]; d_tl = [Dep(), Dep()]
                        pgl = [[ph.ps([128, 512]) for _ in range(2)] for _ in range(2)]; d_pgl = [[Dep(), Dep()], [Dep(), Dep()]]
                        pyy = [ph.ps([128, 512]) for _ in range(3)]; d_pyy = [Dep() for _ in range(3)]
                        cnt = {"j": 0, "y": 0, "a": 0}
                        m.op("dve", lambda e: e.tensor_scalar(out=gate[:, ta:tb_, :], in0=gate[:, ta:tb_, :], scalar1=1.0 / 1.702, scalar2=None, op0=ALU.mult),
                             reads=[d_gate], writes=[d_gate])

                        wtok = []

                        def wdma(out, in_, dep):
                            if len(wtok) >= 2:
                                m._wait("pool", wtok[-2])
                            wtok.append(m.dma(out, in_, writes=[dep], q="pool"))

                        def load_w1(e_):
                            for kc in range(8):
                                wdma(w1[e_ % 2][:, kc, :], w1v[e_, :, kc, :], d_w1[e_ % 2][kc])

                        def load_w2(e_):
                            for kc in range(8):
                                wdma(w2[:, kc, :], w2v[e_, :, kc, :], d_w2[kc])
                        load_w1(0)
                        for e_ in range(n_exp):
                            load_w2(e_)
                            if e_ + 1 < n_exp:
                                load_w1(e_ + 1)
                            W1 = w1[e_ % 2]; dW1 = d_w1[e_ % 2]
                            for b0 in range(0, ntile, 4):
                                nt_ = min(4, ntile - b0); n = nt_ * 128
                                cs_ = slice(b0 * 128, b0 * 128 + n)
                                ai = cnt["a"] % 2; cnt["a"] += 1
                                A_ = act[ai]; dA = d_act[ai]
                                for j in range(8):
                                    k = cnt["j"] % 2; cnt["j"] += 1
                                    for gl in range(2):
                                        for kc in range(8):
                                            m.op("pe", lambda e: e.matmul(pgl[k][gl][:, 0:n], lhsT=W1[:, kc, 256 * j + gl:256 * j + 256:2], rhs=fT[:, kc, cs_],
                                                                          start=(kc == 0), stop=(kc == 7)),
                                                 reads=[dW1[kc], d_fT], writes=[d_pgl[k][gl]], inc=(kc == 7))
                                    m.op("dve", lambda e: e.tensor_scalar(out=tg[k][:, 0:n], in0=pgl[k][0][:, 0:n], scalar1=b1[:, e_, j:j + 1], scalar2=7.0,
                                                                          op0=ALU.add, op1=ALU.min), reads=[d_pgl[k][0], d_c], writes=[d_tg[k]])
                                    m.op("act", lambda e: e.activation(out=tsg[k][:, 0:n], in_=tg[k][:, 0:n], func=AF.Silu, scale=1.702),
                                         reads=[d_tg[k]], writes=[d_tsg[k]])
                                    m.op("dve", lambda e: e.tensor_scalar(out=tl[k][:, 0:n], in0=pgl[k][1][:, 0:n], scalar1=b1[:, e_, 8 + j:9 + j], scalar2=8.0,
                                                                          op0=ALU.add, op1=ALU.min), reads=[d_pgl[k][1], d_c], writes=[d_tl[k]])
                                    m.op("dve", lambda e: e.scalar_tensor_tensor(out=A_[:, j, 0:n], in0=tl[k][:, 0:n], scalar=-6.0, in1=tsg[k][:, 0:n],
                                                                                 op0=ALU.max, op1=ALU.mult), reads=[d_tl[k], d_tsg[k]], writes=[dA])
                                for tt in range(nt_):
                                    ti = b0 + tt
                                    for h in range(2):
                                        yk = cnt["y"] % 3; cnt["y"] += 1
                                        for j in range(8):
                                            m.op("pe", lambda e: e.matmul(pyy[yk][:], lhsT=A_[:, j, tt * 128:(tt + 1) * 128], rhs=w2[:, j, h * 512:(h + 1) * 512],
                                                                          start=(j == 0), stop=(j == 7)),
                                                 reads=[dA, d_w2[j]], writes=[d_pyy[yk]], inc=(j == 7))
                                        m.op("dve", lambda e: e.scalar_tensor_tensor(out=yacc[:, ti, h * 512:(h + 1) * 512], in0=pyy[yk][:], scalar=gate[:, ta + ti, e_:e_ + 1],
                                                                                     in1=yacc[:, ti, h * 512:(h + 1) * 512], op0=ALU.mult, op1=ALU.add),
                                             reads=[d_pyy[yk], d_gate, d_y[ti]], writes=[d_y[ti]])
                    with Phase(m) as ph:
                        xm = [ph.sb([128, D]) for _ in range(2)]; d_xm = [Dep(), Dep()]
                        ot = [ph.sb([128, D]) for _ in range(2)]; d_ot = [Dep(), Dep()]
                        for ti in range(ntile):
                            tile = ta + ti; k = ti % 2
                            m.dma(xm[k][:], self.XM[tile * 128:(tile + 1) * 128, :], writes=[d_xm[k]], q="sp")
                            gi = 1 if tile < SHL // 128 else 3
                            m.op("pool", lambda e: e.tensor_tensor(out=ot[k][:], in0=yacc[:, ti, :], in1=self.gbc[:, gi, :], op=ALU.mult),
                                 reads=[d_y[ti], self.d_gbc], writes=[d_ot[k]])
                            m.op("dve", lambda e: e.tensor_tensor(out=ot[k][:], in0=ot[k][:], in1=xm[k][:], op=ALU.add), reads=[d_ot[k], d_xm[k]], writes=[d_ot[k]])
                            if tile < SHL // 128:
                                dst = self.x_out if self.last else self.X1
                                m.dma(dst[tile * 128:(tile + 1) * 128, :], ot[k][:], reads=[d_ot[k]], q="act")
                            else:
                                r0 = tile * 128 - SHL
                                m.dma(self.C1[r0:r0 + 128, :], ot[k][:], reads=[d_ot[k]], q="act")


_PROG = {}


def _get_prog():
    if "p" not in _PROG:
        _PROG["p"] = LayerProg()
    return _PROG["p"]


def kernel(**inputs):
    inp = {k: np.asarray(v) for k, v in inputs.items()}
    P = _get_prog()
    need = set(P.Iall.keys())
    x = np.ascontiguousarray(inp["x"], dtype=np.float32)
    ctx = np.ascontiguousarray(inp["ctx"], dtype=np.float32)
    maps = []
    for core in range(8):
        b, half = core // 2, core % 2
        xc = x[b][::-1] if half else x[b]
        cc = ctx[b][::-1] if half else ctx[b]
        mp = {}
        for l in range(2):
            d = _prep_core(inp, l, b, half, xc, cc)
            for k, v in d.items():
                name = k if k in SHARED else "%s@%d" % (k, l)
                if name in need and name not in mp:
                    mp[name.replace("@", "_L")] = v
        maps.append(mp)
    res = run_bass_kernel_spmd(P.nc, maps, core_ids=list(range(8)))
    out = np.empty_like(x)
    for core in range(8):
        b, half = core // 2, core % 2
        xo = np.asarray(res.results[core]["x_out"])
        if half == 0:
            out[b, :SH] = xo
        else:
            out[b, SH:] = xo[::-1]
    return out
```

```python
import math
from contextlib import ExitStack
import numpy as np
import ml_dtypes
import concourse.bass as bass
import concourse.mybir as mybir
from concourse.bass_utils import run_bass_kernel_spmd

F32 = mybir.dt.float32
BF16 = mybir.dt.bfloat16
AF = mybir.ActivationFunctionType
ALU = mybir.AluOpType
AX = mybir.AxisListType

D = 1024
S = 8192
SH = 4096
LC = 256
NTOK = SH + LC
NALL = S + LC
NE = 32
EPS = 1e-6
NFFT = 16384


class Dep:
    __slots__ = ("w", "r")

    def __init__(self):
        self.w = None
        self.r = {}


class MK:
    ENG = ("pe", "act", "dve", "pool", "sp")
    EPOCH = 12000

    def __init__(self, nc, n_dma_sems=48):
        self.nc = nc
        self.e = {"pe": nc.tensor, "act": nc.scalar, "dve": nc.vector, "pool": nc.gpsimd, "sp": nc.sync}
        self.sem = {}
        self.cnt = {}
        self.allsems = []
        self.nsem = 0
        for k in self.ENG:
            self._new_epoch(k)
        self.seen = {k: {} for k in self.ENG}
        self.dma_sems = [nc.alloc_semaphore(f"dq{i}") for i in range(n_dma_sems)]
        self.dma_val = [0] * n_dma_sems
        self.dma_rr = 0
        self.n_inst = 0
        self.n_wait = 0
        self._uid = 0

    def _new_epoch(self, k):
        self.sem[k] = self.nc.alloc_semaphore(f"s_{k}_{self.nsem}")
        self.nsem += 1
        self.cnt[k] = 0

    def uid(self, p="t"):
        self._uid += 1
        return f"{p}{self._uid}"

    def _wait(self, eng, tok):
        if tok is None:
            return
        sem, val = tok
        sid = id(sem)
        if self.seen[eng].get(sid, 0) >= val:
            return
        if sem is self.sem.get(eng) and val > self.cnt[eng]:
            return
        self.e[eng].wait_ge(sem, val)
        self.seen[eng][sid] = val
        self.n_wait += 1

    def _deps(self, eng, reads, writes):
        for d in reads:
            self._wait(eng, d.w)
        for d in writes:
            self._wait(eng, d.w)
            for t in d.r.values():
                self._wait(eng, t)

    def op(self, eng, fn, reads=(), writes=(), inc=True):
        self._deps(eng, reads, writes)
        ins = fn(self.e[eng])
        self.n_inst += 1
        if inc:
            self.cnt[eng] += 1
            ins.then_inc(self.sem[eng], 1)
            tok = (self.sem[eng], self.cnt[eng])
            self.seen[eng][id(self.sem[eng])] = self.seen[eng].get(id(self.sem[eng]), 0)
            if self.cnt[eng] >= self.EPOCH:
                self._new_epoch(eng)
        else:
            tok = (self.sem[eng], self.cnt[eng] + 1)
        for d in reads:
            d.r[eng] = tok
        for d in writes:
            d.w = tok
            d.r = {}
        return ins

    def dma(self, out, in_, reads=(), writes=(), q="sp", **kw):
        self._deps(q, reads, writes)
        i = self.dma_rr
        self.dma_rr = (self.dma_rr + 1) % len(self.dma_sems)
        sem = self.dma_sems[i]
        if self.dma_val[i] > 0:
            self._wait(q, (sem, self.dma_val[i]))
        self.dma_val[i] += 16
        ins = self.e[q].dma_start(out=out, in_=in_, **kw)
        ins.then_inc(sem, 16)
        self.n_inst += 1
        tok = (sem, self.dma_val[i])
        for d in reads:
            d.r["dma%d" % i] = tok
        for d in writes:
            d.w = tok
            d.r = {}
        return tok

    def barrier(self, engines=None):
        engines = engines or self.ENG
        toks = [(self.sem[p], self.cnt[p]) for p in self.ENG if self.cnt[p] > 0]
        toks += [(s, v) for s, v in zip(self.dma_sems, self.dma_val) if v > 0]
        for e in engines:
            for t in toks:
                self._wait(e, t)


class Phase:
    def __init__(self, m):
        self.m = m
        self.st = ExitStack()

    def __enter__(self):
        self.st.__enter__()
        return self

    def sb(self, shape, dt=F32):
        return self.st.enter_context(self.m.nc.sbuf_tensor(self.m.uid("sb"), list(shape), dt))

    def ps(self, shape, dt=F32):
        return self.st.enter_context(self.m.nc.psum_tensor(self.m.uid("ps"), list(shape), dt))

    def __exit__(self, *a):
        self.m.barrier()
        return self.st.__exit__(*a)


_CONST = {}


def _consts():
    if _CONST:
        return _CONST
    bf = ml_dtypes.bfloat16
    c = _CONST
    c["ident"] = np.eye(128, dtype=np.float32)
    pr = np.zeros((128, 128), np.float32)
    for blk in range(2):
        for i in range(32):
            pr[blk * 64 + i + 32, blk * 64 + i] = -1.0
            pr[blk * 64 + i, blk * 64 + i + 32] = 1.0
    c["prot"] = pr
    bo = np.zeros((128, 128), np.float32)
    bo[:64, :64] = 1.0 / 64
    bo[64:, 64:] = 1.0 / 64
    c["blk64"] = bo
    sel = np.zeros((65, 64), np.float32)
    sel[64, :] = 1.0
    c["sel"] = sel
    rows = np.repeat(np.arange(S // 64), 64).astype(np.float32)
    cols = np.tile(np.arange(64), S // 64).astype(np.float32)
    inv = (10000.0 ** (-np.arange(16, dtype=np.float32) / 16)).astype(np.float32)
    ang = np.concatenate([rows[:, None] * inv, cols[:, None] * inv], axis=-1).astype(np.float32)
    c["rope_cos"] = np.ascontiguousarray(np.tile(np.cos(ang).T, (4, 1)).astype(np.float32))
    c["rope_sin"] = np.ascontiguousarray(np.tile(np.sin(ang).T, (4, 1)).astype(np.float32))

    def zfeat(L):
        t01 = np.linspace(0.0, 1.0, L, dtype=np.float32)[:, None]
        bands = np.linspace(1e-4, 15, 16, dtype=np.float32)
        a = (np.float32(2.0 * math.pi / L) * np.arange(L, dtype=np.float32)[:, None] * bands).astype(np.float32)
        z = np.concatenate([t01, np.cos(a), -np.sin(a)], axis=-1).astype(np.float32)
        return np.ascontiguousarray(z.T), np.ascontiguousarray(np.broadcast_to(t01[:, 0][None, :], (128, L)))
    c["zT"], c["t01"] = zfeat(S)
    c["zTc"], c["t01c"] = zfeat(LC)
    a = np.arange(64)[:, None, None]
    b = np.arange(128)[None, :, None]
    cc = np.arange(128)[None, None, :]
    th = 2.0 * np.pi * (((128 * a + b) * cc) % NFFT) / NFFT
    c["F1"] = np.ascontiguousarray(np.stack([np.cos(th), -np.sin(th)], axis=2)).astype(bf)
    bd = 2.0 * np.pi * ((np.arange(128)[:, None] * np.arange(128)[None, :]) % 128) / 128
    c["C2"] = np.stack([np.cos(bd), np.sin(bd), -np.sin(bd)], axis=1).astype(bf)
    cI = np.arange(128)[:, None, None]
    bI = np.arange(128)[None, :, None]
    for hh in range(2):
        a32 = (np.arange(32) + 32 * hh)[None, None, :]
        thi = 2.0 * np.pi * (((128 * a32 + bI) * cI) % NFFT) / NFFT
        c["FI%d" % hh] = np.ascontiguousarray(np.stack([np.cos(thi), -np.sin(thi)], axis=2)).astype(bf)
    return c


def _cols(v, n):
    return np.ascontiguousarray(np.asarray(v, np.float32).reshape(n, 128).T)


def _prep_core(inp, l, b, half, x_core, ctx_core):
    c = _consts()
    r = (lambda a, ax=0: np.flip(a, axis=ax)) if half else (lambda a, ax=0: a)
    d = {}
    d["x"] = np.ascontiguousarray(x_core, np.float32)
    d["ctx"] = np.ascontiguousarray(ctx_core, np.float32)
    d["cvec"] = np.ascontiguousarray(np.concatenate([_cols(inp["c"][b], 8), _cols(inp["c_ctx"], 8)], axis=1))
    d["w_mod"] = inp["w_mod"][l]
    d["bmod"] = _cols(inp["b_mod"][l], 48)
    d["gmix"] = _cols(inp["norm_mix_g"][l], 8)
    d["gffn"] = _cols(inp["norm_ffn_g"][l], 8)
    w_in = inp["w_in"][l]
    perm = np.array([(j + 4 * s) * 64 + dd for j in range(4) for s in range(2) for dd in range(64)])
    d["w_in"] = np.ascontiguousarray(np.concatenate([w_in[:, perm], w_in[:, 512:]], axis=1))
    qg = inp["q_norm_g"][l]
    kg = inp["k_norm_g"][l]
    d["qkg"] = np.ascontiguousarray(np.stack([np.tile(qg, 2), np.tile(kg, 2)], axis=1).astype(np.float32))
    d["qkg_row"] = np.ascontiguousarray(np.broadcast_to(np.concatenate([qg, kg])[None, :], (128, 128)).astype(np.float32))
    d["rope_cos"] = np.ascontiguousarray(r(c["rope_cos"], 1))
    d["rope_sin"] = np.ascontiguousarray(r(c["rope_sin"], 1))
    cw = inp["lru_conv_w"][l]
    w5 = np.zeros((5, 256), np.float32)
    if half:
        w5[1:5] = cw[::-1]
    else:
        w5[0:4] = cw
    d["lru_cw"] = np.ascontiguousarray(w5.T.reshape(2, 128, 5).transpose(1, 0, 2))
    d["lru_cb"] = _cols(inp["lru_conv_b"][l], 2)
    gw = inp["lru_gate_w"][l]
    gb = inp["lru_gate_b"][l]
    lam = inp["lru_lambda"][l]
    if half:
        gw, gb, lam = gw[::-1], gb[::-1], lam[::-1]
    wbd = np.zeros((128, 2, 2, 2, 128), np.float32)
    for dd in range(2):
        for g in range(2):
            for ch in range(2):
                wbd[0:64, dd, g, ch, 0:64] = gw[dd, g, 2 * ch]
                wbd[64:128, dd, g, ch, 64:128] = gw[dd, g, 2 * ch + 1]
    d["lru_wbd"] = wbd.reshape(128, 8, 128)
    d["lru_gb"] = np.ascontiguousarray(np.stack([_cols(gb[dd, g], 2) for dd in range(2) for g in range(2)], axis=1).reshape(128, 8))
    d["lru_lam"] = np.ascontiguousarray(np.stack([_cols(lam[dd], 2) for dd in range(2)], axis=1).reshape(128, 4))
    hw = inp["hy_conv_w"][l]
    if half:
        hw = hw[::-1]
    d["hy_cw"] = np.ascontiguousarray(hw.T.reshape(6, 128, 3).transpose(1, 0, 2))
    d["hy_cb"] = _cols(inp["hy_conv_b"][l], 6)
    d["hy_w1"] = inp["hy_w1"][l]
    d["hy_w2"] = inp["hy_w2"][l]
    d["hy_b12f"] = np.ascontiguousarray(np.stack([inp["hy_b1"][l], inp["hy_b2"][l], inp["hy_freq"][l]], axis=1))
    w3 = inp["hy_w3"][l]
    dec = inp["hy_decay"][l]
    wa, wb_, da, db = w3[:, :256], w3[:, 256:], dec[:256], dec[256:]
    if half:
        wa, wb_, da, db = wb_, wa, db, da
    d["hy_w3"] = np.ascontiguousarray(np.concatenate([wa, wb_, w3[:, :256]], axis=1))
    d["hy_dec"] = np.ascontiguousarray(np.concatenate([_cols(da, 2), _cols(db, 2)], axis=1))
    d["hy_bias"] = _cols(inp["hy_bias"][l], 2)
    d["out_g"] = _cols(inp["out_norm_g"][l], 8)
    d["w_out"] = inp["w_out"][l]
    d["router_w"] = inp["router_w"][l]
    d["router_b"] = np.ascontiguousarray(np.broadcast_to(inp["router_b"][l][None, :], (128, NE)).astype(np.float32))
    d["moe_w1"] = inp["moe_w1"][l]
    b1 = inp["moe_b1"][l]
    d["moe_b1"] = np.ascontiguousarray(np.concatenate(
        [b1[:, 0::2].reshape(NE, 8, 128).transpose(2, 0, 1), b1[:, 1::2].reshape(NE, 8, 128).transpose(2, 0, 1)], axis=2))
    d["moe_w2"] = inp["moe_w2"][l]
    d["moe_b2"] = inp["moe_b2"][l]
    for k in ("ident", "prot", "blk64", "sel", "zT", "t01", "zTc", "t01c", "F1", "C2", "FI0", "FI1"):
        d[k] = c[k]
    return d


IN_SPECS = [
    ("x", [S, D], F32), ("ctx", [LC, D], F32), ("cvec", [128, 16], F32), ("w_mod", [D, 6 * D], F32),
    ("bmod", [128, 48], F32), ("gmix", [128, 8], F32), ("gffn", [128, 8], F32), ("w_in", [D, 2048], F32),
    ("qkg", [128, 2], F32), ("qkg_row", [128, 128], F32), ("rope_cos", [128, S], F32), ("rope_sin", [128, S], F32),
    ("lru_cw", [128, 2, 5], F32), ("lru_cb", [128, 2], F32), ("lru_wbd", [128, 8, 128], F32), ("lru_gb", [128, 8], F32),
    ("lru_lam", [128, 4], F32), ("hy_cw", [128, 6, 3], F32), ("hy_cb", [128, 6], F32), ("hy_w1", [33, 64], F32),
    ("hy_w2", [64, 64], F32), ("hy_b12f", [64, 3], F32), ("hy_w3", [64, 768], F32), ("hy_dec", [128, 4], F32),
    ("hy_bias", [128, 2], F32), ("out_g", [128, 8], F32), ("w_out", [D, D], F32), ("router_w", [D, NE], F32),
    ("router_b", [128, NE], F32), ("moe_w1", [NE, D, 2 * D], F32), ("moe_b1", [128, NE, 16], F32),
    ("moe_w2", [NE, D, D], F32), ("moe_b2", [NE, D], F32),
    ("ident", [128, 128], F32), ("prot", [128, 128], F32), ("blk64", [128, 128], F32), ("sel", [65, 64], F32),
    ("zT", [33, S], F32), ("t01", [128, S], F32), ("zTc", [33, LC], F32), ("t01c", [128, LC], F32),
    ("F1", [64, 128, 2, 128], BF16), ("C2", [128, 3, 128], BF16), ("FI0", [128, 128, 2, 32], BF16), ("FI1", [128, 128, 2, 32], BF16),
]


SHARED = ("x", "ctx", "cvec", "rope_cos", "rope_sin", "ident", "prot", "blk64", "sel", "zT", "t01", "zTc", "t01c", "F1", "C2", "FI0", "FI1")


class LayerProg:
    def __init__(self, layers=((0, S, False), (1, SH, True)), stop_after=None, dbg=()):
        self.nc = nc = bass.Bass("TRN2", target_bir_lowering=False)
        self.m = MK(nc)
        specs = {n: (sh, dt) for n, sh, dt in IN_SPECS}
        prog = self

        class _Lazy(dict):
            def __missing__(d, n):
                base = n.split("@")[0]
                sh, dt = specs[base]
                d[n] = nc.dram_tensor(n.replace("@", "_L"), list(sh), dt, kind="ExternalInput").ap()
                return d[n]

        class _View:
            def __getitem__(v, n):
                return prog.Iall[n if n in SHARED else "%s@%d" % (n, prog.l)]
        self.Iall = _Lazy()
        self.I = _View()
        self.stop_after = stop_after
        self.dbg = set(dbg)
        self.out_names = []
        k = "ExternalOutput" if "scratch" in self.dbg else "Internal"
        sc = lambda n, sh, dt=F32: nc.dram_tensor(n, list(sh), dt, kind=k).ap()
        self.U = sc("sU", [10, 128, NALL])
        self.OT = sc("sOT", [D, NALL])
        self.HF = sc("sHF", [2, 128, NALL])
        self.VV = sc("sVV", [256, S])
        self.X0 = sc("sX0", [256, S])
        self.KF = sc("sKF", [512, S])
        self.XM = sc("sXM", [NALL, D])
        self.FT = sc("sFT", [128, 8, NALL], BF16)
        self.QTs = sc("sQT", [4, 128, S], BF16)
        self.VVb = sc("sVVb", [256, S], BF16)
        self.KFb = sc("sKFb", [512, S], BF16)
        self.X1 = sc("sX1", [S, D])
        self.C1 = sc("sC1", [LC, D])
        if k == "ExternalOutput":
            self.out_names += ["sU", "sOT", "sHF", "sVV", "sX0", "sKF", "sXM", "sFT", "sQT", "sX1", "sC1"]
        self.x_out = nc.dram_tensor("x_out", [SH, D], F32, kind="ExternalOutput").ap()
        self.out_names += ["x_out"]
        self.layers = list(layers)
        self.build()

    def build(self):
        m = self.m
        with Phase(m) as G:
            self.G = G
            self.setup_globals()
            steps = ["mod", "inproj_attn", "lru", "hyena", "merge", "moe"]
            done = False
            for li, (l, nown, last) in enumerate(self.layers):
                self.l, self.nown, self.last = l, nown, last
                self.ntok = nown + (0 if last else LC)
                self.xsrc = self.Iall["x"] if li == 0 else self.X1
                self.ctxsrc = self.Iall["ctx"] if li == 0 else self.C1
                for name in steps:
                    getattr(self, "phase_" + name)()
                    m.barrier()
                    if self.stop_after == "%s%d" % (name, l) or (name == "inproj_attn" and self.stop_after == "inproj%d" % l):
                        done = True
                        break
                if done:
                    break
            m.barrier()

    def setup_globals(self):
        m, G, I = self.m, self.G, self.I
        self.identf = G.sb([128, 128]); self.d_const = Dep()
        self.ident = G.sb([128, 128], BF16)
        self.onesf = G.sb([128, 128])
        self.epsT = G.sb([128, 1])
        m.dma(self.identf[:], I["ident"][:, :], writes=[self.d_const])
        m.op("dve", lambda e: e.tensor_copy(out=self.ident[:], in_=self.identf[:]), reads=[self.d_const], writes=[self.d_const])
        m.op("dve", lambda e: e.memset(self.onesf[:], 1.0), writes=[self.d_const])
        m.op("dve", lambda e: e.memset(self.epsT[:], EPS), writes=[self.d_const])
        self.modL = G.sb([128, 48]); self.modC = G.sb([128, 48]); self.d_mod = Dep()
        self.AB = G.sb([128, 4, 8])
        self.gbc = G.sb([128, 4, D])
        self.d_gbc = Dep()

    def phase_mod(self):
        m, I = self.m, self.I
        with Phase(m) as ph:
            cv = ph.sb([128, 16]); d_cv = Dep()
            m.dma(cv[:], I["cvec"][:, :], writes=[d_cv])
            sc = ph.sb([128, 8, 2]); d_sc = Dep()
            m.op("act", lambda e: e.activation(out=sc[:, :, 0], in_=cv[:, 0:8], func=AF.Silu), reads=[d_cv], writes=[d_sc])
            m.op("act", lambda e: e.activation(out=sc[:, :, 1], in_=cv[:, 8:16], func=AF.Silu), reads=[d_cv], writes=[d_sc])
            bm = ph.sb([128, 48]); gm = ph.sb([128, 2, 8]); d_bm = Dep()
            m.dma(bm[:], I["bmod"][:, :], writes=[d_bm])
            m.dma(gm[:, 0, :], I["gmix"][:, :], writes=[d_bm])
            m.dma(gm[:, 1, :], I["gffn"][:, :], writes=[d_bm])
            pm = ph.ps([128, 48, 2]); d_pm = Dep()
            wv = I["w_mod"].rearrange("(kc p) n -> p kc n", p=128)
            wb = [ph.sb([128, 8, 512]) for _ in range(2)]
            d_wb = [Dep(), Dep()]
            for nb in range(12):
                t, dw = wb[nb % 2], d_wb[nb % 2]
                m.dma(t[:], wv[:, :, nb * 512:(nb + 1) * 512], writes=[dw], q=("sp" if nb % 2 == 0 else "act"))
                for j in range(4):
                    col = nb * 4 + j
                    for kc in range(8):
                        m.op("pe", lambda e: e.matmul(pm[:, col, :], lhsT=t[:, kc, j * 128:(j + 1) * 128], rhs=sc[:, kc, :],
                                                      start=(kc == 0), stop=(kc == 7)),
                             reads=[dw, d_sc], writes=[d_pm], inc=(kc == 7))
            m.op("dve", lambda e: e.tensor_tensor(out=self.modL[:], in0=pm[:, :, 0], in1=bm[:], op=ALU.add), reads=[d_pm, d_bm], writes=[self.d_mod])
            m.op("dve", lambda e: e.tensor_tensor(out=self.modC[:], in0=pm[:, :, 1], in1=bm[:], op=ALU.add), reads=[d_pm, d_bm], writes=[self.d_mod])
            tmp = ph.sb([128, 8]); d_tmp = Dep()
            for i, (mt, c0, gi) in enumerate([(self.modL, 8, 0), (self.modL, 32, 1), (self.modC, 8, 0), (self.modC, 32, 1)]):
                m.op("dve", lambda e: e.tensor_scalar(out=tmp[:], in0=mt[:, c0:c0 + 8], scalar1=1.0, scalar2=None, op0=ALU.add),
                     reads=[self.d_mod], writes=[d_tmp])
                m.op("dve", lambda e: e.tensor_tensor(out=self.AB[:, i, :], in0=tmp[:], in1=gm[:, gi, :], op=ALU.mult),
                     reads=[d_tmp, d_bm], writes=[self.d_mod])
            dg = ph.sb([128, 8, 128]); d_dg = Dep()
            pb = ph.ps([128, D]); d_pb = Dep()
            for i, (mt, c0) in enumerate([(self.modL, 16), (self.modL, 40), (self.modC, 16), (self.modC, 40)]):
                for j in range(8):
                    m.op("dve", lambda e: e.tensor_scalar(out=dg[:, j, :], in0=self.identf[:], scalar1=mt[:, c0 + j:c0 + j + 1], scalar2=None,
                                                          op0=ALU.mult), reads=[self.d_mod, self.d_const], writes=[d_dg])
                for h in range(2):
                    m.op("pe", lambda e: e.matmul(pb[:, h * 512:(h + 1) * 512], lhsT=self.onesf[:], rhs=dg[:, 4 * h:4 * h + 4, :],
                                                  start=True, stop=True), reads=[d_dg, self.d_const], writes=[d_pb])
                m.op("act", lambda e: e.activation(out=self.gbc[:, i, :], in_=pb[:], func=AF.Identity), reads=[d_pb], writes=[self.d_gbc])
            if "mod" in self.dbg:
                o = self.nc.dram_tensor("dbg_mod", [128, 96], F32, kind="ExternalOutput").ap(); self.out_names.append("dbg_mod")
                m.dma(o[:, 0:48], self.modL[:], reads=[self.d_mod])
                m.dma(o[:, 48:96], self.modC[:], reads=[self.d_mod])
                o2 = self.nc.dram_tensor("dbg_gbc", [128, 4, D], F32, kind="ExternalOutput").ap(); self.out_names.append("dbg_gbc")
                m.dma(o2[:, :, :], self.gbc[:], reads=[self.d_gbc])

    def phase_inproj_attn(self):
        m, I = self.m, self.I
        with Phase(m) as P:
            KT = P.sb([128, NALL], BF16); d_KT = Dep()
            QT = None; d_QT = Dep()
            QC = P.sb([128, 4, LC], BF16); d_QC = Dep()
            VA = P.sb([128, 66, 2, 65], BF16); d_VA = Dep()
            negM = P.sb([128, 1]); d_negM = Dep()
            m.op("pool", lambda e: e.memset(VA[:, :, :, 64:65], 1.0), writes=[d_VA])
            self._inproj(KT, d_KT, QT, d_QT, QC, d_QC, VA, d_VA, negM, d_negM)
            m.barrier()
            if self.stop_after == "inproj%d" % self.l:
                return
            self._attention(KT, d_KT, QT, d_QT, QC, d_QC, VA, d_VA, negM, d_negM)

    def _inproj(self, KT, d_KT, QT, d_QT, QC, d_QC, VA, d_VA, negM, d_negM):
        m, I = self.m, self.I
        with Phase(m) as ph:
            w_in = ph.sb([128, 8, 2048], BF16); d_w = Dep()
            wv = I["w_in"].rearrange("(kc p) n -> p kc n", p=128)
            for kc in range(8):
                m.dma(w_in[:, kc, :], wv[:, kc, :], writes=[d_w], q="pool")
            cst = ph.sb([128, 2, 128], BF16); d_cst = Dep()
            m.dma(cst[:, 0, :], I["prot"][:, :], writes=[d_cst], q="pool")
            m.dma(cst[:, 1, :], I["blk64"][:, :], writes=[d_cst], q="pool")
            qkg = ph.sb([128, 2]); grow = ph.sb([128, 128])
            m.dma(qkg[:], I["qkg"][:, :], writes=[d_cst])
            m.dma(grow[:], I["qkg_row"][:, :], writes=[d_cst])
            mq = ph.sb([128, 2])
            m.op("dve", lambda e: e.tensor_reduce(out=mq[:, 0:1], in_=grow[:, 0:64], axis=AX.X, op=ALU.max, apply_absolute_value=True),
                 reads=[d_cst], writes=[d_negM])
            m.op("dve", lambda e: e.tensor_reduce(out=mq[:, 1:2], in_=grow[:, 64:128], axis=AX.X, op=ALU.max, apply_absolute_value=True),
                 reads=[d_cst], writes=[d_negM])
            m.op("dve", lambda e: e.tensor_scalar(out=negM[:], in0=mq[:, 0:1], scalar1=mq[:, 1:2], scalar2=-8.0, op0=ALU.mult, op1=ALU.mult),
                 reads=[d_negM], writes=[d_negM])
            xr = [ph.sb([128, D]) for _ in range(8)]; d_xr = [Dep() for _ in range(8)]
            xn = [ph.sb([128, D], BF16) for _ in range(8)]; d_xn = [Dep() for _ in range(8)]
            junk = ph.sb([128, D], BF16); d_junk = Dep()
            hT = [ph.sb([128, 8, 512], BF16) for _ in range(2)]; d_hT = [Dep(), Dep()]
            ss = [ph.sb([128, 4]) for _ in range(2)]; d_ss = [Dep(), Dep()]
            cs = [ph.sb([128, 2, 512]) for _ in range(2)]; d_cs = [Dep(), Dep()]
            stg = [ph.sb([128, 512]) for _ in range(4)]; d_stg = [Dep() for _ in range(4)]
            sq = [ph.sb([128, 512], BF16) for _ in range(2)]; d_sq = [Dep(), Dep()]
            f1 = [ph.sb([128, 512]) for _ in range(2)]; d_f1 = [Dep(), Dep()]
            f2 = [ph.sb([128, 512]) for _ in range(2)]; d_f2 = [Dep(), Dep()]
            qn = [ph.sb([128, 512], BF16) for _ in range(2)]; d_qn = [Dep(), Dep()]
            pT = ph.ps([128, 4, 512], BF16); d_pT = Dep()
            pr = [ph.ps([128, 512]) for _ in range(5)]; d_pr = [Dep() for _ in range(5)]
            pv = ph.ps([128, 4, 128]); d_pv = Dep()
            cnt = {"pr": 0, "stg": 0, "qk": 0, "x": 0, "xn": 0, "qst": 0}
            qst = [ph.sb([128, 512], BF16) for _ in range(3)]; d_qst = [Dep() for _ in range(3)]

            def nxt(key, n):
                i = cnt[key] % n
                cnt[key] += 1
                return i

            blocks = [("lat", i * 512, 512, i * 512 < self.nown) for i in range(16)] + [("ctx", 0, LC, not self.last)]
            for bi, (kind, t0, ntok, own) in enumerate(blocks):
                src = self.xsrc if kind == "lat" else self.ctxsrc
                ntile = ntok // 128
                mi = 0 if kind == "lat" else 2
                modt = self.modL if kind == "lat" else self.modC
                col0 = t0 if kind == "lat" else S
                h = hT[bi % 2]; dh = d_hT[bi % 2]
                s_ = ss[bi % 2]; ds_ = d_ss[bi % 2]
                xs, xns = [], []
                for tt in range(ntile):
                    xi = nxt("x", 8)
                    m.dma(xr[xi][:], src[t0 + tt * 128:t0 + (tt + 1) * 128, :], writes=[d_xr[xi]], q="sp")
                    m.op("act", lambda e: e.activation(out=junk[:], in_=xr[xi][:], func=AF.Square, accum_out=s_[:, tt:tt + 1]),
                         reads=[d_xr[xi]], writes=[d_junk, ds_])
                    xs.append(xi)
                m.op("dve", lambda e: e.tensor_scalar(out=s_[:, 0:ntile], in0=s_[:, 0:ntile], scalar1=1.0 / D, scalar2=EPS, op0=ALU.mult, op1=ALU.add),
                     reads=[ds_], writes=[ds_])
                m.op("act", lambda e: e.activation(out=s_[:, 0:ntile], in_=s_[:, 0:ntile], func=AF.Sqrt), reads=[ds_], writes=[ds_])
                m.op("dve", lambda e: e.reciprocal(out=s_[:, 0:ntile], in_=s_[:, 0:ntile]), reads=[ds_], writes=[ds_])
                for tt in range(ntile):
                    ni = nxt("xn", 8)
                    m.op("dve", lambda e: e.tensor_scalar(out=xn[ni][:], in0=xr[xs[tt]][:], scalar1=s_[:, tt:tt + 1], scalar2=None, op0=ALU.mult),
                         reads=[d_xr[xs[tt]], ds_], writes=[d_xn[ni]])
                    xns.append(ni)
                for hf in range(2):
                    for kcl in range(4):
                        kc = hf * 4 + kcl
                        for tt in range(ntile):
                            m.op("pe", lambda e: e.transpose(out=pT[:, kcl, tt * 128:(tt + 1) * 128], in_=xn[xns[tt]][:, kc * 128:(kc + 1) * 128],
                                                             identity=self.ident[:]),
                                 reads=[d_xn[xns[tt]], self.d_const], writes=[d_pT], inc=(kcl == 3 and tt == ntile - 1))
                    for kcl in range(4):
                        kc = hf * 4 + kcl
                        m.op("dve", lambda e: e.tensor_scalar(out=h[:, kc, 0:ntok], in0=pT[:, kcl, 0:ntok], scalar1=self.AB[:, mi, kc:kc + 1],
                                                              scalar2=modt[:, kc:kc + 1], op0=ALU.mult, op1=ALU.add),
                             reads=[d_pT, self.d_mod], writes=[dh])
                if "hT" in self.dbg and bi in (0, 16):
                    nm = f"dbg_hT{bi}"
                    o = self.nc.dram_tensor(nm, [128, 8, 512], BF16, kind="ExternalOutput").ap(); self.out_names.append(nm)
                    m.dma(o[:, :, 0:ntok], h[:, :, 0:ntok], reads=[dh])
                if kind == "lat":
                    ci = bi % 2
                    m.dma(cs[ci][:, 0, :], I["rope_cos"][:, t0:t0 + 512], writes=[d_cs[ci]], q="sp")
                    m.dma(cs[ci][:, 1, :], I["rope_sin"][:, t0:t0 + 512], writes=[d_cs[ci]], q="sp")

                def proj(c0):
                    pi = nxt("pr", 5)
                    for kc in range(8):
                        m.op("pe", lambda e: e.matmul(pr[pi][:, 0:ntok], lhsT=w_in[:, kc, c0:c0 + 128], rhs=h[:, kc, 0:ntok],
                                                      start=(kc == 0), stop=(kc == 7)),
                             reads=[d_w, dh], writes=[d_pr[pi]], inc=(kc == 7))
                    return pi

                def qk_post(pi, gcol, rope, out_ap, d_out):
                    k_ = nxt("qk", 2)
                    m.op("act", lambda e: e.activation(out=sq[k_][:, 0:ntok], in_=pr[pi][:, 0:ntok], func=AF.Square),
                         reads=[d_pr[pi]], writes=[d_sq[k_]])
                    p2 = nxt("pr", 5)
                    m.op("pe", lambda e: e.matmul(pr[p2][:, 0:ntok], lhsT=cst[:, 1, :], rhs=sq[k_][:, 0:ntok], start=True, stop=True),
                         reads=[d_cst, d_sq[k_]], writes=[d_pr[p2]])
                    m.op("act", lambda e: e.activation(out=f1[k_][:, 0:ntok], in_=pr[p2][:, 0:ntok], func=AF.Sqrt, bias=self.epsT[:, 0:1]),
                         reads=[d_pr[p2], self.d_const], writes=[d_f1[k_]])
                    m.op("dve", lambda e: e.reciprocal(out=f1[k_][:, 0:ntok], in_=f1[k_][:, 0:ntok]), reads=[d_f1[k_]], writes=[d_f1[k_]])
                    if not rope:
                        m.op("dve", lambda e: e.scalar_tensor_tensor(out=out_ap, in0=pr[pi][:, 0:ntok], scalar=qkg[:, gcol:gcol + 1],
                                                                     in1=f1[k_][:, 0:ntok], op0=ALU.mult, op1=ALU.mult),
                             reads=[d_pr[pi], d_f1[k_], d_cst], writes=[d_out])
                        return
                    m.op("dve", lambda e: e.scalar_tensor_tensor(out=qn[k_][:, 0:ntok], in0=pr[pi][:, 0:ntok], scalar=qkg[:, gcol:gcol + 1],
                                                                 in1=f1[k_][:, 0:ntok], op0=ALU.mult, op1=ALU.mult),
                         reads=[d_pr[pi], d_f1[k_], d_cst], writes=[d_qn[k_]])
                    p3 = nxt("pr", 5)
                    m.op("pe", lambda e: e.matmul(pr[p3][:, 0:ntok], lhsT=cst[:, 0, :], rhs=qn[k_][:, 0:ntok], start=True, stop=True),
                         reads=[d_cst, d_qn[k_]], writes=[d_pr[p3]])
                    ci = bi % 2
                    m.op("dve", lambda e: e.tensor_tensor(out=f1[k_][:, 0:ntok], in0=qn[k_][:, 0:ntok], in1=cs[ci][:, 0, 0:ntok], op=ALU.mult),
                         reads=[d_qn[k_], d_cs[ci]], writes=[d_f1[k_]])
                    m.op("dve", lambda e: e.tensor_tensor(out=f2[k_][:, 0:ntok], in0=pr[p3][:, 0:ntok], in1=cs[ci][:, 1, 0:ntok], op=ALU.mult),
                         reads=[d_pr[p3], d_cs[ci]], writes=[d_f2[k_]])
                    m.op("pool", lambda e: e.tensor_tensor(out=out_ap, in0=f1[k_][:, 0:ntok], in1=f2[k_][:, 0:ntok], op=ALU.add),
                         reads=[d_f1[k_], d_f2[k_]], writes=[d_out])

                if own:
                    for j in range(4):
                        pi = proj(j * 128)
                        if kind == "lat":
                            qs = nxt("qst", 3)
                            qk_post(pi, 0, True, qst[qs][:], d_qst[qs])
                            m.dma(self.QTs[j, :, t0:t0 + 512], qst[qs][:], reads=[d_qst[qs]], q="act")
                        else:
                            qk_post(pi, 0, False, QC[:, j, :], d_QC)
                pi = proj(512)
                qk_post(pi, 1, kind == "lat", KT[:, col0:col0 + ntok], d_KT)
                for tt in range(ntile):
                    for kc in range(8):
                        m.op("pe", lambda e: e.matmul(pv[:, tt, :], lhsT=h[:, kc, tt * 128:(tt + 1) * 128], rhs=w_in[:, kc, 640:768],
                                                      start=(kc == 0), stop=(kc == 7)),
                             reads=[dh, d_w], writes=[d_pv], inc=(kc == 7))
                kt0 = col0 // 128
                m.op("act", lambda e: e.activation(out=VA[:, kt0:kt0 + ntile, :, 0:64],
                                                   in_=pv[:, 0:ntile, :].rearrange("p t (h d) -> p t h d", h=2), func=AF.Identity),
                     reads=[d_pv], writes=[d_VA])
                for ci_ in range(10):
                    pi = proj(768 + ci_ * 128)
                    si = nxt("stg", 4)
                    m.op("act", lambda e: e.activation(out=stg[si][:, 0:ntok], in_=pr[pi][:, 0:ntok], func=AF.Identity),
                         reads=[d_pr[pi]], writes=[d_stg[si]])
                    m.dma(self.U[ci_, :, col0:col0 + ntok], stg[si][:, 0:ntok], reads=[d_stg[si]], q="act")

    def _attention(self, KT, d_KT, QT, d_QT, QC, d_QC, VA, d_VA, negM, d_negM):
        m, I = self.m, self.I
        with Phase(m) as ph:
            self_f = ph.sb([65, 64]); d_sel = Dep()
            m.dma(self_f[:], I["sel"][:, :], writes=[d_sel])
            pS = [[ph.ps([128, 512]) for _ in range(2)] for _ in range(3)]
            d_pS = [[Dep(), Dep()] for _ in range(3)]
            pO = [ph.ps([65, 512]) for _ in range(2)]; d_pO = [Dep(), Dep()]
            Pt = [[ph.sb([128, 512], BF16) for _ in range(2)] for _ in range(3)]
            d_Pt = [[Dep(), Dep()] for _ in range(3)]
            osb = [ph.sb([65, 512]) for _ in range(2)]; d_osb = [Dep(), Dep()]
            att = [ph.sb([64, 512]) for _ in range(4)]; d_att = [Dep() for _ in range(4)]
            acnt = [0]

            qtl = [ph.sb([128, 512], BF16) for _ in range(3)]; d_qtl = [Dep() for _ in range(3)]
            qcnt = [0]

            def run(qsrc, d_q, j, q0, nq, keys, out_col0):
                nk = len(keys)

                def issue_S(i):
                    kc0, _ = keys[i]
                    for hb in range(2):
                        lo = hb * 64
                        m.op("pe", lambda e: e.matmul(pS[i % 3][hb][:, 0:nq], lhsT=KT[lo:lo + 64, kc0:kc0 + 128],
                                                      rhs=qsrc[lo:lo + 64, 0:nq], start=True, stop=True),
                             reads=[d_KT, d_q], writes=[d_pS[i % 3][hb]])
                issue_S(0)
                if nk > 1:
                    issue_S(1)
                for i in range(nk):
                    if i + 2 < nk:
                        issue_S(i + 2)
                    _, vt = keys[i]
                    for hb in range(2):
                        P_ = Pt[i % 3][hb]; dP = d_Pt[i % 3][hb]
                        m.op("act", lambda e: e.activation(out=P_[:, 0:nq], in_=pS[i % 3][hb][:, 0:nq], func=AF.Exp, scale=0.125, bias=negM[:, 0:1]),
                             reads=[d_pS[i % 3][hb], d_negM], writes=[dP])
                        m.op("pe", lambda e: e.matmul(pO[hb][:, 0:nq], lhsT=VA[:, vt, hb, :], rhs=P_[:, 0:nq], start=(i == 0), stop=(i == nk - 1)),
                             reads=[d_VA, dP], writes=[d_pO[hb]], inc=(i == nk - 1))
                for hb in range(2):
                    head = j + 4 * hb
                    o_ = osb[hb]; do = d_osb[hb]
                    m.op("act", lambda e: e.activation(out=o_[:, 0:nq], in_=pO[hb][:, 0:nq], func=AF.Identity), reads=[d_pO[hb]], writes=[do])
                    m.op("dve", lambda e: e.reciprocal(out=o_[64:65, 0:nq], in_=o_[64:65, 0:nq]), reads=[do], writes=[do])
                    pB = pS[nk % 3][hb]; d_pB = d_pS[nk % 3][hb]
                    m.op("pe", lambda e: e.matmul(pB[0:64, 0:nq], lhsT=self_f[:], rhs=o_[:, 0:nq], start=True, stop=True),
                         reads=[do, d_sel], writes=[d_pB])
                    ai = acnt[0] % 4; acnt[0] += 1
                    m.op("dve", lambda e: e.tensor_tensor(out=att[ai][:, 0:nq], in0=o_[0:64, 0:nq], in1=pB[0:64, 0:nq], op=ALU.mult),
                         reads=[do, d_pB], writes=[d_att[ai]])
                    m.dma(self.OT[head * 64:(head + 1) * 64, out_col0:out_col0 + nq], att[ai][:, 0:nq], reads=[d_att[ai]], q="sp")

            all_keys = [(kt * 128, kt) for kt in range(66)]
            ctx_keys = [(S + i * 128, 64 + i) for i in range(2)]
            nqb = self.nown // 512 if "att_short" not in self.dbg else 1
            for qb in range(nqb):
                for j in range(4):
                    qi = qcnt[0] % 3; qcnt[0] += 1
                    m.dma(qtl[qi][:], self.QTs[j, :, qb * 512:(qb + 1) * 512], writes=[d_qtl[qi]], q="sp")
                    run(qtl[qi], d_qtl[qi], j, qb * 512, 512, all_keys, qb * 512)
            if not self.last:
                for j in range(4):
                    run(QC[:, j, :], d_QC, j, 0, LC, ctx_keys, self.nown)

    def phase_lru(self):
        m, I = self.m, self.I
        SEG = 2048
        with Phase(m) as ph:
            d_c = Dep()
            cw = ph.sb([128, 2, 5]); cb = ph.sb([128, 2]); gb = ph.sb([128, 8]); lam = ph.sb([128, 4]); cvec = ph.sb([128, 4])
            wbd = ph.sb([128, 8, 128], BF16)
            m.dma(cw[:], I["lru_cw"][:, :, :], writes=[d_c]); m.dma(cb[:], I["lru_cb"][:, :], writes=[d_c])
            m.dma(gb[:], I["lru_gb"][:, :], writes=[d_c]); m.dma(lam[:], I["lru_lam"][:, :], writes=[d_c])
            m.dma(wbd[:], I["lru_wbd"][:, :, :], writes=[d_c], q="pool")
            m.op("act", lambda e: e.activation(out=cvec[:], in_=lam[:], func=AF.Exp, scale=-1.0), reads=[d_c], writes=[d_c])
            m.op("act", lambda e: e.activation(out=cvec[:], in_=cvec[:], func=AF.Ln, bias=1.0), reads=[d_c], writes=[d_c])
            m.op("dve", lambda e: e.tensor_scalar(out=cvec[:], in0=cvec[:], scalar1=-8.0, scalar2=None, op0=ALU.mult), reads=[d_c], writes=[d_c])
            lxp = [ph.sb([128, SEG + 4]) for _ in range(2)]; d_lxp = [Dep(), Dep()]
            xl = ph.sb([128, SEG]); d_xl = Dep()
            xlb = ph.sb([128, SEG], BF16); d_xlb = Dep()
            aS = ph.sb([128, SEG]); d_aS = Dep()
            bS = ph.sb([128, SEG]); d_bS = Dep()
            hS = [ph.sb([128, SEG]) for _ in range(2)]; d_hS = [Dep(), Dep()]
            hfS = [ph.sb([128, SEG]) for _ in range(2)]; d_hfS = [Dep(), Dep()]
            lgS = [ph.sb([128, SEG]) for _ in range(2)]; d_lgS = [Dep(), Dep()]
            g1 = ph.sb([128, SEG]); d_g1 = Dep()
            g2 = ph.sb([128, SEG]); d_g2 = Dep()
            tR = [ph.sb([128, 512]) for _ in range(2)]; d_tR = [Dep(), Dep()]
            tI = [ph.sb([128, 512]) for _ in range(2)]; d_tI = [Dep(), Dep()]
            tA = [ph.sb([128, 512]) for _ in range(2)]; d_tA = [Dep(), Dep()]
            pG = [[ph.ps([128, 512]) for _ in range(2)] for _ in range(2)]; d_pG = [[Dep(), Dep()], [Dep(), Dep()]]
            cr = ph.sb([128, 1]); d_cr = Dep()
            segs_f = [("ctx", 0, LC)] + [("lat", i * SEG, SEG) for i in range(S // SEG)]
            segs_b = [("ctx", 0, LC)] + [("lat", i * SEG, SEG) for i in reversed(range(S // SEG))]
            sc = 0
            for dirn in range(2):
                for c in range(2):
                    m.op("dve", lambda e: e.memset(cr[:], 0.0), writes=[d_cr])
                    for (kind, t0, ln) in (segs_f if dirn == 0 else segs_b):
                        seqlen = S if kind == "lat" else LC
                        base = 0 if kind == "lat" else S
                        bi = sc % 2; sc += 1
                        lp = lxp[bi]; dlp = d_lxp[bi]
                        lo = max(t0 - 2, 0); hi = min(t0 + ln + 2, seqlen)
                        if t0 - 2 < 0:
                            m.op("pool", lambda e: e.memset(lp[:, 0:2], 0.0), writes=[dlp])
                        if t0 + ln + 2 > seqlen:
                            m.op("pool", lambda e: e.memset(lp[:, ln + 2:ln + 4], 0.0), writes=[dlp])
                        m.dma(lp[:, lo - (t0 - 2):hi - (t0 - 2)], self.U[c, :, base + lo:base + hi], writes=[dlp], q="sp")
                        own = (kind == "ctx" and not self.last) or (kind == "lat" and t0 < self.nown)
                        if dirn == 1:
                            m.dma(hfS[bi][:, 0:ln], self.HF[c, :, base + t0:base + t0 + ln], writes=[d_hfS[bi]], q="sp")
                            if own:
                                m.dma(lgS[bi][:, 0:ln], self.U[2 + c, :, base + t0:base + t0 + ln], writes=[d_lgS[bi]], q="sp")
                        m.op("dve", lambda e: e.tensor_scalar(out=xl[:, 0:ln], in0=lp[:, 0:ln], scalar1=cw[:, c, 0:1], scalar2=cb[:, c:c + 1],
                                                              op0=ALU.mult, op1=ALU.add), reads=[dlp, d_c], writes=[d_xl])
                        for o in range(1, 5):
                            m.op("dve", lambda e: e.scalar_tensor_tensor(out=xl[:, 0:ln], in0=lp[:, o:o + ln], scalar=cw[:, c, o:o + 1], in1=xl[:, 0:ln],
                                                                         op0=ALU.mult, op1=ALU.add), reads=[dlp, d_c, d_xl], writes=[d_xl])
                        m.op("act", lambda e: e.activation(out=xlb[:, 0:ln], in_=xl[:, 0:ln], func=AF.Identity), reads=[d_xl], writes=[d_xlb])
                        for sb_ in range((ln + 511) // 512):
                            s0 = sb_ * 512; n = min(512, ln - s0); k_ = sb_ % 2
                            for g in range(2):
                                m.op("pe", lambda e: e.matmul(pG[k_][g][:, 0:n], lhsT=wbd[:, dirn * 4 + g * 2 + c, :], rhs=xlb[:, s0:s0 + n],
                                                              start=True, stop=True), reads=[d_c, d_xlb], writes=[d_pG[k_][g]])
                            gi = dirn * 4 + c
                            m.op("act", lambda e: e.activation(out=tR[k_][:, 0:n], in_=pG[k_][0][:, 0:n], func=AF.Sigmoid, bias=gb[:, gi:gi + 1]),
                                 reads=[d_pG[k_][0], d_c], writes=[d_tR[k_]])
                            m.op("act", lambda e: e.activation(out=tI[k_][:, 0:n], in_=pG[k_][1][:, 0:n], func=AF.Sigmoid, bias=gb[:, gi + 2:gi + 3]),
                                 reads=[d_pG[k_][1], d_c], writes=[d_tI[k_]])
                            m.op("act", lambda e: e.activation(out=aS[:, s0:s0 + n], in_=tR[k_][:, 0:n], func=AF.Exp, scale=cvec[:, dirn * 2 + c:dirn * 2 + c + 1]),
                                 reads=[d_tR[k_], d_c], writes=[d_aS])
                            m.op("pool", lambda e: e.tensor_tensor(out=tA[k_][:, 0:n], in0=aS[:, s0:s0 + n], in1=aS[:, s0:s0 + n], op=ALU.mult),
                                 reads=[d_aS], writes=[d_tA[k_]])
                            m.op("pool", lambda e: e.tensor_scalar(out=tA[k_][:, 0:n], in0=tA[k_][:, 0:n], scalar1=-1.0, scalar2=1.0, op0=ALU.mult, op1=ALU.add),
                                 reads=[d_tA[k_]], writes=[d_tA[k_]])
                            m.op("act", lambda e: e.activation(out=tA[k_][:, 0:n], in_=tA[k_][:, 0:n], func=AF.Sqrt), reads=[d_tA[k_]], writes=[d_tA[k_]])
                            m.op("pool", lambda e: e.tensor_tensor(out=tI[k_][:, 0:n], in0=tI[k_][:, 0:n], in1=xl[:, s0:s0 + n], op=ALU.mult),
                                 reads=[d_tI[k_], d_xl], writes=[d_tI[k_]])
                            m.op("dve", lambda e: e.tensor_tensor(out=bS[:, s0:s0 + n], in0=tA[k_][:, 0:n], in1=tI[k_][:, 0:n], op=ALU.mult),
                                 reads=[d_tA[k_], d_tI[k_]], writes=[d_bS])
                        h_ = hS[bi]; dh = d_hS[bi]
                        if dirn == 0:
                            m.op("dve", lambda e: e.tensor_tensor_scan(out=h_[:, 0:ln], data0=aS[:, 0:ln], data1=bS[:, 0:ln], initial=cr[:, 0:1],
                                                                       op0=ALU.mult, op1=ALU.add), reads=[d_aS, d_bS, d_cr], writes=[dh])
                            m.op("dve", lambda e: e.tensor_copy(out=cr[:], in_=h_[:, ln - 1:ln]), reads=[dh], writes=[d_cr])
                            m.dma(self.HF[c, :, base + t0:base + t0 + ln], h_[:, 0:ln], reads=[dh], q="act")
                        else:
                            m.op("dve", lambda e: e.tensor_tensor_scan(out=h_[:, ln - 1::-1] if ln == SEG else h_[:, ln - 1::-1],
                                                                       data0=aS[:, ln - 1::-1], data1=bS[:, ln - 1::-1], initial=cr[:, 0:1],
                                                                       op0=ALU.mult, op1=ALU.add), reads=[d_aS, d_bS, d_cr], writes=[dh])
                            m.op("dve", lambda e: e.tensor_copy(out=cr[:], in_=h_[:, 0:1]), reads=[dh], writes=[d_cr])
                            if own:
                                lg_ = lgS[bi]; dlg = d_lgS[bi]
                                m.op("pool", lambda e: e.tensor_tensor(out=h_[:, 0:ln], in0=h_[:, 0:ln], in1=hfS[bi][:, 0:ln], op=ALU.add),
                                     reads=[dh, d_hfS[bi]], writes=[dh])
                                m.op("pool", lambda e: e.tensor_tensor(out=g1[:, 0:ln], in0=lg_[:, 0:ln], in1=lg_[:, 0:ln], op=ALU.mult),
                                     reads=[dlg], writes=[d_g1])
                                m.op("pool", lambda e: e.tensor_scalar(out=g1[:, 0:ln], in0=g1[:, 0:ln], scalar1=0.044715, scalar2=1.0, op0=ALU.mult, op1=ALU.add),
                                     reads=[d_g1], writes=[d_g1])
                                m.op("pool", lambda e: e.tensor_tensor(out=g1[:, 0:ln], in0=g1[:, 0:ln], in1=lg_[:, 0:ln], op=ALU.mult),
                                     reads=[d_g1, dlg], writes=[d_g1])
                                m.op("act", lambda e: e.activation(out=g1[:, 0:ln], in_=g1[:, 0:ln], func=AF.Sigmoid, scale=1.5957691216057308),
                                     reads=[d_g1], writes=[d_g1])
                                m.op("dve", lambda e: e.tensor_tensor(out=g1[:, 0:ln], in0=g1[:, 0:ln], in1=lg_[:, 0:ln], op=ALU.mult),
                                     reads=[d_g1, dlg], writes=[d_g1])
                                m.op("dve", lambda e: e.tensor_tensor(out=g2[:, 0:ln], in0=g1[:, 0:ln], in1=h_[:, 0:ln], op=ALU.mult),
                                     reads=[d_g1, dh], writes=[d_g2])
                                oc = t0 if kind == "lat" else self.nown
                                m.dma(self.OT[512 + c * 128:512 + (c + 1) * 128, oc:oc + ln], g2[:, 0:ln], reads=[d_g2], q="act")
                    m.barrier()

    def phase_hyena(self):
        self._hy_filters_and_conv()
        self.m.barrier()
        self._hy_fft()

    def _sin9(self, m, ph, out, psum, n, sc, bi, d_in, d_out, tmp, d_tmp, d_c):
        m.op("act", lambda e: e.activation(out=out, in_=psum, func=AF.Sin, scale=sc, bias=bi), reads=[d_in, d_c], writes=[d_out])
        for _ in range(2):
            m.op("dve", lambda e: e.tensor_tensor(out=tmp, in0=out, in1=out, op=ALU.mult), reads=[d_out], writes=[d_tmp])
            m.op("dve", lambda e: e.tensor_scalar(out=tmp, in0=tmp, scalar1=-4.0, scalar2=3.0, op0=ALU.mult, op1=ALU.add), reads=[d_tmp], writes=[d_tmp])
            m.op("dve", lambda e: e.tensor_tensor(out=out, in0=out, in1=tmp, op=ALU.mult), reads=[d_out, d_tmp], writes=[d_out])

    def _hy_filters_and_conv(self):
        m, I = self.m, self.I
        G = self.G
        self.hyP = Phase(m); P = self.hyP; P.__enter__()
        self.kc_f = P.sb([128, 4, LC]); self.d_kc = Dep()
        self.nrm = P.sb([128, 2, 4]); self.d_nrm = Dep()
        self.vvc = P.sb([128, 2, LC]); self.x0c = P.sb([128, 2, LC]); self.d_vvc = Dep()
        self.hsc = P.sb([128, 2, 2]); self.d_hsc = Dep()
        with Phase(m) as ph:
            d_c = Dep()
            w1 = ph.sb([33, 64]); w2 = ph.sb([64, 64]); w3 = ph.sb([64, 768]); b12f = ph.sb([64, 3]); dec = ph.sb([128, 4])
            for t_, n_ in ((w1, "hy_w1"), (w2, "hy_w2"), (w3, "hy_w3"), (b12f, "hy_b12f"), (dec, "hy_dec")):
                m.dma(t_[:], I[n_][:, :], writes=[d_c])
            scb = ph.sb([64, 3])
            m.op("dve", lambda e: e.tensor_scalar(out=scb[:, 0:1], in0=b12f[:, 2:3], scalar1=1.0 / 9.0, scalar2=None, op0=ALU.mult), reads=[d_c], writes=[d_c])
            m.op("dve", lambda e: e.tensor_scalar(out=scb[:, 1:3], in0=b12f[:, 0:2], scalar1=scb[:, 0:1], scalar2=None, op0=ALU.mult), reads=[d_c], writes=[d_c])
            nad = ph.sb([128, 4])
            m.op("dve", lambda e: e.tensor_scalar(out=nad[:], in0=dec[:], scalar1=-1.0, scalar2=None, op0=ALU.mult), reads=[d_c], writes=[d_c])
            m.op("dve", lambda e: e.tensor_tensor(out=nad[:], in0=nad[:], in1=dec[:], op=ALU.min), reads=[d_c], writes=[d_c])
            m.op("dve", lambda e: e.memset(self.nrm[:], 0.0), writes=[self.d_nrm])
            zb = [ph.sb([33, 512]) for _ in range(2)]; d_zb = [Dep(), Dep()]
            tb = [ph.sb([128, 512]) for _ in range(2)]; d_tb = [Dep(), Dep()]
            h1 = ph.sb([64, 512]); d_h1 = Dep()
            h2 = ph.sb([64, 512]); d_h2 = Dep()
            tmp = ph.sb([64, 512]); d_tmp = Dep()
            ex = [ph.sb([128, 512]) for _ in range(2)]; d_ex = [Dep(), Dep()]
            kk = [ph.sb([128, 512], BF16) for _ in range(3)]; d_kk = [Dep() for _ in range(3)]
            red = ph.sb([128, 1]); d_red = Dep()
            c0t = ph.sb([128, 2, 2]); d_c0 = Dep()
            p1 = ph.ps([64, 512]); d_p1 = Dep()
            p2 = ph.ps([64, 512]); d_p2 = Dep()
            p3 = [ph.ps([128, 512]) for _ in range(2)]; d_p3 = [Dep(), Dep()]
            pc = ph.ps([128, 2, 2]); d_pc = Dep()
            kc_i = 0
            for which, (zname, tname, L) in enumerate((("zT", "t01", S), ("zTc", "t01c", LC))[:(1 if self.last else 2)]):
                for bi_ in range((L + 511) // 512):
                    c0_ = bi_ * 512; n = min(512, L - c0_); r = bi_ % 2
                    m.dma(zb[r][:, 0:n], I[zname][:, c0_:c0_ + n], writes=[d_zb[r]])
                    m.dma(tb[r][:, 0:n], I[tname][:, c0_:c0_ + n], writes=[d_tb[r]])
                    m.op("pe", lambda e: e.matmul(p1[:, 0:n], lhsT=w1[:], rhs=zb[r][:, 0:n], start=True, stop=True), reads=[d_c, d_zb[r]], writes=[d_p1])
                    self._sin9(m, ph, h1[:, 0:n], p1[:, 0:n], n, scb[:, 0:1], scb[:, 1:2], d_p1, d_h1, tmp[:, 0:n], d_tmp, d_c)
                    m.op("pe", lambda e: e.matmul(p2[:, 0:n], lhsT=w2[:], rhs=h1[:, 0:n], start=True, stop=True), reads=[d_c, d_h1], writes=[d_p2])
                    self._sin9(m, ph, h2[:, 0:n], p2[:, 0:n], n, scb[:, 0:1], scb[:, 2:3], d_p2, d_h2, tmp[:, 0:n], d_tmp, d_c)
                    if bi_ == 0:
                        for c in range(2):
                            m.op("pe", lambda e: e.matmul(pc[:, c, :], lhsT=w3[:, 512 + c * 128:512 + (c + 1) * 128], rhs=h2[:, 0:2], start=True, stop=True),
                                 reads=[d_c, d_h2], writes=[d_pc])
                        m.op("act", lambda e: e.activation(out=c0t[:], in_=pc[:], func=AF.Identity), reads=[d_pc], writes=[d_c0])
                    for ci in range(4):
                        pi = ci % 2
                        m.op("pe", lambda e: e.matmul(p3[pi][:, 0:n], lhsT=w3[:, ci * 128:(ci + 1) * 128], rhs=h2[:, 0:n], start=True, stop=True),
                             reads=[d_c, d_h2], writes=[d_p3[pi]])
                        m.op("act", lambda e: e.activation(out=ex[pi][:, 0:n], in_=tb[r][:, 0:n], func=AF.Exp, scale=nad[:, ci:ci + 1]),
                             reads=[d_tb[r], d_c], writes=[d_ex[pi]])
                        if which == 0:
                            ki = kc_i % 3; kc_i += 1
                            kt_ = kk[ki][:, 0:n]; dk = d_kk[ki]
                        else:
                            kt_ = self.kc_f[:, ci, :]; dk = self.d_kc
                        m.op("dve", lambda e: e.tensor_tensor(out=kt_, in0=p3[pi][:, 0:n], in1=ex[pi][:, 0:n], op=ALU.mult),
                             reads=[d_p3[pi], d_ex[pi]], writes=[dk])
                        if bi_ == 0:
                            if ci < 2:
                                m.op("dve", lambda e: e.tensor_copy(out=kt_[:, 0:1], in_=c0t[:, ci, 0:1]), reads=[d_c0, dk], writes=[dk])
                            else:
                                m.op("dve", lambda e: e.memset(kt_[:, 0:1], 0.0), reads=[dk], writes=[dk])
                        m.op("dve", lambda e: e.tensor_reduce(out=red[:], in_=kt_, axis=AX.X, op=ALU.add, apply_absolute_value=True),
                             reads=[dk], writes=[d_red])
                        m.op("dve", lambda e: e.tensor_tensor(out=self.nrm[:, which, ci:ci + 1], in0=self.nrm[:, which, ci:ci + 1], in1=red[:], op=ALU.add),
                             reads=[d_red, self.d_nrm], writes=[self.d_nrm])
                        if which == 0:
                            m.dma(self.KFb[ci * 128:(ci + 1) * 128, c0_:c0_ + n], kt_, reads=[dk], q="act")
            m.op("dve", lambda e: e.tensor_tensor(out=self.hsc[:], in0=self.nrm[:, :, 0:2], in1=self.nrm[:, :, 2:4], op=ALU.add),
                 reads=[self.d_nrm], writes=[self.d_hsc])
            m.op("dve", lambda e: e.tensor_scalar(out=self.hsc[:, 0, :], in0=self.hsc[:, 0, :], scalar1=float(NFFT), scalar2=None, op0=ALU.mult),
                 reads=[self.d_hsc], writes=[self.d_hsc])
            m.op("dve", lambda e: e.reciprocal(out=self.hsc[:], in_=self.hsc[:]), reads=[self.d_hsc], writes=[self.d_hsc])
        with Phase(m) as ph:
            SEG = 2048
            d_c = Dep()
            cw = ph.sb([128, 6, 3]); cb = ph.sb([128, 6])
            m.dma(cw[:], I["hy_cw"][:, :, :], writes=[d_c]); m.dma(cb[:], I["hy_cb"][:, :], writes=[d_c])
            pad = [[ph.sb([128, SEG + 2]) for _ in range(3)] for _ in range(2)]; d_pad = [[Dep() for _ in range(3)] for _ in range(2)]
            uc = [ph.sb([128, SEG]) for _ in range(3)]; d_uc = [Dep() for _ in range(3)]
            vv = [ph.sb([128, SEG]) for _ in range(2)]; d_vv = [Dep(), Dep()]
            vvb = [ph.sb([128, SEG], BF16) for _ in range(2)]; d_vvb = [Dep(), Dep()]
            sc = 0
            segs = [("lat", i * SEG, SEG) for i in range(S // SEG)] + ([] if self.last else [("ctx", 0, LC)])
            for (kind, t0, ln) in segs:
                seqlen = S if kind == "lat" else LC
                base = 0 if kind == "lat" else S
                for c in range(2):
                    bi = sc % 2; sc += 1
                    for k3 in range(3):
                        ci = 2 * k3 + c
                        lp = pad[bi][k3]; dlp = d_pad[bi][k3]
                        lo = max(t0 - 1, 0); hi = min(t0 + ln + 1, seqlen)
                        if t0 - 1 < 0:
                            m.op("pool", lambda e: e.memset(lp[:, 0:1], 0.0), writes=[dlp])
                        if t0 + ln + 1 > seqlen:
                            m.op("pool", lambda e: e.memset(lp[:, ln + 1:ln + 2], 0.0), writes=[dlp])
                        m.dma(lp[:, lo - (t0 - 1):hi - (t0 - 1)], self.U[4 + ci, :, base + lo:base + hi], writes=[dlp], q="sp")
                        eng = "dve" if k3 != 0 else "pool"
                        u_ = uc[k3]; du = d_uc[k3]
                        if kind == "ctx" and k3 == 0:
                            u_ = self.x0c[:, c, :]
                            du = self.d_vvc
                        else:
                            u_ = u_[:, 0:ln]
                        m.op("dve", lambda e: e.tensor_scalar(out=u_, in0=lp[:, 0:ln], scalar1=cw[:, ci, 0:1], scalar2=cb[:, ci:ci + 1],
                                                              op0=ALU.mult, op1=ALU.add), reads=[dlp, d_c], writes=[du])
                        for o in range(1, 3):
                            m.op("dve", lambda e: e.scalar_tensor_tensor(out=u_, in0=lp[:, o:o + ln], scalar=cw[:, ci, o:o + 1], in1=u_,
                                                                         op0=ALU.mult, op1=ALU.add), reads=[dlp, d_c, du], writes=[du])
                    if kind == "lat":
                        m.op("pool", lambda e: e.tensor_tensor(out=vv[bi][:, 0:ln], in0=uc[1][:, 0:ln], in1=uc[2][:, 0:ln], op=ALU.mult),
                             reads=[d_uc[1], d_uc[2]], writes=[d_vv[bi]])
                        m.dma(self.VV[c * 128:(c + 1) * 128, t0:t0 + ln], vv[bi][:, 0:ln], reads=[d_vv[bi]], q="act")
                        m.op("act", lambda e: e.activation(out=vvb[bi][:, 0:ln], in_=vv[bi][:, 0:ln], func=AF.Identity), reads=[d_vv[bi]], writes=[d_vvb[bi]])
                        m.dma(self.VVb[c * 128:(c + 1) * 128, t0:t0 + ln], vvb[bi][:, 0:ln], reads=[d_vvb[bi]], q="act")
                        if t0 < self.nown:
                            m.dma(self.X0[c * 128:(c + 1) * 128, t0:t0 + ln], uc[0][:, 0:ln], reads=[d_uc[0]], q="act")
                    else:
                        m.op("pool", lambda e: e.tensor_tensor(out=self.vvc[:, c, :], in0=uc[1][:, 0:ln], in1=uc[2][:, 0:ln], op=ALU.mult),
                             reads=[d_uc[1], d_uc[2]], writes=[self.d_vvc])
            yc = ph.sb([128, 2, LC]); d_yc = Dep()
            hb = ph.sb([128, 2]); m.dma(hb[:], I["hy_bias"][:, :], writes=[d_c])
            for c in range(0 if self.last else 2):
                y_ = yc[:, c, :]; v_ = self.vvc[:, c, :]
                m.op("dve", lambda e: e.tensor_scalar(out=y_, in0=v_, scalar1=self.kc_f[:, c, 0:1], scalar2=None, op0=ALU.mult),
                     reads=[self.d_vvc, self.d_kc], writes=[d_yc])
                for dl in range(1, LC):
                    m.op("dve", lambda e: e.scalar_tensor_tensor(out=y_[:, dl:], in0=v_[:, 0:LC - dl], scalar=self.kc_f[:, c, dl:dl + 1], in1=y_[:, dl:],
                                                                 op0=ALU.mult, op1=ALU.add), reads=[d_yc], writes=[d_yc])
                    m.op("dve", lambda e: e.scalar_tensor_tensor(out=y_[:, 0:LC - dl], in0=v_[:, dl:], scalar=self.kc_f[:, 2 + c, dl:dl + 1], in1=y_[:, 0:LC - dl],
                                                                 op0=ALU.mult, op1=ALU.add), reads=[d_yc], writes=[d_yc])
                m.op("dve", lambda e: e.tensor_scalar(out=y_, in0=y_, scalar1=self.hsc[:, 1, c:c + 1], scalar2=None, op0=ALU.mult),
                     reads=[d_yc, self.d_hsc], writes=[d_yc])
                m.op("dve", lambda e: e.scalar_tensor_tensor(out=y_, in0=v_, scalar=hb[:, c:c + 1], in1=y_, op0=ALU.mult, op1=ALU.add),
                     reads=[d_yc, d_c, self.d_vvc], writes=[d_yc])
                m.op("dve", lambda e: e.tensor_tensor(out=y_, in0=y_, in1=self.x0c[:, c, :], op=ALU.mult), reads=[d_yc, self.d_vvc], writes=[d_yc])
                m.dma(self.OT[768 + c * 128:768 + (c + 1) * 128, self.nown:self.nown + LC], y_, reads=[d_yc], q="act")

    def _hy_fft(self):
        m, I = self.m, self.I
        GC = 32
        with Phase(m) as ph:
            d_c = Dep()
            C2 = ph.sb([128, 3, 128], BF16); FI = ph.sb([128, 128, 2, 32], BF16); d_FI = Dep()
            m.dma(C2[:], I["C2"][:, :, :], writes=[d_c])
            fi_loaded = [None]
            x1s = [ph.sb([64, GC, 128], BF16) for _ in range(2)]; d_x1 = [Dep(), Dep()]
            f1p = [ph.sb([64, 16, 2, 128], BF16) for _ in range(2)]; d_f1 = [Dep() for _ in range(2)]
            bP = ph.sb([128, 2, 128, GC], BF16); d_bP = Dep()
            bQ = ph.sb([128, 2, GC, 128], BF16); d_bQ = Dep()
            bZ = ph.sb([128, 2, GC, 128], BF16); d_bZ = Dep()
            Kf = ph.sb([128, 2, GC, 128], BF16); d_Kf = Dep()
            Yb = ph.sb([128, 2, GC, 128], BF16); d_Yb = Dep()
            t4 = [ph.sb([128, 512]) for _ in range(4)]; d_t4 = [Dep() for _ in range(4)]
            ysb = ph.sb([GC, 32, 128]); d_ysb = Dep()
            fv = [ph.sb([GC, 512]) for _ in range(2)]; d_fv = [Dep(), Dep()]
            fx = [ph.sb([GC, 512]) for _ in range(2)]; d_fx = [Dep(), Dep()]
            fo = [ph.sb([GC, 512]) for _ in range(2)]; d_fo = [Dep(), Dep()]
            gsc = ph.sb([GC, 2]); d_gsc = Dep()
            pA = [ph.ps([128, 16, GC]) for _ in range(2)]; d_pA = [Dep(), Dep()]
            pT = [ph.ps([128, 8, 128], BF16) for _ in range(2)]; d_pT = [Dep(), Dep()]
            pX = [[ph.ps([128, 512]) for _ in range(2)] for _ in range(2)]; d_pX = [[Dep(), Dep()], [Dep(), Dep()]]
            cnt = {"x1": 0, "f1": 0, "pA": 0, "pT": 0, "pX": 0, "ev": 0, "fin": 0}

            def nxt(k, n):
                i = cnt[k] % n; cnt[k] += 1
                return i

            def evac(out, in_, reads, writes):
                if nxt("ev", 2) == 0:
                    m.op("act", lambda e: e.activation(out=out, in_=in_, func=AF.Identity), reads=reads, writes=writes)
                else:
                    m.op("dve", lambda e: e.tensor_copy(out=out, in_=in_), reads=reads, writes=writes)

            def fwd(src_rows, mode):
                xi = nxt("x1", 2)
                m.dma(x1s[xi][:], src_rows.rearrange("c (a b) -> a c b", b=128), writes=[d_x1[xi]], q="sp")
                for bg in range(8):
                    fi = nxt("f1", 2)
                    m.dma(f1p[fi][:], I["F1"][:, bg * 16:(bg + 1) * 16, :, :], writes=[d_f1[fi]], q="sp")
                    for hb in range(2):
                        pi = nxt("pA", 2)
                        for bl in range(8):
                            b = bg * 16 + hb * 8 + bl
                            for ri in range(2):
                                m.op("pe", lambda e: e.matmul(pA[pi][:, bl * 2 + ri, :], lhsT=f1p[fi][:, hb * 8 + bl, ri, :], rhs=x1s[xi][:, :, b],
                                                              start=True, stop=True),
                                     reads=[d_f1[fi], d_x1[xi]], writes=[d_pA[pi]], inc=(bl == 7 and ri == 1))
                        b0 = bg * 16 + hb * 8
                        evac(bP[:, :, b0:b0 + 8, :], pA[pi][:].rearrange("p (b r) c -> p r b c", r=2), [d_pA[pi]], [d_bP])
                for ri in range(2):
                    for cg in range(GC // 8):
                        ti = nxt("pT", 2)
                        for k in range(8):
                            ch = cg * 8 + k
                            m.op("pe", lambda e: e.transpose(out=pT[ti][:, k, :], in_=bP[:, ri, :, ch], identity=self.ident[:]),
                                 reads=[d_bP, self.d_const], writes=[d_pT[ti]], inc=(k == 7))
                        evac(bQ[:, ri, cg * 8:(cg + 1) * 8, :], pT[ti][:], [d_pT[ti]], [d_bQ])
                for blk in range(GC // 4):
                    xi_ = nxt("pX", 2)
                    cs_ = slice(blk * 4, blk * 4 + 4)
                    are = bQ[:, 0, cs_, :]; aim = bQ[:, 1, cs_, :]
                    m.op("pe", lambda e: e.matmul(pX[xi_][0][:], lhsT=C2[:, 0, :], rhs=are, start=True, stop=False), reads=[d_c, d_bQ], writes=[d_pX[xi_][0]], inc=False)
                    m.op("pe", lambda e: e.matmul(pX[xi_][0][:], lhsT=C2[:, 1, :], rhs=aim, start=False, stop=True), reads=[d_c, d_bQ], writes=[d_pX[xi_][0]])
                    m.op("pe", lambda e: e.matmul(pX[xi_][1][:], lhsT=C2[:, 0, :], rhs=aim, start=True, stop=False), reads=[d_c, d_bQ], writes=[d_pX[xi_][1]], inc=False)
                    m.op("pe", lambda e: e.matmul(pX[xi_][1][:], lhsT=C2[:, 2, :], rhs=are, start=False, stop=True), reads=[d_c, d_bQ], writes=[d_pX[xi_][1]])
                    pre, pim = pX[xi_][0][:], pX[xi_][1][:]
                    dre, dim_ = d_pX[xi_][0], d_pX[xi_][1]
                    kre = Kf[:, 0, cs_, :]; kim = Kf[:, 1, cs_, :]
                    if mode == "A":
                        evac(kre, pre, [dre], [d_Kf]); evac(kim, pim, [dim_], [d_Kf])
                    elif mode == "B":
                        m.op("dve", lambda e: e.tensor_tensor(out=kre, in0=pre, in1=kre, op=ALU.add), reads=[dre, d_Kf], writes=[d_Kf])
                        m.op("dve", lambda e: e.tensor_tensor(out=kim, in0=kim, in1=pim, op=ALU.subtract), reads=[dim_, d_Kf], writes=[d_Kf])
                    else:
                        a_, b_, c_, e_ = t4
                        m.op("dve", lambda e: e.tensor_tensor(out=a_[:], in0=pre, in1=kre, op=ALU.mult), reads=[dre, d_Kf], writes=[d_t4[0]])
                        m.op("dve", lambda e: e.tensor_tensor(out=b_[:], in0=pim, in1=kim, op=ALU.mult), reads=[dim_, d_Kf], writes=[d_t4[1]])
                        m.op("dve", lambda e: e.tensor_tensor(out=c_[:], in0=pre, in1=kim, op=ALU.mult), reads=[dre, d_Kf], writes=[d_t4[2]])
                        m.op("dve", lambda e: e.tensor_tensor(out=e_[:], in0=pim, in1=kre, op=ALU.mult), reads=[dim_, d_Kf], writes=[d_t4[3]])
                        m.op("dve", lambda e: e.tensor_tensor(out=Yb[:, 0, cs_, :], in0=a_[:], in1=b_[:], op=ALU.subtract),
                             reads=[d_t4[0], d_t4[1]], writes=[d_Yb])
                        m.op("pool", lambda e: e.tensor_tensor(out=Yb[:, 1, cs_, :], in0=c_[:], in1=e_[:], op=ALU.add),
                             reads=[d_t4[2], d_t4[3]], writes=[d_Yb])

            def inv(g):
                for blk in range(GC // 4):
                    xi_ = nxt("pX", 2)
                    cs_ = slice(blk * 4, blk * 4 + 4)
                    yre = Yb[:, 0, cs_, :]; yim = Yb[:, 1, cs_, :]
                    m.op("pe", lambda e: e.matmul(pX[xi_][0][:], lhsT=C2[:, 0, :], rhs=yre, start=True, stop=False), reads=[d_c, d_Yb], writes=[d_pX[xi_][0]], inc=False)
                    m.op("pe", lambda e: e.matmul(pX[xi_][0][:], lhsT=C2[:, 2, :], rhs=yim, start=False, stop=True), reads=[d_c, d_Yb], writes=[d_pX[xi_][0]])
                    m.op("pe", lambda e: e.matmul(pX[xi_][1][:], lhsT=C2[:, 1, :], rhs=yre, start=True, stop=False), reads=[d_c, d_Yb], writes=[d_pX[xi_][1]], inc=False)
                    m.op("pe", lambda e: e.matmul(pX[xi_][1][:], lhsT=C2[:, 0, :], rhs=yim, start=False, stop=True), reads=[d_c, d_Yb], writes=[d_pX[xi_][1]])
                    evac(bZ[:, 0, cs_, :], pX[xi_][0][:], [d_pX[xi_][0]], [d_bZ])
                    evac(bZ[:, 1, cs_, :], pX[xi_][1][:], [d_pX[xi_][1]], [d_bZ])
                for ri in range(2):
                    for cg in range(GC // 8):
                        ti = nxt("pT", 2)
                        for k in range(8):
                            ch = cg * 8 + k
                            m.op("pe", lambda e: e.transpose(out=pT[ti][:, k, :], in_=bZ[:, ri, ch, :], identity=self.ident[:]),
                                 reads=[d_bZ, self.d_const], writes=[d_pT[ti]], inc=(k == 7))
                        evac(bQ[:, ri, cg * 8:(cg + 1) * 8, :], pT[ti][:], [d_pT[ti]], [d_bQ])
                c = (g * GC) // 128; r0 = (g * GC) % 128
                m.dma(gsc[:, 0:1], self.hsc[r0:r0 + GC, 0, c:c + 1], reads=[self.d_hsc], writes=[d_gsc], q="sp", allow_slow_non_contiguous=True)
                m.dma(gsc[:, 1:2], I["hy_bias"][r0:r0 + GC, c:c + 1], writes=[d_gsc], q="sp", allow_slow_non_contiguous=True)
                for ah in range(self.nown // SH):
                    if fi_loaded[0] != ah:
                        m.dma(FI[:], I["FI%d" % ah][:, :, :, :], writes=[d_FI], q="sp")
                        fi_loaded[0] = ah
                    for bg in range(8):
                        pi = nxt("pA", 2)
                        for bl in range(16):
                            b = bg * 16 + bl
                            m.op("pe", lambda e: e.matmul(pA[pi][0:GC, bl, :], lhsT=bQ[:, 0, :, b], rhs=FI[:, b, 0, :], start=True, stop=False),
                                 reads=[d_bQ, d_FI], writes=[d_pA[pi]], inc=False)
                            m.op("pe", lambda e: e.matmul(pA[pi][0:GC, bl, :], lhsT=bQ[:, 1, :, b], rhs=FI[:, b, 1, :], start=False, stop=True),
                                 reads=[d_bQ, d_FI], writes=[d_pA[pi]], inc=(bl == 15))
                        evac(ysb[:, :, bg * 16:(bg + 1) * 16], pA[pi][0:GC, :, :].rearrange("p b a -> p a b"), [d_pA[pi]], [d_ysb])
                    yfl = ysb[:].rearrange("p a b -> p (a b)")
                    for q4 in range(8):
                        fi = nxt("fin", 2)
                        sl = slice(q4 * 512, (q4 + 1) * 512)
                        gl = slice(ah * SH + q4 * 512, ah * SH + (q4 + 1) * 512)
                        m.dma(fv[fi][:], self.VV[g * GC:(g + 1) * GC, gl], writes=[d_fv[fi]], q="sp")
                        m.dma(fx[fi][:], self.X0[g * GC:(g + 1) * GC, gl], writes=[d_fx[fi]], q="sp")
                        m.op("act", lambda e: e.activation(out=fo[fi][:], in_=yfl[:, sl], func=AF.Identity, scale=gsc[:, 0:1]),
                             reads=[d_ysb, d_gsc], writes=[d_fo[fi]])
                        m.op("dve", lambda e: e.scalar_tensor_tensor(out=fo[fi][:], in0=fv[fi][:], scalar=gsc[:, 1:2], in1=fo[fi][:], op0=ALU.mult, op1=ALU.add),
                             reads=[d_fv[fi], d_gsc, d_fo[fi]], writes=[d_fo[fi]])
                        m.op("dve", lambda e: e.tensor_tensor(out=fo[fi][:], in0=fo[fi][:], in1=fx[fi][:], op=ALU.mult),
                             reads=[d_fo[fi], d_fx[fi]], writes=[d_fo[fi]])
                        m.dma(self.OT[768 + g * GC:768 + (g + 1) * GC, gl], fo[fi][:], reads=[d_fo[fi]], q="act")

            ngrp = 256 // GC if "hy_short" not in self.dbg else 1
            for g in range(ngrp):
                fwd(self.KFb[g * GC:(g + 1) * GC, :], "A")
                fwd(self.KFb[256 + g * GC:256 + (g + 1) * GC, :], "B")
                fwd(self.VVb[g * GC:(g + 1) * GC, :], "V")
                inv(g)
        self.hyP.__exit__(None, None, None)

    def phase_merge(self):
        m, I = self.m, self.I
        with Phase(m) as ph:
            d_c = Dep()
            og = ph.sb([128, 8]); m.dma(og[:], I["out_g"][:, :], writes=[d_c])
            wo = ph.sb([128, 8, D], BF16); d_wo = Dep()
            wst = [ph.sb([128, D]) for _ in range(2)]; d_wst = [Dep(), Dep()]
            wv = I["w_out"].rearrange("(kc p) n -> p kc n", p=128)
            for kc in range(8):
                m.dma(wst[kc % 2][:], wv[:, kc, :], writes=[d_wst[kc % 2]])
                m.op("dve", lambda e: e.tensor_scalar(out=wo[:, kc, :], in0=wst[kc % 2][:], scalar1=og[:, kc:kc + 1], scalar2=None, op0=ALU.mult),
                     reads=[d_wst[kc % 2], d_c], writes=[d_wo])
            ones2 = ph.sb([128, 2], BF16)
            m.op("dve", lambda e: e.memset(ones2[:], 1.0), writes=[d_c])
            wrec = ph.sb([128, 3])
            for gi, wd in enumerate((512, 256, 256)):
                m.op("dve", lambda e: e.memset(wrec[:, gi:gi + 1], 1.0 / wd), writes=[d_c])
            ob = [ph.sb([128, 8, 512]) for _ in range(2)]; d_ob = [Dep(), Dep()]
            obb = [ph.sb([128, 8, 512], BF16) for _ in range(2)]; d_obb = [Dep(), Dep()]
            sqb = [ph.sb([128, 8, 512], BF16) for _ in range(2)]; d_sqb = [Dep(), Dep()]
            xt = [ph.sb([128, D]) for _ in range(2)]; d_xt = [Dep(), Dep()]
            acc = [ph.sb([128, D]) for _ in range(2)]; d_acc = [Dep(), Dep()]
            xn = [ph.sb([128, D], BF16) for _ in range(2)]; d_xn = [Dep(), Dep()]
            junk = ph.sb([128, D], BF16); d_junk = Dep()
            rs = [ph.sb([128, 4]) for _ in range(2)]; d_rs = [Dep(), Dep()]
            fT = [ph.sb([128, 8, 512], BF16) for _ in range(2)]; d_fT = [Dep(), Dep()]
            pO = [[ph.ps([128, 512]) for _ in range(3)] for _ in range(2)]; d_pO = [[Dep() for _ in range(3)] for _ in range(2)]
            pT = ph.ps([128, 8, 128], BF16); d_pT = Dep()
            pS = ph.ps([128, 3, 2]); d_pS = Dep()
            groups = [(0, 4), (4, 6), (6, 8)]
            OTv = self.OT.rearrange("(c p) t -> p c t", p=128)
            blocks = [("lat", i * 512, 512) for i in range(self.nown // 512)] + ([] if self.last else [("ctx", self.nown, LC)])
            tcount = 0
            pcount = 0
            for bi, (kind, c0, ntok) in enumerate(blocks):
                o_ = ob[bi % 2]; do = d_ob[bi % 2]
                m.dma(o_[:, :, 0:ntok], OTv[:, :, c0:c0 + ntok], writes=[do], q="sp")
                ob_ = obb[bi % 2]; dob = d_obb[bi % 2]
                sq_ = sqb[bi % 2]; dsq = d_sqb[bi % 2]
                for ch in range(8):
                    m.op("act", lambda e: e.activation(out=sq_[:, ch, 0:ntok], in_=o_[:, ch, 0:ntok], func=AF.Square), reads=[do], writes=[dsq])
                    m.op("pool", lambda e: e.tensor_copy(out=ob_[:, ch, 0:ntok], in_=o_[:, ch, 0:ntok]), reads=[do], writes=[dob])
                f_ = fT[bi % 2]; df = d_fT[bi % 2]
                gi_ga = 0 if kind == "lat" else 2
                mi = 1 if kind == "lat" else 3
                modt = self.modL if kind == "lat" else self.modC
                for tt in range(ntok // 128):
                    ts_ = slice(tt * 128, (tt + 1) * 128)
                    k2 = tcount % 2; tcount += 1
                    r_ = rs[k2]; dr = d_rs[k2]
                    if kind == "lat":
                        m.dma(xt[k2][:], self.xsrc[c0 + tt * 128:c0 + (tt + 1) * 128, :], writes=[d_xt[k2]], q="sp")
                    else:
                        m.dma(xt[k2][:], self.ctxsrc[tt * 128:(tt + 1) * 128, :], writes=[d_xt[k2]], q="sp")
                    for gi, (a, b) in enumerate(groups):
                        for ch in range(a, b):
                            m.op("pe", lambda e: e.matmul(pS[:, gi, :], lhsT=sq_[:, ch, ts_], rhs=ones2[:], start=(ch == a), stop=(ch == b - 1)),
                                 reads=[dsq, d_c], writes=[d_pS], inc=(ch == b - 1))
                    m.op("dve", lambda e: e.tensor_tensor(out=r_[:, 0:3], in0=pS[:, :, 0], in1=wrec[:], op=ALU.mult), reads=[d_pS, d_c], writes=[dr])
                    m.op("act", lambda e: e.activation(out=r_[:, 0:3], in_=r_[:, 0:3], func=AF.Sqrt, bias=self.epsT[:, 0:1]), reads=[dr, self.d_const], writes=[dr])
                    m.op("dve", lambda e: e.reciprocal(out=r_[:, 0:3], in_=r_[:, 0:3]), reads=[dr], writes=[dr])
                    a_ = acc[k2]; da = d_acc[k2]
                    for h in range(2):
                        hs = slice(h * 512, (h + 1) * 512)
                        pk = pcount % 2; pcount += 1
                        for gi, (a, b) in enumerate(groups):
                            for ch in range(a, b):
                                m.op("pe", lambda e: e.matmul(pO[pk][gi][:], lhsT=ob_[:, ch, ts_], rhs=wo[:, ch, hs], start=(ch == a), stop=(ch == b - 1)),
                                     reads=[dob, d_wo], writes=[d_pO[pk][gi]], inc=(ch == b - 1))
                        m.op("dve", lambda e: e.tensor_scalar(out=a_[:, hs], in0=pO[pk][0][:], scalar1=r_[:, 0:1], scalar2=None, op0=ALU.mult),
                             reads=[d_pO[pk][0], dr], writes=[da])
                        for gi in (1, 2):
                            m.op("dve", lambda e: e.scalar_tensor_tensor(out=a_[:, hs], in0=pO[pk][gi][:], scalar=r_[:, gi:gi + 1], in1=a_[:, hs],
                                                                         op0=ALU.mult, op1=ALU.add), reads=[d_pO[pk][gi], dr, da], writes=[da])
                    m.op("pool", lambda e: e.tensor_tensor(out=a_[:], in0=a_[:], in1=self.gbc[:, gi_ga, :], op=ALU.mult), reads=[da, self.d_gbc], writes=[da])
                    m.op("pool", lambda e: e.tensor_tensor(out=a_[:], in0=a_[:], in1=xt[k2][:], op=ALU.add), reads=[da, d_xt[k2]], writes=[da])
                    m.dma(self.XM[c0 + tt * 128:c0 + (tt + 1) * 128, :], a_[:], reads=[da], q="act")
                    m.op("act", lambda e: e.activation(out=junk[:], in_=a_[:], func=AF.Square, accum_out=r_[:, 3:4]), reads=[da], writes=[d_junk, dr])
                    m.op("dve", lambda e: e.tensor_scalar(out=r_[:, 3:4], in0=r_[:, 3:4], scalar1=1.0 / D, scalar2=EPS, op0=ALU.mult, op1=ALU.add),
                         reads=[dr], writes=[dr])
                    m.op("act", lambda e: e.activation(out=r_[:, 3:4], in_=r_[:, 3:4], func=AF.Sqrt), reads=[dr], writes=[dr])
                    m.op("dve", lambda e: e.reciprocal(out=r_[:, 3:4], in_=r_[:, 3:4]), reads=[dr], writes=[dr])
                    m.op("dve", lambda e: e.tensor_scalar(out=xn[k2][:], in0=a_[:], scalar1=r_[:, 3:4], scalar2=None, op0=ALU.mult),
                         reads=[da, dr], writes=[d_xn[k2]])
                    for kc in range(8):
                        m.op("pe", lambda e: e.transpose(out=pT[:, kc, :], in_=xn[k2][:, kc * 128:(kc + 1) * 128], identity=self.ident[:]),
                             reads=[d_xn[k2], self.d_const], writes=[d_pT], inc=(kc == 7))
                    for kc in range(8):
                        m.op("dve", lambda e: e.tensor_scalar(out=f_[:, kc, ts_], in0=pT[:, kc, :], scalar1=self.AB[:, mi, kc:kc + 1],
                                                              scalar2=modt[:, 24 + kc:25 + kc], op0=ALU.mult, op1=ALU.add),
                             reads=[d_pT, self.d_mod], writes=[df])
                m.dma(self.FT[:, :, c0:c0 + ntok], f_[:, :, 0:ntok], reads=[df], q="act")

    def phase_moe(self):
        m, I = self.m, self.I
        NT = self.ntok // 128
        NTOK = self.ntok; SHL = self.nown
        with Phase(m) as P:
            d_c = Dep()
            gate = P.sb([128, NT, NE]); d_gate = Dep()
            b1 = P.sb([128, NE, 16])
            m.dma(b1[:], I["moe_b1"][:, :, :], writes=[d_c])
            m.op("dve", lambda e: e.tensor_scalar(out=b1[:, :, 8:16], in0=b1[:, :, 8:16], scalar1=1.0, scalar2=None, op0=ALU.add), reads=[d_c], writes=[d_c])
            with Phase(m) as ph:
                rw = ph.sb([128, 8, NE], BF16); rb = ph.sb([128, NE])
                m.dma(rw[:], I["router_w"].rearrange("(kc p) n -> p kc n", p=128), writes=[d_c], q="pool")
                m.dma(rb[:], I["router_b"][:, :], writes=[d_c])
                fb = [ph.sb([128, 8, 512], BF16) for _ in range(2)]; d_fb = [Dep(), Dep()]
                lg = [ph.sb([128, NE]) for _ in range(2)]; d_lg = [Dep(), Dep()]
                ex = [ph.sb([128, NE]) for _ in range(2)]; d_ex = [Dep(), Dep()]
                m8 = [ph.sb([128, 8]) for _ in range(2)]; d_m8 = [Dep(), Dep()]
                sm = [ph.sb([128, 2]) for _ in range(2)]; d_sm = [Dep(), Dep()]
                pl = [ph.ps([128, NE]) for _ in range(2)]; d_pl = [Dep(), Dep()]
                tcount = 0
                for bi in range((NTOK + 511) // 512):
                    c0 = bi * 512; n = min(512, NTOK - c0)
                    m.dma(fb[bi % 2][:, :, 0:n], self.FT[:, :, c0:c0 + n], writes=[d_fb[bi % 2]], q="sp")
                    for tt in range(n // 128):
                        tile = c0 // 128 + tt
                        k = tcount % 2; tcount += 1
                        for kc in range(8):
                            m.op("pe", lambda e: e.matmul(pl[k][:], lhsT=fb[bi % 2][:, kc, tt * 128:(tt + 1) * 128], rhs=rw[:, kc, :], start=(kc == 0), stop=(kc == 7)),
                                 reads=[d_fb[bi % 2], d_c], writes=[d_pl[k]], inc=(kc == 7))
                        m.op("dve", lambda e: e.tensor_tensor(out=lg[k][:], in0=pl[k][:], in1=rb[:], op=ALU.add), reads=[d_pl[k], d_c], writes=[d_lg[k]])
                        m.op("dve", lambda e: e.max(out=m8[k][:], in_=lg[k][:]), reads=[d_lg[k]], writes=[d_m8[k]])
                        m.op("dve", lambda e: e.tensor_scalar(out=sm[k][:, 0:1], in0=m8[k][:, 0:1], scalar1=-1.0, scalar2=None, op0=ALU.mult),
                             reads=[d_m8[k]], writes=[d_sm[k]])
                        m.op("act", lambda e: e.activation(out=ex[k][:], in_=lg[k][:], func=AF.Exp, bias=sm[k][:, 0:1]), reads=[d_lg[k], d_sm[k]], writes=[d_ex[k]])
                        m.op("dve", lambda e: e.scalar_tensor_tensor(out=ex[k][:], in0=lg[k][:], scalar=m8[k][:, 3:4], in1=ex[k][:], op0=ALU.is_ge, op1=ALU.mult,
                                                                     accum_out=sm[k][:, 1:2]), reads=[d_lg[k], d_m8[k], d_ex[k]], writes=[d_ex[k], d_sm[k]])
                        m.op("dve", lambda e: e.reciprocal(out=sm[k][:, 1:2], in_=sm[k][:, 1:2]), reads=[d_sm[k]], writes=[d_sm[k]])
                        m.op("dve", lambda e: e.tensor_scalar(out=gate[:, tile, :], in0=ex[k][:], scalar1=sm[k][:, 1:2], scalar2=None, op0=ALU.mult),
                             reads=[d_ex[k], d_sm[k]], writes=[d_gate])
            if "gate" in self.dbg:
                o = self.nc.dram_tensor("dbg_gate", [128, NT, NE], F32, kind="ExternalOutput").ap(); self.out_names.append("dbg_gate")
                m.dma(o[:, :, :], gate[:], reads=[d_gate])
            w1v = I["moe_w1"].rearrange("e (kc p) n -> e p kc n", p=128)
            w2v = I["moe_w2"].rearrange("e (kc p) n -> e p kc n", p=128)
            passes = [(a, min(a + 11, NT)) for a in range(0, NT, 11)]
            if "moe_short" in self.dbg:
                passes = [(NT - 2, NT)]
            n_exp = NE
            for (ta, tb_) in passes:
                ntile = tb_ - ta
                with Phase(m) as pp:
                    fT = pp.sb([128, 8, ntile * 128], BF16); d_fT = Dep()
                    yacc = pp.sb([128, ntile, D]); d_y = [Dep() for _ in range(ntile)]
                    m.dma(fT[:], self.FT[:, :, ta * 128:tb_ * 128], writes=[d_fT], q="sp")
                    with Phase(m) as ph:
                        b2 = ph.sb([NE, D])
                        m.dma(b2[:], I["moe_b2"][:, :], writes=[d_c])
                        gT = [ph.sb([NE, 128]) for _ in range(2)]; d_gT = [Dep(), Dep()]
                        pg = [ph.ps([NE, 128]) for _ in range(2)]; d_pg = [Dep(), Dep()]
                        py = [ph.ps([128, 512]) for _ in range(2)]; d_py = [Dep(), Dep()]
                        for ti in range(ntile):
                            k = ti % 2
                            m.op("pe", lambda e: e.transpose(out=pg[k][:], in_=gate[:, ta + ti, :], identity=self.identf[:]),
                                 reads=[d_gate, self.d_const], writes=[d_pg[k]])
                            m.op("act", lambda e: e.activation(out=gT[k][:], in_=pg[k][:], func=AF.Identity), reads=[d_pg[k]], writes=[d_gT[k]])
                            for h in range(2):
                                m.op("pe", lambda e: e.matmul(py[h][:], lhsT=gT[k][:], rhs=b2[:, h * 512:(h + 1) * 512], start=True, stop=True),
                                     reads=[d_gT[k], d_c], writes=[d_py[h]])
                                m.op("dve", lambda e: e.tensor_copy(out=yacc[:, ti, h * 512:(h + 1) * 512], in_=py[h][:]), reads=[d_py[h]], writes=[d_y[ti]])
                    with Phase(m) as ph:
                        w1 = [ph.sb([128, 8, 2 * D], BF16) for _ in range(2)]; d_w1 = [[Dep() for _ in range(8)] for _ in range(2)]
                        w2 = ph.sb([128, 8, D], BF16); d_w2 = [Dep() for _ in range(8)]
                        act = [ph.sb([128, 8, 512], BF16) for _ in range(2)]; d_act = [Dep(), Dep()]
                        tg = [ph.sb([128, 512]) for _ in range(2)]; d_tg = [Dep(), Dep()]
                        tsg = [ph.sb([128, 512]) for _ in range(2)]; d_tsg = [Dep(), Dep()]
                        tl = [ph.sb([128, 512]) for _ in range(2)]; d_tl = [Dep(), Dep()]
                        pgl = [[ph.ps([128, 512]) for _ in range(2)] for _ in range(2)]; d_pgl = [[Dep(), Dep()], [Dep(), Dep()]]
                        pyy = [ph.ps([128, 512]) for _ in range(3)]; d_pyy = [Dep() for _ in range(3)]
                        cnt = {"j": 0, "y": 0, "a": 0}
                        m.op("dve", lambda e: e.tensor_scalar(out=gate[:, ta:tb_, :], in0=gate[:, ta:tb_, :], scalar1=1.0 / 1.702, scalar2=None, op0=ALU.mult),
                             reads=[d_gate], writes=[d_gate])

                        wtok = []

                        def wdma(out, in_, dep):
                            if len(wtok) >= 2:
                                m._wait("pool", wtok[-2])
                            wtok.append(m.dma(out, in_, writes=[dep], q="pool"))

                        def load_w1(e_):
                            for kc in range(8):
                                wdma(w1[e_ % 2][:, kc, :], w1v[e_, :, kc, :], d_w1[e_ % 2][kc])

                        def load_w2(e_):
                            for kc in range(8):
                                wdma(w2[:, kc, :], w2v[e_, :, kc, :], d_w2[kc])
                        load_w1(0)
                        for e_ in range(n_exp):
                            load_w2(e_)
                            if e_ + 1 < n_exp:
                                load_w1(e_ + 1)
                            W1 = w1[e_ % 2]; dW1 = d_w1[e_ % 2]
                            for b0 in range(0, ntile, 4):
                                nt_ = min(4, ntile - b0); n = nt_ * 128
                                cs_ = slice(b0 * 128, b0 * 128 + n)
                                ai = cnt["a"] % 2; cnt["a"] += 1
                                A_ = act[ai]; dA = d_act[ai]
                                for j in range(8):
                                    k = cnt["j"] % 2; cnt["j"] += 1
                                    for gl in range(2):
                                        for kc in range(8):
                                            m.op("pe", lambda e: e.matmul(pgl[k][gl][:, 0:n], lhsT=W1[:, kc, 256 * j + gl:256 * j + 256:2], rhs=fT[:, kc, cs_],
                                                                          start=(kc == 0), stop=(kc == 7)),
                                                 reads=[dW1[kc], d_fT], writes=[d_pgl[k][gl]], inc=(kc == 7))
                                    m.op("dve", lambda e: e.tensor_scalar(out=tg[k][:, 0:n], in0=pgl[k][0][:, 0:n], scalar1=b1[:, e_, j:j + 1], scalar2=7.0,
                                                                          op0=ALU.add, op1=ALU.min), reads=[d_pgl[k][0], d_c], writes=[d_tg[k]])
                                    m.op("act", lambda e: e.activation(out=tsg[k][:, 0:n], in_=tg[k][:, 0:n], func=AF.Silu, scale=1.702),
                                         reads=[d_tg[k]], writes=[d_tsg[k]])
                                    m.op("dve", lambda e: e.tensor_scalar(out=tl[k][:, 0:n], in0=pgl[k][1][:, 0:n], scalar1=b1[:, e_, 8 + j:9 + j], scalar2=8.0,
                                                                          op0=ALU.add, op1=ALU.min), reads=[d_pgl[k][1], d_c], writes=[d_tl[k]])
                                    m.op("dve", lambda e: e.scalar_tensor_tensor(out=A_[:, j, 0:n], in0=tl[k][:, 0:n], scalar=-6.0, in1=tsg[k][:, 0:n],
                                                                                 op0=ALU.max, op1=ALU.mult), reads=[d_tl[k], d_tsg[k]], writes=[dA])
                                for tt in range(nt_):
                                    ti = b0 + tt
                                    for h in range(2):
                                        yk = cnt["y"] % 3; cnt["y"] += 1
                                        for j in range(8):
                                            m.op("pe", lambda e: e.matmul(pyy[yk][:], lhsT=A_[:, j, tt * 128:(tt + 1) * 128], rhs=w2[:, j, h * 512:(h + 1) * 512],
                                                                          start=(j == 0), stop=(j == 7)),
                                                 reads=[dA, d_w2[j]], writes=[d_pyy[yk]], inc=(j == 7))
                                        m.op("dve", lambda e: e.scalar_tensor_tensor(out=yacc[:, ti, h * 512:(h + 1) * 512], in0=pyy[yk][:], scalar=gate[:, ta + ti, e_:e_ + 1],
                                                                                     in1=yacc[:, ti, h * 512:(h + 1) * 512], op0=ALU.mult, op1=ALU.add),
                                             reads=[d_pyy[yk], d_gate, d_y[ti]], writes=[d_y[ti]])
                    with Phase(m) as ph:
                        xm = [ph.sb([128, D]) for _ in range(2)]; d_xm = [Dep(), Dep()]
                        ot = [ph.sb([128, D]) for _ in range(2)]; d_ot = [Dep(), Dep()]
                        for ti in range(ntile):
                            tile = ta + ti; k = ti % 2
                            m.dma(xm[k][:], self.XM[tile * 128:(tile + 1) * 128, :], writes=[d_xm[k]], q="sp")
                            gi = 1 if tile < SHL // 128 else 3
                            m.op("pool", lambda e: e.tensor_tensor(out=ot[k][:], in0=yacc[:, ti, :], in1=self.gbc[:, gi, :], op=ALU.mult),
                                 reads=[d_y[ti], self.d_gbc], writes=[d_ot[k]])
                            m.op("dve", lambda e: e.tensor_tensor(out=ot[k][:], in0=ot[k][:], in1=xm[k][:], op=ALU.add), reads=[d_ot[k], d_xm[k]], writes=[d_ot[k]])
                            if tile < SHL // 128:
                                dst = self.x_out if self.last else self.X1
                                m.dma(dst[tile * 128:(tile + 1) * 128, :], ot[k][:], reads=[d_ot[k]], q="act")
                            else:
                                r0 = tile * 128 - SHL
                                m.dma(self.C1[r0:r0 + 128, :], ot[k][:], reads=[d_ot[k]], q="act")


_PROG = {}


def _get_prog():
    if "p" not in _PROG:
        _PROG["p"] = LayerProg()
    return _PROG["p"]


def kernel(**inputs):
    inp = {k: np.asarray(v) for k, v in inputs.items()}
    P = _get_prog()
    need = set(P.Iall.keys())
    x = np.ascontiguousarray(inp["x"], dtype=np.float32)
    ctx = np.ascontiguousarray(inp["ctx"], dtype=np.float32)
    maps = []
    for core in range(8):
        b, half = core // 2, core % 2
        xc = x[b][::-1] if half else x[b]
        cc = ctx[b][::-1] if half else ctx[b]
        mp = {}
        for l in range(2):
            d = _prep_core(inp, l, b, half, xc, cc)
            for k, v in d.items():
                name = k if k in SHARED else "%s@%d" % (k, l)
                if name in need and name not in mp:
                    mp[name.replace("@", "_L")] = v
        maps.append(mp)
    res = run_bass_kernel_spmd(P.nc, maps, core_ids=list(range(8)))
    out = np.empty_like(x)
    for core in range(8):
        b, half = core // 2, core % 2
        xo = np.asarray(res.results[core]["x_out"])
        if half == 0:
            out[b, :SH] = xo
        else:
            out[b, SH:] = xo[::-1]
    return out
```

```python
import math
from contextlib import ExitStack
import numpy as np
import ml_dtypes
import concourse.bass as bass
import concourse.mybir as mybir
from concourse.bass_utils import run_bass_kernel_spmd

F32 = mybir.dt.float32
BF16 = mybir.dt.bfloat16
AF = mybir.ActivationFunctionType
ALU = mybir.AluOpType
AX = mybir.AxisListType

D = 1024
S = 8192
SH = 4096
LC = 256
NTOK = SH + LC
NALL = S + LC
NE = 32
EPS = 1e-6
NFFT = 16384


class Dep:
    __slots__ = ("w", "r")

    def __init__(self):
        self.w = None
        self.r = {}


class MK:
    ENG = ("pe", "act", "dve", "pool", "sp")
    EPOCH = 12000

    def __init__(self, nc, n_dma_sems=48):
        self.nc = nc
        self.e = {"pe": nc.tensor, "act": nc.scalar, "dve": nc.vector, "pool": nc.gpsimd, "sp": nc.sync}
        self.sem = {}
        self.cnt = {}
        self.allsems = []
        self.nsem = 0
        for k in self.ENG:
            self._new_epoch(k)
        self.seen = {k: {} for k in self.ENG}
        self.dma_sems = [nc.alloc_semaphore(f"dq{i}") for i in range(n_dma_sems)]
        self.dma_val = [0] * n_dma_sems
        self.dma_rr = 0
        self.n_inst = 0
        self.n_wait = 0
        self._uid = 0

    def _new_epoch(self, k):
        self.sem[k] = self.nc.alloc_semaphore(f"s_{k}_{self.nsem}")
        self.nsem += 1
        self.cnt[k] = 0

    def uid(self, p="t"):
        self._uid += 1
        return f"{p}{self._uid}"

    def _wait(self, eng, tok):
        if tok is None:
            return
        sem, val = tok
        sid = id(sem)
        if self.seen[eng].get(sid, 0) >= val:
            return
        if sem is self.sem.get(eng) and val > self.cnt[eng]:
            return
        self.e[eng].wait_ge(sem, val)
        self.seen[eng][sid] = val
        self.n_wait += 1

    def _deps(self, eng, reads, writes):
        for d in reads:
            self._wait(eng, d.w)
        for d in writes:
            self._wait(eng, d.w)
            for t in d.r.values():
                self._wait(eng, t)

    def op(self, eng, fn, reads=(), writes=(), inc=True):
        self._deps(eng, reads, writes)
        ins = fn(self.e[eng])
        self.n_inst += 1
        if inc:
            self.cnt[eng] += 1
            ins.then_inc(self.sem[eng], 1)
            tok = (self.sem[eng], self.cnt[eng])
            self.seen[eng][id(self.sem[eng])] = self.seen[eng].get(id(self.sem[eng]), 0)
            if self.cnt[eng] >= self.EPOCH:
                self._new_epoch(eng)
        else:
            tok = (self.sem[eng], self.cnt[eng] + 1)
        for d in reads:
            d.r[eng] = tok
        for d in writes:
            d.w = tok
            d.r = {}
        return ins

    def dma(self, out, in_, reads=(), writes=(), q="sp", **kw):
        self._deps(q, reads, writes)
        i = self.dma_rr
        self.dma_rr = (self.dma_rr + 1) % len(self.dma_sems)
        sem = self.dma_sems[i]
        if self.dma_val[i] > 0:
            self._wait(q, (sem, self.dma_val[i]))
        self.dma_val[i] += 16
        ins = self.e[q].dma_start(out=out, in_=in_, **kw)
        ins.then_inc(sem, 16)
        self.n_inst += 1
        tok = (sem, self.dma_val[i])
        for d in reads:
            d.r["dma%d" % i] = tok
        for d in writes:
            d.w = tok
            d.r = {}
        return tok

    def barrier(self, engines=None):
        engines = engines or self.ENG
        toks = [(self.sem[p], self.cnt[p]) for p in self.ENG if self.cnt[p] > 0]
        toks += [(s, v) for s, v in zip(self.dma_sems, self.dma_val) if v > 0]
        for e in engines:
            for t in toks:
                self._wait(e, t)


class Phase:
    def __init__(self, m):
        self.m = m
        self.st = ExitStack()

    def __enter__(self):
        self.st.__enter__()
        return self

    def sb(self, shape, dt=F32):
        return self.st.enter_context(self.m.nc.sbuf_tensor(self.m.uid("sb"), list(shape), dt))

    def ps(self, shape, dt=F32):
        return self.st.enter_context(self.m.nc.psum_tensor(self.m.uid("ps"), list(shape), dt))

    def __exit__(self, *a):
        self.m.barrier()
        return self.st.__exit__(*a)


_CONST = {}


def _consts():
    if _CONST:
        return _CONST
    bf = ml_dtypes.bfloat16
    c = _CONST
    c["ident"] = np.eye(128, dtype=np.float32)
    pr = np.zeros((128, 128), np.float32)
    for blk in range(2):
        for i in range(32):
            pr[blk * 64 + i + 32, blk * 64 + i] = -1.0
            pr[blk * 64 + i, blk * 64 + i + 32] = 1.0
    c["prot"] = pr
    bo = np.zeros((128, 128), np.float32)
    bo[:64, :64] = 1.0 / 64
    bo[64:, 64:] = 1.0 / 64
    c["blk64"] = bo
    sel = np.zeros((65, 64), np.float32)
    sel[64, :] = 1.0
    c["sel"] = sel
    rows = np.repeat(np.arange(S // 64), 64).astype(np.float32)
    cols = np.tile(np.arange(64), S // 64).astype(np.float32)
    inv = (10000.0 ** (-np.arange(16, dtype=np.float32) / 16)).astype(np.float32)
    ang = np.concatenate([rows[:, None] * inv, cols[:, None] * inv], axis=-1).astype(np.float32)
    c["rope_cos"] = np.ascontiguousarray(np.tile(np.cos(ang).T, (4, 1)).astype(np.float32))
    c["rope_sin"] = np.ascontiguousarray(np.tile(np.sin(ang).T, (4, 1)).astype(np.float32))

    def zfeat(L):
        t01 = np.linspace(0.0, 1.0, L, dtype=np.float32)[:, None]
        bands = np.linspace(1e-4, 15, 16, dtype=np.float32)
        a = (np.float32(2.0 * math.pi / L) * np.arange(L, dtype=np.float32)[:, None] * bands).astype(np.float32)
        z = np.concatenate([t01, np.cos(a), -np.sin(a)], axis=-1).astype(np.float32)
        return np.ascontiguousarray(z.T), np.ascontiguousarray(np.broadcast_to(t01[:, 0][None, :], (128, L)))
    c["zT"], c["t01"] = zfeat(S)
    c["zTc"], c["t01c"] = zfeat(LC)
    a = np.arange(64)[:, None, None]
    b = np.arange(128)[None, :, None]
    cc = np.arange(128)[None, None, :]
    th = 2.0 * np.pi * (((128 * a + b) * cc) % NFFT) / NFFT
    c["F1"] = np.ascontiguousarray(np.stack([np.cos(th), -np.sin(th)], axis=2)).astype(bf)
    bd = 2.0 * np.pi * ((np.arange(128)[:, None] * np.arange(128)[None, :]) % 128) / 128
    c["C2"] = np.stack([np.cos(bd), np.sin(bd), -np.sin(bd)], axis=1).astype(bf)
    cI = np.arange(128)[:, None, None]
    bI = np.arange(128)[None, :, None]
    for hh in range(2):
        a32 = (np.arange(32) + 32 * hh)[None, None, :]
        thi = 2.0 * np.pi * (((128 * a32 + bI) * cI) % NFFT) / NFFT
        c["FI%d" % hh] = np.ascontiguousarray(np.stack([np.cos(thi), -np.sin(thi)], axis=2)).astype(bf)
    return c


def _cols(v, n):
    return np.ascontiguousarray(np.asarray(v, np.float32).reshape(n, 128).T)


def _prep_core(inp, l, b, half, x_core, ctx_core):
    c = _consts()
    r = (lambda a, ax=0: np.flip(a, axis=ax)) if half else (lambda a, ax=0: a)
    d = {}
    d["x"] = np.ascontiguousarray(x_core, np.float32)
    d["ctx"] = np.ascontiguousarray(ctx_core, np.float32)
    d["cvec"] = np.ascontiguousarray(np.concatenate([_cols(inp["c"][b], 8), _cols(inp["c_ctx"], 8)], axis=1))
    d["w_mod"] = inp["w_mod"][l]
    d["bmod"] = _cols(inp["b_mod"][l], 48)
    d["gmix"] = _cols(inp["norm_mix_g"][l], 8)
    d["gffn"] = _cols(inp["norm_ffn_g"][l], 8)
    w_in = inp["w_in"][l]
    perm = np.array([(j + 4 * s) * 64 + dd for j in range(4) for s in range(2) for dd in range(64)])
    d["w_in"] = np.ascontiguousarray(np.concatenate([w_in[:, perm], w_in[:, 512:]], axis=1))
    qg = inp["q_norm_g"][l]
    kg = inp["k_norm_g"][l]
    d["qkg"] = np.ascontiguousarray(np.stack([np.tile(qg, 2), np.tile(kg, 2)], axis=1).astype(np.float32))
    d["qkg_row"] = np.ascontiguousarray(np.broadcast_to(np.concatenate([qg, kg])[None, :], (128, 128)).astype(np.float32))
    d["rope_cos"] = np.ascontiguousarray(r(c["rope_cos"], 1))
    d["rope_sin"] = np.ascontiguousarray(r(c["rope_sin"], 1))
    cw = inp["lru_conv_w"][l]
    w5 = np.zeros((5, 256), np.float32)
    if half:
        w5[1:5] = cw[::-1]
    else:
        w5[0:4] = cw
    d["lru_cw"] = np.ascontiguousarray(w5.T.reshape(2, 128, 5).transpose(1, 0, 2))
    d["lru_cb"] = _cols(inp["lru_conv_b"][l], 2)
    gw = inp["lru_gate_w"][l]
    gb = inp["lru_gate_b"][l]
    lam = inp["lru_lambda"][l]
    if half:
        gw, gb, lam = gw[::-1], gb[::-1], lam[::-1]
    wbd = np.zeros((128, 2, 2, 2, 128), np.float32)
    for dd in range(2):
        for g in range(2):
            for ch in range(2):
                wbd[0:64, dd, g, ch, 0:64] = gw[dd, g, 2 * ch]
                wbd[64:128, dd, g, ch, 64:128] = gw[dd, g, 2 * ch + 1]
    d["lru_wbd"] = wbd.reshape(128, 8, 128)
    d["lru_gb"] = np.ascontiguousarray(np.stack([_cols(gb[dd, g], 2) for dd in range(2) for g in range(2)], axis=1).reshape(128, 8))
    d["lru_lam"] = np.ascontiguousarray(np.stack([_cols(lam[dd], 2) for dd in range(2)], axis=1).reshape(128, 4))
    hw = inp["hy_conv_w"][l]
    if half:
        hw = hw[::-1]
    d["hy_cw"] = np.ascontiguousarray(hw.T.reshape(6, 128, 3).transpose(1, 0, 2))
    d["hy_cb"] = _cols(inp["hy_conv_b"][l], 6)
    d["hy_w1"] = inp["hy_w1"][l]
    d["hy_w2"] = inp["hy_w2"][l]
    d["hy_b12f"] = np.ascontiguousarray(np.stack([inp["hy_b1"][l], inp["hy_b2"][l], inp["hy_freq"][l]], axis=1))
    w3 = inp["hy_w3"][l]
    dec = inp["hy_decay"][l]
    wa, wb_, da, db = w3[:, :256], w3[:, 256:], dec[:256], dec[256:]
    if half:
        wa, wb_, da, db = wb_, wa, db, da
    d["hy_w3"] = np.ascontiguousarray(np.concatenate([wa, wb_, w3[:, :256]], axis=1))
    d["hy_dec"] = np.ascontiguousarray(np.concatenate([_cols(da, 2), _cols(db, 2)], axis=1))
    d["hy_bias"] = _cols(inp["hy_bias"][l], 2)
    d["out_g"] = _cols(inp["out_norm_g"][l], 8)
    d["w_out"] = inp["w_out"][l]
    d["router_w"] = inp["router_w"][l]
    d["router_b"] = np.ascontiguousarray(np.broadcast_to(inp["router_b"][l][None, :], (128, NE)).astype(np.float32))
    d["moe_w1"] = inp["moe_w1"][l]
    b1 = inp["moe_b1"][l]
    d["moe_b1"] = np.ascontiguousarray(np.concatenate(
        [b1[:, 0::2].reshape(NE, 8, 128).transpose(2, 0, 1), b1[:, 1::2].reshape(NE, 8, 128).transpose(2, 0, 1)], axis=2))
    d["moe_w2"] = inp["moe_w2"][l]
    d["moe_b2"] = inp["moe_b2"][l]
    for k in ("ident", "prot", "blk64", "sel", "zT", "t01", "zTc", "t01c", "F1", "C2", "FI0", "FI1"):
        d[k] = c[k]
    return d


IN_SPECS = [
    ("x", [S, D], F32), ("ctx", [LC, D], F32), ("cvec", [128, 16], F32), ("w_mod", [D, 6 * D], F32),
    ("bmod", [128, 48], F32), ("gmix", [128, 8], F32), ("gffn", [128, 8], F32), ("w_in", [D, 2048], F32),
    ("qkg", [128, 2], F32), ("qkg_row", [128, 128], F32), ("rope_cos", [128, S], F32), ("rope_sin", [128, S], F32),
    ("lru_cw", [128, 2, 5], F32), ("lru_cb", [128, 2], F32), ("lru_wbd", [128, 8, 128], F32), ("lru_gb", [128, 8], F32),
    ("lru_lam", [128, 4], F32), ("hy_cw", [128, 6, 3], F32), ("hy_cb", [128, 6], F32), ("hy_w1", [33, 64], F32),
    ("hy_w2", [64, 64], F32), ("hy_b12f", [64, 3], F32), ("hy_w3", [64, 768], F32), ("hy_dec", [128, 4], F32),
    ("hy_bias", [128, 2], F32), ("out_g", [128, 8], F32), ("w_out", [D, D], F32), ("router_w", [D, NE], F32),
    ("router_b", [128, NE], F32), ("moe_w1", [NE, D, 2 * D], F32), ("moe_b1", [128, NE, 16], F32),
    ("moe_w2", [NE, D, D], F32), ("moe_b2", [NE, D], F32),
    ("ident", [128, 128], F32), ("prot", [128, 128], F32), ("blk64", [128, 128], F32), ("sel", [65, 64], F32),
    ("zT", [33, S], F32), ("t01", [128, S], F32), ("zTc", [33, LC], F32), ("t01c", [128, LC], F32),
    ("F1", [64, 128, 2, 128], BF16), ("C2", [128, 3, 128], BF16), ("FI0", [128, 128, 2, 32], BF16), ("FI1", [128, 128, 2, 32], BF16),
]


SHARED = ("x", "ctx", "cvec", "rope_cos", "rope_sin", "ident", "prot", "blk64", "sel", "zT", "t01", "zTc", "t01c", "F1", "C2", "FI0", "FI1")


class LayerProg:
    def __init__(self, layers=((0, S, False), (1, SH, True)), stop_after=None, dbg=()):
        self.nc = nc = bass.Bass("TRN2", target_bir_lowering=False)
        self.m = MK(nc)
        specs = {n: (sh, dt) for n, sh, dt in IN_SPECS}
        prog = self

        class _Lazy(dict):
            def __missing__(d, n):
                base = n.split("@")[0]
                sh, dt = specs[base]
                d[n] = nc.dram_tensor(n.replace("@", "_L"), list(sh), dt, kind="ExternalInput").ap()
                return d[n]

        class _View:
            def __getitem__(v, n):
                return prog.Iall[n if n in SHARED else "%s@%d" % (n, prog.l)]
        self.Iall = _Lazy()
        self.I = _View()
        self.stop_after = stop_after
        self.dbg = set(dbg)
        self.out_names = []
        k = "ExternalOutput" if "scratch" in self.dbg else "Internal"
        sc = lambda n, sh, dt=F32: nc.dram_tensor(n, list(sh), dt, kind=k).ap()
        self.U = sc("sU", [10, 128, NALL])
        self.OT = sc("sOT", [D, NALL])
        self.HF = sc("sHF", [2, 128, NALL])
        self.VV = sc("sVV", [256, S])
        self.X0 = sc("sX0", [256, S])
        self.KF = sc("sKF", [512, S])
        self.XM = sc("sXM", [NALL, D])
        self.FT = sc("sFT", [128, 8, NALL], BF16)
        self.QTs = sc("sQT", [4, 128, S], BF16)
        self.VVb = sc("sVVb", [256, S], BF16)
        self.KFb = sc("sKFb", [512, S], BF16)
        self.X1 = sc("sX1", [S, D])
        self.C1 = sc("sC1", [LC, D])
        if k == "ExternalOutput":
            self.out_names += ["sU", "sOT", "sHF", "sVV", "sX0", "sKF", "sXM", "sFT", "sQT", "sX1", "sC1"]
        self.x_out = nc.dram_tensor("x_out", [SH, D], F32, kind="ExternalOutput").ap()
        self.out_names += ["x_out"]
        self.layers = list(layers)
        self.build()

    def build(self):
        m = self.m
        with Phase(m) as G:
            self.G = G
            self.setup_globals()
            steps = ["mod", "inproj_attn", "lru", "hyena", "merge", "moe"]
            done = False
            for li, (l, nown, last) in enumerate(self.layers):
                self.l, self.nown, self.last = l, nown, last
                self.ntok = nown + (0 if last else LC)
                self.xsrc = self.Iall["x"] if li == 0 else self.X1
                self.ctxsrc = self.Iall["ctx"] if li == 0 else self.C1
                for name in steps:
                    getattr(self, "phase_" + name)()
                    m.barrier()
                    if self.stop_after == "%s%d" % (name, l) or (name == "inproj_attn" and self.stop_after == "inproj%d" % l):
                        done = True
                        break
                if done:
                    break
            m.barrier()

    def setup_globals(self):
        m, G, I = self.m, self.G, self.I
        self.identf = G.sb([128, 128]); self.d_const = Dep()
        self.ident = G.sb([128, 128], BF16)
        self.onesf = G.sb([128, 128])
        self.epsT = G.sb([128, 1])
        m.dma(self.identf[:], I["ident"][:, :], writes=[self.d_const])
        m.op("dve", lambda e: e.tensor_copy(out=self.ident[:], in_=self.identf[:]), reads=[self.d_const], writes=[self.d_const])
        m.op("dve", lambda e: e.memset(self.onesf[:], 1.0), writes=[self.d_const])
        m.op("dve", lambda e: e.memset(self.epsT[:], EPS), writes=[self.d_const])
        self.modL = G.sb([128, 48]); self.modC = G.sb([128, 48]); self.d_mod = Dep()
        self.AB = G.sb([128, 4, 8])
        self.gbc = G.sb([128, 4, D])
        self.d_gbc = Dep()

    def phase_mod(self):
        m, I = self.m, self.I
        with Phase(m) as ph:
            cv = ph.sb([128, 16]); d_cv = Dep()
            m.dma(cv[:], I["cvec"][:, :], writes=[d_cv])
            sc = ph.sb([128, 8, 2]); d_sc = Dep()
            m.op("act", lambda e: e.activation(out=sc[:, :, 0], in_=cv[:, 0:8], func=AF.Silu), reads=[d_cv], writes=[d_sc])
            m.op("act", lambda e: e.activation(out=sc[:, :, 1], in_=cv[:, 8:16], func=AF.Silu), reads=[d_cv], writes=[d_sc])
            bm = ph.sb([128, 48]); gm = ph.sb([128, 2, 8]); d_bm = Dep()
            m.dma(bm[:], I["bmod"][:, :], writes=[d_bm])
            m.dma(gm[:, 0, :], I["gmix"][:, :], writes=[d_bm])
            m.dma(gm[:, 1, :], I["gffn"][:, :], writes=[d_bm])
            pm = ph.ps([128, 48, 2]); d_pm = Dep()
            wv = I["w_mod"].rearrange("(kc p) n -> p kc n", p=128)
            wb = [ph.sb([128, 8, 512]) for _ in range(2)]
            d_wb = [Dep(), Dep()]
            for nb in range(12):
                t, dw = wb[nb % 2], d_wb[nb % 2]
                m.dma(t[:], wv[:, :, nb * 512:(nb + 1) * 512], writes=[dw], q=("sp" if nb % 2 == 0 else "act"))
                for j in range(4):
                    col = nb * 4 + j
                    for kc in range(8):
                        m.op("pe", lambda e: e.matmul(pm[:, col, :], lhsT=t[:, kc, j * 128:(j + 1) * 128], rhs=sc[:, kc, :],
                                                      start=(kc == 0), stop=(kc == 7)),
                             reads=[dw, d_sc], writes=[d_pm], inc=(kc == 7))
            m.op("dve", lambda e: e.tensor_tensor(out=self.modL[:], in0=pm[:, :, 0], in1=bm[:], op=ALU.add), reads=[d_pm, d_bm], writes=[self.d_mod])
            m.op("dve", lambda e: e.tensor_tensor(out=self.modC[:], in0=pm[:, :, 1], in1=bm[:], op=ALU.add), reads=[d_pm, d_bm], writes=[self.d_mod])
            tmp = ph.sb([128, 8]); d_tmp = Dep()
            for i, (mt, c0, gi) in enumerate([(self.modL, 8, 0), (self.modL, 32, 1), (self.modC, 8, 0), (self.modC, 32, 1)]):
                m.op("dve", lambda e: e.tensor_scalar(out=tmp[:], in0=mt[:, c0:c0 + 8], scalar1=1.0, scalar2=None, op0=ALU.add),
                     reads=[self.d_mod], writes=[d_tmp])
                m.op("dve", lambda e: e.tensor_tensor(out=self.AB[:, i, :], in0=tmp[:], in1=gm[:, gi, :], op=ALU.mult),
                     reads=[d_tmp, d_bm], writes=[self.d_mod])
            dg = ph.sb([128, 8, 128]); d_dg = Dep()
            pb = ph.ps([128, D]); d_pb = Dep()
            for i, (mt, c0) in enumerate([(self.modL, 16), (self.modL, 40), (self.modC, 16), (self.modC, 40)]):
                for j in range(8):
                    m.op("dve", lambda e: e.tensor_scalar(out=dg[:, j, :], in0=self.identf[:], scalar1=mt[:, c0 + j:c0 + j + 1], scalar2=None,
                                                          op0=ALU.mult), reads=[self.d_mod, self.d_const], writes=[d_dg])
                for h in range(2):
                    m.op("pe", lambda e: e.matmul(pb[:, h * 512:(h + 1) * 512], lhsT=self.onesf[:], rhs=dg[:, 4 * h:4 * h + 4, :],
                                                  start=True, stop=True), reads=[d_dg, self.d_const], writes=[d_pb])
                m.op("act", lambda e: e.activation(out=self.gbc[:, i, :], in_=pb[:], func=AF.Identity), reads=[d_pb], writes=[self.d_gbc])
            if "mod" in self.dbg:
                o = self.nc.dram_tensor("dbg_mod", [128, 96], F32, kind="ExternalOutput").ap(); self.out_names.append("dbg_mod")
                m.dma(o[:, 0:48], self.modL[:], reads=[self.d_mod])
                m.dma(o[:, 48:96], self.modC[:], reads=[self.d_mod])
                o2 = self.nc.dram_tensor("dbg_gbc", [128, 4, D], F32, kind="ExternalOutput").ap(); self.out_names.append("dbg_gbc")
                m.dma(o2[:, :, :], self.gbc[:], reads=[self.d_gbc])

    def phase_inproj_attn(self):
        m, I = self.m, self.I
        with Phase(m) as P:
            KT = P.sb([128, NALL], BF16); d_KT = Dep()
            QT = None; d_QT = Dep()
            QC = P.sb([128, 4, LC], BF16); d_QC = Dep()
            VA = P.sb([128, 66, 2, 65], BF16); d_VA = Dep()
            negM = P.sb([128, 1]); d_negM = Dep()
            m.op("pool", lambda e: e.memset(VA[:, :, :, 64:65], 1.0), writes=[d_VA])
            self._inproj(KT, d_KT, QT, d_QT, QC, d_QC, VA, d_VA, negM, d_negM)
            m.barrier()
            if self.stop_after == "inproj%d" % self.l:
                return
            self._attention(KT, d_KT, QT, d_QT, QC, d_QC, VA, d_VA, negM, d_negM)

    def _inproj(self, KT, d_KT, QT, d_QT, QC, d_QC, VA, d_VA, negM, d_negM):
        m, I = self.m, self.I
        with Phase(m) as ph:
            w_in = ph.sb([128, 8, 2048], BF16); d_w = Dep()
            wv = I["w_in"].rearrange("(kc p) n -> p kc n", p=128)
            for kc in range(8):
                m.dma(w_in[:, kc, :], wv[:, kc, :], writes=[d_w], q="pool")
            cst = ph.sb([128, 2, 128], BF16); d_cst = Dep()
            m.dma(cst[:, 0, :], I["prot"][:, :], writes=[d_cst], q="pool")
            m.dma(cst[:, 1, :], I["blk64"][:, :], writes=[d_cst], q="pool")
            qkg = ph.sb([128, 2]); grow = ph.sb([128, 128])
            m.dma(qkg[:], I["qkg"][:, :], writes=[d_cst])
            m.dma(grow[:], I["qkg_row"][:, :], writes=[d_cst])
            mq = ph.sb([128, 2])
            m.op("dve", lambda e: e.tensor_reduce(out=mq[:, 0:1], in_=grow[:, 0:64], axis=AX.X, op=ALU.max, apply_absolute_value=True),
                 reads=[d_cst], writes=[d_negM])
            m.op("dve", lambda e: e.tensor_reduce(out=mq[:, 1:2], in_=grow[:, 64:128], axis=AX.X, op=ALU.max, apply_absolute_value=True),
                 reads=[d_cst], writes=[d_negM])
            m.op("dve", lambda e: e.tensor_scalar(out=negM[:], in0=mq[:, 0:1], scalar1=mq[:, 1:2], scalar2=-8.0, op0=ALU.mult, op1=ALU.mult),
                 reads=[d_negM], writes=[d_negM])
            xr = [ph.sb([128, D]) for _ in range(4)]; d_xr = [Dep() for _ in range(4)]
            xn = [ph.sb([128, D], BF16) for _ in range(8)]; d_xn = [Dep() for _ in range(8)]
            junk = ph.sb([128, D], BF16); d_junk = Dep()
            hT = [ph.sb([128, 8, 512], BF16) for _ in range(2)]; d_hT = [Dep(), Dep()]
            ss = [ph.sb([128, 4]) for _ in range(2)]; d_ss = [Dep(), Dep()]
            cs = [ph.sb([128, 2, 512]) for _ in range(2)]; d_cs = [Dep(), Dep()]
            stg = [ph.sb([128, 512]) for _ in range(4)]; d_stg = [Dep() for _ in range(4)]
            sq = [ph.sb([128, 512], BF16) for _ in range(2)]; d_sq = [Dep(), Dep()]
            f1 = [ph.sb([128, 512]) for _ in range(2)]; d_f1 = [Dep(), Dep()]
            f2 = [ph.sb([128, 512]) for _ in range(2)]; d_f2 = [Dep(), Dep()]
            qn = [ph.sb([128, 512], BF16) for _ in range(2)]; d_qn = [Dep(), Dep()]
            pT = ph.ps([128, 4, 512], BF16); d_pT = Dep()
            pr = [ph.ps([128, 512]) for _ in range(5)]; d_pr = [Dep() for _ in range(5)]
            pv = ph.ps([128, 4, 128]); d_pv = Dep()
            cnt = {"pr": 0, "stg": 0, "qk": 0, "x": 0, "xn": 0, "qst": 0}
            qst = [ph.sb([128, 512], BF16) for _ in range(3)]; d_qst = [Dep() for _ in range(3)]

            def nxt(key, n):
                i = cnt[key] % n
                cnt[key] += 1
                return i

            blocks = [("lat", i * 512, 512, i * 512 < self.nown) for i in range(16)] + [("ctx", 0, LC, not self.last)]
            for bi, (kind, t0, ntok, own) in enumerate(blocks):
                src = self.xsrc if kind == "lat" else self.ctxsrc
                ntile = ntok // 128
                mi = 0 if kind == "lat" else 2
                modt = self.modL if kind == "lat" else self.modC
                col0 = t0 if kind == "lat" else S
                h = hT[bi % 2]; dh = d_hT[bi % 2]
                s_ = ss[bi % 2]; ds_ = d_ss[bi % 2]
                xs, xns = [], []
                for tt in range(ntile):
                    xi = nxt("x", 4)
                    m.dma(xr[xi][:], src[t0 + tt * 128:t0 + (tt + 1) * 128, :], writes=[d_xr[xi]], q="sp")
                    m.op("act", lambda e: e.activation(out=junk[:], in_=xr[xi][:], func=AF.Square, accum_out=s_[:, tt:tt + 1]),
                         reads=[d_xr[xi]], writes=[d_junk, ds_])
                    xs.append(xi)
                m.op("dve", lambda e: e.tensor_scalar(out=s_[:, 0:ntile], in0=s_[:, 0:ntile], scalar1=1.0 / D, scalar2=EPS, op0=ALU.mult, op1=ALU.add),
                     reads=[ds_], writes=[ds_])
                m.op("act", lambda e: e.activation(out=s_[:, 0:ntile], in_=s_[:, 0:ntile], func=AF.Sqrt), reads=[ds_], writes=[ds_])
                m.op("dve", lambda e: e.reciprocal(out=s_[:, 0:ntile], in_=s_[:, 0:ntile]), reads=[ds_], writes=[ds_])
                for tt in range(ntile):
                    ni = nxt("xn", 8)
                    m.op("dve", lambda e: e.tensor_scalar(out=xn[ni][:], in0=xr[xs[tt]][:], scalar1=s_[:, tt:tt + 1], scalar2=None, op0=ALU.mult),
                         reads=[d_xr[xs[tt]], ds_], writes=[d_xn[ni]])
                    xns.append(ni)
                for hf in range(2):
                    for kcl in range(4):
                        kc = hf * 4 + kcl
                        for tt in range(ntile):
                            m.op("pe", lambda e: e.transpose(out=pT[:, kcl, tt * 128:(tt + 1) * 128], in_=xn[xns[tt]][:, kc * 128:(kc + 1) * 128],
                                                             identity=self.ident[:]),
                                 reads=[d_xn[xns[tt]], self.d_const], writes=[d_pT], inc=(kcl == 3 and tt == ntile - 1))
                    for kcl in range(4):
                        kc = hf * 4 + kcl
                        m.op("dve", lambda e: e.tensor_scalar(out=h[:, kc, 0:ntok], in0=pT[:, kcl, 0:ntok], scalar1=self.AB[:, mi, kc:kc + 1],
                                                              scalar2=modt[:, kc:kc + 1], op0=ALU.mult, op1=ALU.add),
                             reads=[d_pT, self.d_mod], writes=[dh])
                if "hT" in self.dbg and bi in (0, 16):
                    nm = f"dbg_hT{bi}"
                    o = self.nc.dram_tensor(nm, [128, 8, 512], BF16, kind="ExternalOutput").ap(); self.out_names.append(nm)
                    m.dma(o[:, :, 0:ntok], h[:, :, 0:ntok], reads=[dh])
                if kind == "lat":
                    ci = bi % 2
                    m.dma(cs[ci][:, 0, :], I["rope_cos"][:, t0:t0 + 512], writes=[d_cs[ci]], q="sp")
                    m.dma(cs[ci][:, 1, :], I["rope_sin"][:, t0:t0 + 512], writes=[d_cs[ci]], q="sp")

                def proj(c0):
                    pi = nxt("pr", 5)
                    for kc in range(8):
                        m.op("pe", lambda e: e.matmul(pr[pi][:, 0:ntok], lhsT=w_in[:, kc, c0:c0 + 128], rhs=h[:, kc, 0:ntok],
                                                      start=(kc == 0), stop=(kc == 7)),
                             reads=[d_w, dh], writes=[d_pr[pi]], inc=(kc == 7))
                    return pi

                def qk_post(pi, gcol, rope, out_ap, d_out):
                    k_ = nxt("qk", 2)
                    m.op("act", lambda e: e.activation(out=sq[k_][:, 0:ntok], in_=pr[pi][:, 0:ntok], func=AF.Square),
                         reads=[d_pr[pi]], writes=[d_sq[k_]])
                    p2 = nxt("pr", 5)
                    m.op("pe", lambda e: e.matmul(pr[p2][:, 0:ntok], lhsT=cst[:, 1, :], rhs=sq[k_][:, 0:ntok], start=True, stop=True),
                         reads=[d_cst, d_sq[k_]], writes=[d_pr[p2]])
                    m.op("act", lambda e: e.activation(out=f1[k_][:, 0:ntok], in_=pr[p2][:, 0:ntok], func=AF.Sqrt, bias=self.epsT[:, 0:1]),
                         reads=[d_pr[p2], self.d_const], writes=[d_f1[k_]])
                    m.op("dve", lambda e: e.reciprocal(out=f1[k_][:, 0:ntok], in_=f1[k_][:, 0:ntok]), reads=[d_f1[k_]], writes=[d_f1[k_]])
                    if not rope:
                        m.op("dve", lambda e: e.scalar_tensor_tensor(out=out_ap, in0=pr[pi][:, 0:ntok], scalar=qkg[:, gcol:gcol + 1],
                                                                     in1=f1[k_][:, 0:ntok], op0=ALU.mult, op1=ALU.mult),
                             reads=[d_pr[pi], d_f1[k_], d_cst], writes=[d_out])
                        return
                    m.op("dve", lambda e: e.scalar_tensor_tensor(out=qn[k_][:, 0:ntok], in0=pr[pi][:, 0:ntok], scalar=qkg[:, gcol:gcol + 1],
                                                                 in1=f1[k_][:, 0:ntok], op0=ALU.mult, op1=ALU.mult),
                         reads=[d_pr[pi], d_f1[k_], d_cst], writes=[d_qn[k_]])
                    p3 = nxt("pr", 5)
                    m.op("pe", lambda e: e.matmul(pr[p3][:, 0:ntok], lhsT=cst[:, 0, :], rhs=qn[k_][:, 0:ntok], start=True, stop=True),
                         reads=[d_cst, d_qn[k_]], writes=[d_pr[p3]])
                    ci = bi % 2
                    m.op("dve", lambda e: e.tensor_tensor(out=f1[k_][:, 0:ntok], in0=qn[k_][:, 0:ntok], in1=cs[ci][:, 0, 0:ntok], op=ALU.mult),
                         reads=[d_qn[k_], d_cs[ci]], writes=[d_f1[k_]])
                    m.op("dve", lambda e: e.tensor_tensor(out=f2[k_][:, 0:ntok], in0=pr[p3][:, 0:ntok], in1=cs[ci][:, 1, 0:ntok], op=ALU.mult),
                         reads=[d_pr[p3], d_cs[ci]], writes=[d_f2[k_]])
                    m.op("pool", lambda e: e.tensor_tensor(out=out_ap, in0=f1[k_][:, 0:ntok], in1=f2[k_][:, 0:ntok], op=ALU.add),
                         reads=[d_f1[k_], d_f2[k_]], writes=[d_out])

                if own:
                    for j in range(4):
                        pi = proj(j * 128)
                        if kind == "lat":
                            qs = nxt("qst", 3)
                            qk_post(pi, 0, True, qst[qs][:], d_qst[qs])
                            m.dma(self.QTs[j, :, t0:t0 + 512], qst[qs][:], reads=[d_qst[qs]], q="act")
                        else:
                            qk_post(pi, 0, False, QC[:, j, :], d_QC)
                pi = proj(512)
                qk_post(pi, 1, kind == "lat", KT[:, col0:col0 + ntok], d_KT)
                for tt in range(ntile):
                    for kc in range(8):
                        m.op("pe", lambda e: e.matmul(pv[:, tt, :], lhsT=h[:, kc, tt * 128:(tt + 1) * 128], rhs=w_in[:, kc, 640:768],
                                                      start=(kc == 0), stop=(kc == 7)),
                             reads=[dh, d_w], writes=[d_pv], inc=(kc == 7))
                kt0 = col0 // 128
                m.op("act", lambda e: e.activation(out=VA[:, kt0:kt0 + ntile, :, 0:64],
                                                   in_=pv[:, 0:ntile, :].rearrange("p t (h d) -> p t h d", h=2), func=AF.Identity),
                     reads=[d_pv], writes=[d_VA])
                for ci_ in range(10):
                    pi = proj(768 + ci_ * 128)
                    si = nxt("stg", 4)
                    m.op("act", lambda e: e.activation(out=stg[si][:, 0:ntok], in_=pr[pi][:, 0:ntok], func=AF.Identity),
                         reads=[d_pr[pi]], writes=[d_stg[si]])
                    m.dma(self.U[ci_, :, col0:col0 + ntok], stg[si][:, 0:ntok], reads=[d_stg[si]], q="act")

    def _attention(self, KT, d_KT, QT, d_QT, QC, d_QC, VA, d_VA, negM, d_negM):
        m, I = self.m, self.I
        with Phase(m) as ph:
            self_f = ph.sb([65, 64]); d_sel = Dep()
            m.dma(self_f[:], I["sel"][:, :], writes=[d_sel])
            pS = [[ph.ps([128, 512]) for _ in range(2)] for _ in range(3)]
            d_pS = [[Dep(), Dep()] for _ in range(3)]
            pO = [ph.ps([65, 512]) for _ in range(2)]; d_pO = [Dep(), Dep()]
            Pt = [[ph.sb([128, 512], BF16) for _ in range(2)] for _ in range(3)]
            d_Pt = [[Dep(), Dep()] for _ in range(3)]
            osb = [ph.sb([65, 512]) for _ in range(2)]; d_osb = [Dep(), Dep()]
            att = [ph.sb([64, 512]) for _ in range(4)]; d_att = [Dep() for _ in range(4)]
            acnt = [0]

            qtl = [ph.sb([128, 512], BF16) for _ in range(3)]; d_qtl = [Dep() for _ in range(3)]
            qcnt = [0]

            def run(qsrc, d_q, j, q0, nq, keys, out_col0):
                nk = len(keys)

                def issue_S(i):
                    kc0, _ = keys[i]
                    for hb in range(2):
                        lo = hb * 64
                        m.op("pe", lambda e: e.matmul(pS[i % 3][hb][:, 0:nq], lhsT=KT[lo:lo + 64, kc0:kc0 + 128],
                                                      rhs=qsrc[lo:lo + 64, 0:nq], start=True, stop=True),
                             reads=[d_KT, d_q], writes=[d_pS[i % 3][hb]])
                issue_S(0)
                if nk > 1:
                    issue_S(1)
                for i in range(nk):
                    if i + 2 < nk:
                        issue_S(i + 2)
                    _, vt = keys[i]
                    for hb in range(2):
                        P_ = Pt[i % 3][hb]; dP = d_Pt[i % 3][hb]
                        m.op("act", lambda e: e.activation(out=P_[:, 0:nq], in_=pS[i % 3][hb][:, 0:nq], func=AF.Exp, scale=0.125, bias=negM[:, 0:1]),
                             reads=[d_pS[i % 3][hb], d_negM], writes=[dP])
                        m.op("pe", lambda e: e.matmul(pO[hb][:, 0:nq], lhsT=VA[:, vt, hb, :], rhs=P_[:, 0:nq], start=(i == 0), stop=(i == nk - 1)),
                             reads=[d_VA, dP], writes=[d_pO[hb]], inc=(i == nk - 1))
                for hb in range(2):
                    head = j + 4 * hb
                    o_ = osb[hb]; do = d_osb[hb]
                    m.op("act", lambda e: e.activation(out=o_[:, 0:nq], in_=pO[hb][:, 0:nq], func=AF.Identity), reads=[d_pO[hb]], writes=[do])
                    m.op("dve", lambda e: e.reciprocal(out=o_[64:65, 0:nq], in_=o_[64:65, 0:nq]), reads=[do], writes=[do])
                    pB = pS[nk % 3][hb]; d_pB = d_pS[nk % 3][hb]
                    m.op("pe", lambda e: e.matmul(pB[0:64, 0:nq], lhsT=self_f[:], rhs=o_[:, 0:nq], start=True, stop=True),
                         reads=[do, d_sel], writes=[d_pB])
                    ai = acnt[0] % 4; acnt[0] += 1
                    m.op("dve", lambda e: e.tensor_tensor(out=att[ai][:, 0:nq], in0=o_[0:64, 0:nq], in1=pB[0:64, 0:nq], op=ALU.mult),
                         reads=[do, d_pB], writes=[d_att[ai]])
                    m.dma(self.OT[head * 64:(head + 1) * 64, out_col0:out_col0 + nq], att[ai][:, 0:nq], reads=[d_att[ai]], q="sp")

            all_keys = [(kt * 128, kt) for kt in range(66)]
            ctx_keys = [(S + i * 128, 64 + i) for i in range(2)]
            nqb = self.nown // 512 if "att_short" not in self.dbg else 1
            for qb in range(nqb):
                for j in range(4):
                    qi = qcnt[0] % 3; qcnt[0] += 1
                    m.dma(qtl[qi][:], self.QTs[j, :, qb * 512:(qb + 1) * 512], writes=[d_qtl[qi]], q="sp")
                    run(qtl[qi], d_qtl[qi], j, qb * 512, 512, all_keys, qb * 512)
            if not self.last:
                for j in range(4):
                    run(QC[:, j, :], d_QC, j, 0, LC, ctx_keys, self.nown)

    def phase_lru(self):
        m, I = self.m, self.I
        SEG = 2048
        with Phase(m) as ph:
            d_c = Dep()
            cw = ph.sb([128, 2, 5]); cb = ph.sb([128, 2]); gb = ph.sb([128, 8]); lam = ph.sb([128, 4]); cvec = ph.sb([128, 4])
            wbd = ph.sb([128, 8, 128], BF16)
            m.dma(cw[:], I["lru_cw"][:, :, :], writes=[d_c]); m.dma(cb[:], I["lru_cb"][:, :], writes=[d_c])
            m.dma(gb[:], I["lru_gb"][:, :], writes=[d_c]); m.dma(lam[:], I["lru_lam"][:, :], writes=[d_c])
            m.dma(wbd[:], I["lru_wbd"][:, :, :], writes=[d_c], q="pool")
            m.op("act", lambda e: e.activation(out=cvec[:], in_=lam[:], func=AF.Exp, scale=-1.0), reads=[d_c], writes=[d_c])
            m.op("act", lambda e: e.activation(out=cvec[:], in_=cvec[:], func=AF.Ln, bias=1.0), reads=[d_c], writes=[d_c])
            m.op("dve", lambda e: e.tensor_scalar(out=cvec[:], in0=cvec[:], scalar1=-8.0, scalar2=None, op0=ALU.mult), reads=[d_c], writes=[d_c])
            lxp = [ph.sb([128, SEG + 4]) for _ in range(2)]; d_lxp = [Dep(), Dep()]
            xl = ph.sb([128, SEG]); d_xl = Dep()
            xlb = ph.sb([128, SEG], BF16); d_xlb = Dep()
            aS = ph.sb([128, SEG]); d_aS = Dep()
            bS = ph.sb([128, SEG]); d_bS = Dep()
            hS = [ph.sb([128, SEG]) for _ in range(2)]; d_hS = [Dep(), Dep()]
            hfS = [ph.sb([128, SEG]) for _ in range(2)]; d_hfS = [Dep(), Dep()]
            lgS = [ph.sb([128, SEG]) for _ in range(2)]; d_lgS = [Dep(), Dep()]
            g1 = ph.sb([128, SEG]); d_g1 = Dep()
            g2 = ph.sb([128, SEG]); d_g2 = Dep()
            tR = [ph.sb([128, 512]) for _ in range(2)]; d_tR = [Dep(), Dep()]
            tI = [ph.sb([128, 512]) for _ in range(2)]; d_tI = [Dep(), Dep()]
            tA = [ph.sb([128, 512]) for _ in range(2)]; d_tA = [Dep(), Dep()]
            pG = [[ph.ps([128, 512]) for _ in range(2)] for _ in range(2)]; d_pG = [[Dep(), Dep()], [Dep(), Dep()]]
            cr = ph.sb([128, 1]); d_cr = Dep()
            segs_f = [("ctx", 0, LC)] + [("lat", i * SEG, SEG) for i in range(S // SEG)]
            segs_b = [("ctx", 0, LC)] + [("lat", i * SEG, SEG) for i in reversed(range(S // SEG))]
            sc = 0
            for dirn in range(2):
                for c in range(2):
                    m.op("dve", lambda e: e.memset(cr[:], 0.0), writes=[d_cr])
                    for (kind, t0, ln) in (segs_f if dirn == 0 else segs_b):
                        seqlen = S if kind == "lat" else LC
                        base = 0 if kind == "lat" else S
                        bi = sc % 2; sc += 1
                        lp = lxp[bi]; dlp = d_lxp[bi]
                        lo = max(t0 - 2, 0); hi = min(t0 + ln + 2, seqlen)
                        if t0 - 2 < 0:
                            m.op("pool", lambda e: e.memset(lp[:, 0:2], 0.0), writes=[dlp])
                        if t0 + ln + 2 > seqlen:
                            m.op("pool", lambda e: e.memset(lp[:, ln + 2:ln + 4], 0.0), writes=[dlp])
                        m.dma(lp[:, lo - (t0 - 2):hi - (t0 - 2)], self.U[c, :, base + lo:base + hi], writes=[dlp], q="sp")
                        own = (kind == "ctx" and not self.last) or (kind == "lat" and t0 < self.nown)
                        if dirn == 1:
                            m.dma(hfS[bi][:, 0:ln], self.HF[c, :, base + t0:base + t0 + ln], writes=[d_hfS[bi]], q="sp")
                            if own:
                                m.dma(lgS[bi][:, 0:ln], self.U[2 + c, :, base + t0:base + t0 + ln], writes=[d_lgS[bi]], q="sp")
                        m.op("dve", lambda e: e.tensor_scalar(out=xl[:, 0:ln], in0=lp[:, 0:ln], scalar1=cw[:, c, 0:1], scalar2=cb[:, c:c + 1],
                                                              op0=ALU.mult, op1=ALU.add), reads=[dlp, d_c], writes=[d_xl])
                        for o in range(1, 5):
                            m.op("dve", lambda e: e.scalar_tensor_tensor(out=xl[:, 0:ln], in0=lp[:, o:o + ln], scalar=cw[:, c, o:o + 1], in1=xl[:, 0:ln],
                                                                         op0=ALU.mult, op1=ALU.add), reads=[dlp, d_c, d_xl], writes=[d_xl])
                        m.op("act", lambda e: e.activation(out=xlb[:, 0:ln], in_=xl[:, 0:ln], func=AF.Identity), reads=[d_xl], writes=[d_xlb])
                        for sb_ in range((ln + 511) // 512):
                            s0 = sb_ * 512; n = min(512, ln - s0); k_ = sb_ % 2
                            for g in range(2):
                                m.op("pe", lambda e: e.matmul(pG[k_][g][:, 0:n], lhsT=wbd[:, dirn * 4 + g * 2 + c, :], rhs=xlb[:, s0:s0 + n],
                                                              start=True, stop=True), reads=[d_c, d_xlb], writes=[d_pG[k_][g]])
                            gi = dirn * 4 + c
                            m.op("act", lambda e: e.activation(out=tR[k_][:, 0:n], in_=pG[k_][0][:, 0:n], func=AF.Sigmoid, bias=gb[:, gi:gi + 1]),
                                 reads=[d_pG[k_][0], d_c], writes=[d_tR[k_]])
                            m.op("act", lambda e: e.activation(out=tI[k_][:, 0:n], in_=pG[k_][1][:, 0:n], func=AF.Sigmoid, bias=gb[:, gi + 2:gi + 3]),
                                 reads=[d_pG[k_][1], d_c], writes=[d_tI[k_]])
                            m.op("act", lambda e: e.activation(out=aS[:, s0:s0 + n], in_=tR[k_][:, 0:n], func=AF.Exp, scale=cvec[:, dirn * 2 + c:dirn * 2 + c + 1]),
                                 reads=[d_tR[k_], d_c], writes=[d_aS])
                            m.op("pool", lambda e: e.tensor_tensor(out=tA[k_][:, 0:n], in0=aS[:, s0:s0 + n], in1=aS[:, s0:s0 + n], op=ALU.mult),
                                 reads=[d_aS], writes=[d_tA[k_]])
                            m.op("pool", lambda e: e.tensor_scalar(out=tA[k_][:, 0:n], in0=tA[k_][:, 0:n], scalar1=-1.0, scalar2=1.0, op0=ALU.mult, op1=ALU.add),
                                 reads=[d_tA[k_]], writes=[d_tA[k_]])
                            m.op("act", lambda e: e.activation(out=tA[k_][:, 0:n], in_=tA[k_][:, 0:n], func=AF.Sqrt), reads=[d_tA[k_]], writes=[d_tA[k_]])
                            m.op("pool", lambda e: e.tensor_tensor(out=tI[k_][:, 0:n], in0=tI[k_][:, 0:n], in1=xl[:, s0:s0 + n], op=ALU.mult),
                                 reads=[d_tI[k_], d_xl], writes=[d_tI[k_]])
                            m.op("dve", lambda e: e.tensor_tensor(out=bS[:, s0:s0 + n], in0=tA[k_][:, 0:n], in1=tI[k_][:, 0:n], op=ALU.mult),
                                 reads=[d_tA[k_], d_tI[k_]], writes=[d_bS])
                        h_ = hS[bi]; dh = d_hS[bi]
                        if dirn == 0:
                            m.op("dve", lambda e: e.tensor_tensor_scan(out=h_[:, 0:ln], data0=aS[:, 0:ln], data1=bS[:, 0:ln], initial=cr[:, 0:1],
                                                                       op0=ALU.mult, op1=ALU.add), reads=[d_aS, d_bS, d_cr], writes=[dh])
                            m.op("dve", lambda e: e.tensor_copy(out=cr[:], in_=h_[:, ln - 1:ln]), reads=[dh], writes=[d_cr])
                            m.dma(self.HF[c, :, base + t0:base + t0 + ln], h_[:, 0:ln], reads=[dh], q="act")
                        else:
                            m.op("dve", lambda e: e.tensor_tensor_scan(out=h_[:, ln - 1::-1] if ln == SEG else h_[:, ln - 1::-1],
                                                                       data0=aS[:, ln - 1::-1], data1=bS[:, ln - 1::-1], initial=cr[:, 0:1],
                                                                       op0=ALU.mult, op1=ALU.add), reads=[d_aS, d_bS, d_cr], writes=[dh])
                            m.op("dve", lambda e: e.tensor_copy(out=cr[:], in_=h_[:, 0:1]), reads=[dh], writes=[d_cr])
                            if own:
                                lg_ = lgS[bi]; dlg = d_lgS[bi]
                                m.op("pool", lambda e: e.tensor_tensor(out=h_[:, 0:ln], in0=h_[:, 0:ln], in1=hfS[bi][:, 0:ln], op=ALU.add),
                                     reads=[dh, d_hfS[bi]], writes=[dh])
                                m.op("pool", lambda e: e.tensor_tensor(out=g1[:, 0:ln], in0=lg_[:, 0:ln], in1=lg_[:, 0:ln], op=ALU.mult),
                                     reads=[dlg], writes=[d_g1])
                                m.op("pool", lambda e: e.tensor_scalar(out=g1[:, 0:ln], in0=g1[:, 0:ln], scalar1=0.044715, scalar2=1.0, op0=ALU.mult, op1=ALU.add),
                                     reads=[d_g1], writes=[d_g1])
                                m.op("pool", lambda e: e.tensor_tensor(out=g1[:, 0:ln], in0=g1[:, 0:ln], in1=lg_[:, 0:ln], op=ALU.mult),
                                     reads=[d_g1, dlg], writes=[d_g1])
                                m.op("act", lambda e: e.activation(out=g1[:, 0:ln], in_=g1[:, 0:ln], func=AF.Sigmoid, scale=1.5957691216057308),
                                     reads=[d_g1], writes=[d_g1])
                                m.op("dve", lambda e: e.tensor_tensor(out=g1[:, 0:ln], in0=g1[:, 0:ln], in1=lg_[:, 0:ln], op=ALU.mult),
                                     reads=[d_g1, dlg], writes=[d_g1])
                                m.op("dve", lambda e: e.tensor_tensor(out=g2[:, 0:ln], in0=g1[:, 0:ln], in1=h_[:, 0:ln], op=ALU.mult),
                                     reads=[d_g1, dh], writes=[d_g2])
                                oc = t0 if kind == "lat" else self.nown
                                m.dma(self.OT[512 + c * 128:512 + (c + 1) * 128, oc:oc + ln], g2[:, 0:ln], reads=[d_g2], q="act")
                    m.barrier()

    def phase_hyena(self):
        self._hy_filters_and_conv()
        self.m.barrier()
        self._hy_fft()

    def _sin9(self, m, ph, out, psum, n, sc, bi, d_in, d_out, tmp, d_tmp, d_c):
        m.op("act", lambda e: e.activation(out=out, in_=psum, func=AF.Sin, scale=sc, bias=bi), reads=[d_in, d_c], writes=[d_out])
        for _ in range(2):
            m.op("dve", lambda e: e.tensor_tensor(out=tmp, in0=out, in1=out, op=ALU.mult), reads=[d_out], writes=[d_tmp])
            m.op("dve", lambda e: e.tensor_scalar(out=tmp, in0=tmp, scalar1=-4.0, scalar2=3.0, op0=ALU.mult, op1=ALU.add), reads=[d_tmp], writes=[d_tmp])
            m.op("dve", lambda e: e.tensor_tensor(out=out, in0=out, in1=tmp, op=ALU.mult), reads=[d_out, d_tmp], writes=[d_out])

    def _hy_filters_and_conv(self):
        m, I = self.m, self.I
        G = self.G
        self.hyP = Phase(m); P = self.hyP; P.__enter__()
        self.kc_f = P.sb([128, 4, LC]); self.d_kc = Dep()
        self.nrm = P.sb([128, 2, 4]); self.d_nrm = Dep()
        self.vvc = P.sb([128, 2, LC]); self.x0c = P.sb([128, 2, LC]); self.d_vvc = Dep()
        self.hsc = P.sb([128, 2, 2]); self.d_hsc = Dep()
        with Phase(m) as ph:
            d_c = Dep()
            w1 = ph.sb([33, 64]); w2 = ph.sb([64, 64]); w3 = ph.sb([64, 768]); b12f = ph.sb([64, 3]); dec = ph.sb([128, 4])
            for t_, n_ in ((w1, "hy_w1"), (w2, "hy_w2"), (w3, "hy_w3"), (b12f, "hy_b12f"), (dec, "hy_dec")):
                m.dma(t_[:], I[n_][:, :], writes=[d_c])
            scb = ph.sb([64, 3])
            m.op("dve", lambda e: e.tensor_scalar(out=scb[:, 0:1], in0=b12f[:, 2:3], scalar1=1.0 / 9.0, scalar2=None, op0=ALU.mult), reads=[d_c], writes=[d_c])
            m.op("dve", lambda e: e.tensor_scalar(out=scb[:, 1:3], in0=b12f[:, 0:2], scalar1=scb[:, 0:1], scalar2=None, op0=ALU.mult), reads=[d_c], writes=[d_c])
            nad = ph.sb([128, 4])
            m.op("dve", lambda e: e.tensor_scalar(out=nad[:], in0=dec[:], scalar1=-1.0, scalar2=None, op0=ALU.mult), reads=[d_c], writes=[d_c])
            m.op("dve", lambda e: e.tensor_tensor(out=nad[:], in0=nad[:], in1=dec[:], op=ALU.min), reads=[d_c], writes=[d_c])
            m.op("dve", lambda e: e.memset(self.nrm[:], 0.0), writes=[self.d_nrm])
            zb = [ph.sb([33, 512]) for _ in range(2)]; d_zb = [Dep(), Dep()]
            tb = [ph.sb([128, 512]) for _ in range(2)]; d_tb = [Dep(), Dep()]
            h1 = ph.sb([64, 512]); d_h1 = Dep()
            h2 = ph.sb([64, 512]); d_h2 = Dep()
            tmp = ph.sb([64, 512]); d_tmp = Dep()
            ex = [ph.sb([128, 512]) for _ in range(2)]; d_ex = [Dep(), Dep()]
            kk = [ph.sb([128, 512], BF16) for _ in range(3)]; d_kk = [Dep() for _ in range(3)]
            red = ph.sb([128, 1]); d_red = Dep()
            c0t = ph.sb([128, 2, 2]); d_c0 = Dep()
            p1 = ph.ps([64, 512]); d_p1 = Dep()
            p2 = ph.ps([64, 512]); d_p2 = Dep()
            p3 = [ph.ps([128, 512]) for _ in range(2)]; d_p3 = [Dep(), Dep()]
            pc = ph.ps([128, 2, 2]); d_pc = Dep()
            kc_i = 0
            for which, (zname, tname, L) in enumerate((("zT", "t01", S), ("zTc", "t01c", LC))[:(1 if self.last else 2)]):
                for bi_ in range((L + 511) // 512):
                    c0_ = bi_ * 512; n = min(512, L - c0_); r = bi_ % 2
                    m.dma(zb[r][:, 0:n], I[zname][:, c0_:c0_ + n], writes=[d_zb[r]])
                    m.dma(tb[r][:, 0:n], I[tname][:, c0_:c0_ + n], writes=[d_tb[r]])
                    m.op("pe", lambda e: e.matmul(p1[:, 0:n], lhsT=w1[:], rhs=zb[r][:, 0:n], start=True, stop=True), reads=[d_c, d_zb[r]], writes=[d_p1])
                    self._sin9(m, ph, h1[:, 0:n], p1[:, 0:n], n, scb[:, 0:1], scb[:, 1:2], d_p1, d_h1, tmp[:, 0:n], d_tmp, d_c)
                    m.op("pe", lambda e: e.matmul(p2[:, 0:n], lhsT=w2[:], rhs=h1[:, 0:n], start=True, stop=True), reads=[d_c, d_h1], writes=[d_p2])
                    self._sin9(m, ph, h2[:, 0:n], p2[:, 0:n], n, scb[:, 0:1], scb[:, 2:3], d_p2, d_h2, tmp[:, 0:n], d_tmp, d_c)
                    if bi_ == 0:
                        for c in range(2):
                            m.op("pe", lambda e: e.matmul(pc[:, c, :], lhsT=w3[:, 512 + c * 128:512 + (c + 1) * 128], rhs=h2[:, 0:2], start=True, stop=True),
                                 reads=[d_c, d_h2], writes=[d_pc])
                        m.op("act", lambda e: e.activation(out=c0t[:], in_=pc[:], func=AF.Identity), reads=[d_pc], writes=[d_c0])
                    for ci in range(4):
                        pi = ci % 2
                        m.op("pe", lambda e: e.matmul(p3[pi][:, 0:n], lhsT=w3[:, ci * 128:(ci + 1) * 128], rhs=h2[:, 0:n], start=True, stop=True),
                             reads=[d_c, d_h2], writes=[d_p3[pi]])
                        m.op("act", lambda e: e.activation(out=ex[pi][:, 0:n], in_=tb[r][:, 0:n], func=AF.Exp, scale=nad[:, ci:ci + 1]),
                             reads=[d_tb[r], d_c], writes=[d_ex[pi]])
                        if which == 0:
                            ki = kc_i % 3; kc_i += 1
                            kt_ = kk[ki][:, 0:n]; dk = d_kk[ki]
                        else:
                            kt_ = self.kc_f[:, ci, :]; dk = self.d_kc
                        m.op("dve", lambda e: e.tensor_tensor(out=kt_, in0=p3[pi][:, 0:n], in1=ex[pi][:, 0:n], op=ALU.mult),
                             reads=[d_p3[pi], d_ex[pi]], writes=[dk])
                        if bi_ == 0:
                            if ci < 2:
                                m.op("dve", lambda e: e.tensor_copy(out=kt_[:, 0:1], in_=c0t[:, ci, 0:1]), reads=[d_c0, dk], writes=[dk])
                            else:
                                m.op("dve", lambda e: e.memset(kt_[:, 0:1], 0.0), reads=[dk], writes=[dk])
                        m.op("dve", lambda e: e.tensor_reduce(out=red[:], in_=kt_, axis=AX.X, op=ALU.add, apply_absolute_value=True),
                             reads=[dk], writes=[d_red])
                        m.op("dve", lambda e: e.tensor_tensor(out=self.nrm[:, which, ci:ci + 1], in0=self.nrm[:, which, ci:ci + 1], in1=red[:], op=ALU.add),
                             reads=[d_red, self.d_nrm], writes=[self.d_nrm])
                        if which == 0:
                            m.dma(self.KFb[ci * 128:(ci + 1) * 128, c0_:c0_ + n], kt_, reads=[dk], q="act")
            m.op("dve", lambda e: e.tensor_tensor(out=self.hsc[:], in0=self.nrm[:, :, 0:2], in1=self.nrm[:, :, 2:4], op=ALU.add),
                 reads=[self.d_nrm], writes=[self.d_hsc])
            m.op("dve", lambda e: e.tensor_scalar(out=self.hsc[:, 0, :], in0=self.hsc[:, 0, :], scalar1=float(NFFT), scalar2=None, op0=ALU.mult),
                 reads=[self.d_hsc], writes=[self.d_hsc])
            m.op("dve", lambda e: e.reciprocal(out=self.hsc[:], in_=self.hsc[:]), reads=[self.d_hsc], writes=[self.d_hsc])
        with Phase(m) as ph:
            SEG = 2048
            d_c = Dep()
            cw = ph.sb([128, 6, 3]); cb = ph.sb([128, 6])
            m.dma(cw[:], I["hy_cw"][:, :, :], writes=[d_c]); m.dma(cb[:], I["hy_cb"][:, :], writes=[d_c])
            pad = [[ph.sb([128, SEG + 2]) for _ in range(3)] for _ in range(2)]; d_pad = [[Dep() for _ in range(3)] for _ in range(2)]
            uc = [ph.sb([128, SEG]) for _ in range(3)]; d_uc = [Dep() for _ in range(3)]
            vv = [ph.sb([128, SEG]) for _ in range(2)]; d_vv = [Dep(), Dep()]
            vvb = [ph.sb([128, SEG], BF16) for _ in range(2)]; d_vvb = [Dep(), Dep()]
            sc = 0
            segs = [("lat", i * SEG, SEG) for i in range(S // SEG)] + ([] if self.last else [("ctx", 0, LC)])
            for (kind, t0, ln) in segs:
                seqlen = S if kind == "lat" else LC
                base = 0 if kind == "lat" else S
                for c in range(2):
                    bi = sc % 2; sc += 1
                    for k3 in range(3):
                        ci = 2 * k3 + c
                        lp = pad[bi][k3]; dlp = d_pad[bi][k3]
                        lo = max(t0 - 1, 0); hi = min(t0 + ln + 1, seqlen)
                        if t0 - 1 < 0:
                            m.op("pool", lambda e: e.memset(lp[:, 0:1], 0.0), writes=[dlp])
                        if t0 + ln + 1 > seqlen:
                            m.op("pool", lambda e: e.memset(lp[:, ln + 1:ln + 2], 0.0), writes=[dlp])
                        m.dma(lp[:, lo - (t0 - 1):hi - (t0 - 1)], self.U[4 + ci, :, base + lo:base + hi], writes=[dlp], q="sp")
                        eng = "dve" if k3 != 0 else "pool"
                        u_ = uc[k3]; du = d_uc[k3]
                        if kind == "ctx" and k3 == 0:
                            u_ = self.x0c[:, c, :]
                            du = self.d_vvc
                        else:
                            u_ = u_[:, 0:ln]
                        m.op("dve", lambda e: e.tensor_scalar(out=u_, in0=lp[:, 0:ln], scalar1=cw[:, ci, 0:1], scalar2=cb[:, ci:ci + 1],
                                                              op0=ALU.mult, op1=ALU.add), reads=[dlp, d_c], writes=[du])
                        for o in range(1, 3):
                            m.op("dve", lambda e: e.scalar_tensor_tensor(out=u_, in0=lp[:, o:o + ln], scalar=cw[:, ci, o:o + 1], in1=u_,
                                                                         op0=ALU.mult, op1=ALU.add), reads=[dlp, d_c, du], writes=[du])
                    if kind == "lat":
                        m.op("pool", lambda e: e.tensor_tensor(out=vv[bi][:, 0:ln], in0=uc[1][:, 0:ln], in1=uc[2][:, 0:ln], op=ALU.mult),
                             reads=[d_uc[1], d_uc[2]], writes=[d_vv[bi]])
                        m.dma(self.VV[c * 128:(c + 1) * 128, t0:t0 + ln], vv[bi][:, 0:ln], reads=[d_vv[bi]], q="act")
                        m.op("act", lambda e: e.activation(out=vvb[bi][:, 0:ln], in_=vv[bi][:, 0:ln], func=AF.Identity), reads=[d_vv[bi]], writes=[d_vvb[bi]])
                        m.dma(self.VVb[c * 128:(c + 1) * 128, t0:t0 + ln], vvb[bi][:, 0:ln], reads=[d_vvb[bi]], q="act")
                        if t0 < self.nown:
                            m.dma(self.X0[c * 128:(c + 1) * 128, t0:t0 + ln], uc[0][:, 0:ln], reads=[d_uc[0]], q="act")
                    else:
                        m.op("pool", lambda e: e.tensor_tensor(out=self.vvc[:, c, :], in0=uc[1][:, 0:ln], in1=uc[2][:, 0:ln], op=ALU.mult),
                             reads=[d_uc[1], d_uc[2]], writes=[self.d_vvc])
            yc = ph.sb([128, 2, LC]); d_yc = Dep()
            hb = ph.sb([128, 2]); m.dma(hb[:], I["hy_bias"][:, :], writes=[d_c])
            for c in range(0 if self.last else 2):
                y_ = yc[:, c, :]; v_ = self.vvc[:, c, :]
                m.op("dve", lambda e: e.tensor_scalar(out=y_, in0=v_, scalar1=self.kc_f[:, c, 0:1], scalar2=None, op0=ALU.mult),
                     reads=[self.d_vvc, self.d_kc], writes=[d_yc])
                for dl in range(1, LC):
                    m.op("dve", lambda e: e.scalar_tensor_tensor(out=y_[:, dl:], in0=v_[:, 0:LC - dl], scalar=self.kc_f[:, c, dl:dl + 1], in1=y_[:, dl:],
                                                                 op0=ALU.mult, op1=ALU.add), reads=[d_yc], writes=[d_yc])
                    m.op("dve", lambda e: e.scalar_tensor_tensor(out=y_[:, 0:LC - dl], in0=v_[:, dl:], scalar=self.kc_f[:, 2 + c, dl:dl + 1], in1=y_[:, 0:LC - dl],
                                                                 op0=ALU.mult, op1=ALU.add), reads=[d_yc], writes=[d_yc])
                m.op("dve", lambda e: e.tensor_scalar(out=y_, in0=y_, scalar1=self.hsc[:, 1, c:c + 1], scalar2=None, op0=ALU.mult),
                     reads=[d_yc, self.d_hsc], writes=[d_yc])
                m.op("dve", lambda e: e.scalar_tensor_tensor(out=y_, in0=v_, scalar=hb[:, c:c + 1], in1=y_, op0=ALU.mult, op1=ALU.add),
                     reads=[d_yc, d_c, self.d_vvc], writes=[d_yc])
                m.op("dve", lambda e: e.tensor_tensor(out=y_, in0=y_, in1=self.x0c[:, c, :], op=ALU.mult), reads=[d_yc, self.d_vvc], writes=[d_yc])
                m.dma(self.OT[768 + c * 128:768 + (c + 1) * 128, self.nown:self.nown + LC], y_, reads=[d_yc], q="act")

    def _hy_fft(self):
        m, I = self.m, self.I
        GC = 32
        with Phase(m) as ph:
            d_c = Dep()
            C2 = ph.sb([128, 3, 128], BF16); FI = ph.sb([128, 128, 2, 32], BF16); d_FI = Dep()
            m.dma(C2[:], I["C2"][:, :, :], writes=[d_c])
            fi_loaded = [None]
            x1s = [ph.sb([64, GC, 128], BF16) for _ in range(2)]; d_x1 = [Dep(), Dep()]
            f1p = [ph.sb([64, 16, 2, 128], BF16) for _ in range(2)]; d_f1 = [Dep() for _ in range(2)]
            bP = ph.sb([128, 2, 128, GC], BF16); d_bP = Dep()
            bQ = ph.sb([128, 2, GC, 128], BF16); d_bQ = Dep()
            bZ = ph.sb([128, 2, GC, 128], BF16); d_bZ = Dep()
            Kf = ph.sb([128, 2, GC, 128], BF16); d_Kf = Dep()
            Yb = ph.sb([128, 2, GC, 128], BF16); d_Yb = Dep()
            t4 = [ph.sb([128, 512]) for _ in range(4)]; d_t4 = [Dep() for _ in range(4)]
            ysb = ph.sb([GC, 32, 128]); d_ysb = Dep()
            fv = [ph.sb([GC, 512]) for _ in range(2)]; d_fv = [Dep(), Dep()]
            fx = [ph.sb([GC, 512]) for _ in range(2)]; d_fx = [Dep(), Dep()]
            fo = [ph.sb([GC, 512]) for _ in range(2)]; d_fo = [Dep(), Dep()]
            gsc = ph.sb([GC, 2]); d_gsc = Dep()
            pA = [ph.ps([128, 16, GC]) for _ in range(2)]; d_pA = [Dep(), Dep()]
            pT = [ph.ps([128, 8, 128], BF16) for _ in range(2)]; d_pT = [Dep(), Dep()]
            pX = [[ph.ps([128, 512]) for _ in range(2)] for _ in range(2)]; d_pX = [[Dep(), Dep()], [Dep(), Dep()]]
            cnt = {"x1": 0, "f1": 0, "pA": 0, "pT": 0, "pX": 0, "ev": 0, "fin": 0}

            def nxt(k, n):
                i = cnt[k] % n; cnt[k] += 1
                return i

            def evac(out, in_, reads, writes):
                if nxt("ev", 2) == 0:
                    m.op("act", lambda e: e.activation(out=out, in_=in_, func=AF.Identity), reads=reads, writes=writes)
                else:
                    m.op("dve", lambda e: e.tensor_copy(out=out, in_=in_), reads=reads, writes=writes)

            def fwd(src_rows, mode):
                xi = nxt("x1", 2)
                m.dma(x1s[xi][:], src_rows.rearrange("c (a b) -> a c b", b=128), writes=[d_x1[xi]], q="sp")
                for bg in range(8):
                    fi = nxt("f1", 2)
                    m.dma(f1p[fi][:], I["F1"][:, bg * 16:(bg + 1) * 16, :, :], writes=[d_f1[fi]], q="sp")
                    for hb in range(2):
                        pi = nxt("pA", 2)
                        for bl in range(8):
                            b = bg * 16 + hb * 8 + bl
                            for ri in range(2):
                                m.op("pe", lambda e: e.matmul(pA[pi][:, bl * 2 + ri, :], lhsT=f1p[fi][:, hb * 8 + bl, ri, :], rhs=x1s[xi][:, :, b],
                                                              start=True, stop=True),
                                     reads=[d_f1[fi], d_x1[xi]], writes=[d_pA[pi]], inc=(bl == 7 and ri == 1))
                        b0 = bg * 16 + hb * 8
                        evac(bP[:, :, b0:b0 + 8, :], pA[pi][:].rearrange("p (b r) c -> p r b c", r=2), [d_pA[pi]], [d_bP])
                for ri in range(2):
                    for cg in range(GC // 8):
                        ti = nxt("pT", 2)
                        for k in range(8):
                            ch = cg * 8 + k
                            m.op("pe", lambda e: e.transpose(out=pT[ti][:, k, :], in_=bP[:, ri, :, ch], identity=self.ident[:]),
                                 reads=[d_bP, self.d_const], writes=[d_pT[ti]], inc=(k == 7))
                        evac(bQ[:, ri, cg * 8:(cg + 1) * 8, :], pT[ti][:], [d_pT[ti]], [d_bQ])
                for blk in range(GC // 4):
                    xi_ = nxt("pX", 2)
                    cs_ = slice(blk * 4, blk * 4 + 4)
                    are = bQ[:, 0, cs_, :]; aim = bQ[:, 1, cs_, :]
                    m.op("pe", lambda e: e.matmul(pX[xi_][0][:], lhsT=C2[:, 0, :], rhs=are, start=True, stop=False), reads=[d_c, d_bQ], writes=[d_pX[xi_][0]], inc=False)
                    m.op("pe", lambda e: e.matmul(pX[xi_][0][:], lhsT=C2[:, 1, :], rhs=aim, start=False, stop=True), reads=[d_c, d_bQ], writes=[d_pX[xi_][0]])
                    m.op("pe", lambda e: e.matmul(pX[xi_][1][:], lhsT=C2[:, 0, :], rhs=aim, start=True, stop=False), reads=[d_c, d_bQ], writes=[d_pX[xi_][1]], inc=False)
                    m.op("pe", lambda e: e.matmul(pX[xi_][1][:], lhsT=C2[:, 2, :], rhs=are, start=False, stop=True), reads=[d_c, d_bQ], writes=[d_pX[xi_][1]])
                    pre, pim = pX[xi_][0][:], pX[xi_][1][:]
                    dre, dim_ = d_pX[xi_][0], d_pX[xi_][1]
                    kre = Kf[:, 0, cs_, :]; kim = Kf[:, 1, cs_, :]
                    if mode == "A":
                        evac(kre, pre, [dre], [d_Kf]); evac(kim, pim, [dim_], [d_Kf])
                    elif mode == "B":
                        m.op("dve", lambda e: e.tensor_tensor(out=kre, in0=pre, in1=kre, op=ALU.add), reads=[dre, d_Kf], writes=[d_Kf])
                        m.op("dve", lambda e: e.tensor_tensor(out=kim, in0=kim, in1=pim, op=ALU.subtract), reads=[dim_, d_Kf], writes=[d_Kf])
                    else:
                        a_, b_, c_, e_ = t4
                        m.op("dve", lambda e: e.tensor_tensor(out=a_[:], in0=pre, in1=kre, op=ALU.mult), reads=[dre, d_Kf], writes=[d_t4[0]])
                        m.op("dve", lambda e: e.tensor_tensor(out=b_[:], in0=pim, in1=kim, op=ALU.mult), reads=[dim_, d_Kf], writes=[d_t4[1]])
                        m.op("dve", lambda e: e.tensor_tensor(out=c_[:], in0=pre, in1=kim, op=ALU.mult), reads=[dre, d_Kf], writes=[d_t4[2]])
                        m.op("dve", lambda e: e.tensor_tensor(out=e_[:], in0=pim, in1=kre, op=ALU.mult), reads=[dim_, d_Kf], writes=[d_t4[3]])
                        m.op("dve", lambda e: e.tensor_tensor(out=Yb[:, 0, cs_, :], in0=a_[:], in1=b_[:], op=ALU.subtract),
                             reads=[d_t4[0], d_t4[1]], writes=[d_Yb])
                        m.op("pool", lambda e: e.tensor_tensor(out=Yb[:, 1, cs_, :], in0=c_[:], in1=e_[:], op=ALU.add),
                             reads=[d_t4[2], d_t4[3]], writes=[d_Yb])

            def inv(g):
                for blk in range(GC // 4):
                    xi_ = nxt("pX", 2)
                    cs_ = slice(blk * 4, blk * 4 + 4)
                    yre = Yb[:, 0, cs_, :]; yim = Yb[:, 1, cs_, :]
                    m.op("pe", lambda e: e.matmul(pX[xi_][0][:], lhsT=C2[:, 0, :], rhs=yre, start=True, stop=False), reads=[d_c, d_Yb], writes=[d_pX[xi_][0]], inc=False)
                    m.op("pe", lambda e: e.matmul(pX[xi_][0][:], lhsT=C2[:, 2, :], rhs=yim, start=False, stop=True), reads=[d_c, d_Yb], writes=[d_pX[xi_][0]])
                    m.op("pe", lambda e: e.matmul(pX[xi_][1][:], lhsT=C2[:, 1, :], rhs=yre, start=True, stop=False), reads=[d_c, d_Yb], writes=[d_pX[xi_][1]], inc=False)
                    m.op("pe", lambda e: e.matmul(pX[xi_][1][:], lhsT=C2[:, 0, :], rhs=yim, start=False, stop=True), reads=[d_c, d_Yb], writes=[d_pX[xi_][1]])
                    evac(bZ[:, 0, cs_, :], pX[xi_][0][:], [d_pX[xi_][0]], [d_bZ])
                    evac(bZ[:, 1, cs_, :], pX[xi_][1][:], [d_pX[xi_][1]], [d_bZ])
                for ri in range(2):
                    for cg in range(GC // 8):
                        ti = nxt("pT", 2)
                        for k in range(8):
                            ch = cg * 8 + k
                            m.op("pe", lambda e: e.transpose(out=pT[ti][:, k, :], in_=bZ[:, ri, ch, :], identity=self.ident[:]),
                                 reads=[d_bZ, self.d_const], writes=[d_pT[ti]], inc=(k == 7))
                        evac(bQ[:, ri, cg * 8:(cg + 1) * 8, :], pT[ti][:], [d_pT[ti]], [d_bQ])
                c = (g * GC) // 128; r0 = (g * GC) % 128
                m.dma(gsc[:, 0:1], self.hsc[r0:r0 + GC, 0, c:c + 1], reads=[self.d_hsc], writes=[d_gsc], q="sp", allow_slow_non_contiguous=True)
                m.dma(gsc[:, 1:2], I["hy_bias"][r0:r0 + GC, c:c + 1], writes=[d_gsc], q="sp", allow_slow_non_contiguous=True)
                for ah in range(self.nown // SH):
                    if fi_loaded[0] != ah:
                        m.dma(FI[:], I["FI%d" % ah][:, :, :, :], writes=[d_FI], q="sp")
                        fi_loaded[0] = ah
                    for bg in range(8):
                        pi = nxt("pA", 2)
                        for bl in range(16):
                            b = bg * 16 + bl
                            m.op("pe", lambda e: e.matmul(pA[pi][0:GC, bl, :], lhsT=bQ[:, 0, :, b], rhs=FI[:, b, 0, :], start=True, stop=False),
                                 reads=[d_bQ, d_FI], writes=[d_pA[pi]], inc=False)
                            m.op("pe", lambda e: e.matmul(pA[pi][0:GC, bl, :], lhsT=bQ[:, 1, :, b], rhs=FI[:, b, 1, :], start=False, stop=True),
                                 reads=[d_bQ, d_FI], writes=[d_pA[pi]], inc=(bl == 15))
                        evac(ysb[:, :, bg * 16:(bg + 1) * 16], pA[pi][0:GC, :, :].rearrange("p b a -> p a b"), [d_pA[pi]], [d_ysb])
                    yfl = ysb[:].rearrange("p a b -> p (a b)")
                    for q4 in range(8):
                        fi = nxt("fin", 2)
                        sl = slice(q4 * 512, (q4 + 1) * 512)
                        gl = slice(ah * SH + q4 * 512, ah * SH + (q4 + 1) * 512)
                        m.dma(fv[fi][:], self.VV[g * GC:(g + 1) * GC, gl], writes=[d_fv[fi]], q="sp")
                        m.dma(fx[fi][:], self.X0[g * GC:(g + 1) * GC, gl], writes=[d_fx[fi]], q="sp")
                        m.op("act", lambda e: e.activation(out=fo[fi][:], in_=yfl[:, sl], func=AF.Identity, scale=gsc[:, 0:1]),
                             reads=[d_ysb, d_gsc], writes=[d_fo[fi]])
                        m.op("dve", lambda e: e.scalar_tensor_tensor(out=fo[fi][:], in0=fv[fi][:], scalar=gsc[:, 1:2], in1=fo[fi][:], op0=ALU.mult, op1=ALU.add),
                             reads=[d_fv[fi], d_gsc, d_fo[fi]], writes=[d_fo[fi]])
                        m.op("dve", lambda e: e.tensor_tensor(out=fo[fi][:], in0=fo[fi][:], in1=fx[fi][:], op=ALU.mult),
                             reads=[d_fo[fi], d_fx[fi]], writes=[d_fo[fi]])
                        m.dma(self.OT[768 + g * GC:768 + (g + 1) * GC, gl], fo[fi][:], reads=[d_fo[fi]], q="act")

            ngrp = 256 // GC if "hy_short" not in self.dbg else 1
            for g in range(ngrp):
                fwd(self.KFb[g * GC:(g + 1) * GC, :], "A")
                fwd(self.KFb[256 + g * GC:256 + (g + 1) * GC, :], "B")
                fwd(self.VVb[g * GC:(g + 1) * GC, :], "V")
                inv(g)
        self.hyP.__exit__(None, None, None)

    def phase_merge(self):
        m, I = self.m, self.I
        with Phase(m) as ph:
            d_c = Dep()
            og = ph.sb([128, 8]); m.dma(og[:], I["out_g"][:, :], writes=[d_c])
            wo = ph.sb([128, 8, D], BF16); d_wo = Dep()
            wst = [ph.sb([128, D]) for _ in range(2)]; d_wst = [Dep(), Dep()]
            wv = I["w_out"].rearrange("(kc p) n -> p kc n", p=128)
            for kc in range(8):
                m.dma(wst[kc % 2][:], wv[:, kc, :], writes=[d_wst[kc % 2]])
                m.op("dve", lambda e: e.tensor_scalar(out=wo[:, kc, :], in0=wst[kc % 2][:], scalar1=og[:, kc:kc + 1], scalar2=None, op0=ALU.mult),
                     reads=[d_wst[kc % 2], d_c], writes=[d_wo])
            ones2 = ph.sb([128, 2], BF16)
            m.op("dve", lambda e: e.memset(ones2[:], 1.0), writes=[d_c])
            wrec = ph.sb([128, 3])
            for gi, wd in enumerate((512, 256, 256)):
                m.op("dve", lambda e: e.memset(wrec[:, gi:gi + 1], 1.0 / wd), writes=[d_c])
            ob = [ph.sb([128, 8, 512]) for _ in range(2)]; d_ob = [Dep(), Dep()]
            obb = [ph.sb([128, 8, 512], BF16) for _ in range(2)]; d_obb = [Dep(), Dep()]
            sqb = [ph.sb([128, 8, 512], BF16) for _ in range(2)]; d_sqb = [Dep(), Dep()]
            xt = [ph.sb([128, D]) for _ in range(2)]; d_xt = [Dep(), Dep()]
            acc = [ph.sb([128, D]) for _ in range(2)]; d_acc = [Dep(), Dep()]
            xn = [ph.sb([128, D], BF16) for _ in range(2)]; d_xn = [Dep(), Dep()]
            junk = ph.sb([128, D], BF16); d_junk = Dep()
            rs = [ph.sb([128, 4]) for _ in range(2)]; d_rs = [Dep(), Dep()]
            fT = [ph.sb([128, 8, 512], BF16) for _ in range(2)]; d_fT = [Dep(), Dep()]
            pO = [[ph.ps([128, 512]) for _ in range(3)] for _ in range(2)]; d_pO = [[Dep() for _ in range(3)] for _ in range(2)]
            pT = ph.ps([128, 8, 128], BF16); d_pT = Dep()
            pS = ph.ps([128, 3, 2]); d_pS = Dep()
            groups = [(0, 4), (4, 6), (6, 8)]
            OTv = self.OT.rearrange("(c p) t -> p c t", p=128)
            blocks = [("lat", i * 512, 512) for i in range(self.nown // 512)] + ([] if self.last else [("ctx", self.nown, LC)])
            tcount = 0
            pcount = 0
            pend = [None]
            for bi, (kind, c0, ntok) in enumerate(blocks):
                o_ = ob[bi % 2]; do = d_ob[bi % 2]
                m.dma(o_[:, :, 0:ntok], OTv[:, :, c0:c0 + ntok], writes=[do], q="sp")
                ob_ = obb[bi % 2]; dob = d_obb[bi % 2]
                sq_ = sqb[bi % 2]; dsq = d_sqb[bi % 2]
                for ch in range(8):
                    m.op("act", lambda e: e.activation(out=sq_[:, ch, 0:ntok], in_=o_[:, ch, 0:ntok], func=AF.Square), reads=[do], writes=[dsq])
                    m.op("pool", lambda e: e.tensor_copy(out=ob_[:, ch, 0:ntok], in_=o_[:, ch, 0:ntok]), reads=[do], writes=[dob])
                f_ = fT[bi % 2]; df = d_fT[bi % 2]
                gi_ga = 0 if kind == "lat" else 2
                mi = 1 if kind == "lat" else 3
                modt = self.modL if kind == "lat" else self.modC
                for tt in range(ntok // 128):
                    ts_ = slice(tt * 128, (tt + 1) * 128)
                    k2 = tcount % 2; tcount += 1
                    r_ = rs[k2]; dr = d_rs[k2]
                    if kind == "lat":
                        m.dma(xt[k2][:], self.xsrc[c0 + tt * 128:c0 + (tt + 1) * 128, :], writes=[d_xt[k2]], q="sp")
                    else:
                        m.dma(xt[k2][:], self.ctxsrc[tt * 128:(tt + 1) * 128, :], writes=[d_xt[k2]], q="sp")
                    for gi, (a, b) in enumerate(groups):
                        for ch in range(a, b):
                            m.op("pe", lambda e: e.matmul(pS[:, gi, :], lhsT=sq_[:, ch, ts_], rhs=ones2[:], start=(ch == a), stop=(ch == b - 1)),
                                 reads=[dsq, d_c], writes=[d_pS], inc=(ch == b - 1))
                    m.op("dve", lambda e: e.tensor_tensor(out=r_[:, 0:3], in0=pS[:, :, 0], in1=wrec[:], op=ALU.mult), reads=[d_pS, d_c], writes=[dr])
                    m.op("act", lambda e: e.activation(out=r_[:, 0:3], in_=r_[:, 0:3], func=AF.Sqrt, bias=self.epsT[:, 0:1]), reads=[dr, self.d_const], writes=[dr])
                    m.op("dve", lambda e: e.reciprocal(out=r_[:, 0:3], in_=r_[:, 0:3]), reads=[dr], writes=[dr])
                    a_ = acc[k2]; da = d_acc[k2]
                    for h in range(2):
                        hs = slice(h * 512, (h + 1) * 512)
                        pk = pcount % 2; pcount += 1
                        for gi, (a, b) in enumerate(groups):
                            for ch in range(a, b):
                                m.op("pe", lambda e: e.matmul(pO[pk][gi][:], lhsT=ob_[:, ch, ts_], rhs=wo[:, ch, hs], start=(ch == a), stop=(ch == b - 1)),
                                     reads=[dob, d_wo], writes=[d_pO[pk][gi]], inc=(ch == b - 1))
                        m.op("dve", lambda e: e.tensor_scalar(out=a_[:, hs], in0=pO[pk][0][:], scalar1=r_[:, 0:1], scalar2=None, op0=ALU.mult),
                             reads=[d_pO[pk][0], dr], writes=[da])
                        for gi in (1, 2):
                            m.op("dve", lambda e: e.scalar_tensor_tensor(out=a_[:, hs], in0=pO[pk][gi][:], scalar=r_[:, gi:gi + 1], in1=a_[:, hs],
                                                                         op0=ALU.mult, op1=ALU.add), reads=[d_pO[pk][gi], dr, da], writes=[da])
                    m.op("pool", lambda e: e.tensor_tensor(out=a_[:], in0=a_[:], in1=self.gbc[:, gi_ga, :], op=ALU.mult), reads=[da, self.d_gbc], writes=[da])
                    m.op("pool", lambda e: e.tensor_tensor(out=a_[:], in0=a_[:], in1=xt[k2][:], op=ALU.add), reads=[da, d_xt[k2]], writes=[da])
                    m.dma(self.XM[c0 + tt * 128:c0 + (tt + 1) * 128, :], a_[:], reads=[da], q="act")
                    def stage_y(a_=a_, da=da, r_=r_, dr=dr, k2=k2, f_=f_, df=df, ts_=ts_, mi=mi, modt=modt,
                                last_tile=(tt == ntok // 128 - 1), c0=c0, ntok=ntok):
                        m.op("act", lambda e: e.activation(out=junk[:], in_=a_[:], func=AF.Square, accum_out=r_[:, 3:4]), reads=[da], writes=[d_junk, dr])
                        m.op("dve", lambda e: e.tensor_scalar(out=r_[:, 3:4], in0=r_[:, 3:4], scalar1=1.0 / D, scalar2=EPS, op0=ALU.mult, op1=ALU.add),
                             reads=[dr], writes=[dr])
                        m.op("act", lambda e: e.activation(out=r_[:, 3:4], in_=r_[:, 3:4], func=AF.Sqrt), reads=[dr], writes=[dr])
                        m.op("dve", lambda e: e.reciprocal(out=r_[:, 3:4], in_=r_[:, 3:4]), reads=[dr], writes=[dr])
                        m.op("dve", lambda e: e.tensor_scalar(out=xn[k2][:], in0=a_[:], scalar1=r_[:, 3:4], scalar2=None, op0=ALU.mult),
                             reads=[da, dr], writes=[d_xn[k2]])
                        for kc in range(8):
                            m.op("pe", lambda e: e.transpose(out=pT[:, kc, :], in_=xn[k2][:, kc * 128:(kc + 1) * 128], identity=self.ident[:]),
                                 reads=[d_xn[k2], self.d_const], writes=[d_pT], inc=(kc == 7))
                        for kc in range(8):
                            m.op("dve", lambda e: e.tensor_scalar(out=f_[:, kc, ts_], in0=pT[:, kc, :], scalar1=self.AB[:, mi, kc:kc + 1],
                                                                  scalar2=modt[:, 24 + kc:25 + kc], op0=ALU.mult, op1=ALU.add),
                                 reads=[d_pT, self.d_mod], writes=[df])
                        if last_tile:
                            m.dma(self.FT[:, :, c0:c0 + ntok], f_[:, :, 0:ntok], reads=[df], q="act")
                    if pend[0] is not None:
                        pend[0]()
                    pend[0] = stage_y
            if pend[0] is not None:
                pend[0]()

    def phase_moe(self):
        m, I = self.m, self.I
        NT = self.ntok // 128
        NTOK = self.ntok; SHL = self.nown
        with Phase(m) as P:
            d_c = Dep()
            gate = P.sb([128, NT, NE]); d_gate = Dep()
            b1 = P.sb([128, NE, 16])
            m.dma(b1[:], I["moe_b1"][:, :, :], writes=[d_c])
            m.op("dve", lambda e: e.tensor_scalar(out=b1[:, :, 8:16], in0=b1[:, :, 8:16], scalar1=1.0, scalar2=None, op0=ALU.add), reads=[d_c], writes=[d_c])
            with Phase(m) as ph:
                rw = ph.sb([128, 8, NE], BF16); rb = ph.sb([128, NE])
                m.dma(rw[:], I["router_w"].rearrange("(kc p) n -> p kc n", p=128), writes=[d_c], q="pool")
                m.dma(rb[:], I["router_b"][:, :], writes=[d_c])
                fb = [ph.sb([128, 8, 512], BF16) for _ in range(2)]; d_fb = [Dep(), Dep()]
                lg = [ph.sb([128, NE]) for _ in range(2)]; d_lg = [Dep(), Dep()]
                ex = [ph.sb([128, NE]) for _ in range(2)]; d_ex = [Dep(), Dep()]
                m8 = [ph.sb([128, 8]) for _ in range(2)]; d_m8 = [Dep(), Dep()]
                sm = [ph.sb([128, 2]) for _ in range(2)]; d_sm = [Dep(), Dep()]
                pl = [ph.ps([128, NE]) for _ in range(2)]; d_pl = [Dep(), Dep()]
                tcount = 0
                for bi in range((NTOK + 511) // 512):
                    c0 = bi * 512; n = min(512, NTOK - c0)
                    m.dma(fb[bi % 2][:, :, 0:n], self.FT[:, :, c0:c0 + n], writes=[d_fb[bi % 2]], q="sp")
                    for tt in range(n // 128):
                        tile = c0 // 128 + tt
                        k = tcount % 2; tcount += 1
                        for kc in range(8):
                            m.op("pe", lambda e: e.matmul(pl[k][:], lhsT=fb[bi % 2][:, kc, tt * 128:(tt + 1) * 128], rhs=rw[:, kc, :], start=(kc == 0), stop=(kc == 7)),
                                 reads=[d_fb[bi % 2], d_c], writes=[d_pl[k]], inc=(kc == 7))
                        m.op("dve", lambda e: e.tensor_tensor(out=lg[k][:], in0=pl[k][:], in1=rb[:], op=ALU.add), reads=[d_pl[k], d_c], writes=[d_lg[k]])
                        m.op("dve", lambda e: e.max(out=m8[k][:], in_=lg[k][:]), reads=[d_lg[k]], writes=[d_m8[k]])
                        m.op("dve", lambda e: e.tensor_scalar(out=sm[k][:, 0:1], in0=m8[k][:, 0:1], scalar1=-1.0, scalar2=None, op0=ALU.mult),
                             reads=[d_m8[k]], writes=[d_sm[k]])
                        m.op("act", lambda e: e.activation(out=ex[k][:], in_=lg[k][:], func=AF.Exp, bias=sm[k][:, 0:1]), reads=[d_lg[k], d_sm[k]], writes=[d_ex[k]])
                        m.op("dve", lambda e: e.scalar_tensor_tensor(out=ex[k][:], in0=lg[k][:], scalar=m8[k][:, 3:4], in1=ex[k][:], op0=ALU.is_ge, op1=ALU.mult,
                                                                     accum_out=sm[k][:, 1:2]), reads=[d_lg[k], d_m8[k], d_ex[k]], writes=[d_ex[k], d_sm[k]])
                        m.op("dve", lambda e: e.reciprocal(out=sm[k][:, 1:2], in_=sm[k][:, 1:2]), reads=[d_sm[k]], writes=[d_sm[k]])
                        m.op("dve", lambda e: e.tensor_scalar(out=gate[:, tile, :], in0=ex[k][:], scalar1=sm[k][:, 1:2], scalar2=None, op0=ALU.mult),
                             reads=[d_ex[k], d_sm[k]], writes=[d_gate])
            if "gate" in self.dbg:
                o = self.nc.dram_tensor("dbg_gate", [128, NT, NE], F32, kind="ExternalOutput").ap(); self.out_names.append("dbg_gate")
                m.dma(o[:, :, :], gate[:], reads=[d_gate])
            w1v = I["moe_w1"].rearrange("e (kc p) n -> e p kc n", p=128)
            w2v = I["moe_w2"].rearrange("e (kc p) n -> e p kc n", p=128)
            passes = [(a, min(a + 11, NT)) for a in range(0, NT, 11)]
            if "moe_short" in self.dbg:
                passes = [(NT - 2, NT)]
            n_exp = NE
            for (ta, tb_) in passes:
                ntile = tb_ - ta
                with Phase(m) as pp:
                    fT = pp.sb([128, 8, ntile * 128], BF16); d_fT = Dep()
                    yacc = pp.sb([128, ntile, D]); d_y = [Dep() for _ in range(ntile)]
                    m.dma(fT[:], self.FT[:, :, ta * 128:tb_ * 128], writes=[d_fT], q="sp")
                    with Phase(m) as ph:
                        b2 = ph.sb([NE, D])
                        m.dma(b2[:], I["moe_b2"][:, :], writes=[d_c])
                        gT = [ph.sb([NE, 128]) for _ in range(2)]; d_gT = [Dep(), Dep()]
                        pg = [ph.ps([NE, 128]) for _ in range(2)]; d_pg = [Dep(), Dep()]
                        py = [ph.ps([128, 512]) for _ in range(2)]; d_py = [Dep(), Dep()]
                        for ti in range(ntile):
                            k = ti % 2
                            m.op("pe", lambda e: e.transpose(out=pg[k][:], in_=gate[:, ta + ti, :], identity=self.identf[:]),
                                 reads=[d_gate, self.d_const], writes=[d_pg[k]])
                            m.op("act", lambda e: e.activation(out=gT[k][:], in_=pg[k][:], func=AF.Identity), reads=[d_pg[k]], writes=[d_gT[k]])
                            for h in range(2):
                                m.op("pe", lambda e: e.matmul(py[h][:], lhsT=gT[k][:], rhs=b2[:, h * 512:(h + 1) * 512], start=True, stop=True),
                                     reads=[d_gT[k], d_c], writes=[d_py[h]])
                                m.op("dve", lambda e: e.tensor_copy(out=yacc[:, ti, h * 512:(h + 1) * 512], in_=py[h][:]), reads=[d_py[h]], writes=[d_y[ti]])
                    with Phase(m) as ph:
                        w1 = [ph.sb([128, 8, 2 * D], BF16) for _ in range(2)]; d_w1 = [[Dep() for _ in range(8)] for _ in range(2)]
                        w2 = ph.sb([128, 8, D], BF16); d_w2 = [Dep() for _ in range(8)]
                        act = [ph.sb([128, 8, 512], BF16) for _ in range(2)]; d_act = [Dep(), Dep()]
                        tg = [ph.sb([128, 512]) for _ in range(2)]; d_tg = [Dep(), Dep()]
                        tsg = [ph.sb([128, 512]) for _ in range(2)]; d_tsg = [Dep(), Dep()]
                        tl = [ph.sb([128, 512]) for _ in range(2)]; d_tl = [Dep(), Dep()]
                        pgl = [[ph.ps([128, 512]) for _ in range(2)] for _ in range(2)]; d_pgl = [[Dep(), Dep()], [Dep(), Dep()]]
                        pyy = [ph.ps([128, 512]) for _ in range(3)]; d_pyy = [Dep() for _ in range(3)]
                        cnt = {"j": 0, "y": 0, "a": 0}
                        m.op("dve", lambda e: e.tensor_scalar(out=gate[:, ta:tb_, :], in0=gate[:, ta:tb_, :], scalar1=1.0 / 1.702, scalar2=None, op0=ALU.mult),
                             reads=[d_gate], writes=[d_gate])

                        wtok = []

                        def wdma(out, in_, dep):
                            if len(wtok) >= 2:
                                m._wait("pool", wtok[-2])
                            wtok.append(m.dma(out, in_, writes=[dep], q="pool"))

                        def load_w1(e_):
                            for kc in range(8):
                                wdma(w1[e_ % 2][:, kc, :], w1v[e_, :, kc, :], d_w1[e_ % 2][kc])

                        def load_w2(e_):
                            for kc in range(8):
                                wdma(w2[:, kc, :], w2v[e_, :, kc, :], d_w2[kc])
                        load_w1(0)
                        for e_ in range(n_exp):
                            load_w2(e_)
                            if e_ + 1 < n_exp:
                                load_w1(e_ + 1)
                            W1 = w1[e_ % 2]; dW1 = d_w1[e_ % 2]
                            for b0 in range(0, ntile, 4):
                                nt_ = min(4, ntile - b0); n = nt_ * 128
                                cs_ = slice(b0 * 128, b0 * 128 + n)
                                ai = cnt["a"] % 2; cnt["a"] += 1
                                A_ = act[ai]; dA = d_act[ai]
                                for j in range(8):
                                    k = cnt["j"] % 2; cnt["j"] += 1
                                    for gl in range(2):
                                        for kc in range(8):
                                            m.op("pe", lambda e: e.matmul(pgl[k][gl][:, 0:n], lhsT=W1[:, kc, 256 * j + gl:256 * j + 256:2], rhs=fT[:, kc, cs_],
                                                                          start=(kc == 0), stop=(kc == 7)),
                                                 reads=[dW1[kc], d_fT], writes=[d_pgl[k][gl]], inc=(kc == 7))
                                    m.op("dve", lambda e: e.tensor_scalar(out=tg[k][:, 0:n], in0=pgl[k][0][:, 0:n], scalar1=b1[:, e_, j:j + 1], scalar2=7.0,
                                                                          op0=ALU.add, op1=ALU.min), reads=[d_pgl[k][0], d_c], writes=[d_tg[k]])
                                    m.op("act", lambda e: e.activation(out=tsg[k][:, 0:n], in_=tg[k][:, 0:n], func=AF.Silu, scale=1.702),
                                         reads=[d_tg[k]], writes=[d_tsg[k]])
                                    m.op("dve", lambda e: e.tensor_scalar(out=tl[k][:, 0:n], in0=pgl[k][1][:, 0:n], scalar1=b1[:, e_, 8 + j:9 + j], scalar2=8.0,
                                                                          op0=ALU.add, op1=ALU.min), reads=[d_pgl[k][1], d_c], writes=[d_tl[k]])
                                    m.op("dve", lambda e: e.scalar_tensor_tensor(out=A_[:, j, 0:n], in0=tl[k][:, 0:n], scalar=-6.0, in1=tsg[k][:, 0:n],
                                                                                 op0=ALU.max, op1=ALU.mult), reads=[d_tl[k], d_tsg[k]], writes=[dA])
                                for tt in range(nt_):
                                    ti = b0 + tt
                                    for h in range(2):
                                        yk = cnt["y"] % 3; cnt["y"] += 1
                                        for j in range(8):
                                            m.op("pe", lambda e: e.matmul(pyy[yk][:], lhsT=A_[:, j, tt * 128:(tt + 1) * 128], rhs=w2[:, j, h * 512:(h + 1) * 512],
                                                                          start=(j == 0), stop=(j == 7)),
                                                 reads=[dA, d_w2[j]], writes=[d_pyy[yk]], inc=(j == 7))
                                        m.op("dve", lambda e: e.scalar_tensor_tensor(out=yacc[:, ti, h * 512:(h + 1) * 512], in0=pyy[yk][:], scalar=gate[:, ta + ti, e_:e_ + 1],
                                                                                     in1=yacc[:, ti, h * 512:(h + 1) * 512], op0=ALU.mult, op1=ALU.add),
                                             reads=[d_pyy[yk], d_gate, d_y[ti]], writes=[d_y[ti]])
                    with Phase(m) as ph:
                        xm = [ph.sb([128, D]) for _ in range(2)]; d_xm = [Dep(), Dep()]
                        ot = [ph.sb([128, D]) for _ in range(2)]; d_ot = [Dep(), Dep()]
                        for ti in range(ntile):
                            tile = ta + ti; k = ti % 2
                            m.dma(xm[k][:], self.XM[tile * 128:(tile + 1) * 128, :], writes=[d_xm[k]], q="sp")
                            gi = 1 if tile < SHL // 128 else 3
                            m.op("pool", lambda e: e.tensor_tensor(out=ot[k][:], in0=yacc[:, ti, :], in1=self.gbc[:, gi, :], op=ALU.mult),
                                 reads=[d_y[ti], self.d_gbc], writes=[d_ot[k]])
                            m.op("dve", lambda e: e.tensor_tensor(out=ot[k][:], in0=ot[k][:], in1=xm[k][:], op=ALU.add), reads=[d_ot[k], d_xm[k]], writes=[d_ot[k]])
                            if tile < SHL // 128:
                                dst = self.x_out if self.last else self.X1
                                m.dma(dst[tile * 128:(tile + 1) * 128, :], ot[k][:], reads=[d_ot[k]], q="act")
                            else:
                                r0 = tile * 128 - SHL
                                m.dma(self.C1[r0:r0 + 128, :], ot[k][:], reads=[d_ot[k]], q="act")


_PROG = {}


def _get_prog():
    if "p" not in _PROG:
        _PROG["p"] = LayerProg()
    return _PROG["p"]


def kernel(**inputs):
    inp = {k: np.asarray(v) for k, v in inputs.items()}
    P = _get_prog()
    need = set(P.Iall.keys())
    x = np.ascontiguousarray(inp["x"], dtype=np.float32)
    ctx = np.ascontiguousarray(inp["ctx"], dtype=np.float32)
    maps = []
    for core in range(8):
        b, half = core // 2, core % 2
        xc = x[b][::-1] if half else x[b]
        cc = ctx[b][::-1] if half else ctx[b]
        mp = {}
        for l in range(2):
            d = _prep_core(inp, l, b, half, xc, cc)
            for k, v in d.items():
                name = k if k in SHARED else "%s@%d" % (k, l)
                if name in need and name not in mp:
                    mp[name.replace("@", "_L")] = v
        maps.append(mp)
    res = run_bass_kernel_spmd(P.nc, maps, core_ids=list(range(8)))
    out = np.empty_like(x)
    for core in range(8):
        b, half = core // 2, core % 2
        xo = np.asarray(res.results[core]["x_out"])
        if half == 0:
            out[b, :SH] = xo
        else:
            out[b, SH:] = xo[::-1]
    return out
```

```python
import math
from contextlib import ExitStack
import numpy as np
import ml_dtypes
import concourse.bass as bass
import concourse.mybir as mybir
from concourse.bass_utils import run_bass_kernel_spmd

F32 = mybir.dt.float32
BF16 = mybir.dt.bfloat16
AF = mybir.ActivationFunctionType
ALU = mybir.AluOpType
AX = mybir.AxisListType

D = 1024
S = 8192
SH = 4096
LC = 256
NTOK = SH + LC
NALL = S + LC
NE = 32
EPS = 1e-6
NFFT = 16384


class Dep:
    __slots__ = ("w", "r")

    def __init__(self):
        self.w = None
        self.r = {}


class MK:
    ENG = ("pe", "act", "dve", "pool", "sp")
    EPOCH = 12000

    def __init__(self, nc, n_dma_sems=48):
        self.nc = nc
        self.e = {"pe": nc.tensor, "act": nc.scalar, "dve": nc.vector, "pool": nc.gpsimd, "sp": nc.sync}
        self.sem = {}
        self.cnt = {}
        self.allsems = []
        self.nsem = 0
        for k in self.ENG:
            self._new_epoch(k)
        self.seen = {k: {} for k in self.ENG}
        self.dma_sems = [nc.alloc_semaphore(f"dq{i}") for i in range(n_dma_sems)]
        self.dma_val = [0] * n_dma_sems
        self.dma_rr = 0
        self.n_inst = 0
        self.n_wait = 0
        self._uid = 0

    def _new_epoch(self, k):
        self.sem[k] = self.nc.alloc_semaphore(f"s_{k}_{self.nsem}")
        self.nsem += 1
        self.cnt[k] = 0

    def uid(self, p="t"):
        self._uid += 1
        return f"{p}{self._uid}"

    def _wait(self, eng, tok):
        if tok is None:
            return
        sem, val = tok
        sid = id(sem)
        if self.seen[eng].get(sid, 0) >= val:
            return
        if sem is self.sem.get(eng) and val > self.cnt[eng]:
            return
        self.e[eng].wait_ge(sem, val)
        self.seen[eng][sid] = val
        self.n_wait += 1

    def _deps(self, eng, reads, writes):
        for d in reads:
            self._wait(eng, d.w)
        for d in writes:
            self._wait(eng, d.w)
            for t in d.r.values():
                self._wait(eng, t)

    def op(self, eng, fn, reads=(), writes=(), inc=True):
        self._deps(eng, reads, writes)
        ins = fn(self.e[eng])
        self.n_inst += 1
        if inc:
            self.cnt[eng] += 1
            ins.then_inc(self.sem[eng], 1)
            tok = (self.sem[eng], self.cnt[eng])
            self.seen[eng][id(self.sem[eng])] = self.seen[eng].get(id(self.sem[eng]), 0)
            if self.cnt[eng] >= self.EPOCH:
                self._new_epoch(eng)
        else:
            tok = (self.sem[eng], self.cnt[eng] + 1)
        for d in reads:
            d.r[eng] = tok
        for d in writes:
            d.w = tok
            d.r = {}
        return ins

    def dma(self, out, in_, reads=(), writes=(), q="sp", **kw):
        self._deps(q, reads, writes)
        i = self.dma_rr
        self.dma_rr = (self.dma_rr + 1) % len(self.dma_sems)
        sem = self.dma_sems[i]
        if self.dma_val[i] > 0:
            self._wait(q, (sem, self.dma_val[i]))
        self.dma_val[i] += 16
        ins = self.e[q].dma_start(out=out, in_=in_, **kw)
        ins.then_inc(sem, 16)
        self.n_inst += 1
        tok = (sem, self.dma_val[i])
        for d in reads:
            d.r["dma%d" % i] = tok
        for d in writes:
            d.w = tok
            d.r = {}
        return tok

    def barrier(self, engines=None):
        engines = engines or self.ENG
        toks = [(self.sem[p], self.cnt[p]) for p in self.ENG if self.cnt[p] > 0]
        toks += [(s, v) for s, v in zip(self.dma_sems, self.dma_val) if v > 0]
        for e in engines:
            for t in toks:
                self._wait(e, t)


class Phase:
    def __init__(self, m):
        self.m = m
        self.st = ExitStack()

    def __enter__(self):
        self.st.__enter__()
        return self

    def sb(self, shape, dt=F32):
        return self.st.enter_context(self.m.nc.sbuf_tensor(self.m.uid("sb"), list(shape), dt))

    def ps(self, shape, dt=F32):
        return self.st.enter_context(self.m.nc.psum_tensor(self.m.uid("ps"), list(shape), dt))

    def __exit__(self, *a):
        self.m.barrier()
        return self.st.__exit__(*a)


_CONST = {}


def _consts():
    if _CONST:
        return _CONST
    bf = ml_dtypes.bfloat16
    c = _CONST
    c["ident"] = np.eye(128, dtype=np.float32)
    pr = np.zeros((128, 128), np.float32)
    for blk in range(2):
        for i in range(32):
            pr[blk * 64 + i + 32, blk * 64 + i] = -1.0
            pr[blk * 64 + i, blk * 64 + i + 32] = 1.0
    c["prot"] = pr
    bo = np.zeros((128, 128), np.float32)
    bo[:64, :64] = 1.0 / 64
    bo[64:, 64:] = 1.0 / 64
    c["blk64"] = bo
    sel = np.zeros((65, 64), np.float32)
    sel[64, :] = 1.0
    c["sel"] = sel
    rows = np.repeat(np.arange(S // 64), 64).astype(np.float32)
    cols = np.tile(np.arange(64), S // 64).astype(np.float32)
    inv = (10000.0 ** (-np.arange(16, dtype=np.float32) / 16)).astype(np.float32)
    ang = np.concatenate([rows[:, None] * inv, cols[:, None] * inv], axis=-1).astype(np.float32)
    c["rope_cos"] = np.ascontiguousarray(np.tile(np.cos(ang).T, (4, 1)).astype(np.float32))
    c["rope_sin"] = np.ascontiguousarray(np.tile(np.sin(ang).T, (4, 1)).astype(np.float32))

    def zfeat(L):
        t01 = np.linspace(0.0, 1.0, L, dtype=np.float32)[:, None]
        bands = np.linspace(1e-4, 15, 16, dtype=np.float32)
        a = (np.float32(2.0 * math.pi / L) * np.arange(L, dtype=np.float32)[:, None] * bands).astype(np.float32)
        z = np.concatenate([t01, np.cos(a), -np.sin(a)], axis=-1).astype(np.float32)
        return np.ascontiguousarray(z.T), np.ascontiguousarray(np.broadcast_to(t01[:, 0][None, :], (128, L)))
    c["zT"], c["t01"] = zfeat(S)
    c["zTc"], c["t01c"] = zfeat(LC)
    a = np.arange(64)[:, None, None]
    b = np.arange(128)[None, :, None]
    cc = np.arange(128)[None, None, :]
    th = 2.0 * np.pi * (((128 * a + b) * cc) % NFFT) / NFFT
    c["F1"] = np.ascontiguousarray(np.stack([np.cos(th), -np.sin(th)], axis=2)).astype(bf)
    bd = 2.0 * np.pi * ((np.arange(128)[:, None] * np.arange(128)[None, :]) % 128) / 128
    c["C2"] = np.stack([np.cos(bd), np.sin(bd), -np.sin(bd)], axis=1).astype(bf)
    cI = np.arange(128)[:, None, None]
    bI = np.arange(128)[None, :, None]
    for hh in range(2):
        a32 = (np.arange(32) + 32 * hh)[None, None, :]
        thi = 2.0 * np.pi * (((128 * a32 + bI) * cI) % NFFT) / NFFT
        c["FI%d" % hh] = np.ascontiguousarray(np.stack([np.cos(thi), -np.sin(thi)], axis=2)).astype(bf)
    return c


def _cols(v, n):
    return np.ascontiguousarray(np.asarray(v, np.float32).reshape(n, 128).T)


def _prep_core(inp, l, b, half, x_core, ctx_core):
    c = _consts()
    r = (lambda a, ax=0: np.flip(a, axis=ax)) if half else (lambda a, ax=0: a)
    d = {}
    d["x"] = np.ascontiguousarray(x_core, np.float32)
    d["ctx"] = np.ascontiguousarray(ctx_core, np.float32)
    d["cvec"] = np.ascontiguousarray(np.concatenate([_cols(inp["c"][b], 8), _cols(inp["c_ctx"], 8)], axis=1))
    d["w_mod"] = inp["w_mod"][l]
    d["bmod"] = _cols(inp["b_mod"][l], 48)
    d["gmix"] = _cols(inp["norm_mix_g"][l], 8)
    d["gffn"] = _cols(inp["norm_ffn_g"][l], 8)
    w_in = inp["w_in"][l]
    perm = np.array([(j + 4 * s) * 64 + dd for j in range(4) for s in range(2) for dd in range(64)])
    d["w_in"] = np.ascontiguousarray(np.concatenate([w_in[:, perm], w_in[:, 512:]], axis=1))
    qg = inp["q_norm_g"][l]
    kg = inp["k_norm_g"][l]
    d["qkg"] = np.ascontiguousarray(np.stack([np.tile(qg, 2), np.tile(kg, 2)], axis=1).astype(np.float32))
    d["qkg_row"] = np.ascontiguousarray(np.broadcast_to(np.concatenate([qg, kg])[None, :], (128, 128)).astype(np.float32))
    d["rope_cos"] = np.ascontiguousarray(r(c["rope_cos"], 1))
    d["rope_sin"] = np.ascontiguousarray(r(c["rope_sin"], 1))
    cw = inp["lru_conv_w"][l]
    w5 = np.zeros((5, 256), np.float32)
    if half:
        w5[1:5] = cw[::-1]
    else:
        w5[0:4] = cw
    d["lru_cw"] = np.ascontiguousarray(w5.T.reshape(2, 128, 5).transpose(1, 0, 2))
    d["lru_cb"] = _cols(inp["lru_conv_b"][l], 2)
    gw = inp["lru_gate_w"][l]
    gb = inp["lru_gate_b"][l]
    lam = inp["lru_lambda"][l]
    if half:
        gw, gb, lam = gw[::-1], gb[::-1], lam[::-1]
    wbd = np.zeros((128, 2, 2, 2, 128), np.float32)
    for dd in range(2):
        for g in range(2):
            for ch in range(2):
                wbd[0:64, dd, g, ch, 0:64] = gw[dd, g, 2 * ch]
                wbd[64:128, dd, g, ch, 64:128] = gw[dd, g, 2 * ch + 1]
    d["lru_wbd"] = wbd.reshape(128, 8, 128)
    d["lru_gb"] = np.ascontiguousarray(np.stack([_cols(gb[dd, g], 2) for dd in range(2) for g in range(2)], axis=1).reshape(128, 8))
    d["lru_lam"] = np.ascontiguousarray(np.stack([_cols(lam[dd], 2) for dd in range(2)], axis=1).reshape(128, 4))
    hw = inp["hy_conv_w"][l]
    if half:
        hw = hw[::-1]
    d["hy_cw"] = np.ascontiguousarray(hw.T.reshape(6, 128, 3).transpose(1, 0, 2))
    d["hy_cb"] = _cols(inp["hy_conv_b"][l], 6)
    d["hy_w1"] = inp["hy_w1"][l]
    d["hy_w2"] = inp["hy_w2"][l]
    d["hy_b12f"] = np.ascontiguousarray(np.stack([inp["hy_b1"][l], inp["hy_b2"][l], inp["hy_freq"][l]], axis=1))
    w3 = inp["hy_w3"][l]
    dec = inp["hy_decay"][l]
    wa, wb_, da, db = w3[:, :256], w3[:, 256:], dec[:256], dec[256:]
    if half:
        wa, wb_, da, db = wb_, wa, db, da
    d["hy_w3"] = np.ascontiguousarray(np.concatenate([wa, wb_, w3[:, :256]], axis=1))
    d["hy_dec"] = np.ascontiguousarray(np.concatenate([_cols(da, 2), _cols(db, 2)], axis=1))
    d["hy_bias"] = _cols(inp["hy_bias"][l], 2)
    d["out_g"] = _cols(inp["out_norm_g"][l], 8)
    d["w_out"] = inp["w_out"][l]
    d["router_w"] = inp["router_w"][l]
    d["router_b"] = np.ascontiguousarray(np.broadcast_to(inp["router_b"][l][None, :], (128, NE)).astype(np.float32))
    d["moe_w1"] = inp["moe_w1"][l]
    b1 = inp["moe_b1"][l]
    d["moe_b1"] = np.ascontiguousarray(np.concatenate(
        [b1[:, 0::2].reshape(NE, 8, 128).transpose(2, 0, 1), b1[:, 1::2].reshape(NE, 8, 128).transpose(2, 0, 1)], axis=2))
    d["moe_w2"] = inp["moe_w2"][l]
    d["moe_b2"] = inp["moe_b2"][l]
    for k in ("ident", "prot", "blk64", "sel", "zT", "t01", "zTc", "t01c", "F1", "C2", "FI0", "FI1"):
        d[k] = c[k]
    return d


IN_SPECS = [
    ("x", [S, D], F32), ("ctx", [LC, D], F32), ("cvec", [128, 16], F32), ("w_mod", [D, 6 * D], F32),
    ("bmod", [128, 48], F32), ("gmix", [128, 8], F32), ("gffn", [128, 8], F32), ("w_in", [D, 2048], F32),
    ("qkg", [128, 2], F32), ("qkg_row", [128, 128], F32), ("rope_cos", [128, S], F32), ("rope_sin", [128, S], F32),
    ("lru_cw", [128, 2, 5], F32), ("lru_cb", [128, 2], F32), ("lru_wbd", [128, 8, 128], F32), ("lru_gb", [128, 8], F32),
    ("lru_lam", [128, 4], F32), ("hy_cw", [128, 6, 3], F32), ("hy_cb", [128, 6], F32), ("hy_w1", [33, 64], F32),
    ("hy_w2", [64, 64], F32), ("hy_b12f", [64, 3], F32), ("hy_w3", [64, 768], F32), ("hy_dec", [128, 4], F32),
    ("hy_bias", [128, 2], F32), ("out_g", [128, 8], F32), ("w_out", [D, D], F32), ("router_w", [D, NE], F32),
    ("router_b", [128, NE], F32), ("moe_w1", [NE, D, 2 * D], F32), ("moe_b1", [128, NE, 16], F32),
    ("moe_w2", [NE, D, D], F32), ("moe_b2", [NE, D], F32),
    ("ident", [128, 128], F32), ("prot", [128, 128], F32), ("blk64", [128, 128], F32), ("sel", [65, 64], F32),
    ("zT", [33, S], F32), ("t01", [128, S], F32), ("zTc", [33, LC], F32), ("t01c", [128, LC], F32),
    ("F1", [64, 128, 2, 128], BF16), ("C2", [128, 3, 128], BF16), ("FI0", [128, 128, 2, 32], BF16), ("FI1", [128, 128, 2, 32], BF16),
]


SHARED = ("x", "ctx", "cvec", "rope_cos", "rope_sin", "ident", "prot", "blk64", "sel", "zT", "t01", "zTc", "t01c", "F1", "C2", "FI0", "FI1")


class LayerProg:
    def __init__(self, layers=((0, S, False), (1, SH, True)), stop_after=None, dbg=()):
        self.nc = nc = bass.Bass("TRN2", target_bir_lowering=False)
        self.m = MK(nc)
        specs = {n: (sh, dt) for n, sh, dt in IN_SPECS}
        prog = self

        class _Lazy(dict):
            def __missing__(d, n):
                base = n.split("@")[0]
                sh, dt = specs[base]
                d[n] = nc.dram_tensor(n.replace("@", "_L"), list(sh), dt, kind="ExternalInput").ap()
                return d[n]

        class _View:
            def __getitem__(v, n):
                return prog.Iall[n if n in SHARED else "%s@%d" % (n, prog.l)]
        self.Iall = _Lazy()
        self.I = _View()
        self.stop_after = stop_after
        self.dbg = set(dbg)
        self.out_names = []
        k = "ExternalOutput" if "scratch" in self.dbg else "Internal"
        sc = lambda n, sh, dt=F32: nc.dram_tensor(n, list(sh), dt, kind=k).ap()
        self.U = sc("sU", [10, 128, NALL])
        self.OT = sc("sOT", [D, NALL])
        self.HF = sc("sHF", [2, 128, NALL])
        self.VV = sc("sVV", [256, S])
        self.X0 = sc("sX0", [256, S])
        self.KF = sc("sKF", [512, S])
        self.XM = sc("sXM", [NALL, D])
        self.FT = sc("sFT", [128, 8, NALL], BF16)
        self.QTs = sc("sQT", [4, 128, S], BF16)
        self.VVb = sc("sVVb", [256, S], BF16)
        self.KFb = sc("sKFb", [512, S], BF16)
        self.X1 = sc("sX1", [S, D])
        self.C1 = sc("sC1", [LC, D])
        if k == "ExternalOutput":
            self.out_names += ["sU", "sOT", "sHF", "sVV", "sX0", "sKF", "sXM", "sFT", "sQT", "sX1", "sC1"]
        self.x_out = nc.dram_tensor("x_out", [SH, D], F32, kind="ExternalOutput").ap()
        self.out_names += ["x_out"]
        self.layers = list(layers)
        self.build()

    def build(self):
        m = self.m
        with Phase(m) as G:
            self.G = G
            self.setup_globals()
            steps = ["mod", "inproj_attn", "lru", "hyena", "merge", "moe"]
            done = False
            for li, (l, nown, last) in enumerate(self.layers):
                self.l, self.nown, self.last = l, nown, last
                self.ntok = nown + (0 if last else LC)
                self.xsrc = self.Iall["x"] if li == 0 else self.X1
                self.ctxsrc = self.Iall["ctx"] if li == 0 else self.C1
                for name in steps:
                    getattr(self, "phase_" + name)()
                    m.barrier()
                    if self.stop_after == "%s%d" % (name, l) or (name == "inproj_attn" and self.stop_after == "inproj%d" % l):
                        done = True
                        break
                if done:
                    break
            m.barrier()

    def setup_globals(self):
        m, G, I = self.m, self.G, self.I
        self.identf = G.sb([128, 128]); self.d_const = Dep()
        self.ident = G.sb([128, 128], BF16)
        self.onesf = G.sb([128, 128])
        self.epsT = G.sb([128, 1])
        m.dma(self.identf[:], I["ident"][:, :], writes=[self.d_const])
        m.op("dve", lambda e: e.tensor_copy(out=self.ident[:], in_=self.identf[:]), reads=[self.d_const], writes=[self.d_const])
        m.op("dve", lambda e: e.memset(self.onesf[:], 1.0), writes=[self.d_const])
        m.op("dve", lambda e: e.memset(self.epsT[:], EPS), writes=[self.d_const])
        self.modL = G.sb([128, 48]); self.modC = G.sb([128, 48]); self.d_mod = Dep()
        self.AB = G.sb([128, 4, 8])
        self.gbc = G.sb([128, 4, D])
        self.d_gbc = Dep()

    def phase_mod(self):
        m, I = self.m, self.I
        with Phase(m) as ph:
            cv = ph.sb([128, 16]); d_cv = Dep()
            m.dma(cv[:], I["cvec"][:, :], writes=[d_cv])
            sc = ph.sb([128, 8, 2]); d_sc = Dep()
            m.op("act", lambda e: e.activation(out=sc[:, :, 0], in_=cv[:, 0:8], func=AF.Silu), reads=[d_cv], writes=[d_sc])
            m.op("act", lambda e: e.activation(out=sc[:, :, 1], in_=cv[:, 8:16], func=AF.Silu), reads=[d_cv], writes=[d_sc])
            bm = ph.sb([128, 48]); gm = ph.sb([128, 2, 8]); d_bm = Dep()
            m.dma(bm[:], I["bmod"][:, :], writes=[d_bm])
            m.dma(gm[:, 0, :], I["gmix"][:, :], writes=[d_bm])
            m.dma(gm[:, 1, :], I["gffn"][:, :], writes=[d_bm])
            pm = ph.ps([128, 48, 2]); d_pm = Dep()
            wv = I["w_mod"].rearrange("(kc p) n -> p kc n", p=128)
            wb = [ph.sb([128, 8, 512]) for _ in range(2)]
            d_wb = [Dep(), Dep()]
            for nb in range(12):
                t, dw = wb[nb % 2], d_wb[nb % 2]
                m.dma(t[:], wv[:, :, nb * 512:(nb + 1) * 512], writes=[dw], q=("sp" if nb % 2 == 0 else "act"))
                for j in range(4):
                    col = nb * 4 + j
                    for kc in range(8):
                        m.op("pe", lambda e: e.matmul(pm[:, col, :], lhsT=t[:, kc, j * 128:(j + 1) * 128], rhs=sc[:, kc, :],
                                                      start=(kc == 0), stop=(kc == 7)),
                             reads=[dw, d_sc], writes=[d_pm], inc=(kc == 7))
            m.op("dve", lambda e: e.tensor_tensor(out=self.modL[:], in0=pm[:, :, 0], in1=bm[:], op=ALU.add), reads=[d_pm, d_bm], writes=[self.d_mod])
            m.op("dve", lambda e: e.tensor_tensor(out=self.modC[:], in0=pm[:, :, 1], in1=bm[:], op=ALU.add), reads=[d_pm, d_bm], writes=[self.d_mod])
            tmp = ph.sb([128, 8]); d_tmp = Dep()
            for i, (mt, c0, gi) in enumerate([(self.modL, 8, 0), (self.modL, 32, 1), (self.modC, 8, 0), (self.modC, 32, 1)]):
                m.op("dve", lambda e: e.tensor_scalar(out=tmp[:], in0=mt[:, c0:c0 + 8], scalar1=1.0, scalar2=None, op0=ALU.add),
                     reads=[self.d_mod], writes=[d_tmp])
                m.op("dve", lambda e: e.tensor_tensor(out=self.AB[:, i, :], in0=tmp[:], in1=gm[:, gi, :], op=ALU.mult),
                     reads=[d_tmp, d_bm], writes=[self.d_mod])
            dg = ph.sb([128, 8, 128]); d_dg = Dep()
            pb = ph.ps([128, D]); d_pb = Dep()
            for i, (mt, c0) in enumerate([(self.modL, 16), (self.modL, 40), (self.modC, 16), (self.modC, 40)]):
                for j in range(8):
                    m.op("dve", lambda e: e.tensor_scalar(out=dg[:, j, :], in0=self.identf[:], scalar1=mt[:, c0 + j:c0 + j + 1], scalar2=None,
                                                          op0=ALU.mult), reads=[self.d_mod, self.d_const], writes=[d_dg])
                for h in range(2):
                    m.op("pe", lambda e: e.matmul(pb[:, h * 512:(h + 1) * 512], lhsT=self.onesf[:], rhs=dg[:, 4 * h:4 * h + 4, :],
                                                  start=True, stop=True), reads=[d_dg, self.d_const], writes=[d_pb])
                m.op("act", lambda e: e.activation(out=self.gbc[:, i, :], in_=pb[:], func=AF.Identity), reads=[d_pb], writes=[self.d_gbc])
            if "mod" in self.dbg:
                o = self.nc.dram_tensor("dbg_mod", [128, 96], F32, kind="ExternalOutput").ap(); self.out_names.append("dbg_mod")
                m.dma(o[:, 0:48], self.modL[:], reads=[self.d_mod])
                m.dma(o[:, 48:96], self.modC[:], reads=[self.d_mod])
                o2 = self.nc.dram_tensor("dbg_gbc", [128, 4, D], F32, kind="ExternalOutput").ap(); self.out_names.append("dbg_gbc")
                m.dma(o2[:, :, :], self.gbc[:], reads=[self.d_gbc])

    def phase_inproj_attn(self):
        m, I = self.m, self.I
        with Phase(m) as P:
            KT = P.sb([128, NALL], BF16); d_KT = Dep()
            QT = None; d_QT = Dep()
            QC = P.sb([128, 4, LC], BF16); d_QC = Dep()
            VA = P.sb([128, 66, 2, 65], BF16); d_VA = Dep()
            negM = P.sb([128, 1]); d_negM = Dep()
            m.op("pool", lambda e: e.memset(VA[:, :, :, 64:65], 1.0), writes=[d_VA])
            self._inproj(KT, d_KT, QT, d_QT, QC, d_QC, VA, d_VA, negM, d_negM)
            m.barrier()
            if self.stop_after == "inproj%d" % self.l:
                return
            self._attention(KT, d_KT, QT, d_QT, QC, d_QC, VA, d_VA, negM, d_negM)

    def _inproj(self, KT, d_KT, QT, d_QT, QC, d_QC, VA, d_VA, negM, d_negM):
        m, I = self.m, self.I
        with Phase(m) as ph:
            w_in = ph.sb([128, 8, 2048], BF16); d_w = Dep()
            wv = I["w_in"].rearrange("(kc p) n -> p kc n", p=128)
            for kc in range(8):
                m.dma(w_in[:, kc, :], wv[:, kc, :], writes=[d_w], q="pool")
            cst = ph.sb([128, 2, 128], BF16); d_cst = Dep()
            m.dma(cst[:, 0, :], I["prot"][:, :], writes=[d_cst], q="pool")
            m.dma(cst[:, 1, :], I["blk64"][:, :], writes=[d_cst], q="pool")
            qkg = ph.sb([128, 2]); grow = ph.sb([128, 128])
            m.dma(qkg[:], I["qkg"][:, :], writes=[d_cst])
            m.dma(grow[:], I["qkg_row"][:, :], writes=[d_cst])
            mq = ph.sb([128, 2])
            m.op("dve", lambda e: e.tensor_reduce(out=mq[:, 0:1], in_=grow[:, 0:64], axis=AX.X, op=ALU.max, apply_absolute_value=True),
                 reads=[d_cst], writes=[d_negM])
            m.op("dve", lambda e: e.tensor_reduce(out=mq[:, 1:2], in_=grow[:, 64:128], axis=AX.X, op=ALU.max, apply_absolute_value=True),
                 reads=[d_cst], writes=[d_negM])
            m.op("dve", lambda e: e.tensor_scalar(out=negM[:], in0=mq[:, 0:1], scalar1=mq[:, 1:2], scalar2=-8.0, op0=ALU.mult, op1=ALU.mult),
                 reads=[d_negM], writes=[d_negM])
            xr = [ph.sb([128, D]) for _ in range(4)]; d_xr = [Dep() for _ in range(4)]
            xn = [ph.sb([128, D], BF16) for _ in range(8)]; d_xn = [Dep() for _ in range(8)]
            junk = ph.sb([128, D], BF16); d_junk = Dep()
            hT = [ph.sb([128, 8, 512], BF16) for _ in range(2)]; d_hT = [Dep(), Dep()]
            ss = [ph.sb([128, 4]) for _ in range(2)]; d_ss = [Dep(), Dep()]
            cs = [ph.sb([128, 2, 512]) for _ in range(2)]; d_cs = [Dep(), Dep()]
            stg = [ph.sb([128, 512]) for _ in range(4)]; d_stg = [Dep() for _ in range(4)]
            sq = [ph.sb([128, 512], BF16) for _ in range(2)]; d_sq = [Dep(), Dep()]
            f1 = [ph.sb([128, 512]) for _ in range(2)]; d_f1 = [Dep(), Dep()]
            f2 = [ph.sb([128, 512]) for _ in range(2)]; d_f2 = [Dep(), Dep()]
            qn = [ph.sb([128, 512], BF16) for _ in range(2)]; d_qn = [Dep(), Dep()]
            pT = ph.ps([128, 4, 512], BF16); d_pT = Dep()
            pr = [ph.ps([128, 512]) for _ in range(5)]; d_pr = [Dep() for _ in range(5)]
            pv = ph.ps([128, 4, 128]); d_pv = Dep()
            cnt = {"pr": 0, "stg": 0, "qk": 0, "x": 0, "xn": 0, "qst": 0}
            qst = [ph.sb([128, 512], BF16) for _ in range(3)]; d_qst = [Dep() for _ in range(3)]

            def nxt(key, n):
                i = cnt[key] % n
                cnt[key] += 1
                return i

            blocks = [("lat", i * 512, 512, i * 512 < self.nown) for i in range(16)] + [("ctx", 0, LC, not self.last)]
            for bi, (kind, t0, ntok, own) in enumerate(blocks):
                src = self.xsrc if kind == "lat" else self.ctxsrc
                ntile = ntok // 128
                mi = 0 if kind == "lat" else 2
                modt = self.modL if kind == "lat" else self.modC
                col0 = t0 if kind == "lat" else S
                h = hT[bi % 2]; dh = d_hT[bi % 2]
                s_ = ss[bi % 2]; ds_ = d_ss[bi % 2]
                xs, xns = [], []
                for tt in range(ntile):
                    xi = nxt("x", 4)
                    m.dma(xr[xi][:], src[t0 + tt * 128:t0 + (tt + 1) * 128, :], writes=[d_xr[xi]], q="sp")
                    m.op("act", lambda e: e.activation(out=junk[:], in_=xr[xi][:], func=AF.Square, accum_out=s_[:, tt:tt + 1]),
                         reads=[d_xr[xi]], writes=[d_junk, ds_])
                    xs.append(xi)
                m.op("dve", lambda e: e.tensor_scalar(out=s_[:, 0:ntile], in0=s_[:, 0:ntile], scalar1=1.0 / D, scalar2=EPS, op0=ALU.mult, op1=ALU.add),
                     reads=[ds_], writes=[ds_])
                m.op("act", lambda e: e.activation(out=s_[:, 0:ntile], in_=s_[:, 0:ntile], func=AF.Sqrt), reads=[ds_], writes=[ds_])
                m.op("dve", lambda e: e.reciprocal(out=s_[:, 0:ntile], in_=s_[:, 0:ntile]), reads=[ds_], writes=[ds_])
                for tt in range(ntile):
                    ni = nxt("xn", 8)
                    m.op("dve", lambda e: e.tensor_scalar(out=xn[ni][:], in0=xr[xs[tt]][:], scalar1=s_[:, tt:tt + 1], scalar2=None, op0=ALU.mult),
                         reads=[d_xr[xs[tt]], ds_], writes=[d_xn[ni]])
                    xns.append(ni)
                for hf in range(2):
                    for kcl in range(4):
                        kc = hf * 4 + kcl
                        for tt in range(ntile):
                            m.op("pe", lambda e: e.transpose(out=pT[:, kcl, tt * 128:(tt + 1) * 128], in_=xn[xns[tt]][:, kc * 128:(kc + 1) * 128],
                                                             identity=self.ident[:]),
                                 reads=[d_xn[xns[tt]], self.d_const], writes=[d_pT], inc=(kcl == 3 and tt == ntile - 1))
                    for kcl in range(4):
                        kc = hf * 4 + kcl
                        m.op("dve", lambda e: e.tensor_scalar(out=h[:, kc, 0:ntok], in0=pT[:, kcl, 0:ntok], scalar1=self.AB[:, mi, kc:kc + 1],
                                                              scalar2=modt[:, kc:kc + 1], op0=ALU.mult, op1=ALU.add),
                             reads=[d_pT, self.d_mod], writes=[dh])
                if "hT" in self.dbg and bi in (0, 16):
                    nm = f"dbg_hT{bi}"
                    o = self.nc.dram_tensor(nm, [128, 8, 512], BF16, kind="ExternalOutput").ap(); self.out_names.append(nm)
                    m.dma(o[:, :, 0:ntok], h[:, :, 0:ntok], reads=[dh])
                if kind == "lat":
                    ci = bi % 2
                    m.dma(cs[ci][:, 0, :], I["rope_cos"][:, t0:t0 + 512], writes=[d_cs[ci]], q="sp")
                    m.dma(cs[ci][:, 1, :], I["rope_sin"][:, t0:t0 + 512], writes=[d_cs[ci]], q="sp")

                def proj(c0):
                    pi = nxt("pr", 5)
                    for kc in range(8):
                        m.op("pe", lambda e: e.matmul(pr[pi][:, 0:ntok], lhsT=w_in[:, kc, c0:c0 + 128], rhs=h[:, kc, 0:ntok],
                                                      start=(kc == 0), stop=(kc == 7)),
                             reads=[d_w, dh], writes=[d_pr[pi]], inc=(kc == 7))
                    return pi

                def qk_post(pi, gcol, rope, out_ap, d_out):
                    k_ = nxt("qk", 2)
                    m.op("act", lambda e: e.activation(out=sq[k_][:, 0:ntok], in_=pr[pi][:, 0:ntok], func=AF.Square),
                         reads=[d_pr[pi]], writes=[d_sq[k_]])
                    p2 = nxt("pr", 5)
                    m.op("pe", lambda e: e.matmul(pr[p2][:, 0:ntok], lhsT=cst[:, 1, :], rhs=sq[k_][:, 0:ntok], start=True, stop=True),
                         reads=[d_cst, d_sq[k_]], writes=[d_pr[p2]])
                    m.op("act", lambda e: e.activation(out=f1[k_][:, 0:ntok], in_=pr[p2][:, 0:ntok], func=AF.Sqrt, bias=self.epsT[:, 0:1]),
                         reads=[d_pr[p2], self.d_const], writes=[d_f1[k_]])
                    m.op("dve", lambda e: e.reciprocal(out=f1[k_][:, 0:ntok], in_=f1[k_][:, 0:ntok]), reads=[d_f1[k_]], writes=[d_f1[k_]])
                    if not rope:
                        m.op("dve", lambda e: e.scalar_tensor_tensor(out=out_ap, in0=pr[pi][:, 0:ntok], scalar=qkg[:, gcol:gcol + 1],
                                                                     in1=f1[k_][:, 0:ntok], op0=ALU.mult, op1=ALU.mult),
                             reads=[d_pr[pi], d_f1[k_], d_cst], writes=[d_out])
                        return
                    m.op("dve", lambda e: e.scalar_tensor_tensor(out=qn[k_][:, 0:ntok], in0=pr[pi][:, 0:ntok], scalar=qkg[:, gcol:gcol + 1],
                                                                 in1=f1[k_][:, 0:ntok], op0=ALU.mult, op1=ALU.mult),
                         reads=[d_pr[pi], d_f1[k_], d_cst], writes=[d_qn[k_]])
                    p3 = nxt("pr", 5)
                    m.op("pe", lambda e: e.matmul(pr[p3][:, 0:ntok], lhsT=cst[:, 0, :], rhs=qn[k_][:, 0:ntok], start=True, stop=True),
                         reads=[d_cst, d_qn[k_]], writes=[d_pr[p3]])
                    ci = bi % 2
                    m.op("dve", lambda e: e.tensor_tensor(out=f1[k_][:, 0:ntok], in0=qn[k_][:, 0:ntok], in1=cs[ci][:, 0, 0:ntok], op=ALU.mult),
                         reads=[d_qn[k_], d_cs[ci]], writes=[d_f1[k_]])
                    m.op("dve", lambda e: e.tensor_tensor(out=f2[k_][:, 0:ntok], in0=pr[p3][:, 0:ntok], in1=cs[ci][:, 1, 0:ntok], op=ALU.mult),
                         reads=[d_pr[p3], d_cs[ci]], writes=[d_f2[k_]])
                    m.op("pool", lambda e: e.tensor_tensor(out=out_ap, in0=f1[k_][:, 0:ntok], in1=f2[k_][:, 0:ntok], op=ALU.add),
                         reads=[d_f1[k_], d_f2[k_]], writes=[d_out])

                if own:
                    for j in range(4):
                        pi = proj(j * 128)
                        if kind == "lat":
                            qs = nxt("qst", 3)
                            qk_post(pi, 0, True, qst[qs][:], d_qst[qs])
                            m.dma(self.QTs[j, :, t0:t0 + 512], qst[qs][:], reads=[d_qst[qs]], q="act")
                        else:
                            qk_post(pi, 0, False, QC[:, j, :], d_QC)
                pi = proj(512)
                qk_post(pi, 1, kind == "lat", KT[:, col0:col0 + ntok], d_KT)
                for tt in range(ntile):
                    for kc in range(8):
                        m.op("pe", lambda e: e.matmul(pv[:, tt, :], lhsT=h[:, kc, tt * 128:(tt + 1) * 128], rhs=w_in[:, kc, 640:768],
                                                      start=(kc == 0), stop=(kc == 7)),
                             reads=[dh, d_w], writes=[d_pv], inc=(kc == 7))
                kt0 = col0 // 128
                m.op("act", lambda e: e.activation(out=VA[:, kt0:kt0 + ntile, :, 0:64],
                                                   in_=pv[:, 0:ntile, :].rearrange("p t (h d) -> p t h d", h=2), func=AF.Identity),
                     reads=[d_pv], writes=[d_VA])
                for ci_ in range(10):
                    pi = proj(768 + ci_ * 128)
                    si = nxt("stg", 4)
                    m.op("act", lambda e: e.activation(out=stg[si][:, 0:ntok], in_=pr[pi][:, 0:ntok], func=AF.Identity),
                         reads=[d_pr[pi]], writes=[d_stg[si]])
                    m.dma(self.U[ci_, :, col0:col0 + ntok], stg[si][:, 0:ntok], reads=[d_stg[si]], q="act")

    def _attention(self, KT, d_KT, QT, d_QT, QC, d_QC, VA, d_VA, negM, d_negM):
        m, I = self.m, self.I
        with Phase(m) as ph:
            self_f = ph.sb([65, 64]); d_sel = Dep()
            m.dma(self_f[:], I["sel"][:, :], writes=[d_sel])
            pS = [ph.ps([128, 2, 512]) for _ in range(3)]
            d_pS = [Dep() for _ in range(3)]
            pO = [ph.ps([65, 512]) for _ in range(2)]; d_pO = [Dep(), Dep()]
            Pt = [ph.sb([128, 2, 512], BF16) for _ in range(3)]
            d_Pt = [Dep() for _ in range(3)]
            osb = [ph.sb([65, 512]) for _ in range(2)]; d_osb = [Dep(), Dep()]
            att = [ph.sb([64, 512]) for _ in range(4)]; d_att = [Dep() for _ in range(4)]
            acnt = [0]

            qtl = [ph.sb([128, 512], BF16) for _ in range(3)]; d_qtl = [Dep() for _ in range(3)]
            qcnt = [0]

            def run(qsrc, d_q, j, q0, nq, keys, out_col0):
                nk = len(keys)

                def issue_S(i):
                    kc0, _ = keys[i]
                    for hb in range(2):
                        lo = hb * 64
                        m.op("pe", lambda e: e.matmul(pS[i % 3][:, hb, 0:nq], lhsT=KT[lo:lo + 64, kc0:kc0 + 128],
                                                      rhs=qsrc[lo:lo + 64, 0:nq], start=True, stop=True),
                             reads=[d_KT, d_q], writes=[d_pS[i % 3]], inc=(hb == 1))
                issue_S(0)
                if nk > 1:
                    issue_S(1)
                for i in range(nk):
                    if i + 2 < nk:
                        issue_S(i + 2)
                    _, vt = keys[i]
                    P_ = Pt[i % 3]; dP = d_Pt[i % 3]
                    m.op("act", lambda e: e.activation(out=P_[:, :, 0:nq], in_=pS[i % 3][:, :, 0:nq], func=AF.Exp, scale=0.125, bias=negM[:, 0:1]),
                         reads=[d_pS[i % 3], d_negM], writes=[dP])
                    for hb in range(2):
                        m.op("pe", lambda e: e.matmul(pO[hb][:, 0:nq], lhsT=VA[:, vt, hb, :], rhs=P_[:, hb, 0:nq], start=(i == 0), stop=(i == nk - 1)),
                             reads=[d_VA, dP], writes=[d_pO[hb]], inc=(i == nk - 1))
                for hb in range(2):
                    head = j + 4 * hb
                    o_ = osb[hb]; do = d_osb[hb]
                    m.op("act", lambda e: e.activation(out=o_[:, 0:nq], in_=pO[hb][:, 0:nq], func=AF.Identity), reads=[d_pO[hb]], writes=[do])
                    m.op("dve", lambda e: e.reciprocal(out=o_[64:65, 0:nq], in_=o_[64:65, 0:nq]), reads=[do], writes=[do])
                    pB = pS[nk % 3]; d_pB = d_pS[nk % 3]
                    m.op("pe", lambda e: e.matmul(pB[0:64, hb, 0:nq], lhsT=self_f[:], rhs=o_[:, 0:nq], start=True, stop=True),
                         reads=[do, d_sel], writes=[d_pB])
                    ai = acnt[0] % 4; acnt[0] += 1
                    m.op("dve", lambda e: e.tensor_tensor(out=att[ai][:, 0:nq], in0=o_[0:64, 0:nq], in1=pB[0:64, hb, 0:nq], op=ALU.mult),
                         reads=[do, d_pB], writes=[d_att[ai]])
                    m.dma(self.OT[head * 64:(head + 1) * 64, out_col0:out_col0 + nq], att[ai][:, 0:nq], reads=[d_att[ai]], q="sp")

            all_keys = [(kt * 128, kt) for kt in range(66)]
            ctx_keys = [(S + i * 128, 64 + i) for i in range(2)]
            nqb = self.nown // 512 if "att_short" not in self.dbg else 1
            for qb in range(nqb):
                for j in range(4):
                    qi = qcnt[0] % 3; qcnt[0] += 1
                    m.dma(qtl[qi][:], self.QTs[j, :, qb * 512:(qb + 1) * 512], writes=[d_qtl[qi]], q="sp")
                    run(qtl[qi], d_qtl[qi], j, qb * 512, 512, all_keys, qb * 512)
            if not self.last:
                for j in range(4):
                    run(QC[:, j, :], d_QC, j, 0, LC, ctx_keys, self.nown)

    def phase_lru(self):
        m, I = self.m, self.I
        SEG = 2048
        with Phase(m) as ph:
            d_c = Dep()
            cw = ph.sb([128, 2, 5]); cb = ph.sb([128, 2]); gb = ph.sb([128, 8]); lam = ph.sb([128, 4]); cvec = ph.sb([128, 4])
            wbd = ph.sb([128, 8, 128], BF16)
            m.dma(cw[:], I["lru_cw"][:, :, :], writes=[d_c]); m.dma(cb[:], I["lru_cb"][:, :], writes=[d_c])
            m.dma(gb[:], I["lru_gb"][:, :], writes=[d_c]); m.dma(lam[:], I["lru_lam"][:, :], writes=[d_c])
            m.dma(wbd[:], I["lru_wbd"][:, :, :], writes=[d_c], q="pool")
            m.op("act", lambda e: e.activation(out=cvec[:], in_=lam[:], func=AF.Exp, scale=-1.0), reads=[d_c], writes=[d_c])
            m.op("act", lambda e: e.activation(out=cvec[:], in_=cvec[:], func=AF.Ln, bias=1.0), reads=[d_c], writes=[d_c])
            m.op("dve", lambda e: e.tensor_scalar(out=cvec[:], in0=cvec[:], scalar1=-8.0, scalar2=None, op0=ALU.mult), reads=[d_c], writes=[d_c])
            lxp = [ph.sb([128, SEG + 4]) for _ in range(2)]; d_lxp = [Dep(), Dep()]
            xl = ph.sb([128, SEG]); d_xl = Dep()
            xlb = ph.sb([128, SEG], BF16); d_xlb = Dep()
            aS = ph.sb([128, SEG]); d_aS = Dep()
            bS = ph.sb([128, SEG]); d_bS = Dep()
            hS = [ph.sb([128, SEG]) for _ in range(2)]; d_hS = [Dep(), Dep()]
            hfS = [ph.sb([128, SEG]) for _ in range(2)]; d_hfS = [Dep(), Dep()]
            lgS = [ph.sb([128, SEG]) for _ in range(2)]; d_lgS = [Dep(), Dep()]
            g1 = ph.sb([128, SEG]); d_g1 = Dep()
            g2 = ph.sb([128, SEG]); d_g2 = Dep()
            tR = [ph.sb([128, 512]) for _ in range(2)]; d_tR = [Dep(), Dep()]
            tI = [ph.sb([128, 512]) for _ in range(2)]; d_tI = [Dep(), Dep()]
            tA = [ph.sb([128, 512]) for _ in range(2)]; d_tA = [Dep(), Dep()]
            pG = [[ph.ps([128, 512]) for _ in range(2)] for _ in range(2)]; d_pG = [[Dep(), Dep()], [Dep(), Dep()]]
            cr = ph.sb([128, 1]); d_cr = Dep()
            segs_f = [("ctx", 0, LC)] + [("lat", i * SEG, SEG) for i in range(S // SEG)]
            segs_b = [("ctx", 0, LC)] + [("lat", i * SEG, SEG) for i in reversed(range(S // SEG))]
            sc = 0
            for dirn in range(2):
                for c in range(2):
                    m.op("dve", lambda e: e.memset(cr[:], 0.0), writes=[d_cr])
                    for (kind, t0, ln) in (segs_f if dirn == 0 else segs_b):
                        seqlen = S if kind == "lat" else LC
                        base = 0 if kind == "lat" else S
                        bi = sc % 2; sc += 1
                        lp = lxp[bi]; dlp = d_lxp[bi]
                        lo = max(t0 - 2, 0); hi = min(t0 + ln + 2, seqlen)
                        if t0 - 2 < 0:
                            m.op("pool", lambda e: e.memset(lp[:, 0:2], 0.0), writes=[dlp])
                        if t0 + ln + 2 > seqlen:
                            m.op("pool", lambda e: e.memset(lp[:, ln + 2:ln + 4], 0.0), writes=[dlp])
                        m.dma(lp[:, lo - (t0 - 2):hi - (t0 - 2)], self.U[c, :, base + lo:base + hi], writes=[dlp], q="sp")
                        own = (kind == "ctx" and not self.last) or (kind == "lat" and t0 < self.nown)
                        if dirn == 1:
                            m.dma(hfS[bi][:, 0:ln], self.HF[c, :, base + t0:base + t0 + ln], writes=[d_hfS[bi]], q="sp")
                            if own:
                                m.dma(lgS[bi][:, 0:ln], self.U[2 + c, :, base + t0:base + t0 + ln], writes=[d_lgS[bi]], q="sp")
                        m.op("dve", lambda e: e.tensor_scalar(out=xl[:, 0:ln], in0=lp[:, 0:ln], scalar1=cw[:, c, 0:1], scalar2=cb[:, c:c + 1],
                                                              op0=ALU.mult, op1=ALU.add), reads=[dlp, d_c], writes=[d_xl])
                        for o in range(1, 5):
                            m.op("dve", lambda e: e.scalar_tensor_tensor(out=xl[:, 0:ln], in0=lp[:, o:o + ln], scalar=cw[:, c, o:o + 1], in1=xl[:, 0:ln],
                                                                         op0=ALU.mult, op1=ALU.add), reads=[dlp, d_c, d_xl], writes=[d_xl])
                        m.op("act", lambda e: e.activation(out=xlb[:, 0:ln], in_=xl[:, 0:ln], func=AF.Identity), reads=[d_xl], writes=[d_xlb])
                        for sb_ in range((ln + 511) // 512):
                            s0 = sb_ * 512; n = min(512, ln - s0); k_ = sb_ % 2
                            for g in range(2):
                                m.op("pe", lambda e: e.matmul(pG[k_][g][:, 0:n], lhsT=wbd[:, dirn * 4 + g * 2 + c, :], rhs=xlb[:, s0:s0 + n],
                                                              start=True, stop=True), reads=[d_c, d_xlb], writes=[d_pG[k_][g]])
                            gi = dirn * 4 + c
                            m.op("act", lambda e: e.activation(out=tR[k_][:, 0:n], in_=pG[k_][0][:, 0:n], func=AF.Sigmoid, bias=gb[:, gi:gi + 1]),
                                 reads=[d_pG[k_][0], d_c], writes=[d_tR[k_]])
                            m.op("act", lambda e: e.activation(out=tI[k_][:, 0:n], in_=pG[k_][1][:, 0:n], func=AF.Sigmoid, bias=gb[:, gi + 2:gi + 3]),
                                 reads=[d_pG[k_][1], d_c], writes=[d_tI[k_]])
                            m.op("act", lambda e: e.activation(out=aS[:, s0:s0 + n], in_=tR[k_][:, 0:n], func=AF.Exp, scale=cvec[:, dirn * 2 + c:dirn * 2 + c + 1]),
                                 reads=[d_tR[k_], d_c], writes=[d_aS])
                            m.op("pool", lambda e: e.tensor_tensor(out=tA[k_][:, 0:n], in0=aS[:, s0:s0 + n], in1=aS[:, s0:s0 + n], op=ALU.mult),
                                 reads=[d_aS], writes=[d_tA[k_]])
                            m.op("pool", lambda e: e.tensor_scalar(out=tA[k_][:, 0:n], in0=tA[k_][:, 0:n], scalar1=-1.0, scalar2=1.0, op0=ALU.mult, op1=ALU.add),
                                 reads=[d_tA[k_]], writes=[d_tA[k_]])
                            m.op("act", lambda e: e.activation(out=tA[k_][:, 0:n], in_=tA[k_][:, 0:n], func=AF.Sqrt), reads=[d_tA[k_]], writes=[d_tA[k_]])
                            m.op("pool", lambda e: e.tensor_tensor(out=tI[k_][:, 0:n], in0=tI[k_][:, 0:n], in1=xl[:, s0:s0 + n], op=ALU.mult),
                                 reads=[d_tI[k_], d_xl], writes=[d_tI[k_]])
                            m.op("dve", lambda e: e.tensor_tensor(out=bS[:, s0:s0 + n], in0=tA[k_][:, 0:n], in1=tI[k_][:, 0:n], op=ALU.mult),
                                 reads=[d_tA[k_], d_tI[k_]], writes=[d_bS])
                        h_ = hS[bi]; dh = d_hS[bi]
                        if dirn == 0:
                            m.op("dve", lambda e: e.tensor_tensor_scan(out=h_[:, 0:ln], data0=aS[:, 0:ln], data1=bS[:, 0:ln], initial=cr[:, 0:1],
                                                                       op0=ALU.mult, op1=ALU.add), reads=[d_aS, d_bS, d_cr], writes=[dh])
                            m.op("dve", lambda e: e.tensor_copy(out=cr[:], in_=h_[:, ln - 1:ln]), reads=[dh], writes=[d_cr])
                            m.dma(self.HF[c, :, base + t0:base + t0 + ln], h_[:, 0:ln], reads=[dh], q="act")
                        else:
                            m.op("dve", lambda e: e.tensor_tensor_scan(out=h_[:, ln - 1::-1] if ln == SEG else h_[:, ln - 1::-1],
                                                                       data0=aS[:, ln - 1::-1], data1=bS[:, ln - 1::-1], initial=cr[:, 0:1],
                                                                       op0=ALU.mult, op1=ALU.add), reads=[d_aS, d_bS, d_cr], writes=[dh])
                            m.op("dve", lambda e: e.tensor_copy(out=cr[:], in_=h_[:, 0:1]), reads=[dh], writes=[d_cr])
                            if own:
                                lg_ = lgS[bi]; dlg = d_lgS[bi]
                                m.op("pool", lambda e: e.tensor_tensor(out=h_[:, 0:ln], in0=h_[:, 0:ln], in1=hfS[bi][:, 0:ln], op=ALU.add),
                                     reads=[dh, d_hfS[bi]], writes=[dh])
                                m.op("pool", lambda e: e.tensor_tensor(out=g1[:, 0:ln], in0=lg_[:, 0:ln], in1=lg_[:, 0:ln], op=ALU.mult),
                                     reads=[dlg], writes=[d_g1])
                                m.op("pool", lambda e: e.tensor_scalar(out=g1[:, 0:ln], in0=g1[:, 0:ln], scalar1=0.044715, scalar2=1.0, op0=ALU.mult, op1=ALU.add),
                                     reads=[d_g1], writes=[d_g1])
                                m.op("pool", lambda e: e.tensor_tensor(out=g1[:, 0:ln], in0=g1[:, 0:ln], in1=lg_[:, 0:ln], op=ALU.mult),
                                     reads=[d_g1, dlg], writes=[d_g1])
                                m.op("act", lambda e: e.activation(out=g1[:, 0:ln], in_=g1[:, 0:ln], func=AF.Sigmoid, scale=1.5957691216057308),
                                     reads=[d_g1], writes=[d_g1])
                                m.op("dve", lambda e: e.tensor_tensor(out=g1[:, 0:ln], in0=g1[:, 0:ln], in1=lg_[:, 0:ln], op=ALU.mult),
                                     reads=[d_g1, dlg], writes=[d_g1])
                                m.op("dve", lambda e: e.tensor_tensor(out=g2[:, 0:ln], in0=g1[:, 0:ln], in1=h_[:, 0:ln], op=ALU.mult),
                                     reads=[d_g1, dh], writes=[d_g2])
                                oc = t0 if kind == "lat" else self.nown
                                m.dma(self.OT[512 + c * 128:512 + (c + 1) * 128, oc:oc + ln], g2[:, 0:ln], reads=[d_g2], q="act")
                    m.barrier()

    def phase_hyena(self):
        self._hy_filters_and_conv()
        self.m.barrier()
        self._hy_fft()

    def _sin9(self, m, ph, out, psum, n, sc, bi, d_in, d_out, tmp, d_tmp, d_c):
        m.op("act", lambda e: e.activation(out=out, in_=psum, func=AF.Sin, scale=sc, bias=bi), reads=[d_in, d_c], writes=[d_out])
        for _ in range(2):
            m.op("dve", lambda e: e.tensor_tensor(out=tmp, in0=out, in1=out, op=ALU.mult), reads=[d_out], writes=[d_tmp])
            m.op("dve", lambda e: e.tensor_scalar(out=tmp, in0=tmp, scalar1=-4.0, scalar2=3.0, op0=ALU.mult, op1=ALU.add), reads=[d_tmp], writes=[d_tmp])
            m.op("dve", lambda e: e.tensor_tensor(out=out, in0=out, in1=tmp, op=ALU.mult), reads=[d_out, d_tmp], writes=[d_out])

    def _hy_filters_and_conv(self):
        m, I = self.m, self.I
        G = self.G
        self.hyP = Phase(m); P = self.hyP; P.__enter__()
        self.kc_f = P.sb([128, 4, LC]); self.d_kc = Dep()
        self.nrm = P.sb([128, 2, 4]); self.d_nrm = Dep()
        self.vvc = P.sb([128, 2, LC]); self.x0c = P.sb([128, 2, LC]); self.d_vvc = Dep()
        self.hsc = P.sb([128, 2, 2]); self.d_hsc = Dep()
        with Phase(m) as ph:
            d_c = Dep()
            w1 = ph.sb([33, 64]); w2 = ph.sb([64, 64]); w3 = ph.sb([64, 768]); b12f = ph.sb([64, 3]); dec = ph.sb([128, 4])
            for t_, n_ in ((w1, "hy_w1"), (w2, "hy_w2"), (w3, "hy_w3"), (b12f, "hy_b12f"), (dec, "hy_dec")):
                m.dma(t_[:], I[n_][:, :], writes=[d_c])
            scb = ph.sb([64, 3])
            m.op("dve", lambda e: e.tensor_scalar(out=scb[:, 0:1], in0=b12f[:, 2:3], scalar1=1.0 / 9.0, scalar2=None, op0=ALU.mult), reads=[d_c], writes=[d_c])
            m.op("dve", lambda e: e.tensor_scalar(out=scb[:, 1:3], in0=b12f[:, 0:2], scalar1=scb[:, 0:1], scalar2=None, op0=ALU.mult), reads=[d_c], writes=[d_c])
            nad = ph.sb([128, 4])
            m.op("dve", lambda e: e.tensor_scalar(out=nad[:], in0=dec[:], scalar1=-1.0, scalar2=None, op0=ALU.mult), reads=[d_c], writes=[d_c])
            m.op("dve", lambda e: e.tensor_tensor(out=nad[:], in0=nad[:], in1=dec[:], op=ALU.min), reads=[d_c], writes=[d_c])
            m.op("dve", lambda e: e.memset(self.nrm[:], 0.0), writes=[self.d_nrm])
            zb = [ph.sb([33, 512]) for _ in range(2)]; d_zb = [Dep(), Dep()]
            tb = [ph.sb([128, 512]) for _ in range(2)]; d_tb = [Dep(), Dep()]
            h1 = ph.sb([64, 512]); d_h1 = Dep()
            h2 = ph.sb([64, 512]); d_h2 = Dep()
            tmp = ph.sb([64, 512]); d_tmp = Dep()
            ex = [ph.sb([128, 512]) for _ in range(2)]; d_ex = [Dep(), Dep()]
            kk = [ph.sb([128, 512], BF16) for _ in range(3)]; d_kk = [Dep() for _ in range(3)]
            red = ph.sb([128, 1]); d_red = Dep()
            c0t = ph.sb([128, 2, 2]); d_c0 = Dep()
            p1 = ph.ps([64, 512]); d_p1 = Dep()
            p2 = ph.ps([64, 512]); d_p2 = Dep()
            p3 = [ph.ps([128, 512]) for _ in range(2)]; d_p3 = [Dep(), Dep()]
            pc = ph.ps([128, 2, 2]); d_pc = Dep()
            kc_i = 0
            for which, (zname, tname, L) in enumerate((("zT", "t01", S), ("zTc", "t01c", LC))[:(1 if self.last else 2)]):
                for bi_ in range((L + 511) // 512):
                    c0_ = bi_ * 512; n = min(512, L - c0_); r = bi_ % 2
                    m.dma(zb[r][:, 0:n], I[zname][:, c0_:c0_ + n], writes=[d_zb[r]])
                    m.dma(tb[r][:, 0:n], I[tname][:, c0_:c0_ + n], writes=[d_tb[r]])
                    m.op("pe", lambda e: e.matmul(p1[:, 0:n], lhsT=w1[:], rhs=zb[r][:, 0:n], start=True, stop=True), reads=[d_c, d_zb[r]], writes=[d_p1])
                    self._sin9(m, ph, h1[:, 0:n], p1[:, 0:n], n, scb[:, 0:1], scb[:, 1:2], d_p1, d_h1, tmp[:, 0:n], d_tmp, d_c)
                    m.op("pe", lambda e: e.matmul(p2[:, 0:n], lhsT=w2[:], rhs=h1[:, 0:n], start=True, stop=True), reads=[d_c, d_h1], writes=[d_p2])
                    self._sin9(m, ph, h2[:, 0:n], p2[:, 0:n], n, scb[:, 0:1], scb[:, 2:3], d_p2, d_h2, tmp[:, 0:n], d_tmp, d_c)
                    if bi_ == 0:
                        for c in range(2):
                            m.op("pe", lambda e: e.matmul(pc[:, c, :], lhsT=w3[:, 512 + c * 128:512 + (c + 1) * 128], rhs=h2[:, 0:2], start=True, stop=True),
                                 reads=[d_c, d_h2], writes=[d_pc])
                        m.op("act", lambda e: e.activation(out=c0t[:], in_=pc[:], func=AF.Identity), reads=[d_pc], writes=[d_c0])
                    for ci in range(4):
                        pi = ci % 2
                        m.op("pe", lambda e: e.matmul(p3[pi][:, 0:n], lhsT=w3[:, ci * 128:(ci + 1) * 128], rhs=h2[:, 0:n], start=True, stop=True),
                             reads=[d_c, d_h2], writes=[d_p3[pi]])
                        m.op("act", lambda e: e.activation(out=ex[pi][:, 0:n], in_=tb[r][:, 0:n], func=AF.Exp, scale=nad[:, ci:ci + 1]),
                             reads=[d_tb[r], d_c], writes=[d_ex[pi]])
                        if which == 0:
                            ki = kc_i % 3; kc_i += 1
                            kt_ = kk[ki][:, 0:n]; dk = d_kk[ki]
                        else:
                            kt_ = self.kc_f[:, ci, :]; dk = self.d_kc
                        m.op("dve", lambda e: e.tensor_tensor(out=kt_, in0=p3[pi][:, 0:n], in1=ex[pi][:, 0:n], op=ALU.mult),
                             reads=[d_p3[pi], d_ex[pi]], writes=[dk])
                        if bi_ == 0:
                            if ci < 2:
                                m.op("dve", lambda e: e.tensor_copy(out=kt_[:, 0:1], in_=c0t[:, ci, 0:1]), reads=[d_c0, dk], writes=[dk])
                            else:
                                m.op("dve", lambda e: e.memset(kt_[:, 0:1], 0.0), reads=[dk], writes=[dk])
                        m.op("dve", lambda e: e.tensor_reduce(out=red[:], in_=kt_, axis=AX.X, op=ALU.add, apply_absolute_value=True),
                             reads=[dk], writes=[d_red])
                        m.op("dve", lambda e: e.tensor_tensor(out=self.nrm[:, which, ci:ci + 1], in0=self.nrm[:, which, ci:ci + 1], in1=red[:], op=ALU.add),
                             reads=[d_red, self.d_nrm], writes=[self.d_nrm])
                        if which == 0:
                            m.dma(self.KFb[ci * 128:(ci + 1) * 128, c0_:c0_ + n], kt_, reads=[dk], q="act")
            m.op("dve", lambda e: e.tensor_tensor(out=self.hsc[:], in0=self.nrm[:, :, 0:2], in1=self.nrm[:, :, 2:4], op=ALU.add),
                 reads=[self.d_nrm], writes=[self.d_hsc])
            m.op("dve", lambda e: e.tensor_scalar(out=self.hsc[:, 0, :], in0=self.hsc[:, 0, :], scalar1=float(NFFT), scalar2=None, op0=ALU.mult),
                 reads=[self.d_hsc], writes=[self.d_hsc])
            m.op("dve", lambda e: e.reciprocal(out=self.hsc[:], in_=self.hsc[:]), reads=[self.d_hsc], writes=[self.d_hsc])
        with Phase(m) as ph:
            SEG = 2048
            d_c = Dep()
            cw = ph.sb([128, 6, 3]); cb = ph.sb([128, 6])
            m.dma(cw[:], I["hy_cw"][:, :, :], writes=[d_c]); m.dma(cb[:], I["hy_cb"][:, :], writes=[d_c])
            pad = [[ph.sb([128, SEG + 2]) for _ in range(3)] for _ in range(2)]; d_pad = [[Dep() for _ in range(3)] for _ in range(2)]
            uc = [ph.sb([128, SEG]) for _ in range(3)]; d_uc = [Dep() for _ in range(3)]
            vv = [ph.sb([128, SEG]) for _ in range(2)]; d_vv = [Dep(), Dep()]
            vvb = [ph.sb([128, SEG], BF16) for _ in range(2)]; d_vvb = [Dep(), Dep()]
            sc = 0
            segs = [("lat", i * SEG, SEG) for i in range(S // SEG)] + ([] if self.last else [("ctx", 0, LC)])
            for (kind, t0, ln) in segs:
                seqlen = S if kind == "lat" else LC
                base = 0 if kind == "lat" else S
                for c in range(2):
                    bi = sc % 2; sc += 1
                    for k3 in range(3):
                        ci = 2 * k3 + c
                        lp = pad[bi][k3]; dlp = d_pad[bi][k3]
                        lo = max(t0 - 1, 0); hi = min(t0 + ln + 1, seqlen)
                        if t0 - 1 < 0:
                            m.op("pool", lambda e: e.memset(lp[:, 0:1], 0.0), writes=[dlp])
                        if t0 + ln + 1 > seqlen:
                            m.op("pool", lambda e: e.memset(lp[:, ln + 1:ln + 2], 0.0), writes=[dlp])
                        m.dma(lp[:, lo - (t0 - 1):hi - (t0 - 1)], self.U[4 + ci, :, base + lo:base + hi], writes=[dlp], q="sp")
                        eng = "dve" if k3 != 0 else "pool"
                        u_ = uc[k3]; du = d_uc[k3]
                        if kind == "ctx" and k3 == 0:
                            u_ = self.x0c[:, c, :]
                            du = self.d_vvc
                        else:
                            u_ = u_[:, 0:ln]
                        m.op("dve", lambda e: e.tensor_scalar(out=u_, in0=lp[:, 0:ln], scalar1=cw[:, ci, 0:1], scalar2=cb[:, ci:ci + 1],
                                                              op0=ALU.mult, op1=ALU.add), reads=[dlp, d_c], writes=[du])
                        for o in range(1, 3):
                            m.op("dve", lambda e: e.scalar_tensor_tensor(out=u_, in0=lp[:, o:o + ln], scalar=cw[:, ci, o:o + 1], in1=u_,
                                                                         op0=ALU.mult, op1=ALU.add), reads=[dlp, d_c, du], writes=[du])
                    if kind == "lat":
                        m.op("pool", lambda e: e.tensor_tensor(out=vv[bi][:, 0:ln], in0=uc[1][:, 0:ln], in1=uc[2][:, 0:ln], op=ALU.mult),
                             reads=[d_uc[1], d_uc[2]], writes=[d_vv[bi]])
                        m.dma(self.VV[c * 128:(c + 1) * 128, t0:t0 + ln], vv[bi][:, 0:ln], reads=[d_vv[bi]], q="act")
                        m.op("act", lambda e: e.activation(out=vvb[bi][:, 0:ln], in_=vv[bi][:, 0:ln], func=AF.Identity), reads=[d_vv[bi]], writes=[d_vvb[bi]])
                        m.dma(self.VVb[c * 128:(c + 1) * 128, t0:t0 + ln], vvb[bi][:, 0:ln], reads=[d_vvb[bi]], q="act")
                        if t0 < self.nown:
                            m.dma(self.X0[c * 128:(c + 1) * 128, t0:t0 + ln], uc[0][:, 0:ln], reads=[d_uc[0]], q="act")
                    else:
                        m.op("pool", lambda e: e.tensor_tensor(out=self.vvc[:, c, :], in0=uc[1][:, 0:ln], in1=uc[2][:, 0:ln], op=ALU.mult),
                             reads=[d_uc[1], d_uc[2]], writes=[self.d_vvc])
            yc = ph.sb([128, 2, LC]); d_yc = Dep()
            hb = ph.sb([128, 2]); m.dma(hb[:], I["hy_bias"][:, :], writes=[d_c])
            for c in range(0 if self.last else 2):
                y_ = yc[:, c, :]; v_ = self.vvc[:, c, :]
                m.op("dve", lambda e: e.tensor_scalar(out=y_, in0=v_, scalar1=self.kc_f[:, c, 0:1], scalar2=None, op0=ALU.mult),
                     reads=[self.d_vvc, self.d_kc], writes=[d_yc])
                for dl in range(1, LC):
                    m.op("dve", lambda e: e.scalar_tensor_tensor(out=y_[:, dl:], in0=v_[:, 0:LC - dl], scalar=self.kc_f[:, c, dl:dl + 1], in1=y_[:, dl:],
                                                                 op0=ALU.mult, op1=ALU.add), reads=[d_yc], writes=[d_yc])
                    m.op("dve", lambda e: e.scalar_tensor_tensor(out=y_[:, 0:LC - dl], in0=v_[:, dl:], scalar=self.kc_f[:, 2 + c, dl:dl + 1], in1=y_[:, 0:LC - dl],
                                                                 op0=ALU.mult, op1=ALU.add), reads=[d_yc], writes=[d_yc])
                m.op("dve", lambda e: e.tensor_scalar(out=y_, in0=y_, scalar1=self.hsc[:, 1, c:c + 1], scalar2=None, op0=ALU.mult),
                     reads=[d_yc, self.d_hsc], writes=[d_yc])
                m.op("dve", lambda e: e.scalar_tensor_tensor(out=y_, in0=v_, scalar=hb[:, c:c + 1], in1=y_, op0=ALU.mult, op1=ALU.add),
                     reads=[d_yc, d_c, self.d_vvc], writes=[d_yc])
                m.op("dve", lambda e: e.tensor_tensor(out=y_, in0=y_, in1=self.x0c[:, c, :], op=ALU.mult), reads=[d_yc, self.d_vvc], writes=[d_yc])
                m.dma(self.OT[768 + c * 128:768 + (c + 1) * 128, self.nown:self.nown + LC], y_, reads=[d_yc], q="act")

    def _hy_fft(self):
        m, I = self.m, self.I
        GC = 32
        with Phase(m) as ph:
            d_c = Dep()
            C2 = ph.sb([128, 3, 128], BF16); FI = ph.sb([128, 128, 2, 32], BF16); d_FI = Dep()
            m.dma(C2[:], I["C2"][:, :, :], writes=[d_c])
            fi_loaded = [None]
            x1s = [ph.sb([64, GC, 128], BF16) for _ in range(2)]; d_x1 = [Dep(), Dep()]
            f1p = [ph.sb([64, 16, 2, 128], BF16) for _ in range(2)]; d_f1 = [Dep() for _ in range(2)]
            bP = ph.sb([128, 2, 128, GC], BF16); d_bP = Dep()
            bQ = ph.sb([128, 2, GC, 128], BF16); d_bQ = Dep()
            bZ = ph.sb([128, 2, GC, 128], BF16); d_bZ = Dep()
            Kf = ph.sb([128, 2, GC, 128], BF16); d_Kf = Dep()
            Yb = ph.sb([128, 2, GC, 128], BF16); d_Yb = Dep()
            t4 = [ph.sb([128, 512]) for _ in range(4)]; d_t4 = [Dep() for _ in range(4)]
            ysb = ph.sb([GC, 32, 128]); d_ysb = Dep()
            fv = [ph.sb([GC, 512]) for _ in range(2)]; d_fv = [Dep(), Dep()]
            fx = [ph.sb([GC, 512]) for _ in range(2)]; d_fx = [Dep(), Dep()]
            fo = [ph.sb([GC, 512]) for _ in range(2)]; d_fo = [Dep(), Dep()]
            gsc = ph.sb([GC, 2]); d_gsc = Dep()
            pA = [ph.ps([128, 16, GC]) for _ in range(2)]; d_pA = [Dep(), Dep()]
            pT = [ph.ps([128, 8, 128], BF16) for _ in range(2)]; d_pT = [Dep(), Dep()]
            pX = [[ph.ps([128, 512]) for _ in range(2)] for _ in range(2)]; d_pX = [[Dep(), Dep()], [Dep(), Dep()]]
            cnt = {"x1": 0, "f1": 0, "pA": 0, "pT": 0, "pX": 0, "ev": 0, "fin": 0}

            def nxt(k, n):
                i = cnt[k] % n; cnt[k] += 1
                return i

            def evac(out, in_, reads, writes):
                if nxt("ev", 2) == 0:
                    m.op("act", lambda e: e.activation(out=out, in_=in_, func=AF.Identity), reads=reads, writes=writes)
                else:
                    m.op("dve", lambda e: e.tensor_copy(out=out, in_=in_), reads=reads, writes=writes)

            def fwd(src_rows, mode):
                xi = nxt("x1", 2)
                m.dma(x1s[xi][:], src_rows.rearrange("c (a b) -> a c b", b=128), writes=[d_x1[xi]], q="sp")
                for bg in range(8):
                    fi = nxt("f1", 2)
                    m.dma(f1p[fi][:], I["F1"][:, bg * 16:(bg + 1) * 16, :, :], writes=[d_f1[fi]], q="sp")
                    for hb in range(2):
                        pi = nxt("pA", 2)
                        for bl in range(8):
                            b = bg * 16 + hb * 8 + bl
                            for ri in range(2):
                                m.op("pe", lambda e: e.matmul(pA[pi][:, bl * 2 + ri, :], lhsT=f1p[fi][:, hb * 8 + bl, ri, :], rhs=x1s[xi][:, :, b],
                                                              start=True, stop=True),
                                     reads=[d_f1[fi], d_x1[xi]], writes=[d_pA[pi]], inc=(bl == 7 and ri == 1))
                        b0 = bg * 16 + hb * 8
                        evac(bP[:, :, b0:b0 + 8, :], pA[pi][:].rearrange("p (b r) c -> p r b c", r=2), [d_pA[pi]], [d_bP])
                for ri in range(2):
                    for cg in range(GC // 8):
                        ti = nxt("pT", 2)
                        for k in range(8):
                            ch = cg * 8 + k
                            m.op("pe", lambda e: e.transpose(out=pT[ti][:, k, :], in_=bP[:, ri, :, ch], identity=self.ident[:]),
                                 reads=[d_bP, self.d_const], writes=[d_pT[ti]], inc=(k == 7))
                        evac(bQ[:, ri, cg * 8:(cg + 1) * 8, :], pT[ti][:], [d_pT[ti]], [d_bQ])
                for blk in range(GC // 4):
                    xi_ = nxt("pX", 2)
                    cs_ = slice(blk * 4, blk * 4 + 4)
                    are = bQ[:, 0, cs_, :]; aim = bQ[:, 1, cs_, :]
                    m.op("pe", lambda e: e.matmul(pX[xi_][0][:], lhsT=C2[:, 0, :], rhs=are, start=True, stop=False), reads=[d_c, d_bQ], writes=[d_pX[xi_][0]], inc=False)
                    m.op("pe", lambda e: e.matmul(pX[xi_][0][:], lhsT=C2[:, 1, :], rhs=aim, start=False, stop=True), reads=[d_c, d_bQ], writes=[d_pX[xi_][0]])
                    m.op("pe", lambda e: e.matmul(pX[xi_][1][:], lhsT=C2[:, 0, :], rhs=aim, start=True, stop=False), reads=[d_c, d_bQ], writes=[d_pX[xi_][1]], inc=False)
                    m.op("pe", lambda e: e.matmul(pX[xi_][1][:], lhsT=C2[:, 2, :], rhs=are, start=False, stop=True), reads=[d_c, d_bQ], writes=[d_pX[xi_][1]])
                    pre, pim = pX[xi_][0][:], pX[xi_][1][:]
                    dre, dim_ = d_pX[xi_][0], d_pX[xi_][1]
                    kre = Kf[:, 0, cs_, :]; kim = Kf[:, 1, cs_, :]
                    if mode == "A":
                        evac(kre, pre, [dre], [d_Kf]); evac(kim, pim, [dim_], [d_Kf])
                    elif mode == "B":
                        m.op("dve", lambda e: e.tensor_tensor(out=kre, in0=pre, in1=kre, op=ALU.add), reads=[dre, d_Kf], writes=[d_Kf])
                        m.op("dve", lambda e: e.tensor_tensor(out=kim, in0=kim, in1=pim, op=ALU.subtract), reads=[dim_, d_Kf], writes=[d_Kf])
                    else:
                        a_, b_, c_, e_ = t4
                        m.op("dve", lambda e: e.tensor_tensor(out=a_[:], in0=pre, in1=kre, op=ALU.mult), reads=[dre, d_Kf], writes=[d_t4[0]])
                        m.op("dve", lambda e: e.tensor_tensor(out=b_[:], in0=pim, in1=kim, op=ALU.mult), reads=[dim_, d_Kf], writes=[d_t4[1]])
                        m.op("dve", lambda e: e.tensor_tensor(out=c_[:], in0=pre, in1=kim, op=ALU.mult), reads=[dre, d_Kf], writes=[d_t4[2]])
                        m.op("dve", lambda e: e.tensor_tensor(out=e_[:], in0=pim, in1=kre, op=ALU.mult), reads=[dim_, d_Kf], writes=[d_t4[3]])
                        m.op("dve", lambda e: e.tensor_tensor(out=Yb[:, 0, cs_, :], in0=a_[:], in1=b_[:], op=ALU.subtract),
                             reads=[d_t4[0], d_t4[1]], writes=[d_Yb])
                        m.op("pool", lambda e: e.tensor_tensor(out=Yb[:, 1, cs_, :], in0=c_[:], in1=e_[:], op=ALU.add),
                             reads=[d_t4[2], d_t4[3]], writes=[d_Yb])

            def inv(g):
                for blk in range(GC // 4):
                    xi_ = nxt("pX", 2)
                    cs_ = slice(blk * 4, blk * 4 + 4)
                    yre = Yb[:, 0, cs_, :]; yim = Yb[:, 1, cs_, :]
                    m.op("pe", lambda e: e.matmul(pX[xi_][0][:], lhsT=C2[:, 0, :], rhs=yre, start=True, stop=False), reads=[d_c, d_Yb], writes=[d_pX[xi_][0]], inc=False)
                    m.op("pe", lambda e: e.matmul(pX[xi_][0][:], lhsT=C2[:, 2, :], rhs=yim, start=False, stop=True), reads=[d_c, d_Yb], writes=[d_pX[xi_][0]])
                    m.op("pe", lambda e: e.matmul(pX[xi_][1][:], lhsT=C2[:, 1, :], rhs=yre, start=True, stop=False), reads=[d_c, d_Yb], writes=[d_pX[xi_][1]], inc=False)
                    m.op("pe", lambda e: e.matmul(pX[xi_][1][:], lhsT=C2[:, 0, :], rhs=yim, start=False, stop=True), reads=[d_c, d_Yb], writes=[d_pX[xi_][1]])
                    evac(bZ[:, 0, cs_, :], pX[xi_][0][:], [d_pX[xi_][0]], [d_bZ])
                    evac(bZ[:, 1, cs_, :], pX[xi_][1][:], [d_pX[xi_][1]], [d_bZ])
                for ri in range(2):
                    for cg in range(GC // 8):
                        ti = nxt("pT", 2)
                        for k in range(8):
                            ch = cg * 8 + k
                            m.op("pe", lambda e: e.transpose(out=pT[ti][:, k, :], in_=bZ[:, ri, ch, :], identity=self.ident[:]),
                                 reads=[d_bZ, self.d_const], writes=[d_pT[ti]], inc=(k == 7))
                        evac(bQ[:, ri, cg * 8:(cg + 1) * 8, :], pT[ti][:], [d_pT[ti]], [d_bQ])
                c = (g * GC) // 128; r0 = (g * GC) % 128
                m.dma(gsc[:, 0:1], self.hsc[r0:r0 + GC, 0, c:c + 1], reads=[self.d_hsc], writes=[d_gsc], q="sp", allow_slow_non_contiguous=True)
                m.dma(gsc[:, 1:2], I["hy_bias"][r0:r0 + GC, c:c + 1], writes=[d_gsc], q="sp", allow_slow_non_contiguous=True)
                for ah in range(self.nown // SH):
                    if fi_loaded[0] != ah:
                        m.dma(FI[:], I["FI%d" % ah][:, :, :, :], writes=[d_FI], q="sp")
                        fi_loaded[0] = ah
                    for bg in range(8):
                        pi = nxt("pA", 2)
                        for bl in range(16):
                            b = bg * 16 + bl
                            m.op("pe", lambda e: e.matmul(pA[pi][0:GC, bl, :], lhsT=bQ[:, 0, :, b], rhs=FI[:, b, 0, :], start=True, stop=False),
                                 reads=[d_bQ, d_FI], writes=[d_pA[pi]], inc=False)
                            m.op("pe", lambda e: e.matmul(pA[pi][0:GC, bl, :], lhsT=bQ[:, 1, :, b], rhs=FI[:, b, 1, :], start=False, stop=True),
                                 reads=[d_bQ, d_FI], writes=[d_pA[pi]], inc=(bl == 15))
                        evac(ysb[:, :, bg * 16:(bg + 1) * 16], pA[pi][0:GC, :, :].rearrange("p b a -> p a b"), [d_pA[pi]], [d_ysb])
                    yfl = ysb[:].rearrange("p a b -> p (a b)")
                    for q4 in range(8):
                        fi = nxt("fin", 2)
                        sl = slice(q4 * 512, (q4 + 1) * 512)
                        gl = slice(ah * SH + q4 * 512, ah * SH + (q4 + 1) * 512)
                        m.dma(fv[fi][:], self.VV[g * GC:(g + 1) * GC, gl], writes=[d_fv[fi]], q="sp")
                        m.dma(fx[fi][:], self.X0[g * GC:(g + 1) * GC, gl], writes=[d_fx[fi]], q="sp")
                        m.op("act", lambda e: e.activation(out=fo[fi][:], in_=yfl[:, sl], func=AF.Identity, scale=gsc[:, 0:1]),
                             reads=[d_ysb, d_gsc], writes=[d_fo[fi]])
                        m.op("dve", lambda e: e.scalar_tensor_tensor(out=fo[fi][:], in0=fv[fi][:], scalar=gsc[:, 1:2], in1=fo[fi][:], op0=ALU.mult, op1=ALU.add),
                             reads=[d_fv[fi], d_gsc, d_fo[fi]], writes=[d_fo[fi]])
                        m.op("dve", lambda e: e.tensor_tensor(out=fo[fi][:], in0=fo[fi][:], in1=fx[fi][:], op=ALU.mult),
                             reads=[d_fo[fi], d_fx[fi]], writes=[d_fo[fi]])
                        m.dma(self.OT[768 + g * GC:768 + (g + 1) * GC, gl], fo[fi][:], reads=[d_fo[fi]], q="act")

            ngrp = 256 // GC if "hy_short" not in self.dbg else 1
            for g in range(ngrp):
                fwd(self.KFb[g * GC:(g + 1) * GC, :], "A")
                fwd(self.KFb[256 + g * GC:256 + (g + 1) * GC, :], "B")
                fwd(self.VVb[g * GC:(g + 1) * GC, :], "V")
                inv(g)
        self.hyP.__exit__(None, None, None)

    def phase_merge(self):
        m, I = self.m, self.I
        with Phase(m) as ph:
            d_c = Dep()
            og = ph.sb([128, 8]); m.dma(og[:], I["out_g"][:, :], writes=[d_c])
            wo = ph.sb([128, 8, D], BF16); d_wo = Dep()
            wst = [ph.sb([128, D]) for _ in range(2)]; d_wst = [Dep(), Dep()]
            wv = I["w_out"].rearrange("(kc p) n -> p kc n", p=128)
            for kc in range(8):
                m.dma(wst[kc % 2][:], wv[:, kc, :], writes=[d_wst[kc % 2]])
                m.op("dve", lambda e: e.tensor_scalar(out=wo[:, kc, :], in0=wst[kc % 2][:], scalar1=og[:, kc:kc + 1], scalar2=None, op0=ALU.mult),
                     reads=[d_wst[kc % 2], d_c], writes=[d_wo])
            ones2 = ph.sb([128, 2], BF16)
            m.op("dve", lambda e: e.memset(ones2[:], 1.0), writes=[d_c])
            wrec = ph.sb([128, 3])
            for gi, wd in enumerate((512, 256, 256)):
                m.op("dve", lambda e: e.memset(wrec[:, gi:gi + 1], 1.0 / wd), writes=[d_c])
            ob = [ph.sb([128, 8, 512]) for _ in range(2)]; d_ob = [Dep(), Dep()]
            obb = [ph.sb([128, 8, 512], BF16) for _ in range(2)]; d_obb = [Dep(), Dep()]
            sqb = [ph.sb([128, 8, 512], BF16) for _ in range(2)]; d_sqb = [Dep(), Dep()]
            xt = [ph.sb([128, D]) for _ in range(2)]; d_xt = [Dep(), Dep()]
            acc = [ph.sb([128, D]) for _ in range(2)]; d_acc = [Dep(), Dep()]
            xn = [ph.sb([128, D], BF16) for _ in range(2)]; d_xn = [Dep(), Dep()]
            junk = ph.sb([128, D], BF16); d_junk = Dep()
            rs = [ph.sb([128, 4]) for _ in range(2)]; d_rs = [Dep(), Dep()]
            fT = [ph.sb([128, 8, 512], BF16) for _ in range(2)]; d_fT = [Dep(), Dep()]
            pO = [[ph.ps([128, 512]) for _ in range(3)] for _ in range(2)]; d_pO = [[Dep() for _ in range(3)] for _ in range(2)]
            pT = ph.ps([128, 8, 128], BF16); d_pT = Dep()
            pS = ph.ps([128, 3, 2]); d_pS = Dep()
            groups = [(0, 4), (4, 6), (6, 8)]
            OTv = self.OT.rearrange("(c p) t -> p c t", p=128)
            blocks = [("lat", i * 512, 512) for i in range(self.nown // 512)] + ([] if self.last else [("ctx", self.nown, LC)])
            tcount = 0
            pcount = 0
            pend = [None]
            for bi, (kind, c0, ntok) in enumerate(blocks):
                o_ = ob[bi % 2]; do = d_ob[bi % 2]
                m.dma(o_[:, :, 0:ntok], OTv[:, :, c0:c0 + ntok], writes=[do], q="sp")
                ob_ = obb[bi % 2]; dob = d_obb[bi % 2]
                sq_ = sqb[bi % 2]; dsq = d_sqb[bi % 2]
                for ch in range(8):
                    m.op("act", lambda e: e.activation(out=sq_[:, ch, 0:ntok], in_=o_[:, ch, 0:ntok], func=AF.Square), reads=[do], writes=[dsq])
                    m.op("pool", lambda e: e.tensor_copy(out=ob_[:, ch, 0:ntok], in_=o_[:, ch, 0:ntok]), reads=[do], writes=[dob])
                f_ = fT[bi % 2]; df = d_fT[bi % 2]
                gi_ga = 0 if kind == "lat" else 2
                mi = 1 if kind == "lat" else 3
                modt = self.modL if kind == "lat" else self.modC
                for tt in range(ntok // 128):
                    ts_ = slice(tt * 128, (tt + 1) * 128)
                    k2 = tcount % 2; tcount += 1
                    r_ = rs[k2]; dr = d_rs[k2]
                    if kind == "lat":
                        m.dma(xt[k2][:], self.xsrc[c0 + tt * 128:c0 + (tt + 1) * 128, :], writes=[d_xt[k2]], q="sp")
                    else:
                        m.dma(xt[k2][:], self.ctxsrc[tt * 128:(tt + 1) * 128, :], writes=[d_xt[k2]], q="sp")
                    for gi, (a, b) in enumerate(groups):
                        for ch in range(a, b):
                            m.op("pe", lambda e: e.matmul(pS[:, gi, :], lhsT=sq_[:, ch, ts_], rhs=ones2[:], start=(ch == a), stop=(ch == b - 1)),
                                 reads=[dsq, d_c], writes=[d_pS], inc=(ch == b - 1))
                    m.op("dve", lambda e: e.tensor_tensor(out=r_[:, 0:3], in0=pS[:, :, 0], in1=wrec[:], op=ALU.mult), reads=[d_pS, d_c], writes=[dr])
                    m.op("act", lambda e: e.activation(out=r_[:, 0:3], in_=r_[:, 0:3], func=AF.Sqrt, bias=self.epsT[:, 0:1]), reads=[dr, self.d_const], writes=[dr])
                    m.op("dve", lambda e: e.reciprocal(out=r_[:, 0:3], in_=r_[:, 0:3]), reads=[dr], writes=[dr])
                    a_ = acc[k2]; da = d_acc[k2]
                    for h in range(2):
                        hs = slice(h * 512, (h + 1) * 512)
                        pk = pcount % 2; pcount += 1
                        for gi, (a, b) in enumerate(groups):
                            for ch in range(a, b):
                                m.op("pe", lambda e: e.matmul(pO[pk][gi][:], lhsT=ob_[:, ch, ts_], rhs=wo[:, ch, hs], start=(ch == a), stop=(ch == b - 1)),
                                     reads=[dob, d_wo], writes=[d_pO[pk][gi]], inc=(ch == b - 1))
                        m.op("dve", lambda e: e.tensor_scalar(out=a_[:, hs], in0=pO[pk][0][:], scalar1=r_[:, 0:1], scalar2=None, op0=ALU.mult),
                             reads=[d_pO[pk][0], dr], writes=[da])
                        for gi in (1, 2):
                            m.op("dve", lambda e: e.scalar_tensor_tensor(out=a_[:, hs], in0=pO[pk][gi][:], scalar=r_[:, gi:gi + 1], in1=a_[:, hs],
                                                                         op0=ALU.mult, op1=ALU.add), reads=[d_pO[pk][gi], dr, da], writes=[da])
                    m.op("pool", lambda e: e.tensor_tensor(out=a_[:], in0=a_[:], in1=self.gbc[:, gi_ga, :], op=ALU.mult), reads=[da, self.d_gbc], writes=[da])
                    m.op("pool", lambda e: e.tensor_tensor(out=a_[:], in0=a_[:], in1=xt[k2][:], op=ALU.add), reads=[da, d_xt[k2]], writes=[da])
                    m.dma(self.XM[c0 + tt * 128:c0 + (tt + 1) * 128, :], a_[:], reads=[da], q="act")
                    def stage_y(a_=a_, da=da, r_=r_, dr=dr, k2=k2, f_=f_, df=df, ts_=ts_, mi=mi, modt=modt,
                                last_tile=(tt == ntok // 128 - 1), c0=c0, ntok=ntok):
                        m.op("act", lambda e: e.activation(out=junk[:], in_=a_[:], func=AF.Square, accum_out=r_[:, 3:4]), reads=[da], writes=[d_junk, dr])
                        m.op("dve", lambda e: e.tensor_scalar(out=r_[:, 3:4], in0=r_[:, 3:4], scalar1=1.0 / D, scalar2=EPS, op0=ALU.mult, op1=ALU.add),
                             reads=[dr], writes=[dr])
                        m.op("act", lambda e: e.activation(out=r_[:, 3:4], in_=r_[:, 3:4], func=AF.Sqrt), reads=[dr], writes=[dr])
                        m.op("dve", lambda e: e.reciprocal(out=r_[:, 3:4], in_=r_[:, 3:4]), reads=[dr], writes=[dr])
                        m.op("dve", lambda e: e.tensor_scalar(out=xn[k2][:], in0=a_[:], scalar1=r_[:, 3:4], scalar2=None, op0=ALU.mult),
                             reads=[da, dr], writes=[d_xn[k2]])
                        for kc in range(8):
                            m.op("pe", lambda e: e.transpose(out=pT[:, kc, :], in_=xn[k2][:, kc * 128:(kc + 1) * 128], identity=self.ident[:]),
                                 reads=[d_xn[k2], self.d_const], writes=[d_pT], inc=(kc == 7))
                        for kc in range(8):
                            m.op("dve", lambda e: e.tensor_scalar(out=f_[:, kc, ts_], in0=pT[:, kc, :], scalar1=self.AB[:, mi, kc:kc + 1],
                                                                  scalar2=modt[:, 24 + kc:25 + kc], op0=ALU.mult, op1=ALU.add),
                                 reads=[d_pT, self.d_mod], writes=[df])
                        if last_tile:
                            m.dma(self.FT[:, :, c0:c0 + ntok], f_[:, :, 0:ntok], reads=[df], q="act")
                    if pend[0] is not None:
                        pend[0]()
                    pend[0] = stage_y
            if pend[0] is not None:
                pend[0]()

    def phase_moe(self):
        m, I = self.m, self.I
        NT = self.ntok // 128
        NTOK = self.ntok; SHL = self.nown
        with Phase(m) as P:
            d_c = Dep()
            gate = P.sb([128, NT, NE]); d_gate = Dep()
            b1 = P.sb([128, NE, 16])
            m.dma(b1[:], I["moe_b1"][:, :, :], writes=[d_c])
            m.op("dve", lambda e: e.tensor_scalar(out=b1[:, :, 8:16], in0=b1[:, :, 8:16], scalar1=1.0, scalar2=None, op0=ALU.add), reads=[d_c], writes=[d_c])
            with Phase(m) as ph:
                rw = ph.sb([128, 8, NE], BF16); rb = ph.sb([128, NE])
                m.dma(rw[:], I["router_w"].rearrange("(kc p) n -> p kc n", p=128), writes=[d_c], q="pool")
                m.dma(rb[:], I["router_b"][:, :], writes=[d_c])
                fb = [ph.sb([128, 8, 512], BF16) for _ in range(2)]; d_fb = [Dep(), Dep()]
                lg = [ph.sb([128, NE]) for _ in range(2)]; d_lg = [Dep(), Dep()]
                ex = [ph.sb([128, NE]) for _ in range(2)]; d_ex = [Dep(), Dep()]
                m8 = [ph.sb([128, 8]) for _ in range(2)]; d_m8 = [Dep(), Dep()]
                sm = [ph.sb([128, 2]) for _ in range(2)]; d_sm = [Dep(), Dep()]
                pl = [ph.ps([128, NE]) for _ in range(2)]; d_pl = [Dep(), Dep()]
                tcount = 0
                for bi in range((NTOK + 511) // 512):
                    c0 = bi * 512; n = min(512, NTOK - c0)
                    m.dma(fb[bi % 2][:, :, 0:n], self.FT[:, :, c0:c0 + n], writes=[d_fb[bi % 2]], q="sp")
                    for tt in range(n // 128):
                        tile = c0 // 128 + tt
                        k = tcount % 2; tcount += 1
                        for kc in range(8):
                            m.op("pe", lambda e: e.matmul(pl[k][:], lhsT=fb[bi % 2][:, kc, tt * 128:(tt + 1) * 128], rhs=rw[:, kc, :], start=(kc == 0), stop=(kc == 7)),
                                 reads=[d_fb[bi % 2], d_c], writes=[d_pl[k]], inc=(kc == 7))
                        m.op("dve", lambda e: e.tensor_tensor(out=lg[k][:], in0=pl[k][:], in1=rb[:], op=ALU.add), reads=[d_pl[k], d_c], writes=[d_lg[k]])
                        m.op("dve", lambda e: e.max(out=m8[k][:], in_=lg[k][:]), reads=[d_lg[k]], writes=[d_m8[k]])
                        m.op("dve", lambda e: e.tensor_scalar(out=sm[k][:, 0:1], in0=m8[k][:, 0:1], scalar1=-1.0, scalar2=None, op0=ALU.mult),
                             reads=[d_m8[k]], writes=[d_sm[k]])
                        m.op("act", lambda e: e.activation(out=ex[k][:], in_=lg[k][:], func=AF.Exp, bias=sm[k][:, 0:1]), reads=[d_lg[k], d_sm[k]], writes=[d_ex[k]])
                        m.op("dve", lambda e: e.scalar_tensor_tensor(out=ex[k][:], in0=lg[k][:], scalar=m8[k][:, 3:4], in1=ex[k][:], op0=ALU.is_ge, op1=ALU.mult,
                                                                     accum_out=sm[k][:, 1:2]), reads=[d_lg[k], d_m8[k], d_ex[k]], writes=[d_ex[k], d_sm[k]])
                        m.op("dve", lambda e: e.reciprocal(out=sm[k][:, 1:2], in_=sm[k][:, 1:2]), reads=[d_sm[k]], writes=[d_sm[k]])
                        m.op("dve", lambda e: e.tensor_scalar(out=gate[:, tile, :], in0=ex[k][:], scalar1=sm[k][:, 1:2], scalar2=None, op0=ALU.mult),
                             reads=[d_ex[k], d_sm[k]], writes=[d_gate])
            if "gate" in self.dbg:
                o = self.nc.dram_tensor("dbg_gate", [128, NT, NE], F32, kind="ExternalOutput").ap(); self.out_names.append("dbg_gate")
                m.dma(o[:, :, :], gate[:], reads=[d_gate])
            w1v = I["moe_w1"].rearrange("e (kc p) n -> e p kc n", p=128)
            w2v = I["moe_w2"].rearrange("e (kc p) n -> e p kc n", p=128)
            passes = [(a, min(a + 11, NT)) for a in range(0, NT, 11)]
            if "moe_short" in self.dbg:
                passes = [(NT - 2, NT)]
            n_exp = NE
            for (ta, tb_) in passes:
                ntile = tb_ - ta
                with Phase(m) as pp:
                    fT = pp.sb([128, 8, ntile * 128], BF16); d_fT = Dep()
                    yacc = pp.sb([128, ntile, D]); d_y = [Dep() for _ in range(ntile)]
                    m.dma(fT[:], self.FT[:, :, ta * 128:tb_ * 128], writes=[d_fT], q="sp")
                    with Phase(m) as ph:
                        b2 = ph.sb([NE, D])
                        m.dma(b2[:], I["moe_b2"][:, :], writes=[d_c])
                        gT = [ph.sb([NE, 128]) for _ in range(2)]; d_gT = [Dep(), Dep()]
                        pg = [ph.ps([NE, 128]) for _ in range(2)]; d_pg = [Dep(), Dep()]
                        py = [ph.ps([128, 512]) for _ in range(2)]; d_py = [Dep(), Dep()]
                        for ti in range(ntile):
                            k = ti % 2
                            m.op("pe", lambda e: e.transpose(out=pg[k][:], in_=gate[:, ta + ti, :], identity=self.identf[:]),
                                 reads=[d_gate, self.d_const], writes=[d_pg[k]])
                            m.op("act", lambda e: e.activation(out=gT[k][:], in_=pg[k][:], func=AF.Identity), reads=[d_pg[k]], writes=[d_gT[k]])
                            for h in range(2):
                                m.op("pe", lambda e: e.matmul(py[h][:], lhsT=gT[k][:], rhs=b2[:, h * 512:(h + 1) * 512], start=True, stop=True),
                                     reads=[d_gT[k], d_c], writes=[d_py[h]])
                                m.op("dve", lambda e: e.tensor_copy(out=yacc[:, ti, h * 512:(h + 1) * 512], in_=py[h][:]), reads=[d_py[h]], writes=[d_y[ti]])
                    with Phase(m) as ph:
                        w1 = [ph.sb([128, 8, 2 * D], BF16) for _ in range(2)]; d_w1 = [[Dep() for _ in range(8)] for _ in range(2)]
                        w2 = ph.sb([128, 8, D], BF16); d_w2 = [Dep() for _ in range(8)]
                        act = [ph.sb([128, 8, 512], BF16) for _ in range(2)]; d_act = [Dep(), Dep()]
                        tg = [ph.sb([128, 512]) for _ in range(2)]; d_tg = [Dep(), Dep()]
                        tsg = [ph.sb([128, 512]) for _ in range(2)]; d_tsg = [Dep(), Dep()]
                        tl = [ph.sb([128, 512]) for _ in range(2)]; d_tl = [Dep(), Dep()]
                        pgl = [[ph.ps([128, 512]) for _ in range(2)] for _ in range(2)]; d_pgl = [[Dep(), Dep()], [Dep(), Dep()]]
                        pyy = [ph.ps([128, 512]) for _ in range(3)]; d_pyy = [Dep() for _ in range(3)]
                        cnt = {"j": 0, "y": 0, "a": 0}
                        m.op("dve", lambda e: e.tensor_scalar(out=gate[:, ta:tb_, :], in0=gate[:, ta:tb_, :], scalar1=1.0 / 1.702, scalar2=None, op0=ALU.mult),
                             reads=[d_gate], writes=[d_gate])

                        wtok = []

                        def wdma(out, in_, dep):
                            if len(wtok) >= 2:
                                m._wait("pool", wtok[-2])
                            wtok.append(m.dma(out, in_, writes=[dep], q="pool"))

                        def load_w1(e_):
                            for kc in range(8):
                                wdma(w1[e_ % 2][:, kc, :], w1v[e_, :, kc, :], d_w1[e_ % 2][kc])

                        def load_w2(e_):
                            for kc in range(8):
                                wdma(w2[:, kc, :], w2v[e_, :, kc, :], d_w2[kc])
                        load_w1(0)
                        for e_ in range(n_exp):
                            load_w2(e_)
                            if e_ + 1 < n_exp:
                                load_w1(e_ + 1)
                            W1 = w1[e_ % 2]; dW1 = d_w1[e_ % 2]
                            for b0 in range(0, ntile, 4):
                                nt_ = min(4, ntile - b0); n = nt_ * 128
                                cs_ = slice(b0 * 128, b0 * 128 + n)
                                ai = cnt["a"] % 2; cnt["a"] += 1
                                A_ = act[ai]; dA = d_act[ai]
                                for j in range(8):
                                    k = cnt["j"] % 2; cnt["j"] += 1
                                    for gl in range(2):
                                        for kc in range(8):
                                            m.op("pe", lambda e: e.matmul(pgl[k][gl][:, 0:n], lhsT=W1[:, kc, 256 * j + gl:256 * j + 256:2], rhs=fT[:, kc, cs_],
                                                                          start=(kc == 0), stop=(kc == 7)),
                                                 reads=[dW1[kc], d_fT], writes=[d_pgl[k][gl]], inc=(kc == 7))
                                    m.op("dve", lambda e: e.tensor_scalar(out=tg[k][:, 0:n], in0=pgl[k][0][:, 0:n], scalar1=b1[:, e_, j:j + 1], scalar2=7.0,
                                                                          op0=ALU.add, op1=ALU.min), reads=[d_pgl[k][0], d_c], writes=[d_tg[k]])
                                    m.op("act", lambda e: e.activation(out=tsg[k][:, 0:n], in_=tg[k][:, 0:n], func=AF.Silu, scale=1.702),
                                         reads=[d_tg[k]], writes=[d_tsg[k]])
                                    m.op("dve", lambda e: e.tensor_scalar(out=tl[k][:, 0:n], in0=pgl[k][1][:, 0:n], scalar1=b1[:, e_, 8 + j:9 + j], scalar2=8.0,
                                                                          op0=ALU.add, op1=ALU.min), reads=[d_pgl[k][1], d_c], writes=[d_tl[k]])
                                    m.op("dve", lambda e: e.scalar_tensor_tensor(out=A_[:, j, 0:n], in0=tl[k][:, 0:n], scalar=-6.0, in1=tsg[k][:, 0:n],
                                                                                 op0=ALU.max, op1=ALU.mult), reads=[d_tl[k], d_tsg[k]], writes=[dA])
                                for tt in range(nt_):
                                    ti = b0 + tt
                                    for h in range(2):
                                        yk = cnt["y"] % 3; cnt["y"] += 1
                                        for j in range(8):
                                            m.op("pe", lambda e: e.matmul(pyy[yk][:], lhsT=A_[:, j, tt * 128:(tt + 1) * 128], rhs=w2[:, j, h * 512:(h + 1) * 512],
                                                                          start=(j == 0), stop=(j == 7)),
                                                 reads=[dA, d_w2[j]], writes=[d_pyy[yk]], inc=(j == 7))
                                        m.op("dve", lambda e: e.scalar_tensor_tensor(out=yacc[:, ti, h * 512:(h + 1) * 512], in0=pyy[yk][:], scalar=gate[:, ta + ti, e_:e_ + 1],
                                                                                     in1=yacc[:, ti, h * 512:(h + 1) * 512], op0=ALU.mult, op1=ALU.add),
                                             reads=[d_pyy[yk], d_gate, d_y[ti]], writes=[d_y[ti]])
                    with Phase(m) as ph:
                        xm = [ph.sb([128, D]) for _ in range(2)]; d_xm = [Dep(), Dep()]
                        ot = [ph.sb([128, D]) for _ in range(2)]; d_ot = [Dep(), Dep()]
                        for ti in range(ntile):
                            tile = ta + ti; k = ti % 2
                            m.dma(xm[k][:], self.XM[tile * 128:(tile + 1) * 128, :], writes=[d_xm[k]], q="sp")
                            gi = 1 if tile < SHL // 128 else 3
                            m.op("pool", lambda e: e.tensor_tensor(out=ot[k][:], in0=yacc[:, ti, :], in1=self.gbc[:, gi, :], op=ALU.mult),
                                 reads=[d_y[ti], self.d_gbc], writes=[d_ot[k]])
                            m.op("dve", lambda e: e.tensor_tensor(out=ot[k][:], in0=ot[k][:], in1=xm[k][:], op=ALU.add), reads=[d_ot[k], d_xm[k]], writes=[d_ot[k]])
                            if tile < SHL // 128:
                                dst = self.x_out if self.last else self.X1
                                m.dma(dst[tile * 128:(tile + 1) * 128, :], ot[k][:], reads=[d_ot[k]], q="act")
                            else:
                                r0 = tile * 128 - SHL
                                m.dma(self.C1[r0:r0 + 128, :], ot[k][:], reads=[d_ot[k]], q="act")


_PROG = {}


def _get_prog():
    if "p" not in _PROG:
        _PROG["p"] = LayerProg()
    return _PROG["p"]


def kernel(**inputs):
    inp = {k: np.asarray(v) for k, v in inputs.items()}
    P = _get_prog()
    need = set(P.Iall.keys())
    x = np.ascontiguousarray(inp["x"], dtype=np.float32)
    ctx = np.ascontiguousarray(inp["ctx"], dtype=np.float32)
    maps = []
    for core in range(8):
        b, half = core // 2, core % 2
        xc = x[b][::-1] if half else x[b]
        cc = ctx[b][::-1] if half else ctx[b]
        mp = {}
        for l in range(2):
            d = _prep_core(inp, l, b, half, xc, cc)
            for k, v in d.items():
                name = k if k in SHARED else "%s@%d" % (k, l)
                if name in need and name not in mp:
                    mp[name.replace("@", "_L")] = v
        maps.append(mp)
    res = run_bass_kernel_spmd(P.nc, maps, core_ids=list(range(8)))
    out = np.empty_like(x)
    for core in range(8):
        b, half = core // 2, core % 2
        xo = np.asarray(res.results[core]["x_out"])
        if half == 0:
            out[b, :SH] = xo
        else:
            out[b, SH:] = xo[::-1]
    return out
```
